# Optimizing a Trainium2 kernel written in Bass

```python
import jax
import jax.numpy as jnp
from jax import lax
import numpy as np

D_MODEL = 1024
BATCH = 16
SEQ = 2048
DEPTH = 1

GRID_W = 64
CTX_LEN = 256
EPS = 1e-6

D_MIX = D_MODEL
D_A = D_MIX // 2
D_B = D_MIX - D_A
CHUNK_A = 128
ROWS_PER_CHUNK = CHUNK_A // GRID_W
HEADS_A = 4
HEAD_DIM_A = D_A // HEADS_A
HEADS_B = 4
HEAD_DIM_B = D_B // HEADS_B
CONV_W = 5
DELTA_CHUNK = 64
N_DIR = 2
N_IN = 2 * D_A + 4 * D_B + 2 * N_DIR * HEADS_B
N_GROUPS = 4
EXPERTS_PER_GROUP = 4
N_EXPERTS = N_GROUPS * EXPERTS_PER_GROUP
TOP_K = 2
D_EXPERT = 512
MOE_BLOCK = 128

kernel_name = 'hymba_chunkmlp_gdn_hmoe_dit'


def rmsnorm(x, g):
    xf = x.astype(jnp.float32)
    y = xf * lax.rsqrt(jnp.mean(xf * xf, axis=-1, keepdims=True) + EPS)
    return (y * g).astype(x.dtype)


def layernorm(x, g, b):
    xf = x.astype(jnp.float32)
    mu = jnp.mean(xf, axis=-1, keepdims=True)
    var = jnp.mean(jnp.square(xf - mu), axis=-1, keepdims=True)
    return ((xf - mu) * lax.rsqrt(var + EPS) * g + b).astype(x.dtype)


def l2norm(x):
    return x * lax.rsqrt(jnp.sum(x * x, axis=-1, keepdims=True) + EPS)


def modulation(cond, w_ada, b_ada):
    m = jax.nn.silu(cond) @ w_ada + b_ada
    return jnp.split(m[..., None, :], 6, axis=-1)


def centred_dwconv(x, w):
    pad = (CONV_W - 1) // 2
    return lax.conv_general_dilated(
        x, w[:, None, :].astype(x.dtype), window_strides=(1,),
        padding=[(pad, CONV_W - 1 - pad)],
        dimension_numbers=('NWC', 'WIO', 'NWC'),
        feature_group_count=x.shape[-1])


def chunk_mlp_mixer(p, n_chunks, ln_g, ln_b, w_s, b_s):
    B_, L_, _ = p.shape
    u, v = jnp.split(jax.nn.gelu(p), 2, axis=-1)
    v = layernorm(v, ln_g, ln_b)
    vc = v.reshape(B_, n_chunks, CHUNK_A, HEADS_A, HEAD_DIM_A)
    s = jnp.einsum('hij,bnjhd->bnihd', w_s, vc) + b_s.T[None, None, :, :, None]
    return u * s.reshape(B_, L_, D_A)


def chunk_gated_delta(q, k, v, g, beta, s0):
    R, L_, DK = k.shape
    DV = v.shape[-1]
    C = DELTA_CHUNK
    N = L_ // C
    q = q.reshape(R, N, C, DK)
    k = k.reshape(R, N, C, DK)
    v = v.reshape(R, N, C, DV)
    beta = beta.reshape(R, N, C)
    gc = jnp.cumsum(g.reshape(R, N, C), axis=-1)
    incl = jnp.tril(jnp.ones((C, C), dtype=bool))
    strict = jnp.tril(jnp.ones((C, C), dtype=bool), -1)
    diff = gc[..., :, None] - gc[..., None, :]
    decay = jnp.where(incl, jnp.exp(jnp.where(incl, diff, 0.0)), 0.0)
    kb = k * beta[..., None]
    lmat = jnp.where(strict, jnp.einsum('rnid,rnjd->rnij', kb, k) * decay, 0.0)
    amat = lmat + jnp.eye(C, dtype=jnp.float32)
    rhs = jnp.concatenate([v * beta[..., None], kb * jnp.exp(gc)[..., None]], axis=-1)
    sol = lax.linalg.triangular_solve(amat, rhs, left_side=True, lower=True, unit_diagonal=True)
    u, w = sol[..., :DV], sol[..., DV:]
    qk = jnp.where(incl, jnp.einsum('rnid,rnjd->rnij', q, k) * decay, 0.0)
    qd = q * jnp.exp(gc)[..., None]
    kd = k * jnp.exp(gc[..., -1:] - gc)[..., None]
    glast = jnp.exp(gc[..., -1])

    def step(s, xs):
        qk_c, qd_c, w_c, u_c, kd_c, gl_c = xs
        v_new = u_c - jnp.einsum('rcd,rde->rce', w_c, s)
        o_c = jnp.einsum('rcd,rde->rce', qd_c, s) + jnp.einsum('rcj,rje->rce', qk_c, v_new)
        s = s * gl_c[:, None, None] + jnp.einsum('rcd,rce->rde', kd_c, v_new)
        return s, o_c

    xs = tuple(jnp.moveaxis(t, 1, 0) for t in (qk, qd, w, u, kd, glast))
    s_final, o = lax.scan(step, s0, xs)
    return jnp.moveaxis(o, 0, 1).reshape(R, L_, DV), s_final


def delta_mixer(p, s0, conv_w, a_log, dt_bias, onorm_g):
    B_, L_, _ = p.shape
    qkv = jax.nn.silu(centred_dwconv(p[..., :3 * D_B], conv_w))
    z = p[..., 3 * D_B:4 * D_B]
    off = 4 * D_B
    a = p[..., off:off + N_DIR * HEADS_B].astype(jnp.float32).reshape(B_, L_, N_DIR, HEADS_B)
    b = p[..., off + N_DIR * HEADS_B:].astype(jnp.float32).reshape(B_, L_, N_DIR, HEADS_B)

    def to_heads(t):
        return t.astype(jnp.float32).reshape(B_, L_, HEADS_B, HEAD_DIM_B).transpose(0, 2, 1, 3)

    q, k, v = jnp.split(qkv, 3, axis=-1)
    q = l2norm(to_heads(q)) * HEAD_DIM_B ** -0.5
    k = l2norm(to_heads(k))
    v = to_heads(v)
    g = (-jnp.exp(a_log) * jax.nn.softplus(a + dt_bias)).transpose(2, 0, 3, 1)
    beta = jax.nn.sigmoid(b).transpose(2, 0, 3, 1)

    def bidir(t):
        return jnp.stack([t, jnp.flip(t, axis=2)]).reshape((N_DIR * B_ * HEADS_B,) + t.shape[2:])

    def per_dir(t):
        return jnp.stack([t[0], jnp.flip(t[1], axis=-1)]).reshape(N_DIR * B_ * HEADS_B, L_)

    o, s_final = chunk_gated_delta(bidir(q), bidir(k), bidir(v), per_dir(g), per_dir(beta), s0)
    o = o.reshape(N_DIR, B_, HEADS_B, L_, HEAD_DIM_B)
    o = o[0] + jnp.flip(o[1], axis=2)
    y = rmsnorm(o, onorm_g) * jax.nn.silu(to_heads(z))
    return y.transpose(0, 2, 1, 3).reshape(B_, L_, D_B).astype(p.dtype), s_final


def hier_moe(h, w_group, b_group, w_router, b_router, w_gate_up, w_down):
    B_, L_, D = h.shape
    t = h.reshape(-1, D)
    T = t.shape[0]
    glog = (t @ w_group + b_group).astype(jnp.float32)
    gprob = jax.nn.softmax(glog, axis=-1)
    g_sel = jnp.argmax(glog, axis=-1)
    p_g = jnp.max(gprob, axis=-1, keepdims=True)
    elog = (t @ w_router + b_router).astype(jnp.float32).reshape(T, N_GROUPS, EXPERTS_PER_GROUP)
    idx = jnp.broadcast_to(g_sel[:, None, None], (T, 1, EXPERTS_PER_GROUP))
    elog_sel = jnp.take_along_axis(elog, idx, axis=1)[:, 0]
    top_p, top_i = lax.top_k(jax.nn.softmax(elog_sel, axis=-1), TOP_K)
    top_p = top_p / jnp.sum(top_p, axis=-1, keepdims=True) * p_g
    expert_idx = g_sel[:, None] * EXPERTS_PER_GROUP + top_i
    combine = jnp.sum(jax.nn.one_hot(expert_idx, N_EXPERTS, dtype=jnp.float32) * top_p[..., None], axis=1)

    def block(args):
        tb, cb = args
        gate, up = jnp.split(jnp.einsum('td,edf->tef', tb, w_gate_up), 2, axis=-1)
        act = jax.nn.silu(gate) * up * cb[..., None].astype(tb.dtype)
        return jnp.einsum('tef,efd->td', act, w_down)

    out = lax.map(block, (t.reshape(-1, MOE_BLOCK, D), combine.reshape(-1, MOE_BLOCK, N_EXPERTS)))
    return out.reshape(B_, L_, D)


def setup_inputs(seed: int = 0) -> dict:
    key = jax.random.key(seed)
    ks = jax.random.split(key, 26)
    f32 = jnp.float32

    def nrm(k, shape, scale):
        return scale * jax.random.normal(k, shape, f32)

    dt = jnp.exp(jax.random.uniform(ks[14], (DEPTH, N_DIR, HEADS_B), f32,
                                    float(np.log(1e-3)), float(np.log(1e-1))))
    return {
        'x': nrm(ks[0], (BATCH, SEQ, D_MODEL), 1.0),
        'c': nrm(ks[1], (BATCH, D_MODEL), 1.0),
        'ctx': nrm(ks[2], (BATCH, CTX_LEN, D_MODEL), 1.0),
        'c_ctx': nrm(ks[3], (D_MODEL,), 1.0),
        'w_ada': nrm(ks[4], (DEPTH, D_MODEL, 6 * D_MODEL), 0.5 * D_MODEL ** -0.5),
        'b_ada': nrm(ks[5], (DEPTH, 6 * D_MODEL), 0.01),
        'norm1_g': 1.0 + nrm(ks[6], (DEPTH, D_MODEL), 0.01),
        'w_in': nrm(ks[7], (DEPTH, D_MODEL, N_IN), D_MODEL ** -0.5),
        'ln_a_g': 1.0 + nrm(ks[8], (DEPTH, D_A), 0.01),
        'ln_a_b': nrm(ks[9], (DEPTH, D_A), 0.01),
        'w_spatial': nrm(ks[10], (DEPTH, HEADS_A, CHUNK_A, CHUNK_A), CHUNK_A ** -0.5),
        'b_spatial': 1.0 + nrm(ks[11], (DEPTH, HEADS_A, CHUNK_A), 0.01),
        'conv_qkv': nrm(ks[12], (DEPTH, CONV_W, 3 * D_B), CONV_W ** -0.5),
        'a_log': jnp.log(jax.random.uniform(ks[13], (DEPTH, N_DIR, HEADS_B), f32, 1.0, 16.0)),
        'dt_bias': dt + jnp.log(-jnp.expm1(-dt)),
        'onorm_g': 1.0 + nrm(ks[15], (DEPTH, HEAD_DIM_B), 0.01),
        'w_out': nrm(ks[16], (DEPTH, D_MIX, D_MODEL), D_MIX ** -0.5),
        'norm2_g': 1.0 + nrm(ks[17], (DEPTH, D_MODEL), 0.01),
        'w_group': nrm(ks[18], (DEPTH, D_MODEL, N_GROUPS), D_MODEL ** -0.5),
        'b_group': nrm(ks[19], (DEPTH, N_GROUPS), 0.01),
        'w_router': nrm(ks[20], (DEPTH, D_MODEL, N_EXPERTS), D_MODEL ** -0.5),
        'b_router': nrm(ks[21], (DEPTH, N_EXPERTS), 0.01),
        'w_gate_up': nrm(ks[22], (DEPTH, N_EXPERTS, D_MODEL, 2 * D_EXPERT), D_MODEL ** -0.5),
        'w_down': nrm(ks[23], (DEPTH, N_EXPERTS, D_EXPERT, D_MODEL), D_EXPERT ** -0.5),
        'final_g': 1.0 + nrm(ks[24], (D_MODEL,), 0.01),
    }


def reference(x, c, ctx, c_ctx, w_ada, b_ada, norm1_g, w_in, ln_a_g, ln_a_b, w_spatial, b_spatial,
              conv_qkv, a_log, dt_bias, onorm_g, w_out, norm2_g, w_group, b_group, w_router, b_router,
              w_gate_up, w_down, final_g):
    B_, L_, _ = x.shape
    ROWS = L_ // GRID_W
    n_lat_chunks = ROWS // ROWS_PER_CHUNK
    n_ctx_chunks = ctx.shape[1] // CHUNK_A
    s_zero = jnp.zeros((N_DIR * B_ * HEADS_B, HEAD_DIM_B, HEAD_DIM_B), jnp.float32)
    h_ctx = ctx
    for l in range(DEPTH):
        last = l == DEPTH - 1
        sh1, sc1, gt1, sh2, sc2, gt2 = modulation(c, w_ada[l], b_ada[l])
        csh1, csc1, cgt1, csh2, csc2, cgt2 = modulation(c_ctx, w_ada[l], b_ada[l])

        p_lat = (rmsnorm(x, norm1_g[l]) * (1.0 + sc1) + sh1) @ w_in[l]
        p_ctx = (rmsnorm(h_ctx, norm1_g[l]) * (1.0 + csc1) + csh1) @ w_in[l]
        yb_ctx, s_ctx = delta_mixer(p_ctx[..., 2 * D_A:], s_zero, conv_qkv[l], a_log[l], dt_bias[l], onorm_g[l])
        yb_lat, _ = delta_mixer(p_lat[..., 2 * D_A:], s_ctx, conv_qkv[l], a_log[l], dt_bias[l], onorm_g[l])
        ya_lat = chunk_mlp_mixer(p_lat[..., :2 * D_A], n_lat_chunks, ln_a_g[l], ln_a_b[l], w_spatial[l], b_spatial[l])
        x = x + gt1 * (jnp.concatenate([ya_lat, yb_lat], axis=-1) @ w_out[l])
        if not last:
            ya_ctx = chunk_mlp_mixer(p_ctx[..., :2 * D_A], n_ctx_chunks, ln_a_g[l], ln_a_b[l], w_spatial[l], b_spatial[l])
            h_ctx = h_ctx + cgt1 * (jnp.concatenate([ya_ctx, yb_ctx], axis=-1) @ w_out[l])

        x = x + gt2 * hier_moe(rmsnorm(x, norm2_g[l]) * (1.0 + sc2) + sh2, w_group[l], b_group[l],
                               w_router[l], b_router[l], w_gate_up[l], w_down[l])
        if not last:
            h_ctx = h_ctx + cgt2 * hier_moe(rmsnorm(h_ctx, norm2_g[l]) * (1.0 + csc2) + csh2, w_group[l], b_group[l],
                                            w_router[l], b_router[l], w_gate_up[l], w_down[l])
    return rmsnorm(x, final_g)
```

```python
from contextlib import ExitStack
import os
import numpy as np
import concourse.bass as bass
import concourse.mybir as mybir
from concourse.bass_utils import run_bass_kernel_spmd

F32 = mybir.dt.float32
BF16 = mybir.dt.bfloat16
U8 = mybir.dt.uint8
AF = mybir.ActivationFunctionType
ALU = mybir.AluOpType
AX = mybir.AxisListType

N_CORES = 8
KLAT = int(os.environ.get('KLAT', '1'))
KNOQK = int(os.environ.get('KNOQK', '0'))
SAME_ENG_SYNC = int(os.environ.get('KSES', '1'))
EPS = 1e-6


class Buf:
    __slots__ = ("name", "last_w", "readers", "excl")

    def __init__(self, name, excl=False):
        self.name = name
        self.excl = excl
        self.last_w = None
        self.readers = []


class Prog:
    ENG = ("pe", "act", "dve", "pool", "sp")

    def __init__(self, nc):
        self.nc = nc
        self.es = ExitStack()
        self.ops = {e: [] for e in self.ENG}
        self.count = {e: 0 for e in self.ENG}
        self.sems = {}
        for e in self.ENG:
            self.sems[e] = self.es.enter_context(nc.semaphore("s_" + e))
        self.waited = {e: {} for e in self.ENG}
        self.dma_sem_cnt = {}
        self.nbuf = 0
        self.capture = None

    def sbuf(self, name, shape, dtype=F32):
        return self.es.enter_context(self.nc.sbuf_tensor(name, list(shape), dtype))

    def psum(self, name, shape, dtype=F32):
        return self.es.enter_context(self.nc.psum_tensor(name, list(shape), dtype))

    def buf(self, name=None, excl=False):
        self.nbuf += 1
        return Buf(name or f"b{self.nbuf}", excl)

    def dma_sem(self, name):
        key = "d_" + name
        self.sems[key] = self.es.enter_context(self.nc.semaphore(key))
        self.dma_sem_cnt[key] = 0
        return key

    def _deps(self, reads, writes):
        toks = []
        for b in reads:
            if b.last_w is not None:
                toks.append(b.last_w)
        for b in writes:
            if b.last_w is not None:
                toks.append(b.last_w)
            toks.extend(b.readers)
        return toks

    def _emit_waits(self, eng, toks):
        need = {}
        for (k, v) in toks:
            if k == eng and (eng in ("pe", "sp") or not SAME_ENG_SYNC):
                continue
            if v > need.get(k, 0):
                need[k] = v
        for k, v in need.items():
            if self.waited[eng].get(k, 0) >= v:
                continue
            self.waited[eng][k] = v
            self.ops[eng].append(("wait", self.sems[k], v))

    def _commit(self, tok, reads, writes):
        for b in writes:
            b.last_w = tok
            b.readers = []
        for b in reads:
            b.readers.append(tok)

    def op(self, eng, fn, reads=(), writes=()):
        if self.capture is not None:
            self.capture.append(("op", eng, fn, list(reads), list(writes)))
            return None
        writes = [b for b in writes if b is not None] + [b for b in reads if b is not None and b.excl]
        reads = [b for b in reads if b is not None and not b.excl]
        self._emit_waits(eng, self._deps(reads, writes))
        self.count[eng] += 1
        tok = (eng, self.count[eng])
        self.ops[eng].append(("op", fn, self.sems[eng], 1))
        self._commit(tok, reads, writes)
        return tok

    def dma(self, queue, semkey, out_ap, in_ap, reads=(), writes=()):
        if self.capture is not None:
            self.capture.append(("dma", queue, semkey, out_ap, in_ap, list(reads), list(writes)))
            return None
        reads = [b for b in reads if b is not None]
        writes = [b for b in writes if b is not None]
        self._emit_waits(queue, self._deps(reads, writes))
        if self.dma_sem_cnt[semkey] > 0:
            self._emit_waits(queue, [(semkey, self.dma_sem_cnt[semkey])])
        self.dma_sem_cnt[semkey] += 16
        tok = (semkey, self.dma_sem_cnt[semkey])

        def fn(e, out_ap=out_ap, in_ap=in_ap):
            return e.dma_start(out=out_ap, in_=in_ap)
        self.ops[queue].append(("op", fn, self.sems[semkey], 16))
        self._commit(tok, reads, writes)
        return tok

    def replay_merged(self, A, B):
        la, lb = len(A), len(B)
        ia = ib = 0
        while ia < la or ib < lb:
            if ib >= lb or (ia < la and ia * lb <= ib * la):
                it = A[ia]; ia += 1
            else:
                it = B[ib]; ib += 1
            if it[0] == "op":
                self.op(it[1], it[2], it[3], it[4])
            else:
                self.dma(it[1], it[2], it[3], it[4], it[5], it[6])

    def barrier(self):
        toks = [(e, self.count[e]) for e in self.ENG if e != "sp" and self.count[e] > 0]
        toks += [(k, v) for k, v in self.dma_sem_cnt.items() if v > 0]
        for e in self.ENG:
            self._emit_waits(e, toks)

    def emit(self):
        nc = self.nc
        P = self
        with nc.Block() as block:
            def run(e, engine):
                for item in P.ops[e]:
                    if item[0] == "wait":
                        engine.wait_ge(item[1], item[2])
                    else:
                        _, fn, sem, inc = item
                        fn(engine).then_inc(sem, inc)

            @block.sync
            def _(eng):
                run("sp", eng)

            @block.tensor
            def _(eng):
                run("pe", eng)

            @block.scalar
            def _(eng):
                run("act", eng)

            @block.vector
            def _(eng):
                run("dve", eng)

            @block.gpsimd
            def _(eng):
                run("pool", eng)

    def close(self):
        self.es.close()


def I(name, *a, **kw):
    return lambda e: getattr(e, name)(*a, **kw)


def build(NB=2, dbg=False, stop=99, KSTEPS=36, KSUB=99):
    nc = bass.Bass("TRN2", target_bir_lowering=False)

    def din(name, shape):
        return nc.dram_tensor(name, list(shape), F32, kind="ExternalInput").ap()
    x_d = din("x", [NB, 2048, 1024]); ctx_d = din("ctx", [NB, 256, 1024]); c_d = din("c", [NB, 1024])
    cctx_d = din("c_ctx", [1, 1024]); wada_d = din("w_ada", [1024, 6144]); bada_d = din("b_ada", [1, 6144])
    n1g_d = din("norm1_g", [1, 1024]); win_d = din("w_in", [1024, 3088]); lng_d = din("ln_a_g", [1, 512])
    lnb_d = din("ln_a_b", [1, 512]); wsp_d = din("w_spatial", [4, 128, 128]); bsp_d = din("b_spatial", [4, 128])
    conv_d = din("conv_qkv", [5, 1536]); alog_d = din("a_log", [1, 8]); dtb_d = din("dt_bias", [1, 8])
    ong_d = din("onorm_g", [1, 128]); wout_d = din("w_out", [1024, 1024]); n2g_d = din("norm2_g", [1, 1024])
    wgrp_d = din("w_group", [1024, 4]); bgrp_d = din("b_group", [1, 4]); wrt_d = din("w_router", [1024, 16])
    brt_d = din("b_router", [1, 16]); wgu_d = din("w_gate_up", [16, 1024, 1024]); wdn_d = din("w_down", [16, 512, 1024])
    fg_d = din("final_g", [1, 1024])
    out_d = nc.dram_tensor("out", [NB, 2048, 1024], F32, kind="ExternalOutput").ap()
    dbg_d = nc.dram_tensor("dbg", [128, 16, 1024], F32, kind="ExternalOutput").ap() if dbg else None

    P = Prog(nc)
    ARENA = 192 * 1024
    arena = P.sbuf("arena", [128, ARENA], U8)
    pers = P.sbuf("pers", [128, 15 * 1024], U8)
    banks = [P.psum(f"bank{i}", [128, 512]) for i in range(8)]

    def V(base, off, shape, dt, parts=128):
        esz = 2 if dt == BF16 else 4
        n = 1
        for s in shape:
            n *= s
        ap = base[0:parts, off:off + n * esz].bitcast(dt)
        if len(shape) == 2:
            ap = ap.rearrange("p (a b) -> p a b", a=shape[0])
        elif len(shape) == 3:
            ap = ap.rearrange("p (a b c) -> p a b c", a=shape[0], b=shape[1])
        return ap

    class Alloc:
        def __init__(self, base, size):
            self.base, self.size, self.off = base, size, 0

        def __call__(self, shape, dt, parts=128):
            esz = 2 if dt == BF16 else 4
            n = esz
            for s in shape:
                n *= s
            n = (n + 31) // 32 * 32
            off = self.off
            self.off += n
            assert self.off <= self.size, (self.off, self.size)
            return V(self.base, off, shape, dt, parts)

    PA = Alloc(pers, 15 * 1024)
    KB = 1024
    sem_ld = P.dma_sem("ld")
    ident = PA([128], F32); ident_bf = PA([128], BF16); ones_bf = PA([128], BF16)
    m_ui = PA([128], F32); m_us = PA([128], F32); m_li = PA([128], F32); m_ls = PA([128], F32); m_one = PA([128], F32)
    m_ubd = V(arena, 150 * 1024, [128], F32); m_ux = V(arena, 150 * 1024 + 512, [128], F32); m_lbd = V(arena, 150 * 1024 + 1024, [128], F32); m_lx = V(arena, 150 * 1024 + 1536, [128], F32)
    mb_ubd = PA([128], BF16); mb_ux = PA([128], BF16); mb_lbd = PA([128], BF16); mb_lx = PA([128], BF16)
    b_const = P.buf("const")

    def pool_op(fn, reads=(), writes=()):
        return P.op("pool", fn, reads, writes)

    def mk_mask(ap, pattern_step, chmul, cmp):
        pool_op(lambda e: e.memset(ap, 1.0), writes=[b_const])
        pool_op(lambda e: e.affine_select(out=ap, in_=ap, pattern=[[pattern_step, 128]], compare_op=cmp, fill=0.0,
                                          base=0, channel_multiplier=chmul), reads=[b_const], writes=[b_const])
    pool_op(lambda e: e.memset(ident, 0.0), writes=[b_const])
    pool_op(lambda e: e.affine_select(out=ident, in_=ident, pattern=[[-1, 128]], compare_op=ALU.not_equal, fill=1.0,
                                      base=0, channel_multiplier=1), reads=[b_const], writes=[b_const])
    pool_op(lambda e: e.tensor_copy(out=ident_bf, in_=ident), reads=[b_const], writes=[b_const])
    pool_op(lambda e: e.memset(ones_bf, 1.0), writes=[b_const])
    pool_op(lambda e: e.memset(m_one, 1.0), writes=[b_const])
    mk_mask(m_ui, 1, -1, ALU.is_ge)
    mk_mask(m_us, 1, -1, ALU.is_gt)
    mk_mask(m_li, -1, 1, ALU.is_ge)
    mk_mask(m_ls, -1, 1, ALU.is_gt)
    pool_op(lambda e: e.tensor_copy(out=m_ubd, in_=m_us), reads=[b_const], writes=[b_const])
    pool_op(lambda e: e.memset(m_ubd[0:64, 64:128], 0.0), reads=[b_const], writes=[b_const])
    pool_op(lambda e: e.memset(m_ux, 0.0), writes=[b_const])
    pool_op(lambda e: e.memset(m_ux[0:64, 64:128], 1.0), reads=[b_const], writes=[b_const])
    pool_op(lambda e: e.tensor_copy(out=m_lbd, in_=m_ls), reads=[b_const], writes=[b_const])
    pool_op(lambda e: e.memset(m_lbd[64:128, 0:64], 0.0), reads=[b_const], writes=[b_const])
    pool_op(lambda e: e.memset(m_lx, 0.0), writes=[b_const])
    pool_op(lambda e: e.memset(m_lx[64:128, 0:64], 1.0), reads=[b_const], writes=[b_const])

    mb_ui = PA([128], BF16); mb_us = PA([128], BF16); mb_li = PA([128], BF16); mb_ls = PA([128], BF16)
    for dst_, src_ in ((mb_ui, m_ui), (mb_us, m_us), (mb_li, m_li), (mb_ls, m_ls), (mb_ubd, m_ubd), (mb_ux, m_ux), (mb_lbd, m_lbd), (mb_lx, m_lx)):
        pool_op(lambda e, dst_=dst_, src_=src_: e.tensor_copy(out=dst_, in_=src_), reads=[b_const], writes=[b_const])
    eps_t = PA([1], F32)
    pool_op(lambda e: e.memset(eps_t, EPS), writes=[b_const])
    negA = PA([8], F32); dtb_bc = PA([8], F32); onorm_bc = PA([128], F32)
    lng_bc = PA([512], F32); lnb_bc = PA([512], F32)
    wsT = PA([4, 128], BF16); bsT = PA([4], F32); convw = PA([12, 5], F32)
    Wr32 = PA([8, 20], F32); brt_bc = PA([20], F32)
    modT = PA([48, 4], F32)
    b_par = P.buf("params")

    def load(dst, src, reads=(), writes=(), q="sp"):
        return P.dma(q, sem_ld, dst, src, reads=reads, writes=list(writes))

    load(negA, alog_d.partition_broadcast(128), writes=[b_par])
    load(dtb_bc, dtb_d.partition_broadcast(128), writes=[b_par])
    load(onorm_bc, ong_d.partition_broadcast(128), writes=[b_par])
    load(lng_bc, lng_d.partition_broadcast(128), writes=[b_par])
    load(lnb_bc, lnb_d.partition_broadcast(128), writes=[b_par])
    load(brt_bc[:, 0:4], bgrp_d.partition_broadcast(128), writes=[b_par])
    load(brt_bc[:, 4:20], brt_d.partition_broadcast(128), writes=[b_par])
    load(Wr32[:, :, 0:4], wgrp_d.rearrange("(kc p) n -> p kc n", p=128), writes=[b_par])
    load(Wr32[:, :, 4:20], wrt_d.rearrange("(kc p) n -> p kc n", p=128), writes=[b_par])
    P.op("act", lambda e: e.activation(out=negA, in_=negA, func=AF.Exp), reads=[b_par], writes=[b_par])
    P.op("dve", lambda e: e.tensor_scalar(out=negA, in0=negA, scalar1=-1.0, scalar2=None, op0=ALU.mult), reads=[b_par], writes=[b_par])

    A0 = Alloc(arena, ARENA)
    b_tmp = P.buf("setup_tmp")
    b_ps = [P.buf(f"bank{i}", excl=True) for i in range(8)]
    wsp_sb = A0([4, 128], F32); bsp_sb = A0([128], F32, parts=4); conv_sb = A0([1536], F32, parts=5)
    load(wsp_sb, wsp_d.rearrange("h i j -> i h j"), writes=[b_tmp])
    load(bsp_sb, bsp_d, writes=[b_tmp])
    load(conv_sb, conv_d, writes=[b_tmp])
    for h in range(4):
        P.op("pe", lambda e, h=h: e.transpose(out=banks[0][:, h * 128:(h + 1) * 128], in_=wsp_sb[:, h, :], identity=ident),
             reads=[b_tmp, b_const], writes=[b_ps[0]])
    P.op("act", lambda e: e.activation(out=wsT, in_=banks[0][:, 0:512].rearrange("p (a b) -> p a b", a=4), func=AF.Copy),
         reads=[b_ps[0]], writes=[b_par])
    P.op("pe", lambda e: e.transpose(out=banks[1][:, 0:4], in_=bsp_sb, identity=ident[0:4, 0:4]), reads=[b_tmp, b_const], writes=[b_ps[1]])
    P.op("act", lambda e: e.activation(out=bsT, in_=banks[1][:, 0:4], func=AF.Copy), reads=[b_ps[1]], writes=[b_par])
    for cc in range(12):
        P.op("pe", lambda e, cc=cc: e.transpose(out=banks[2][:, cc * 8:cc * 8 + 5], in_=conv_sb[:, cc * 128:(cc + 1) * 128],
                                                identity=ident[0:5, 0:5]), reads=[b_tmp, b_const], writes=[b_ps[2]])
    P.op("act", lambda e: e.activation(out=convw, in_=banks[2][:, 0:96].rearrange("p (a b) -> p a b", a=12)[:, :, 0:5], func=AF.Copy),
         reads=[b_ps[2]], writes=[b_par])

    cT = A0([3, 8], F32); cTb = A0([3, 8], BF16)
    for j in range(NB):
        load(cT[:, j, :], c_d[j].rearrange("(p kc) -> p kc", kc=8), writes=[b_tmp])
    if NB < 2:
        P.op("dve", lambda e: e.memset(cT[:, 1, :], 0.0), writes=[b_tmp])
    load(cT[:, 2, :], cctx_d[0].rearrange("(p kc) -> p kc", kc=8), writes=[b_tmp])
    P.op("act", lambda e: e.activation(out=cTb, in_=cT, func=AF.Silu), reads=[b_tmp], writes=[b_tmp])
    wada_v = wada_d.rearrange("(p kc) n -> p kc n", kc=8)
    wa = [A0([8, 512], BF16) for _ in range(2)]
    b_wa = [P.buf() for _ in range(2)]
    sem_wa = [P.dma_sem(f"wa{i}") for i in range(2)]
    for nb_ in range(12):
        s = nb_ % 2
        P.dma("pool", sem_wa[s], wa[s], wada_v[:, :, nb_ * 512:(nb_ + 1) * 512], writes=[b_wa[s]])
        for c4 in range(4):
            ch = nb_ * 4 + c4
            for kc in range(8):
                P.op("pe", lambda e, s=s, c4=c4, kc=kc, ch=ch: e.matmul(banks[3][:, ch * 4:ch * 4 + 3], wa[s][:, kc, c4 * 128:(c4 + 1) * 128],
                                                                      cTb[:, :, kc], start=(kc == 0), stop=(kc == 7)),
                     reads=[b_wa[s], b_tmp], writes=[b_ps[3]])
    P.op("act", lambda e: e.activation(out=modT[:, :, 0:3], in_=banks[3][:, 0:192].rearrange("p (a b) -> p a b", a=48)[:, :, 0:3], func=AF.Copy),
         reads=[b_ps[3]], writes=[b_par])
    P.barrier()

    def make_bc(dst, j, which, b_dst, tmp_bias, b_tmpb, g_bc=None, b_g=None):
        load(tmp_bias, bada_d[:, which * 1024:(which + 1) * 1024].partition_broadcast(128), writes=[b_tmpb])
        for c8 in range(8):
            ch = which * 8 + c8
            bk = 6 + c8 // 4
            P.op("pe", lambda e, c8=c8, ch=ch, bk=bk: e.matmul(banks[bk][:, (c8 % 4) * 128:(c8 % 4 + 1) * 128],
                                                               modT[:, ch, j:j + 1].to_broadcast([128, 128]), ident, start=True, stop=True),
                 reads=[b_par, b_const], writes=[b_ps[bk]])
        for hf in range(2):
            P.op("dve", lambda e, hf=hf: e.tensor_tensor(out=dst[:, hf * 512:(hf + 1) * 512], in0=banks[6 + hf][:, :],
                                                        in1=tmp_bias[:, hf * 512:(hf + 1) * 512], op=ALU.add),
                 reads=[b_ps[6 + hf], b_tmpb], writes=[b_dst])
        if g_bc is not None:
            P.op("dve", lambda e: e.scalar_tensor_tensor(out=dst, in0=dst, scalar=1.0, in1=g_bc, op0=ALU.add, op1=ALU.mult),
                 reads=[b_dst, b_g], writes=[b_dst])

    def rstd_from_ss(ss, n, b_s):
        P.op("dve", lambda e: e.tensor_scalar(out=ss, in0=ss, scalar1=1.0 / n, scalar2=EPS, op0=ALU.mult, op1=ALU.add), reads=[b_s], writes=[b_s])
        P.op("act", lambda e: e.activation(out=ss, in_=ss, func=AF.Sqrt), reads=[b_s], writes=[b_s])
        P.op("dve", lambda e: e.reciprocal(out=ss, in_=ss), reads=[b_s], writes=[b_s])

    sem_x = [P.dma_sem(f"x{i}") for i in range(2)]
    sem_w = [P.dma_sem(f"w{i}") for i in range(4)]
    sem_o = [P.dma_sem(f"o{i}") for i in range(2)]
    out_toks = []

    def do_batch(b):
        A = Alloc(arena, ARENA)
        RA = 0
        x_res = V(arena, RA, [16, 1024], F32)
        b_xres = [P.buf(f"xres{t}") for t in range(16)]
        xT = V(arena, RA, [8, 2304], BF16)
        b_xT = [P.buf(f"xT{t}") for t in range(18)]
        CT = RA + 36 * KB
        RB = 64 * KB
        qkvT = V(arena, RB, [12, 2304], BF16)
        b_qkv = [[P.buf() for _ in range(18)] for _ in range(12)]
        RC = RB + 54 * KB
        o_acc = V(arena, RC, [16, 512], F32)
        b_oacc = [[P.buf() for _ in range(4)] for _ in range(16)]
        Wqkv = V(arena, RC, [8, 1536], BF16)
        b_wqkv = P.buf()
        RD = RC + 32 * KB
        AD = Alloc(arena[:, RD:ARENA], ARENA - RD)
        gb = AD([18, 16], F32); cumE = AD([18, 40], F32); negE = AD([18, 40], F32)
        b_gb = [P.buf() for _ in range(18)]
        bcA = AD([1024], F32); bcS = AD([1024], F32); bcG = AD([1024], F32)
        b_bcA, b_bcS, b_bcG = P.buf(), P.buf(), P.buf()
        Wab = AD([8, 16], BF16); b_wab = P.buf()
        xin = [AD([1024], F32) for _ in range(2)]; b_xin = [P.buf() for _ in range(2)]
        xmb = AD([1024], BF16); b_xmb = P.buf()
        small = AD([16], F32); b_small0 = P.buf()
        g1_bc = xin[0]
        load(g1_bc, n1g_d.partition_broadcast(128), writes=[b_xin[0]])
        P.dma("pool", sem_w[0], Wab, win_d.rearrange("(kc p) n -> p kc n", p=128)[:, :, 3072:3088], writes=[b_wab])

        def norm_mod_T(src, b_src, A_bc, S_bc, dstT, b_dstT, bank, junk, b_junk, f32T=None, xm_f32=None, tmps=None):
            small_, b_small, junk_f32, b_jf = tmps if tmps is not None else (small, b_small0, junk_f320, b_jf0)
            ss = small_[:, 0:1]
            P.op("act", lambda e: e.activation(out=junk, in_=src, func=AF.Square, accum_out=ss), reads=[b_src], writes=[b_junk, b_small])
            rstd_from_ss(ss, 1024.0, b_small)
            if xm_f32 is None:
                tmp = junk_f32
                P.op("dve", lambda e: e.scalar_tensor_tensor(out=tmp, in0=src, scalar=ss, in1=A_bc, op0=ALU.mult, op1=ALU.mult),
                     reads=[b_src, b_small, b_bcA], writes=[b_jf])
                P.op("dve", lambda e: e.tensor_tensor(out=junk, in0=tmp, in1=S_bc, op=ALU.add), reads=[b_jf, b_bcS], writes=[b_junk])
                pv = banks[bank][:, 0:512].bitcast(BF16)
                for kc in range(8):
                    P.op("pe", lambda e, kc=kc: e.transpose(out=pv[:, kc * 128:(kc + 1) * 128], in_=junk[:, kc * 128:(kc + 1) * 128], identity=ident_bf),
                         reads=[b_junk, b_const], writes=[b_ps[bank]])
                P.op("act", lambda e: e.activation(out=dstT, in_=pv.rearrange("p (a b) -> p a b", a=8), func=AF.Copy),
                     reads=[b_ps[bank]], writes=b_dstT)
            else:
                P.op("dve", lambda e: e.scalar_tensor_tensor(out=xm_f32, in0=src, scalar=ss, in1=A_bc, op0=ALU.mult, op1=ALU.mult),
                     reads=[b_src, b_small, b_bcA], writes=[b_jf])
                P.op("dve", lambda e: e.tensor_tensor(out=xm_f32, in0=xm_f32, in1=S_bc, op=ALU.add), reads=[b_jf, b_bcS], writes=[b_jf])
                for kc in range(8):
                    bk = bank + kc // 4
                    P.op("pe", lambda e, kc=kc, bk=bk: e.transpose(out=banks[bk][:, (kc % 4) * 128:(kc % 4 + 1) * 128],
                                                                   in_=xm_f32[:, kc * 128:(kc + 1) * 128], identity=ident),
                         reads=[b_jf, b_const], writes=[b_ps[bk]])
                for hf in range(2):
                    P.op("act", lambda e, hf=hf: e.activation(out=dstT[:, hf * 4:(hf + 1) * 4, :], in_=banks[bank + hf][:, :].rearrange("p (a b) -> p a b", a=4), func=AF.Copy),
                         reads=[b_ps[bank + hf]], writes=b_dstT)
                    P.op("dve", lambda e, hf=hf: e.tensor_copy(out=f32T[:, hf * 4:(hf + 1) * 4, :], in_=banks[bank + hf][:, :].rearrange("p (a b) -> p a b", a=4)),
                         reads=[b_ps[bank + hf]], writes=[b_f32T])

        junk_f320 = AD([1024], F32); b_jf0 = P.buf()
        junk_f32 = junk_f320; b_jf = b_jf0

        def p1_tile(t, ev):
            if t < 2:
                src_d = ctx_d[b, t * 128:(t + 1) * 128, :]
            else:
                src_d = x_d[b, (t - 2) * 128:(t - 1) * 128, :]
            xt = ev["xin"]; bk0, bk1, bk2 = ev["banks"]
            ghf_ = ev["ghf"]
            P.dma("sp", ev["sem"], xt, src_d, writes=[ev["b_xin"]])
            norm_mod_T(xt, ev["b_xin"], bcA, bcS, xT[:, :, t * 128:(t + 1) * 128], [b_xT[t]], bk0, ev["xmb"], ev["b_xmb"], tmps=ev["tmps"])
            for kc in range(8):
                P.op("pe", I("matmul", banks[bk1][:, 0:16], xT[:, kc, t * 128:(t + 1) * 128], Wab[:, kc, :], start=(kc == 0), stop=(kc == 7)),
                     reads=[b_xT[t], b_wab], writes=[b_ps[bk1]])
            g8 = gb[:, t, 0:8]
            P.op("dve", I("tensor_tensor", out=g8, in0=banks[bk1][:, 0:8], in1=dtb_bc, op=ALU.add), reads=[b_ps[bk1], b_par], writes=[b_gb[t]])
            P.op("act", I("activation", out=gb[:, t, 8:16], in_=banks[bk1][:, 8:16], func=AF.Sigmoid), reads=[b_ps[bk1]], writes=[b_gb[t]])
            P.op("act", I("activation", out=g8, in_=g8, func=AF.Exp), reads=[b_gb[t]], writes=[b_gb[t]])
            P.op("act", I("activation", out=g8, in_=g8, func=AF.Ln, bias=1.0), reads=[b_gb[t]], writes=[b_gb[t]])
            P.op("dve", I("tensor_tensor", out=g8, in0=g8, in1=negA, op=ALU.mult), reads=[b_gb[t], b_par], writes=[b_gb[t]])
            P.op("dve", I("tensor_copy", out=ghb[:, t, 0:8], in_=g8), reads=[b_gb[t]], writes=[b_gb[t]])
            P.op("dve", I("tensor_copy", out=ghf_, in_=ghb[:, t, 0:8]), reads=[b_gb[t]], writes=[b_gb[t]])
            P.op("dve", I("tensor_tensor", out=ghb[:, t, 8:16], in0=g8, in1=ghf_, op=ALU.subtract), reads=[b_gb[t]], writes=[b_gb[t]])
            for mi, mk in enumerate((m_ui, m_ls, m_li, m_us, m_one)):
                P.op("pe", I("matmul", banks[bk2][:, mi * 8:(mi + 1) * 8], mk, g8, start=True, stop=True), reads=[b_gb[t], b_const], writes=[b_ps[bk2]])
            P.op("act", I("activation", out=cumE[:, t, :], in_=banks[bk2][:, 0:40], func=AF.Exp), reads=[b_ps[bk2]], writes=[b_gb[t]])
            P.op("dve", I("tensor_scalar", out=negE[:, t, :], in0=cumE[:, t, :], scalar1=-1.0, scalar2=None, op0=ALU.mult), reads=[b_gb[t]], writes=[b_gb[t]])

        def phase1_tiles(tiles, j):
            make_bc(bcA, j, 1, b_bcA, xin[1], b_xin[1], g_bc=g1_bc, b_g=b_xin[0])
            make_bc(bcS, j, 0, b_bcS, xin[1], b_xin[1])
            for i in range(0, len(tiles), 2):
                P.capture = []
                p1_tile(tiles[i], envs1[0])
                A_ = P.capture
                P.capture = []
                p1_tile(tiles[i + 1], envs1[1])
                B_ = P.capture
                P.capture = None
                P.replay_merged(A_, B_)

        if stop <= 0:
            return
        _x2 = AD([1024], F32); _bx2 = P.buf()
        xin2 = [_x2, _x2]; b_xin2 = [_bx2, _bx2]
        ghb = AD([18, 16], BF16); ghf = AD([8], F32)
        E1 = Alloc(arena[:, RB:RB + 16 * KB], 16 * KB)
        envs1 = [
            {"xin": xin2[0], "b_xin": b_xin2[0], "sem": sem_x[0], "xmb": xmb, "b_xmb": b_xmb, "tmps": None, "ghf": ghf, "banks": (0, 1, 2)},
            {"xin": E1([1024], F32), "b_xin": P.buf(), "sem": sem_x[1], "xmb": E1([1024], BF16), "b_xmb": P.buf(),
             "tmps": (E1([16], F32), P.buf(), E1([1024], F32), P.buf()), "ghf": E1([8], F32), "banks": (3, 4, 5)},
        ]
        phase1_tiles([0, 1], 2)
        phase1_tiles(list(range(2, 18)), b)

        if stop <= 1:
            return
        P.dma("pool", sem_w[1], Wqkv, win_d.rearrange("(kc p) n -> p kc n", p=128)[:, :, 1024:2560], writes=[b_wqkv])
        C5 = Alloc(arena[:, CT:CT + 28 * KB], 28 * KB)
        PTb = [C5([2320], BF16) for _ in range(2)]; b_PTb = [P.buf() for _ in range(2)]
        ACC = C5([2304], F32); b_ACC = P.buf()
        SQB = C5([2304], BF16); b_SQB = P.buf()
        RNb = [C5([512], F32) for _ in range(2)]; b_RNb = [P.buf() for _ in range(2)]
        DG = C5([5, 128], BF16); b_DG = P.buf()
        for i_ in range(2):
            P.op("pool", I("memset", PTb[i_], 0.0), writes=[b_PTb[i_]])
        blocks = [(0, 256)] + [(256 + i * 512, 512) for i in range(4)]
        for cc in range(12):
            pi_ = cc % 2
            PT_ = PTb[pi_]; bPT = b_PTb[pi_]
            for tp in range(5):
                P.op("dve", I("tensor_scalar", out=DG[:, tp, :], in0=ident_bf, scalar1=convw[:, cc, tp:tp + 1], scalar2=None, op0=ALU.mult),
                     reads=[b_par, b_const], writes=[b_DG])
            for bi, (t0, n) in enumerate(blocks):
                bk = bi % 2
                tl = list(range(t0 // 128, (t0 + n) // 128))
                for kc in range(8):
                    P.op("pe", I("matmul", banks[bk][:, 0:n], Wqkv[:, kc, cc * 128:(cc + 1) * 128], xT[:, kc, t0:t0 + n], start=(kc == 0), stop=(kc == 7)),
                         reads=[b_wqkv] + [b_xT[t] for t in tl], writes=[b_ps[bk]])
                po = (2 + t0) if t0 < 256 else (262 + t0 - 256)
                P.op("act", I("activation", out=PT_[:, po:po + n], in_=banks[bk][:, 0:n], func=AF.Copy), reads=[b_ps[bk]], writes=[bPT])
            allq = [b_qkv[cc][t] for t in range(18)]
            for bi, (t0, n) in enumerate(blocks):
                bk = 2 + bi % 2
                po = (2 + t0) if t0 < 256 else (262 + t0 - 256)
                for tp in range(5):
                    P.op("pe", I("matmul", banks[bk][:, 0:n], DG[:, tp, :], PT_[:, po - 2 + tp:po - 2 + tp + n], start=(tp == 0), stop=(tp == 4)),
                         reads=[b_DG, bPT], writes=[b_ps[bk]])
                if cc >= 8:
                    P.op("act", I("activation", out=qkvT[:, cc, t0:t0 + n], in_=banks[bk][:, 0:n], func=AF.Silu), reads=[b_ps[bk]], writes=allq)
                else:
                    P.op("act", I("activation", out=ACC[:, t0:t0 + n], in_=banks[bk][:, 0:n], func=AF.Silu), reads=[b_ps[bk]], writes=[b_ACC])
                    P.op("dve", I("tensor_tensor", out=SQB[:, t0:t0 + n], in0=ACC[:, t0:t0 + n], in1=ACC[:, t0:t0 + n], op=ALU.mult), reads=[b_ACC], writes=[b_SQB])
            if cc < 8:
                sc = (128.0 ** -0.5) if cc < 4 else 1.0
                for bi, (t0, n) in enumerate(blocks):
                    bk = 4 + bi % 2
                    ri = bi % 2
                    P.op("pe", I("matmul", banks[bk][:, 0:n], ones_bf, SQB[:, t0:t0 + n], start=True, stop=True), reads=[b_SQB, b_const], writes=[b_ps[bk]])
                    P.op("act", I("activation", out=RNb[ri][:, 0:n], in_=banks[bk][:, 0:n], func=AF.Sqrt, bias=eps_t), reads=[b_ps[bk], b_const], writes=[b_RNb[ri]])
                    P.op("dve", I("reciprocal", out=RNb[ri][:, 0:n], in_=RNb[ri][:, 0:n]), reads=[b_RNb[ri]], writes=[b_RNb[ri]])
                    P.op("dve", I("scalar_tensor_tensor", out=qkvT[:, cc, t0:t0 + n], in0=ACC[:, t0:t0 + n], scalar=sc, in1=RNb[ri][:, 0:n], op0=ALU.mult, op1=ALU.mult),
                         reads=[b_ACC, b_RNb[ri]], writes=allq)
        P.barrier()
        if stop <= 5:
            return
        SA = Alloc(arena[:, RA:RA + 64 * KB], 64 * KB)
        seqs = []
        for d in range(2):
            for h in range(4):
                q = {"d": d, "h": h}
                for nm in ("gmask", "decT", "dmbd", "dmx", "dmq", "vtok"):
                    q[nm] = SA([128], F32)
                for nm in ("Mbd", "Nbd", "XT", "QKd", "R0", "R1", "RT0", "RT1", "P0", "P1", "PT0", "PT1", "ZT", "AinvT", "kd", "r", "vnew", "Sbf"):
                    q[nm] = SA([128], BF16)
                q["S"] = SA([128], F32)
                q["b"] = {}
                seqs.append(q)

        def sb(q, nm):
            if nm not in q["b"]:
                q["b"][nm] = P.buf()
            return q["b"][nm]
        slots = [(i % 8, i // 8) for i in range(32)]
        b_slot = [b_ps[i % 8] for i in range(32)]
        slot_i = [0]

        def pslot():
            i = slot_i[0] % 32
            slot_i[0] += 1
            bk, c = slots[i]
            if os.environ.get('KDBG') and slot_i[0] <= 24:
                print('pslot', i, bk, c, banks[bk][:, c * 128:(c + 1) * 128].offset, banks[bk][:, c * 128:(c + 1) * 128].bitcast(BF16)[:, 0:128].offset)
            return banks[bk][:, c * 128:(c + 1) * 128], b_slot[i]

        for q in seqs:
            P.op("pool", I("memset", q["S"], 0.0), writes=[sb(q, "S")])
            P.op("pool", I("memset", q["Sbf"], 0.0), writes=[sb(q, "Sbf")])
        orders = [list(range(18)), [1, 0] + list(range(17, 1, -1))]
        o_written = [[False] * 4 for _ in range(16)]

        def mmq(out, lhsT, rhs, reads, bw):
            P.op("pe", lambda e: e.matmul(out, lhsT, rhs, start=True, stop=True), reads=reads, writes=[bw])

        def scan_step(step2):
            step = step2 // 2
            st = []
            for q in seqs:
                d, h = q["d"], q["h"]
                if d != step2 % 2:
                    continue
                t = orders[d][step]
                if KLAT == 0:
                    t = t % 2
                st.append((q, d, h, t, (t >= 2) and KNOQK == 0))
            tsl = lambda t: slice(t * 128, (t + 1) * 128)
            for (q, d, h, t, lat) in st:
                kT = qkvT[:, 4 + h, tsl(t)]; qT = qkvT[:, h, tsl(t)]; vT = qkvT[:, 8 + h, tsl(t)]
                q["kT"], q["qT"] = kT, qT
                q["bk"], q["bq"], q["bv"] = b_qkv[4 + h][t], b_qkv[h][t], b_qkv[8 + h][t]
                col = d * 4 + h
                q["beta"] = gb[:, t, 8 + col:9 + col]
                q["gcol"] = gb[:, t, col:col + 1]
                q["eg"] = cumE[:, t, (h if d == 0 else 20 + h):(h if d == 0 else 20 + h) + 1]
                q["neg"] = negE[:, t, (h if d == 0 else 20 + h):(h if d == 0 else 20 + h) + 1]
                q["ekd"] = cumE[:, t, (8 + h if d == 0 else 28 + h):(8 + h if d == 0 else 28 + h) + 1]
                q["gl"] = cumE[:, t, 32 + col:33 + col]
                q["bg"] = b_gb[t]
                ks, q["bks"] = pslot(); q["ks"] = ks.bitcast(BF16)[:, 0:128]
                vs, q["bvs"] = pslot(); q["vs"] = vs.bitcast(BF16)[:, 0:128]
                P.op("pe", I("transpose", out=q["ks"], in_=kT, identity=ident_bf), reads=[q["bk"], b_const], writes=[q["bks"]])
                P.op("pe", I("transpose", out=q["vs"], in_=vT, identity=ident_bf), reads=[q["bv"], b_const], writes=[q["bvs"]])
                ml = mb_ls if d == 0 else mb_us
                gmv = q["gmask"].bitcast(BF16)
                q["gmh"], q["gml"] = gmv[:, 0:128], gmv[:, 128:256]
                P.op("dve", I("tensor_scalar", out=q["gmh"], in0=ml, scalar1=ghb[:, t, col:col + 1], scalar2=None, op0=ALU.mult),
                     reads=[q["bg"], b_const], writes=[sb(q, "gmask")])
                P.op("dve", I("tensor_scalar", out=q["gml"], in0=ml, scalar1=ghb[:, t, 8 + col:9 + col], scalar2=None, op0=ALU.mult),
                     reads=[q["bg"], b_const], writes=[sb(q, "gmask")])
            if KSUB < 2:
                return
            for (q, d, h, t, lat) in st:
                P.op("act", I("activation", out=q["vtok"], in_=q["vs"], func=AF.Copy), reads=[q["bvs"]], writes=[sb(q, "vtok")])
                P.op("act", I("activation", out=q["kd"], in_=q["ks"], func=AF.Copy, scale=q["ekd"]), reads=[q["bks"], q["bg"]], writes=[sb(q, "kd")])
            for (q, d, h, t, lat) in st:
                q["G"], q["bG"] = pslot()
                mmq(q["G"], q["kT"], q["kT"], [q["bk"]], q["bG"])
                if lat:
                    q["QK"], q["bQK"] = pslot()
                    mmq(q["QK"], q["kT"], q["qT"], [q["bk"], q["bq"]], q["bQK"])
            for (q, d, h, t, lat) in st:
                mr = mb_ui if d == 0 else mb_li
                q["df"], q["bdf"] = pslot()
                P.op("pe", I("matmul", q["df"], q["gmh"], mr, start=True, stop=False), reads=[sb(q, "gmask"), b_const], writes=[q["bdf"]])
                P.op("pe", I("matmul", q["df"], q["gml"], mr, start=False, stop=True), reads=[sb(q, "gmask"), b_const], writes=[q["bdf"]])
            if KSUB < 3:
                return
            for (q, d, h, t, lat) in st:
                P.op("act", I("activation", out=q["decT"], in_=q["df"], func=AF.Exp), reads=[q["bdf"]], writes=[sb(q, "decT")])
            if KSUB < 5:
                return
            for (q, d, h, t, lat) in st:
                mbd, mx, mi = (mb_ubd, mb_ux, mb_ui) if d == 0 else (mb_lbd, mb_lx, mb_li)
                t1 = q["dmbd"].bitcast(BF16)[:, 0:128]
                P.op("dve", I("scalar_tensor_tensor", out=t1, in0=q["G"], scalar=q["beta"], in1=q["decT"], op0=ALU.mult, op1=ALU.mult),
                     reads=[q["bG"], q["bg"], sb(q, "decT")], writes=[sb(q, "dmbd")])
                P.op("dve", I("tensor_tensor", out=q["Mbd"], in0=t1, in1=mbd, op=ALU.mult), reads=[sb(q, "dmbd"), b_const], writes=[sb(q, "Mbd")])
                P.op("dve", I("tensor_tensor", out=q["XT"], in0=t1, in1=mx, op=ALU.mult), reads=[sb(q, "dmbd"), b_const], writes=[sb(q, "XT")])
                if lat:
                    t2 = q["dmq"].bitcast(BF16)[:, 0:128]
                    P.op("dve", I("tensor_tensor", out=t2, in0=q["QK"], in1=q["decT"], op=ALU.mult), reads=[q["bQK"], sb(q, "decT")], writes=[sb(q, "dmq")])
                    P.op("dve", I("tensor_tensor", out=q["QKd"], in0=t2, in1=mi, op=ALU.mult), reads=[sb(q, "dmq"), b_const], writes=[sb(q, "QKd")])
            if KSUB < 6:
                return
            for (q, d, h, t, lat) in st:
                ns, q["bns"] = pslot(); q["ns"] = ns.bitcast(BF16)[:, 0:128]
                P.op("pe", I("transpose", out=q["ns"], in_=q["Mbd"], identity=ident_bf), reads=[sb(q, "Mbd"), b_const], writes=[q["bns"]])
                P.op("act", I("activation", out=q["Nbd"], in_=q["ns"], func=AF.Copy), reads=[q["bns"]], writes=[sb(q, "Nbd")])
                P.op("dve", I("tensor_tensor", out=q["R0"], in0=ident_bf, in1=q["Mbd"], op=ALU.subtract), reads=[sb(q, "Mbd"), b_const], writes=[sb(q, "R0")])
                q["cur"] = ("Mbd", "Nbd", "R0", "RT0")
            if KSUB < 7:
                return
            for lvl in range(5):
                pn, ptn = ("P0", "PT0") if lvl % 2 == 0 else ("P1", "PT1")
                rn_ = "R1" if lvl % 2 == 0 else "R0"
                last = lvl == 4
                for (q, d, h, t, lat) in st:
                    pw, pwt, r_, _ = q["cur"]
                    if not last:
                        q["p2"], q["bp2"] = pslot()
                        mmq(q["p2"], q[pwt], q[pw], [sb(q, pw), sb(q, pwt)], q["bp2"])
                    q["p2t"], q["bp2t"] = pslot()
                    mmq(q["p2t"], q[pw], q[pwt], [sb(q, pw), sb(q, pwt)], q["bp2t"])
                for (q, d, h, t, lat) in st:
                    if not last:
                        P.op("act", I("activation", out=q[pn], in_=q["p2"], func=AF.Copy), reads=[q["bp2"]], writes=[sb(q, pn)])
                    P.op("act", I("activation", out=q[ptn], in_=q["p2t"], func=AF.Copy), reads=[q["bp2t"]], writes=[sb(q, ptn)])
                for (q, d, h, t, lat) in st:
                    pw, pwt, r_, _ = q["cur"]
                    q["ra"], q["bra"] = pslot()
                    mmq(q["ra"], q[ptn], q[r_], [sb(q, r_), sb(q, ptn)], q["bra"])
                for (q, d, h, t, lat) in st:
                    pw, pwt, r_, _ = q["cur"]
                    P.op("dve", I("tensor_tensor", out=q[rn_], in0=q["ra"], in1=q[r_], op=ALU.add),
                         reads=[q["bra"], sb(q, r_)], writes=[sb(q, rn_)])
                    q["cur"] = (pn, ptn, rn_, None)
            for (q, d, h, t, lat) in st:
                _, _, r_, _ = q["cur"]
                rts, q["brts"] = pslot(); q["rts"] = rts.bitcast(BF16)[:, 0:128]
                P.op("pe", I("transpose", out=q["rts"], in_=q[r_], identity=ident_bf), reads=[sb(q, r_), b_const], writes=[q["brts"]])
            for (q, d, h, t, lat) in st:
                _, _, r_, _ = q["cur"]
                P.op("act", I("activation", out=q["RT0"], in_=q["rts"], func=AF.Copy), reads=[q["brts"]], writes=[sb(q, "RT0")])
                q["cur"] = (None, None, r_, "RT0")
            if KSUB < 8:
                return
            for (q, d, h, t, lat) in st:
                _, _, r_, rt_ = q["cur"]
                q["z"], q["bz"] = pslot()
                mmq(q["z"], q["XT"], q[rt_], [sb(q, "XT"), sb(q, rt_)], q["bz"])
            for (q, d, h, t, lat) in st:
                P.op("act", I("activation", out=q["ZT"], in_=q["z"], func=AF.Copy), reads=[q["bz"]], writes=[sb(q, "ZT")])
            for (q, d, h, t, lat) in st:
                _, _, r_, rt_ = q["cur"]
                q["w"], q["bw"] = pslot()
                mmq(q["w"], q["ZT"], q[r_], [sb(q, "ZT"), sb(q, r_)], q["bw"])
            for (q, d, h, t, lat) in st:
                _, _, r_, rt_ = q["cur"]
                P.op("dve", I("scalar_tensor_tensor", out=q["AinvT"], in0=q["w"], scalar=-1.0, in1=q[r_], op0=ALU.mult, op1=ALU.add),
                     reads=[q["bw"], sb(q, r_)], writes=[sb(q, "AinvT")])
            if KSUB < 9:
                return
            for (q, d, h, t, lat) in st:
                q["a"], q["ba"] = pslot()
                mmq(q["a"], q["kT"], q["Sbf"], [q["bk"], sb(q, "Sbf")], q["ba"])
            for (q, d, h, t, lat) in st:
                P.op("dve", I("scalar_tensor_tensor", out=q["r"], in0=q["a"], scalar=q["neg"], in1=q["vtok"], op0=ALU.mult, op1=ALU.add),
                     reads=[q["ba"], q["bg"], sb(q, "vtok")], writes=[sb(q, "r")])
            for (q, d, h, t, lat) in st:
                q["bb"], q["bbb"] = pslot()
                mmq(q["bb"], q["AinvT"], q["r"], [sb(q, "AinvT"), sb(q, "r")], q["bbb"])
            for (q, d, h, t, lat) in st:
                P.op("act", I("activation", out=q["vnew"], in_=q["bb"], func=AF.Copy, scale=q["beta"]), reads=[q["bbb"], q["bg"]], writes=[sb(q, "vnew")])
            for (q, d, h, t, lat) in st:
                if lat:
                    q["o1"], q["bo1"] = pslot()
                    mmq(q["o1"], q["qT"], q["Sbf"], [q["bq"], sb(q, "Sbf")], q["bo1"])
                    q["o2"], q["bo2"] = pslot()
                    mmq(q["o2"], q["QKd"], q["vnew"], [sb(q, "QKd"), sb(q, "vnew")], q["bo2"])
                q["sp"], q["bsp"] = pslot()
                mmq(q["sp"], q["kd"], q["vnew"], [sb(q, "kd"), sb(q, "vnew")], q["bsp"])
            for (q, d, h, t, lat) in st:
                if lat:
                    oa = o_acc[:, t - 2, h * 128:(h + 1) * 128]
                    bo = b_oacc[t - 2][h]
                    tmp = q["gmask"]
                    if not o_written[t - 2][h]:
                        P.op("act", I("activation", out=tmp, in_=q["o2"], func=AF.Copy), reads=[q["bo2"]], writes=[sb(q, "gmask")])
                        o_written[t - 2][h] = True
                    else:
                        P.op("dve", I("tensor_tensor", out=tmp, in0=q["o2"], in1=oa, op=ALU.add), reads=[q["bo2"], bo], writes=[sb(q, "gmask")])
                    P.op("dve", I("scalar_tensor_tensor", out=oa, in0=q["o1"], scalar=q["eg"], in1=tmp, op0=ALU.mult, op1=ALU.add),
                         reads=[q["bo1"], q["bg"], sb(q, "gmask")], writes=[bo])
                P.op("dve", I("scalar_tensor_tensor", out=q["S"], in0=q["S"], scalar=q["gl"], in1=q["sp"], op0=ALU.mult, op1=ALU.add),
                     reads=[sb(q, "S"), q["bg"], q["bsp"]], writes=[sb(q, "S")])
                P.op("act", I("activation", out=q["Sbf"], in_=q["S"], func=AF.Copy), reads=[sb(q, "S")], writes=[sb(q, "Sbf")])
        for step2 in range(min(36, KSTEPS)):
            scan_step(step2)
        P.barrier()

        if stop <= 6:
            return
        WB = Alloc(arena[:, RB:RB + 54 * KB], 54 * KB)
        WinA = WB([8, 1024], BF16); Wz = WB([8, 512], BF16); Wout = WB([8, 1024], BF16)
        b_w7 = P.buf()
        sets7 = []
        for i7 in range(2):
            d7 = {}
            if i7 == 0:
                d7["xTt"] = WB([8, 128], BF16); d7["u"] = WB([512], F32); d7["v"] = WB([512], F32); d7["vn"] = WB([512], BF16)
                d7["y"] = WB([1024], BF16); d7["yT"] = WB([8, 128], BF16); d7["sz"] = WB([512], F32); d7["st6"] = WB([8], F32); d7["ss4"] = WB([4], F32)
            else:
                d7["u"] = xin2[0][:, 0:512]; d7["v"] = xin2[0][:, 512:1024]
                d7["sz"] = xin[0][:, 0:512]
                d7["y"] = xin[0][:, 512:1024].bitcast(BF16)
                d7["xTt"] = V(arena, RD + 1152, [8, 128], BF16); d7["vn"] = V(arena, RD + 1152 + 2048, [512], BF16)
                d7["yT"] = V(arena, RD + 4224, [8, 128], BF16)
                d7["st6"] = WB([8], F32); d7["ss4"] = WB([4], F32)
            for nm in ("xTt", "u", "v", "vn", "y", "yT", "sz", "st"):
                d7["b_" + nm] = P.buf()
            sets7.append(d7)
        winv = win_d.rearrange("(kc p) n -> p kc n", p=128)
        P.dma("pool", sem_w[0], WinA, winv[:, :, 0:1024], writes=[b_w7])
        P.dma("pool", sem_w[1], Wz, winv[:, :, 2560:3072], writes=[b_w7])
        P.dma("pool", sem_w[2], Wout, wout_d.rearrange("(kc p) n -> p kc n", p=128), writes=[b_w7])
        make_bc(bcG, b, 2, b_bcG, xin[1], b_xin[1])
        def p7_a(tt, xTt, u_sb, v_sb, vn_bf, y_bf, yTt, sz, st6, ss4, b_xTt, b_u, b_v, b_vn, b_y, b_yT, b_sz, b_st):
            xt = x_res[:, tt, :]
            P.dma("sp", sem_x[tt % 2], xt, x_d[b, tt * 128:(tt + 1) * 128, :], writes=[b_xres[tt]])
            norm_mod_T(xt, b_xres[tt], bcA, bcS, xTt, [b_xTt], 0, xmb, b_xmb)
            for hf, bk in ((0, 1), (1, 2)):
                for kc in range(8):
                    P.op("pe", lambda e, kc=kc, hf=hf, bk=bk: e.matmul(banks[bk][:, :], xTt[:, kc, :], WinA[:, kc, hf * 512:(hf + 1) * 512],
                                                                     start=(kc == 0), stop=(kc == 7)), reads=[b_xTt, b_w7], writes=[b_ps[bk]])
            for kc in range(8):
                P.op("pe", lambda e, kc=kc: e.matmul(banks[3][:, :], xTt[:, kc, :], Wz[:, kc, :], start=(kc == 0), stop=(kc == 7)),
                     reads=[b_xTt, b_w7], writes=[b_ps[3]])
            P.op("act", lambda e: e.activation(out=u_sb, in_=banks[1][:, :], func=AF.Gelu_apprx_tanh), reads=[b_ps[1]], writes=[b_u])
            P.op("act", lambda e: e.activation(out=v_sb, in_=banks[2][:, :], func=AF.Gelu_apprx_tanh), reads=[b_ps[2]], writes=[b_v])
            P.op("act", lambda e: e.activation(out=sz, in_=banks[3][:, :], func=AF.Silu), reads=[b_ps[3]], writes=[b_sz])
            P.op("dve", lambda e: e.bn_stats(out=st6[:, 0:6], in_=v_sb), reads=[b_v], writes=[b_st])
            P.op("dve", lambda e: e.bn_aggr(out=st6[:, 6:8], in_=st6[:, 0:6]), reads=[b_st], writes=[b_st])
            P.op("dve", lambda e: e.tensor_scalar(out=st6[:, 7:8], in0=st6[:, 7:8], scalar1=EPS, scalar2=None, op0=ALU.add), reads=[b_st], writes=[b_st])
            P.op("act", lambda e: e.activation(out=st6[:, 7:8], in_=st6[:, 7:8], func=AF.Sqrt), reads=[b_st], writes=[b_st])
            P.op("dve", lambda e: e.reciprocal(out=st6[:, 7:8], in_=st6[:, 7:8]), reads=[b_st], writes=[b_st])
            P.op("dve", lambda e: e.tensor_scalar(out=v_sb, in0=v_sb, scalar1=st6[:, 6:7], scalar2=st6[:, 7:8], op0=ALU.subtract, op1=ALU.mult),
                 reads=[b_v, b_st], writes=[b_v])
            P.op("dve", lambda e: e.tensor_tensor(out=v_sb, in0=v_sb, in1=lng_bc, op=ALU.mult), reads=[b_v, b_par], writes=[b_v])
            P.op("dve", lambda e: e.tensor_tensor(out=vn_bf, in0=v_sb, in1=lnb_bc, op=ALU.add), reads=[b_v, b_par], writes=[b_vn])
        def p7_b(tt, xTt, u_sb, v_sb, vn_bf, y_bf, yTt, sz, st6, ss4, b_xTt, b_u, b_v, b_vn, b_y, b_yT, b_sz, b_st):
            xt = x_res[:, tt, :]
            for h in range(4):
                P.op("pe", lambda e, h=h: e.matmul(banks[4][:, h * 128:(h + 1) * 128], wsT[:, h, :], vn_bf[:, h * 128:(h + 1) * 128], start=True, stop=True),
                     reads=[b_vn, b_par], writes=[b_ps[4]])
            for h in range(4):
                hs = slice(h * 128, (h + 1) * 128)
                P.op("dve", lambda e, h=h, hs=hs: e.scalar_tensor_tensor(out=y_bf[:, hs], in0=banks[4][:, hs], scalar=bsT[:, h:h + 1], in1=u_sb[:, hs],
                                                                       op0=ALU.add, op1=ALU.mult), reads=[b_ps[4], b_par, b_u], writes=[b_y])
            for h in range(4):
                hs = slice(h * 128, (h + 1) * 128)
                P.op("act", lambda e, h=h, hs=hs, tt=tt: e.activation(out=u_sb[:, hs], in_=o_acc[:, tt, hs], func=AF.Square, accum_out=ss4[:, h:h + 1]),
                     reads=[b_oacc[tt][h], b_y], writes=[b_u, b_st])
            rstd_from_ss(ss4, 128.0, b_st)
            for h in range(4):
                hs = slice(h * 128, (h + 1) * 128)
                P.op("dve", lambda e, h=h, hs=hs, tt=tt: e.scalar_tensor_tensor(out=u_sb[:, hs], in0=o_acc[:, tt, hs], scalar=ss4[:, h:h + 1], in1=onorm_bc,
                                                                              op0=ALU.mult, op1=ALU.mult), reads=[b_oacc[tt][h], b_st, b_par, b_u], writes=[b_u])
            P.op("dve", lambda e: e.tensor_tensor(out=y_bf[:, 512:1024], in0=u_sb, in1=sz, op=ALU.mult), reads=[b_u, b_sz], writes=[b_y])
            pv = banks[5][:, 0:512].bitcast(BF16)
            for ch in range(8):
                P.op("pe", lambda e, ch=ch: e.transpose(out=pv[:, ch * 128:(ch + 1) * 128], in_=y_bf[:, ch * 128:(ch + 1) * 128], identity=ident_bf),
                     reads=[b_y, b_const], writes=[b_ps[5]])
            P.op("act", lambda e: e.activation(out=yTt, in_=pv.rearrange("p (a b) -> p a b", a=8), func=AF.Copy), reads=[b_ps[5]], writes=[b_yT])
            for hf in range(2):
                bk = 6 + hf
                for ch in range(8):
                    P.op("pe", lambda e, ch=ch, hf=hf, bk=bk: e.matmul(banks[bk][:, :], yTt[:, ch, :], Wout[:, ch, hf * 512:(hf + 1) * 512],
                                                                     start=(ch == 0), stop=(ch == 7)), reads=[b_yT, b_w7], writes=[b_ps[bk]])
            for hf in range(2):
                hs = slice(hf * 512, (hf + 1) * 512)
                P.op("dve", lambda e, hf=hf, hs=hs: e.tensor_tensor(out=junk_f32[:, hs], in0=banks[6 + hf][:, :], in1=bcG[:, hs], op=ALU.mult),
                     reads=[b_ps[6 + hf], b_bcG], writes=[b_jf])
            P.op("pool", lambda e, xt=xt: e.tensor_tensor(out=xt, in0=xt, in1=junk_f32, op=ALU.add), reads=[b_jf, b_xres[tt]], writes=[b_xres[tt]])
        def args7(tt):
            d7 = sets7[tt % 2]
            return (tt, d7["xTt"], d7["u"], d7["v"], d7["vn"], d7["y"], d7["yT"], d7["sz"], d7["st6"], d7["ss4"],
                    d7["b_xTt"], d7["b_u"], d7["b_v"], d7["b_vn"], d7["b_y"], d7["b_yT"], d7["b_sz"], d7["b_st"])
        def cap(fn, *a):
            P.capture = []
            fn(*a)
            lst = P.capture
            P.capture = None
            return lst
        P.replay_merged(cap(p7_a, *args7(0)), [])
        for tt in range(1, 16):
            P.replay_merged(cap(p7_a, *args7(tt)), cap(p7_b, *args7(tt - 1)))
        P.replay_merged([], cap(p7_b, *args7(15)))
        P.barrier()
        if dbg and b == 0:
            out_toks.append(P.dma("sp", sem_o[0], dbg_d, x_res, reads=b_xres))
            P.barrier()

        if stop <= 7:
            return
        MA = Alloc(arena[:, RB:ARENA], ARENA - RB)
        h2T = MA([8, 2048], BF16)
        b_h2T = [P.buf() for _ in range(16)]
        GU = [MA([8, 1024], BF16) for _ in range(2)]; DW = [MA([4, 1024], BF16) for _ in range(2)]
        b_GU = [P.buf() for _ in range(2)]; b_DW = [P.buf() for _ in range(2)]
        act_t = [MA([4, 512], BF16) for _ in range(2)]; b_act = [P.buf() for _ in range(2)]
        sg = [MA([512], F32) for _ in range(2)]; b_sg = [P.buf() for _ in range(2)]
        h2f = MA([1024], F32); f32T = MA([8, 128], F32); b_f32T = P.buf()
        cb = MA([16, 16], F32); b_cb = [P.buf() for _ in range(16)]
        lg = MA([20], F32); rt = MA([32], F32); b_rt = P.buf()
        small2 = MA([16], F32); b_small2 = P.buf()
        bc2A = MA([1024], F32); bc2S = MA([1024], F32); bc2G = MA([1024], F32); tb_ = MA([1024], F32)
        b_2A, b_2S, b_2G, b_tb = P.buf(), P.buf(), P.buf(), P.buf()
        jb = MA([1024], BF16); b_jb = P.buf()
        load(tb_, n2g_d.partition_broadcast(128), writes=[b_tb])
        P.op("dve", lambda e: e.tensor_copy(out=h2f, in_=tb_), reads=[b_tb], writes=[b_jf])
        g2_bc = h2f
        make_bc(bc2A, b, 4, b_2A, tb_, b_tb, g_bc=g2_bc, b_g=b_jf)
        make_bc(bc2S, b, 3, b_2S, tb_, b_tb)
        make_bc(bc2G, b, 5, b_2G, tb_, b_tb)
        def route_tile(tt, ev):
            small2, b_small2, jb, b_jb, h2f, b_jf, f32T, b_f32T, lg, rt, b_rt, bkA, bkB, bkC = ev
            ss = small2[:, 0:1]
            src = x_res[:, tt, :]
            P.op("act", lambda e, src=src: e.activation(out=jb, in_=src, func=AF.Square, accum_out=ss), reads=[b_xres[tt]], writes=[b_jb, b_small2])
            rstd_from_ss(ss, 1024.0, b_small2)
            P.op("dve", lambda e, src=src: e.scalar_tensor_tensor(out=h2f, in0=src, scalar=ss, in1=bc2A, op0=ALU.mult, op1=ALU.mult),
                 reads=[b_xres[tt], b_small2, b_2A], writes=[b_jf])
            P.op("dve", lambda e: e.tensor_tensor(out=h2f, in0=h2f, in1=bc2S, op=ALU.add), reads=[b_jf, b_2S], writes=[b_jf])
            for kc in range(8):
                bk = (bkA, bkB)[kc // 4]
                P.op("pe", lambda e, kc=kc, bk=bk: e.transpose(out=banks[bk][:, (kc % 4) * 128:(kc % 4 + 1) * 128], in_=h2f[:, kc * 128:(kc + 1) * 128], identity=ident),
                     reads=[b_jf, b_const], writes=[b_ps[bk]])
            for hf in range(2):
                P.op("act", lambda e, hf=hf, tt=tt: e.activation(out=h2T[:, hf * 4:(hf + 1) * 4, tt * 128:(tt + 1) * 128],
                                                                 in_=banks[(bkA, bkB)[hf]][:, :].rearrange("p (a b) -> p a b", a=4), func=AF.Copy),
                     reads=[b_ps[(bkA, bkB)[hf]]], writes=[b_h2T[tt]])
                P.op("dve", lambda e, hf=hf: e.tensor_copy(out=f32T[:, hf * 4:(hf + 1) * 4, :], in_=banks[(bkA, bkB)[hf]][:, :].rearrange("p (a b) -> p a b", a=4)),
                     reads=[b_ps[(bkA, bkB)[hf]]], writes=[b_f32T])
            for kc in range(8):
                P.op("pe", lambda e, kc=kc: e.matmul(banks[bkC][:, 0:20], f32T[:, kc, :], Wr32[:, kc, :], start=(kc == 0), stop=(kc == 7)),
                     reads=[b_f32T, b_par], writes=[b_ps[bkC]])
            R_ = [b_rt]

            def dv(fn, extra_r=()):
                P.op("dve", fn, reads=R_ + list(extra_r), writes=R_)
            gmx, ngm, gsum, pg, m1, m2, dd, w1g, w2g = (rt[:, i:i + 1] for i in range(9))
            ohg = rt[:, 12:16]; es = rt[:, 16:20]; oh1 = rt[:, 20:24]; es2 = rt[:, 24:28]; oh2 = rt[:, 28:32]
            P.op("dve", lambda e: e.tensor_tensor(out=lg, in0=banks[bkC][:, 0:20], in1=brt_bc, op=ALU.add), reads=[b_ps[bkC], b_par], writes=R_)
            dv(lambda e: e.tensor_reduce(out=gmx, in_=lg[:, 0:4], axis=AX.X, op=ALU.max))
            dv(lambda e: e.tensor_scalar(out=ohg, in0=lg[:, 0:4], scalar1=gmx, scalar2=None, op0=ALU.is_equal))
            dv(lambda e: e.tensor_scalar(out=ngm, in0=gmx, scalar1=-1.0, scalar2=None, op0=ALU.mult))
            P.op("act", lambda e: e.activation(out=es2, in_=lg[:, 0:4], func=AF.Exp, bias=ngm, accum_out=gsum), reads=R_, writes=R_)
            dv(lambda e: e.reciprocal(out=pg, in_=gsum))
            dv(lambda e: e.tensor_scalar(out=es, in0=lg[:, 4:8], scalar1=ohg[:, 0:1], scalar2=None, op0=ALU.mult))
            for g in range(1, 4):
                dv(lambda e, g=g: e.scalar_tensor_tensor(out=es, in0=lg[:, 4 + 4 * g:8 + 4 * g], scalar=ohg[:, g:g + 1], in1=es, op0=ALU.mult, op1=ALU.add))
            dv(lambda e: e.tensor_reduce(out=m1, in_=es, axis=AX.X, op=ALU.max))
            dv(lambda e: e.tensor_scalar(out=oh1, in0=es, scalar1=m1, scalar2=None, op0=ALU.is_equal))
            dv(lambda e: e.scalar_tensor_tensor(out=es2, in0=oh1, scalar=-1e30, in1=es, op0=ALU.mult, op1=ALU.add))
            dv(lambda e: e.tensor_reduce(out=m2, in_=es2, axis=AX.X, op=ALU.max))
            dv(lambda e: e.tensor_scalar(out=oh2, in0=es2, scalar1=m2, scalar2=None, op0=ALU.is_equal))
            dv(lambda e: e.tensor_tensor(out=dd, in0=m1, in1=m2, op=ALU.subtract))
            P.op("act", lambda e: e.activation(out=dd, in_=dd, func=AF.Sigmoid), reads=R_, writes=R_)
            dv(lambda e: e.tensor_tensor(out=w1g, in0=dd, in1=pg, op=ALU.mult))
            dv(lambda e: e.tensor_tensor(out=w2g, in0=pg, in1=w1g, op=ALU.subtract))
            dv(lambda e: e.tensor_scalar(out=es, in0=oh1, scalar1=w1g, scalar2=None, op0=ALU.mult))
            dv(lambda e: e.scalar_tensor_tensor(out=es, in0=oh2, scalar=w2g, in1=es, op0=ALU.mult, op1=ALU.add))
            for g in range(4):
                P.op("dve", lambda e, g=g, tt=tt: e.tensor_scalar(out=cb[:, tt, 4 * g:4 * g + 4], in0=es, scalar1=ohg[:, g:g + 1], scalar2=None, op0=ALU.mult),
                     reads=R_, writes=[b_cb[tt]])
        f32T2 = MA([8, 128], F32); jb2 = MA([1024], BF16); lg2 = MA([20], F32); rt2 = MA([32], F32); small3 = MA([16], F32)
        ev_r = [(small2, b_small2, jb, b_jb, h2f, b_jf, f32T, b_f32T, lg, rt, b_rt, 0, 1, 2),
                (small3, P.buf(), jb2, P.buf(), tb_, b_tb, f32T2, P.buf(), lg2, rt2, P.buf(), 3, 4, 5)]
        for tt in range(0, 16, 2):
            P.capture = []
            route_tile(tt, ev_r[0])
            A_ = P.capture
            P.capture = []
            route_tile(tt + 1, ev_r[1])
            B_ = P.capture
            P.capture = None
            P.replay_merged(A_, B_)
        if stop <= 8:
            return
        def emit_GU(ex, s, tb4, a_s):
            for fc in range(4):
                gs = fc % 2
                bg_, bu_ = (0, 1) if gs == 0 else (2, 3)
                for kc in range(8):
                    P.op("pe", I("matmul", banks[bg_][:, :], GU[s][:, kc, fc * 128:(fc + 1) * 128], h2T[:, kc, tb4 * 512:(tb4 + 1) * 512], start=(kc == 0), stop=(kc == 7)),
                         reads=[b_GU[s]] + b_h2T[tb4 * 4:tb4 * 4 + 4], writes=[b_ps[bg_]])
                for kc in range(8):
                    P.op("pe", I("matmul", banks[bu_][:, :], GU[s][:, kc, 512 + fc * 128:512 + (fc + 1) * 128], h2T[:, kc, tb4 * 512:(tb4 + 1) * 512], start=(kc == 0), stop=(kc == 7)),
                         reads=[b_GU[s]] + b_h2T[tb4 * 4:tb4 * 4 + 4], writes=[b_ps[bu_]])
                P.op("act", I("activation", out=sg[gs], in_=banks[bg_][:, :], func=AF.Silu), reads=[b_ps[bg_]], writes=[b_sg[gs]])
                P.op("dve", I("tensor_tensor", out=act_t[a_s][:, fc, :], in0=sg[gs], in1=banks[bu_][:, :], op=ALU.mult),
                     reads=[b_sg[gs], b_ps[bu_]], writes=[b_act[a_s]])

        def emit_DOWN(ex, s, tb4, a_s):
            for t4 in range(4):
                tt = tb4 * 4 + t4
                ds = t4 % 2
                for hf in range(2):
                    bk = 4 + ds * 2 + hf
                    for fc in range(4):
                        P.op("pe", I("matmul", banks[bk][:, :], act_t[a_s][:, fc, t4 * 128:(t4 + 1) * 128], DW[s][:, fc, hf * 512:(hf + 1) * 512], start=(fc == 0), stop=(fc == 3)),
                             reads=[b_act[a_s], b_DW[s]], writes=[b_ps[bk]])
                for hf in range(2):
                    bk = 4 + ds * 2 + hf
                    hs = slice(hf * 512, (hf + 1) * 512)
                    P.op("dve", I("scalar_tensor_tensor", out=x_res[:, tt, hs], in0=banks[bk][:, :], scalar=cb[:, tt, ex:ex + 1], in1=x_res[:, tt, hs], op0=ALU.mult, op1=ALU.add),
                         reads=[b_ps[bk], b_cb[tt], b_xres[tt]], writes=[b_xres[tt]])

        pending = None
        gcount = 0
        for ex in range(16):
            s = ex % 2
            P.dma("pool", sem_w[s], GU[s], wgu_d[ex].rearrange("(kc p) n -> p kc n", p=128), writes=[b_GU[s]])
            P.dma("pool", sem_w[2 + s], DW[s], wdn_d[ex].rearrange("(fc p) n -> p fc n", p=128), writes=[b_DW[s]])
            for fc in range(4):
                P.op("pool", I("tensor_tensor", out=DW[s][:, fc, :], in0=DW[s][:, fc, :], in1=bc2G, op=ALU.mult),
                     reads=[b_DW[s], b_2G], writes=[b_DW[s]])
            for tb4 in range(4):
                a_s = gcount % 2
                emit_GU(ex, s, tb4, a_s)
                if pending is not None:
                    emit_DOWN(*pending)
                pending = (ex, s, tb4, a_s)
                gcount += 1
        emit_DOWN(*pending)
        if stop <= 9:
            return
        load(tb_, fg_d.partition_broadcast(128), writes=[b_tb])
        ot = [bc2A, bc2S]; b_ot = [b_2A, b_2S]
        for tt in range(16):
            s = tt % 2
            ss = small2[:, 0:1]
            src = x_res[:, tt, :]
            P.op("act", lambda e, src=src: e.activation(out=jb, in_=src, func=AF.Square, accum_out=ss), reads=[b_xres[tt]], writes=[b_jb, b_small2])
            rstd_from_ss(ss, 1024.0, b_small2)
            P.op("dve", lambda e, src=src, s=s: e.scalar_tensor_tensor(out=ot[s], in0=src, scalar=ss, in1=tb_, op0=ALU.mult, op1=ALU.mult),
                 reads=[b_xres[tt], b_small2, b_tb], writes=[b_ot[s]])
            out_toks.append(P.dma("sp", sem_o[s], out_d[b, tt * 128:(tt + 1) * 128, :], ot[s], reads=[b_ot[s]]))
        P.barrier()

    for b in range(NB):
        do_batch(b)
        P.barrier()

    P._emit_waits("sp", out_toks)
    P.emit()
    P.close()
    return nc


_NC_CACHE = {}


def kernel(**inputs):
    NB = 2
    if "nc" not in _NC_CACHE:
        _NC_CACHE["nc"] = build(NB)
    nc = _NC_CACHE["nc"]
    f = lambda a: np.ascontiguousarray(np.asarray(a, dtype=np.float32))
    shared = {
        "c_ctx": f(inputs["c_ctx"]).reshape(1, 1024), "w_ada": f(inputs["w_ada"])[0], "b_ada": f(inputs["b_ada"]).reshape(1, 6144),
        "norm1_g": f(inputs["norm1_g"]).reshape(1, 1024), "w_in": f(inputs["w_in"])[0], "ln_a_g": f(inputs["ln_a_g"]).reshape(1, 512),
        "ln_a_b": f(inputs["ln_a_b"]).reshape(1, 512), "w_spatial": f(inputs["w_spatial"])[0], "b_spatial": f(inputs["b_spatial"])[0],
        "conv_qkv": f(inputs["conv_qkv"])[0], "a_log": f(inputs["a_log"]).reshape(1, 8), "dt_bias": f(inputs["dt_bias"]).reshape(1, 8),
        "onorm_g": f(inputs["onorm_g"]).reshape(1, 128), "w_out": f(inputs["w_out"])[0], "norm2_g": f(inputs["norm2_g"]).reshape(1, 1024),
        "w_group": f(inputs["w_group"])[0], "b_group": f(inputs["b_group"]).reshape(1, 4), "w_router": f(inputs["w_router"])[0],
        "b_router": f(inputs["b_router"]).reshape(1, 16), "w_gate_up": f(inputs["w_gate_up"])[0], "w_down": f(inputs["w_down"])[0],
        "final_g": f(inputs["final_g"]).reshape(1, 1024),
    }
    x = f(inputs["x"]); c = f(inputs["c"]); ctx = f(inputs["ctx"])
    in_maps = []
    for i in range(N_CORES):
        m = dict(shared)
        m["x"] = x[i * NB:(i + 1) * NB]; m["c"] = c[i * NB:(i + 1) * NB]; m["ctx"] = ctx[i * NB:(i + 1) * NB]
        in_maps.append(m)
    res = run_bass_kernel_spmd(nc, in_maps, core_ids=list(range(N_CORES)))
    return np.concatenate([r["out"] for r in res.results], axis=0).astype(np.float32)
```

```python
from contextlib import ExitStack
import os
import numpy as np
import concourse.bass as bass
import concourse.mybir as mybir
from concourse.bass_utils import run_bass_kernel_spmd

F32 = mybir.dt.float32
BF16 = mybir.dt.bfloat16
U8 = mybir.dt.uint8
AF = mybir.ActivationFunctionType
ALU = mybir.AluOpType
AX = mybir.AxisListType

N_CORES = 8
KLAT = int(os.environ.get('KLAT', '1'))
KNOQK = int(os.environ.get('KNOQK', '0'))
SAME_ENG_SYNC = int(os.environ.get('KSES', '1'))
EPS = 1e-6


class Buf:
    __slots__ = ("name", "last_w", "readers", "excl")

    def __init__(self, name, excl=False):
        self.name = name
        self.excl = excl
        self.last_w = None
        self.readers = []


class Prog:
    ENG = ("pe", "act", "dve", "pool", "sp")

    def __init__(self, nc):
        self.nc = nc
        self.es = ExitStack()
        self.ops = {e: [] for e in self.ENG}
        self.count = {e: 0 for e in self.ENG}
        self.sems = {}
        for e in self.ENG:
            self.sems[e] = self.es.enter_context(nc.semaphore("s_" + e))
        self.waited = {e: {} for e in self.ENG}
        self.dma_sem_cnt = {}
        self.nbuf = 0
        self.capture = None

    def sbuf(self, name, shape, dtype=F32):
        return self.es.enter_context(self.nc.sbuf_tensor(name, list(shape), dtype))

    def psum(self, name, shape, dtype=F32):
        return self.es.enter_context(self.nc.psum_tensor(name, list(shape), dtype))

    def buf(self, name=None, excl=False):
        self.nbuf += 1
        return Buf(name or f"b{self.nbuf}", excl)

    def dma_sem(self, name):
        key = "d_" + name
        self.sems[key] = self.es.enter_context(self.nc.semaphore(key))
        self.dma_sem_cnt[key] = 0
        return key

    def _deps(self, reads, writes):
        toks = []
        for b in reads:
            if b.last_w is not None:
                toks.append(b.last_w)
        for b in writes:
            if b.last_w is not None:
                toks.append(b.last_w)
            toks.extend(b.readers)
        return toks

    def _emit_waits(self, eng, toks):
        need = {}
        for (k, v) in toks:
            if k == eng and (eng in ("pe", "sp") or not SAME_ENG_SYNC):
                continue
            if v > need.get(k, 0):
                need[k] = v
        for k, v in need.items():
            if self.waited[eng].get(k, 0) >= v:
                continue
            self.waited[eng][k] = v
            self.ops[eng].append(("wait", self.sems[k], v))

    def _commit(self, tok, reads, writes):
        for b in writes:
            b.last_w = tok
            b.readers = []
        for b in reads:
            b.readers.append(tok)

    def op(self, eng, fn, reads=(), writes=()):
        if self.capture is not None:
            self.capture.append(("op", eng, fn, list(reads), list(writes)))
            return None
        writes = [b for b in writes if b is not None] + [b for b in reads if b is not None and b.excl]
        reads = [b for b in reads if b is not None and not b.excl]
        self._emit_waits(eng, self._deps(reads, writes))
        self.count[eng] += 1
        tok = (eng, self.count[eng])
        self.ops[eng].append(("op", fn, self.sems[eng], 1))
        self._commit(tok, reads, writes)
        return tok

    def dma(self, queue, semkey, out_ap, in_ap, reads=(), writes=()):
        if self.capture is not None:
            self.capture.append(("dma", queue, semkey, out_ap, in_ap, list(reads), list(writes)))
            return None
        reads = [b for b in reads if b is not None]
        writes = [b for b in writes if b is not None]
        self._emit_waits(queue, self._deps(reads, writes))
        if self.dma_sem_cnt[semkey] > 0:
            self._emit_waits(queue, [(semkey, self.dma_sem_cnt[semkey])])
        self.dma_sem_cnt[semkey] += 16
        tok = (semkey, self.dma_sem_cnt[semkey])

        def fn(e, out_ap=out_ap, in_ap=in_ap):
            return e.dma_start(out=out_ap, in_=in_ap)
        self.ops[queue].append(("op", fn, self.sems[semkey], 16))
        self._commit(tok, reads, writes)
        return tok

    def replay_merged(self, A, B):
        la, lb = len(A), len(B)
        ia = ib = 0
        while ia < la or ib < lb:
            if ib >= lb or (ia < la and ia * lb <= ib * la):
                it = A[ia]; ia += 1
            else:
                it = B[ib]; ib += 1
            if it[0] == "op":
                self.op(it[1], it[2], it[3], it[4])
            else:
                self.dma(it[1], it[2], it[3], it[4], it[5], it[6])

    def barrier(self):
        toks = [(e, self.count[e]) for e in self.ENG if e != "sp" and self.count[e] > 0]
        toks += [(k, v) for k, v in self.dma_sem_cnt.items() if v > 0]
        for e in self.ENG:
            self._emit_waits(e, toks)

    def emit(self):
        nc = self.nc
        P = self
        with nc.Block() as block:
            def run(e, engine):
                for item in P.ops[e]:
                    if item[0] == "wait":
                        engine.wait_ge(item[1], item[2])
                    else:
                        _, fn, sem, inc = item
                        fn(engine).then_inc(sem, inc)

            @block.sync
            def _(eng):
                run("sp", eng)

            @block.tensor
            def _(eng):
                run("pe", eng)

            @block.scalar
            def _(eng):
                run("act", eng)

            @block.vector
            def _(eng):
                run("dve", eng)

            @block.gpsimd
            def _(eng):
                run("pool", eng)

    def close(self):
        self.es.close()


def I(name, *a, **kw):
    return lambda e: getattr(e, name)(*a, **kw)


def build(NB=2, dbg=False, stop=99, KSTEPS=36, KSUB=99):
    nc = bass.Bass("TRN2", target_bir_lowering=False)

    def din(name, shape):
        return nc.dram_tensor(name, list(shape), F32, kind="ExternalInput").ap()
    x_d = din("x", [NB, 2048, 1024]); ctx_d = din("ctx", [NB, 256, 1024]); c_d = din("c", [NB, 1024])
    cctx_d = din("c_ctx", [1, 1024]); wada_d = din("w_ada", [1024, 6144]); bada_d = din("b_ada", [1, 6144])
    n1g_d = din("norm1_g", [1, 1024]); win_d = din("w_in", [1024, 3088]); lng_d = din("ln_a_g", [1, 512])
    lnb_d = din("ln_a_b", [1, 512]); wsp_d = din("w_spatial", [4, 128, 128]); bsp_d = din("b_spatial", [4, 128])
    conv_d = din("conv_qkv", [5, 1536]); alog_d = din("a_log", [1, 8]); dtb_d = din("dt_bias", [1, 8])
    ong_d = din("onorm_g", [1, 128]); wout_d = din("w_out", [1024, 1024]); n2g_d = din("norm2_g", [1, 1024])
    wgrp_d = din("w_group", [1024, 4]); bgrp_d = din("b_group", [1, 4]); wrt_d = din("w_router", [1024, 16])
    brt_d = din("b_router", [1, 16]); wgu_d = din("w_gate_up", [16, 1024, 1024]); wdn_d = din("w_down", [16, 512, 1024])
    fg_d = din("final_g", [1, 1024])
    out_d = nc.dram_tensor("out", [NB, 2048, 1024], F32, kind="ExternalOutput").ap()
    dbg_d = nc.dram_tensor("dbg", [128, 16, 1024], F32, kind="ExternalOutput").ap() if dbg else None

    P = Prog(nc)
    ARENA = 192 * 1024
    arena = P.sbuf("arena", [128, ARENA], U8)
    pers = P.sbuf("pers", [128, 15 * 1024], U8)
    banks = [P.psum(f"bank{i}", [128, 512]) for i in range(8)]

    def V(base, off, shape, dt, parts=128):
        esz = 2 if dt == BF16 else 4
        n = 1
        for s in shape:
            n *= s
        ap = base[0:parts, off:off + n * esz].bitcast(dt)
        if len(shape) == 2:
            ap = ap.rearrange("p (a b) -> p a b", a=shape[0])
        elif len(shape) == 3:
            ap = ap.rearrange("p (a b c) -> p a b c", a=shape[0], b=shape[1])
        return ap

    class Alloc:
        def __init__(self, base, size):
            self.base, self.size, self.off = base, size, 0

        def __call__(self, shape, dt, parts=128):
            esz = 2 if dt == BF16 else 4
            n = esz
            for s in shape:
                n *= s
            n = (n + 31) // 32 * 32
            off = self.off
            self.off += n
            assert self.off <= self.size, (self.off, self.size)
            return V(self.base, off, shape, dt, parts)

    PA = Alloc(pers, 15 * 1024)
    KB = 1024
    sem_ld = P.dma_sem("ld")
    ident = PA([128], F32); ident_bf = PA([128], BF16); ones_bf = PA([128], BF16)
    m_ui = PA([128], F32); m_us = PA([128], F32); m_li = PA([128], F32); m_ls = PA([128], F32); m_one = PA([128], F32)
    m_ubd = V(arena, 150 * 1024, [128], F32); m_ux = V(arena, 150 * 1024 + 512, [128], F32); m_lbd = V(arena, 150 * 1024 + 1024, [128], F32); m_lx = V(arena, 150 * 1024 + 1536, [128], F32)
    mb_ubd = PA([128], BF16); mb_ux = PA([128], BF16); mb_lbd = PA([128], BF16); mb_lx = PA([128], BF16)
    b_const = P.buf("const")

    def pool_op(fn, reads=(), writes=()):
        return P.op("pool", fn, reads, writes)

    def mk_mask(ap, pattern_step, chmul, cmp):
        pool_op(lambda e: e.memset(ap, 1.0), writes=[b_const])
        pool_op(lambda e: e.affine_select(out=ap, in_=ap, pattern=[[pattern_step, 128]], compare_op=cmp, fill=0.0,
                                          base=0, channel_multiplier=chmul), reads=[b_const], writes=[b_const])
    pool_op(lambda e: e.memset(ident, 0.0), writes=[b_const])
    pool_op(lambda e: e.affine_select(out=ident, in_=ident, pattern=[[-1, 128]], compare_op=ALU.not_equal, fill=1.0,
                                      base=0, channel_multiplier=1), reads=[b_const], writes=[b_const])
    pool_op(lambda e: e.tensor_copy(out=ident_bf, in_=ident), reads=[b_const], writes=[b_const])
    pool_op(lambda e: e.memset(ones_bf, 1.0), writes=[b_const])
    pool_op(lambda e: e.memset(m_one, 1.0), writes=[b_const])
    mk_mask(m_ui, 1, -1, ALU.is_ge)
    mk_mask(m_us, 1, -1, ALU.is_gt)
    mk_mask(m_li, -1, 1, ALU.is_ge)
    mk_mask(m_ls, -1, 1, ALU.is_gt)
    pool_op(lambda e: e.tensor_copy(out=m_ubd, in_=m_us), reads=[b_const], writes=[b_const])
    pool_op(lambda e: e.memset(m_ubd[0:64, 64:128], 0.0), reads=[b_const], writes=[b_const])
    pool_op(lambda e: e.memset(m_ux, 0.0), writes=[b_const])
    pool_op(lambda e: e.memset(m_ux[0:64, 64:128], 1.0), reads=[b_const], writes=[b_const])
    pool_op(lambda e: e.tensor_copy(out=m_lbd, in_=m_ls), reads=[b_const], writes=[b_const])
    pool_op(lambda e: e.memset(m_lbd[64:128, 0:64], 0.0), reads=[b_const], writes=[b_const])
    pool_op(lambda e: e.memset(m_lx, 0.0), writes=[b_const])
    pool_op(lambda e: e.memset(m_lx[64:128, 0:64], 1.0), reads=[b_const], writes=[b_const])

    mb_ui = PA([128], BF16); mb_us = PA([128], BF16); mb_li = PA([128], BF16); mb_ls = PA([128], BF16)
    for dst_, src_ in ((mb_ui, m_ui), (mb_us, m_us), (mb_li, m_li), (mb_ls, m_ls), (mb_ubd, m_ubd), (mb_ux, m_ux), (mb_lbd, m_lbd), (mb_lx, m_lx)):
        pool_op(lambda e, dst_=dst_, src_=src_: e.tensor_copy(out=dst_, in_=src_), reads=[b_const], writes=[b_const])
    eps_t = PA([1], F32)
    pool_op(lambda e: e.memset(eps_t, EPS), writes=[b_const])
    negA = PA([8], F32); dtb_bc = PA([8], F32); onorm_bc = PA([128], F32)
    lng_bc = PA([512], F32); lnb_bc = PA([512], F32)
    wsT = PA([4, 128], BF16); bsT = PA([4], F32); convw = PA([12, 5], F32)
    Wr32 = PA([8, 20], F32); brt_bc = PA([20], F32)
    modT = PA([48, 4], F32)
    b_par = P.buf("params")

    def load(dst, src, reads=(), writes=(), q="sp"):
        return P.dma(q, sem_ld, dst, src, reads=reads, writes=list(writes))

    load(negA, alog_d.partition_broadcast(128), writes=[b_par])
    load(dtb_bc, dtb_d.partition_broadcast(128), writes=[b_par])
    load(onorm_bc, ong_d.partition_broadcast(128), writes=[b_par])
    load(lng_bc, lng_d.partition_broadcast(128), writes=[b_par])
    load(lnb_bc, lnb_d.partition_broadcast(128), writes=[b_par])
    load(brt_bc[:, 0:4], bgrp_d.partition_broadcast(128), writes=[b_par])
    load(brt_bc[:, 4:20], brt_d.partition_broadcast(128), writes=[b_par])
    load(Wr32[:, :, 0:4], wgrp_d.rearrange("(kc p) n -> p kc n", p=128), writes=[b_par])
    load(Wr32[:, :, 4:20], wrt_d.rearrange("(kc p) n -> p kc n", p=128), writes=[b_par])
    P.op("act", lambda e: e.activation(out=negA, in_=negA, func=AF.Exp), reads=[b_par], writes=[b_par])
    P.op("dve", lambda e: e.tensor_scalar(out=negA, in0=negA, scalar1=-1.0, scalar2=None, op0=ALU.mult), reads=[b_par], writes=[b_par])

    A0 = Alloc(arena, ARENA)
    b_tmp = P.buf("setup_tmp")
    b_ps = [P.buf(f"bank{i}", excl=True) for i in range(8)]
    wsp_sb = A0([4, 128], F32); bsp_sb = A0([128], F32, parts=4); conv_sb = A0([1536], F32, parts=5)
    load(wsp_sb, wsp_d.rearrange("h i j -> i h j"), writes=[b_tmp])
    load(bsp_sb, bsp_d, writes=[b_tmp])
    load(conv_sb, conv_d, writes=[b_tmp])
    for h in range(4):
        P.op("pe", lambda e, h=h: e.transpose(out=banks[0][:, h * 128:(h + 1) * 128], in_=wsp_sb[:, h, :], identity=ident),
             reads=[b_tmp, b_const], writes=[b_ps[0]])
    P.op("act", lambda e: e.activation(out=wsT, in_=banks[0][:, 0:512].rearrange("p (a b) -> p a b", a=4), func=AF.Copy),
         reads=[b_ps[0]], writes=[b_par])
    P.op("pe", lambda e: e.transpose(out=banks[1][:, 0:4], in_=bsp_sb, identity=ident[0:4, 0:4]), reads=[b_tmp, b_const], writes=[b_ps[1]])
    P.op("act", lambda e: e.activation(out=bsT, in_=banks[1][:, 0:4], func=AF.Copy), reads=[b_ps[1]], writes=[b_par])
    for cc in range(12):
        P.op("pe", lambda e, cc=cc: e.transpose(out=banks[2][:, cc * 8:cc * 8 + 5], in_=conv_sb[:, cc * 128:(cc + 1) * 128],
                                                identity=ident[0:5, 0:5]), reads=[b_tmp, b_const], writes=[b_ps[2]])
    P.op("act", lambda e: e.activation(out=convw, in_=banks[2][:, 0:96].rearrange("p (a b) -> p a b", a=12)[:, :, 0:5], func=AF.Copy),
         reads=[b_ps[2]], writes=[b_par])

    cT = A0([3, 8], F32); cTb = A0([3, 8], BF16)
    for j in range(NB):
        load(cT[:, j, :], c_d[j].rearrange("(p kc) -> p kc", kc=8), writes=[b_tmp])
    if NB < 2:
        P.op("dve", lambda e: e.memset(cT[:, 1, :], 0.0), writes=[b_tmp])
    load(cT[:, 2, :], cctx_d[0].rearrange("(p kc) -> p kc", kc=8), writes=[b_tmp])
    P.op("act", lambda e: e.activation(out=cTb, in_=cT, func=AF.Silu), reads=[b_tmp], writes=[b_tmp])
    wada_v = wada_d.rearrange("(p kc) n -> p kc n", kc=8)
    wa = [A0([8, 512], BF16) for _ in range(2)]
    b_wa = [P.buf() for _ in range(2)]
    sem_wa = [P.dma_sem(f"wa{i}") for i in range(2)]
    for nb_ in range(12):
        s = nb_ % 2
        P.dma("pool", sem_wa[s], wa[s], wada_v[:, :, nb_ * 512:(nb_ + 1) * 512], writes=[b_wa[s]])
        for c4 in range(4):
            ch = nb_ * 4 + c4
            for kc in range(8):
                P.op("pe", lambda e, s=s, c4=c4, kc=kc, ch=ch: e.matmul(banks[3][:, ch * 4:ch * 4 + 3], wa[s][:, kc, c4 * 128:(c4 + 1) * 128],
                                                                      cTb[:, :, kc], start=(kc == 0), stop=(kc == 7)),
                     reads=[b_wa[s], b_tmp], writes=[b_ps[3]])
    P.op("act", lambda e: e.activation(out=modT[:, :, 0:3], in_=banks[3][:, 0:192].rearrange("p (a b) -> p a b", a=48)[:, :, 0:3], func=AF.Copy),
         reads=[b_ps[3]], writes=[b_par])
    P.barrier()

    def make_bc(dst, j, which, b_dst, tmp_bias, b_tmpb, g_bc=None, b_g=None):
        load(tmp_bias, bada_d[:, which * 1024:(which + 1) * 1024].partition_broadcast(128), writes=[b_tmpb])
        for c8 in range(8):
            ch = which * 8 + c8
            bk = 6 + c8 // 4
            P.op("pe", lambda e, c8=c8, ch=ch, bk=bk: e.matmul(banks[bk][:, (c8 % 4) * 128:(c8 % 4 + 1) * 128],
                                                               modT[:, ch, j:j + 1].to_broadcast([128, 128]), ident, start=True, stop=True),
                 reads=[b_par, b_const], writes=[b_ps[bk]])
        for hf in range(2):
            P.op("dve", lambda e, hf=hf: e.tensor_tensor(out=dst[:, hf * 512:(hf + 1) * 512], in0=banks[6 + hf][:, :],
                                                        in1=tmp_bias[:, hf * 512:(hf + 1) * 512], op=ALU.add),
                 reads=[b_ps[6 + hf], b_tmpb], writes=[b_dst])
        if g_bc is not None:
            P.op("dve", lambda e: e.scalar_tensor_tensor(out=dst, in0=dst, scalar=1.0, in1=g_bc, op0=ALU.add, op1=ALU.mult),
                 reads=[b_dst, b_g], writes=[b_dst])

    def rstd_from_ss(ss, n, b_s):
        P.op("dve", lambda e: e.tensor_scalar(out=ss, in0=ss, scalar1=1.0 / n, scalar2=EPS, op0=ALU.mult, op1=ALU.add), reads=[b_s], writes=[b_s])
        P.op("act", lambda e: e.activation(out=ss, in_=ss, func=AF.Sqrt), reads=[b_s], writes=[b_s])
        P.op("dve", lambda e: e.reciprocal(out=ss, in_=ss), reads=[b_s], writes=[b_s])

    sem_x = [P.dma_sem(f"x{i}") for i in range(2)]
    sem_w = [P.dma_sem(f"w{i}") for i in range(4)]
    sem_o = [P.dma_sem(f"o{i}") for i in range(2)]
    out_toks = []

    def do_batch(b):
        A = Alloc(arena, ARENA)
        RA = 0
        x_res = V(arena, RA, [16, 1024], F32)
        b_xres = [P.buf(f"xres{t}") for t in range(16)]
        xT = V(arena, RA, [8, 2304], BF16)
        b_xT = [P.buf(f"xT{t}") for t in range(18)]
        CT = RA + 36 * KB
        RB = 64 * KB
        qkvT = V(arena, RB, [12, 2304], BF16)
        b_qkv = [[P.buf() for _ in range(18)] for _ in range(12)]
        RC = RB + 54 * KB
        o_acc = V(arena, RC, [16, 512], F32)
        b_oacc = [[P.buf() for _ in range(4)] for _ in range(16)]
        Wqkv = V(arena, RC, [8, 1536], BF16)
        b_wqkv = P.buf()
        RD = RC + 32 * KB
        AD = Alloc(arena[:, RD:ARENA], ARENA - RD)
        gb = AD([18, 16], F32); cumE = AD([18, 40], F32); negE = AD([18, 40], F32)
        b_gb = [P.buf() for _ in range(18)]
        bcA = AD([1024], F32); bcS = AD([1024], F32); bcG = AD([1024], F32)
        b_bcA, b_bcS, b_bcG = P.buf(), P.buf(), P.buf()
        Wab = AD([8, 16], BF16); b_wab = P.buf()
        xin = [AD([1024], F32) for _ in range(2)]; b_xin = [P.buf() for _ in range(2)]
        xmb = AD([1024], BF16); b_xmb = P.buf()
        small = AD([16], F32); b_small0 = P.buf()
        g1_bc = xin[0]
        load(g1_bc, n1g_d.partition_broadcast(128), writes=[b_xin[0]])
        P.dma("pool", sem_w[0], Wab, win_d.rearrange("(kc p) n -> p kc n", p=128)[:, :, 3072:3088], writes=[b_wab])

        def norm_mod_T(src, b_src, A_bc, S_bc, dstT, b_dstT, bank, junk, b_junk, f32T=None, xm_f32=None, tmps=None):
            small_, b_small, junk_f32, b_jf = tmps if tmps is not None else (small, b_small0, junk_f320, b_jf0)
            ss = small_[:, 0:1]
            P.op("act", lambda e: e.activation(out=junk, in_=src, func=AF.Square, accum_out=ss), reads=[b_src], writes=[b_junk, b_small])
            rstd_from_ss(ss, 1024.0, b_small)
            if xm_f32 is None:
                tmp = junk_f32
                P.op("dve", lambda e: e.scalar_tensor_tensor(out=tmp, in0=src, scalar=ss, in1=A_bc, op0=ALU.mult, op1=ALU.mult),
                     reads=[b_src, b_small, b_bcA], writes=[b_jf])
                P.op("dve", lambda e: e.tensor_tensor(out=junk, in0=tmp, in1=S_bc, op=ALU.add), reads=[b_jf, b_bcS], writes=[b_junk])
                pv = banks[bank][:, 0:512].bitcast(BF16)
                for kc in range(8):
                    P.op("pe", lambda e, kc=kc: e.transpose(out=pv[:, kc * 128:(kc + 1) * 128], in_=junk[:, kc * 128:(kc + 1) * 128], identity=ident_bf),
                         reads=[b_junk, b_const], writes=[b_ps[bank]])
                P.op("act", lambda e: e.activation(out=dstT, in_=pv.rearrange("p (a b) -> p a b", a=8), func=AF.Copy),
                     reads=[b_ps[bank]], writes=b_dstT)
            else:
                P.op("dve", lambda e: e.scalar_tensor_tensor(out=xm_f32, in0=src, scalar=ss, in1=A_bc, op0=ALU.mult, op1=ALU.mult),
                     reads=[b_src, b_small, b_bcA], writes=[b_jf])
                P.op("dve", lambda e: e.tensor_tensor(out=xm_f32, in0=xm_f32, in1=S_bc, op=ALU.add), reads=[b_jf, b_bcS], writes=[b_jf])
                for kc in range(8):
                    bk = bank + kc // 4
                    P.op("pe", lambda e, kc=kc, bk=bk: e.transpose(out=banks[bk][:, (kc % 4) * 128:(kc % 4 + 1) * 128],
                                                                   in_=xm_f32[:, kc * 128:(kc + 1) * 128], identity=ident),
                         reads=[b_jf, b_const], writes=[b_ps[bk]])
                for hf in range(2):
                    P.op("act", lambda e, hf=hf: e.activation(out=dstT[:, hf * 4:(hf + 1) * 4, :], in_=banks[bank + hf][:, :].rearrange("p (a b) -> p a b", a=4), func=AF.Copy),
                         reads=[b_ps[bank + hf]], writes=b_dstT)
                    P.op("dve", lambda e, hf=hf: e.tensor_copy(out=f32T[:, hf * 4:(hf + 1) * 4, :], in_=banks[bank + hf][:, :].rearrange("p (a b) -> p a b", a=4)),
                         reads=[b_ps[bank + hf]], writes=[b_f32T])

        junk_f320 = AD([1024], F32); b_jf0 = P.buf()
        junk_f32 = junk_f320; b_jf = b_jf0

        def p1_tile(t, ev):
            if t < 2:
                src_d = ctx_d[b, t * 128:(t + 1) * 128, :]
            else:
                src_d = x_d[b, (t - 2) * 128:(t - 1) * 128, :]
            xt = ev["xin"]; bk0, bk1, bk2 = ev["banks"]
            ghf_ = ev["ghf"]
            P.dma("sp", ev["sem"], xt, src_d, writes=[ev["b_xin"]])
            norm_mod_T(xt, ev["b_xin"], bcA, bcS, xT[:, :, t * 128:(t + 1) * 128], [b_xT[t]], bk0, ev["xmb"], ev["b_xmb"], tmps=ev["tmps"])
            for kc in range(8):
                P.op("pe", I("matmul", banks[bk1][:, 0:16], xT[:, kc, t * 128:(t + 1) * 128], Wab[:, kc, :], start=(kc == 0), stop=(kc == 7)),
                     reads=[b_xT[t], b_wab], writes=[b_ps[bk1]])
            g8 = gb[:, t, 0:8]
            P.op("dve", I("tensor_tensor", out=g8, in0=banks[bk1][:, 0:8], in1=dtb_bc, op=ALU.add), reads=[b_ps[bk1], b_par], writes=[b_gb[t]])
            P.op("act", I("activation", out=gb[:, t, 8:16], in_=banks[bk1][:, 8:16], func=AF.Sigmoid), reads=[b_ps[bk1]], writes=[b_gb[t]])
            P.op("act", I("activation", out=g8, in_=g8, func=AF.Exp), reads=[b_gb[t]], writes=[b_gb[t]])
            P.op("act", I("activation", out=g8, in_=g8, func=AF.Ln, bias=1.0), reads=[b_gb[t]], writes=[b_gb[t]])
            P.op("dve", I("tensor_tensor", out=g8, in0=g8, in1=negA, op=ALU.mult), reads=[b_gb[t], b_par], writes=[b_gb[t]])
            P.op("dve", I("tensor_copy", out=ghb[:, t, 0:8], in_=g8), reads=[b_gb[t]], writes=[b_gb[t]])
            P.op("dve", I("tensor_copy", out=ghf_, in_=ghb[:, t, 0:8]), reads=[b_gb[t]], writes=[b_gb[t]])
            P.op("dve", I("tensor_tensor", out=ghb[:, t, 8:16], in0=g8, in1=ghf_, op=ALU.subtract), reads=[b_gb[t]], writes=[b_gb[t]])
            for mi, mk in enumerate((m_ui, m_ls, m_li, m_us, m_one)):
                P.op("pe", I("matmul", banks[bk2][:, mi * 8:(mi + 1) * 8], mk, g8, start=True, stop=True), reads=[b_gb[t], b_const], writes=[b_ps[bk2]])
            P.op("act", I("activation", out=cumE[:, t, :], in_=banks[bk2][:, 0:40], func=AF.Exp), reads=[b_ps[bk2]], writes=[b_gb[t]])
            P.op("dve", I("tensor_scalar", out=negE[:, t, :], in0=cumE[:, t, :], scalar1=-1.0, scalar2=None, op0=ALU.mult), reads=[b_gb[t]], writes=[b_gb[t]])

        def phase1_tiles(tiles, j):
            make_bc(bcA, j, 1, b_bcA, xin[1], b_xin[1], g_bc=g1_bc, b_g=b_xin[0])
            make_bc(bcS, j, 0, b_bcS, xin[1], b_xin[1])
            for i in range(0, len(tiles), 2):
                P.capture = []
                p1_tile(tiles[i], envs1[0])
                A_ = P.capture
                P.capture = []
                p1_tile(tiles[i + 1], envs1[1])
                B_ = P.capture
                P.capture = None
                P.replay_merged(A_, B_)

        if stop <= 0:
            return
        _x2 = AD([1024], F32); _bx2 = P.buf()
        xin2 = [_x2, _x2]; b_xin2 = [_bx2, _bx2]
        ghb = AD([18, 16], BF16); ghf = AD([8], F32)
        E1 = Alloc(arena[:, RB:RB + 16 * KB], 16 * KB)
        envs1 = [
            {"xin": xin2[0], "b_xin": b_xin2[0], "sem": sem_x[0], "xmb": xmb, "b_xmb": b_xmb, "tmps": None, "ghf": ghf, "banks": (0, 1, 2)},
            {"xin": E1([1024], F32), "b_xin": P.buf(), "sem": sem_x[1], "xmb": E1([1024], BF16), "b_xmb": P.buf(),
             "tmps": (E1([16], F32), P.buf(), E1([1024], F32), P.buf()), "ghf": E1([8], F32), "banks": (3, 4, 5)},
        ]
        phase1_tiles([0, 1], 2)
        phase1_tiles(list(range(2, 18)), b)

        if stop <= 1:
            return
        P.dma("pool", sem_w[1], Wqkv, win_d.rearrange("(kc p) n -> p kc n", p=128)[:, :, 1024:2560], writes=[b_wqkv])
        C5 = Alloc(arena[:, CT:CT + 28 * KB], 28 * KB)
        PTb = [C5([2320], BF16) for _ in range(2)]; b_PTb = [P.buf() for _ in range(2)]
        ACC = C5([2304], F32); b_ACC = P.buf()
        SQB = C5([2304], BF16); b_SQB = P.buf()
        RNb = [C5([512], F32) for _ in range(2)]; b_RNb = [P.buf() for _ in range(2)]
        DG = C5([5, 128], BF16); b_DG = P.buf()
        for i_ in range(2):
            P.op("pool", I("memset", PTb[i_], 0.0), writes=[b_PTb[i_]])
        blocks = [(0, 256)] + [(256 + i * 512, 512) for i in range(4)]
        for cc in range(12):
            pi_ = cc % 2
            PT_ = PTb[pi_]; bPT = b_PTb[pi_]
            for tp in range(5):
                P.op("dve", I("tensor_scalar", out=DG[:, tp, :], in0=ident_bf, scalar1=convw[:, cc, tp:tp + 1], scalar2=None, op0=ALU.mult),
                     reads=[b_par, b_const], writes=[b_DG])
            for bi, (t0, n) in enumerate(blocks):
                bk = bi % 2
                tl = list(range(t0 // 128, (t0 + n) // 128))
                for kc in range(8):
                    P.op("pe", I("matmul", banks[bk][:, 0:n], Wqkv[:, kc, cc * 128:(cc + 1) * 128], xT[:, kc, t0:t0 + n], start=(kc == 0), stop=(kc == 7)),
                         reads=[b_wqkv] + [b_xT[t] for t in tl], writes=[b_ps[bk]])
                po = (2 + t0) if t0 < 256 else (262 + t0 - 256)
                P.op("act", I("activation", out=PT_[:, po:po + n], in_=banks[bk][:, 0:n], func=AF.Copy), reads=[b_ps[bk]], writes=[bPT])
            allq = [b_qkv[cc][t] for t in range(18)]
            for bi, (t0, n) in enumerate(blocks):
                bk = 2 + bi % 2
                po = (2 + t0) if t0 < 256 else (262 + t0 - 256)
                for tp in range(5):
                    P.op("pe", I("matmul", banks[bk][:, 0:n], DG[:, tp, :], PT_[:, po - 2 + tp:po - 2 + tp + n], start=(tp == 0), stop=(tp == 4)),
                         reads=[b_DG, bPT], writes=[b_ps[bk]])
                if cc >= 8:
                    P.op("act", I("activation", out=qkvT[:, cc, t0:t0 + n], in_=banks[bk][:, 0:n], func=AF.Silu), reads=[b_ps[bk]], writes=allq)
                else:
                    P.op("act", I("activation", out=ACC[:, t0:t0 + n], in_=banks[bk][:, 0:n], func=AF.Silu), reads=[b_ps[bk]], writes=[b_ACC])
                    P.op("dve", I("tensor_tensor", out=SQB[:, t0:t0 + n], in0=ACC[:, t0:t0 + n], in1=ACC[:, t0:t0 + n], op=ALU.mult), reads=[b_ACC], writes=[b_SQB])
            if cc < 8:
                sc = (128.0 ** -0.5) if cc < 4 else 1.0
                for bi, (t0, n) in enumerate(blocks):
                    bk = 4 + bi % 2
                    ri = bi % 2
                    P.op("pe", I("matmul", banks[bk][:, 0:n], ones_bf, SQB[:, t0:t0 + n], start=True, stop=True), reads=[b_SQB, b_const], writes=[b_ps[bk]])
                    P.op("act", I("activation", out=RNb[ri][:, 0:n], in_=banks[bk][:, 0:n], func=AF.Sqrt, bias=eps_t), reads=[b_ps[bk], b_const], writes=[b_RNb[ri]])
                    P.op("dve", I("reciprocal", out=RNb[ri][:, 0:n], in_=RNb[ri][:, 0:n]), reads=[b_RNb[ri]], writes=[b_RNb[ri]])
                    P.op("dve", I("scalar_tensor_tensor", out=qkvT[:, cc, t0:t0 + n], in0=ACC[:, t0:t0 + n], scalar=sc, in1=RNb[ri][:, 0:n], op0=ALU.mult, op1=ALU.mult),
                         reads=[b_ACC, b_RNb[ri]], writes=allq)
        P.barrier()
        if stop <= 5:
            return
        SA = Alloc(arena[:, RA:RA + 64 * KB], 64 * KB)
        seqs = []
        for d in range(2):
            for h in range(4):
                q = {"d": d, "h": h}
                for nm in ("gmask", "decT", "dmbd", "dmx", "dmq", "vtok"):
                    q[nm] = SA([128], F32)
                for nm in ("Mbd", "Nbd", "XT", "QKd", "R0", "R1", "RT0", "RT1", "P0", "P1", "PT0", "PT1", "ZT", "AinvT", "kd", "r", "vnew", "Sbf"):
                    q[nm] = SA([128], BF16)
                q["S"] = SA([128], F32)
                q["b"] = {}
                seqs.append(q)

        def sb(q, nm):
            if nm not in q["b"]:
                q["b"][nm] = P.buf()
            return q["b"][nm]
        slots = [(i % 8, i // 8) for i in range(32)]
        b_slot = [b_ps[i % 8] for i in range(32)]
        slot_i = [0]

        def pslot():
            i = slot_i[0] % 32
            slot_i[0] += 1
            bk, c = slots[i]
            if os.environ.get('KDBG') and slot_i[0] <= 24:
                print('pslot', i, bk, c, banks[bk][:, c * 128:(c + 1) * 128].offset, banks[bk][:, c * 128:(c + 1) * 128].bitcast(BF16)[:, 0:128].offset)
            return banks[bk][:, c * 128:(c + 1) * 128], b_slot[i]

        for q in seqs:
            P.op("pool", I("memset", q["S"], 0.0), writes=[sb(q, "S")])
            P.op("pool", I("memset", q["Sbf"], 0.0), writes=[sb(q, "Sbf")])
        orders = [list(range(18)), [1, 0] + list(range(17, 1, -1))]
        o_written = [[False] * 4 for _ in range(16)]

        def mmq(out, lhsT, rhs, reads, bw):
            P.op("pe", lambda e: e.matmul(out, lhsT, rhs, start=True, stop=True), reads=reads, writes=[bw])

        def scan_step(step2):
            step = step2 // 2
            st = []
            for q in seqs:
                d, h = q["d"], q["h"]
                if d != step2 % 2:
                    continue
                t = orders[d][step]
                if KLAT == 0:
                    t = t % 2
                st.append((q, d, h, t, (t >= 2) and KNOQK == 0))
            tsl = lambda t: slice(t * 128, (t + 1) * 128)
            for (q, d, h, t, lat) in st:
                kT = qkvT[:, 4 + h, tsl(t)]; qT = qkvT[:, h, tsl(t)]; vT = qkvT[:, 8 + h, tsl(t)]
                q["kT"], q["qT"] = kT, qT
                q["bk"], q["bq"], q["bv"] = b_qkv[4 + h][t], b_qkv[h][t], b_qkv[8 + h][t]
                col = d * 4 + h
                q["beta"] = gb[:, t, 8 + col:9 + col]
                q["gcol"] = gb[:, t, col:col + 1]
                q["eg"] = cumE[:, t, (h if d == 0 else 20 + h):(h if d == 0 else 20 + h) + 1]
                q["neg"] = negE[:, t, (h if d == 0 else 20 + h):(h if d == 0 else 20 + h) + 1]
                q["ekd"] = cumE[:, t, (8 + h if d == 0 else 28 + h):(8 + h if d == 0 else 28 + h) + 1]
                q["gl"] = cumE[:, t, 32 + col:33 + col]
                q["bg"] = b_gb[t]
                ks, q["bks"] = pslot(); q["ks"] = ks.bitcast(BF16)[:, 0:128]
                vs, q["bvs"] = pslot(); q["vs"] = vs.bitcast(BF16)[:, 0:128]
                P.op("pe", I("transpose", out=q["ks"], in_=kT, identity=ident_bf), reads=[q["bk"], b_const], writes=[q["bks"]])
                P.op("pe", I("transpose", out=q["vs"], in_=vT, identity=ident_bf), reads=[q["bv"], b_const], writes=[q["bvs"]])
                ml = mb_ls if d == 0 else mb_us
                gmv = q["gmask"].bitcast(BF16)
                q["gmh"], q["gml"] = gmv[:, 0:128], gmv[:, 128:256]
                P.op("dve", I("tensor_scalar", out=q["gmh"], in0=ml, scalar1=ghb[:, t, col:col + 1], scalar2=None, op0=ALU.mult),
                     reads=[q["bg"], b_const], writes=[sb(q, "gmask")])
                P.op("dve", I("tensor_scalar", out=q["gml"], in0=ml, scalar1=ghb[:, t, 8 + col:9 + col], scalar2=None, op0=ALU.mult),
                     reads=[q["bg"], b_const], writes=[sb(q, "gmask")])
            if KSUB < 2:
                return
            for (q, d, h, t, lat) in st:
                P.op("act", I("activation", out=q["vtok"], in_=q["vs"], func=AF.Copy), reads=[q["bvs"]], writes=[sb(q, "vtok")])
                P.op("act", I("activation", out=q["kd"], in_=q["ks"], func=AF.Copy, scale=q["ekd"]), reads=[q["bks"], q["bg"]], writes=[sb(q, "kd")])
            for (q, d, h, t, lat) in st:
                q["G"], q["bG"] = pslot()
                mmq(q["G"], q["kT"], q["kT"], [q["bk"]], q["bG"])
                if lat:
                    q["QK"], q["bQK"] = pslot()
                    mmq(q["QK"], q["kT"], q["qT"], [q["bk"], q["bq"]], q["bQK"])
            for (q, d, h, t, lat) in st:
                mr = mb_ui if d == 0 else mb_li
                q["df"], q["bdf"] = pslot()
                P.op("pe", I("matmul", q["df"], q["gmh"], mr, start=True, stop=False), reads=[sb(q, "gmask"), b_const], writes=[q["bdf"]])
                P.op("pe", I("matmul", q["df"], q["gml"], mr, start=False, stop=True), reads=[sb(q, "gmask"), b_const], writes=[q["bdf"]])
            if KSUB < 3:
                return
            for (q, d, h, t, lat) in st:
                P.op("act", I("activation", out=q["decT"], in_=q["df"], func=AF.Exp), reads=[q["bdf"]], writes=[sb(q, "decT")])
            if KSUB < 5:
                return
            for (q, d, h, t, lat) in st:
                mbd, mx, mi = (mb_ubd, mb_ux, mb_ui) if d == 0 else (mb_lbd, mb_lx, mb_li)
                t1 = q["dmbd"].bitcast(BF16)[:, 0:128]
                P.op("dve", I("scalar_tensor_tensor", out=t1, in0=q["G"], scalar=q["beta"], in1=q["decT"], op0=ALU.mult, op1=ALU.mult),
                     reads=[q["bG"], q["bg"], sb(q, "decT")], writes=[sb(q, "dmbd")])
                P.op("dve", I("tensor_tensor", out=q["Mbd"], in0=t1, in1=mbd, op=ALU.mult), reads=[sb(q, "dmbd"), b_const], writes=[sb(q, "Mbd")])
                P.op("dve", I("tensor_tensor", out=q["XT"], in0=t1, in1=mx, op=ALU.mult), reads=[sb(q, "dmbd"), b_const], writes=[sb(q, "XT")])
                if lat:
                    t2 = q["dmq"].bitcast(BF16)[:, 0:128]
                    P.op("dve", I("tensor_tensor", out=t2, in0=q["QK"], in1=q["decT"], op=ALU.mult), reads=[q["bQK"], sb(q, "decT")], writes=[sb(q, "dmq")])
                    P.op("dve", I("tensor_tensor", out=q["QKd"], in0=t2, in1=mi, op=ALU.mult), reads=[sb(q, "dmq"), b_const], writes=[sb(q, "QKd")])
            if KSUB < 6:
                return
            for (q, d, h, t, lat) in st:
                ns, q["bns"] = pslot(); q["ns"] = ns.bitcast(BF16)[:, 0:128]
                P.op("pe", I("transpose", out=q["ns"], in_=q["Mbd"], identity=ident_bf), reads=[sb(q, "Mbd"), b_const], writes=[q["bns"]])
                P.op("act", I("activation", out=q["Nbd"], in_=q["ns"], func=AF.Copy), reads=[q["bns"]], writes=[sb(q, "Nbd")])
                P.op("dve", I("tensor_tensor", out=q["R0"], in0=ident_bf, in1=q["Mbd"], op=ALU.subtract), reads=[sb(q, "Mbd"), b_const], writes=[sb(q, "R0")])
                q["cur"] = ("Mbd", "Nbd", "R0", "RT0")
            if KSUB < 7:
                return
            for lvl in range(5):
                pn, ptn = ("P0", "PT0") if lvl % 2 == 0 else ("P1", "PT1")
                rn_ = "R1" if lvl % 2 == 0 else "R0"
                last = lvl == 4
                for (q, d, h, t, lat) in st:
                    pw, pwt, r_, _ = q["cur"]
                    if not last:
                        q["p2"], q["bp2"] = pslot()
                        mmq(q["p2"], q[pwt], q[pw], [sb(q, pw), sb(q, pwt)], q["bp2"])
                    q["p2t"], q["bp2t"] = pslot()
                    mmq(q["p2t"], q[pw], q[pwt], [sb(q, pw), sb(q, pwt)], q["bp2t"])
                for (q, d, h, t, lat) in st:
                    if not last:
                        P.op("act", I("activation", out=q[pn], in_=q["p2"], func=AF.Copy), reads=[q["bp2"]], writes=[sb(q, pn)])
                    P.op("act", I("activation", out=q[ptn], in_=q["p2t"], func=AF.Copy), reads=[q["bp2t"]], writes=[sb(q, ptn)])
                for (q, d, h, t, lat) in st:
                    pw, pwt, r_, _ = q["cur"]
                    q["ra"], q["bra"] = pslot()
                    mmq(q["ra"], q[ptn], q[r_], [sb(q, r_), sb(q, ptn)], q["bra"])
                for (q, d, h, t, lat) in st:
                    pw, pwt, r_, _ = q["cur"]
                    P.op("dve", I("tensor_tensor", out=q[rn_], in0=q["ra"], in1=q[r_], op=ALU.add),
                         reads=[q["bra"], sb(q, r_)], writes=[sb(q, rn_)])
                    q["cur"] = (pn, ptn, rn_, None)
            for (q, d, h, t, lat) in st:
                _, _, r_, _ = q["cur"]
                rts, q["brts"] = pslot(); q["rts"] = rts.bitcast(BF16)[:, 0:128]
                P.op("pe", I("transpose", out=q["rts"], in_=q[r_], identity=ident_bf), reads=[sb(q, r_), b_const], writes=[q["brts"]])
            for (q, d, h, t, lat) in st:
                _, _, r_, _ = q["cur"]
                P.op("act", I("activation", out=q["RT0"], in_=q["rts"], func=AF.Copy), reads=[q["brts"]], writes=[sb(q, "RT0")])
                q["cur"] = (None, None, r_, "RT0")
            if KSUB < 8:
                return
            for (q, d, h, t, lat) in st:
                _, _, r_, rt_ = q["cur"]
                q["z"], q["bz"] = pslot()
                mmq(q["z"], q["XT"], q[rt_], [sb(q, "XT"), sb(q, rt_)], q["bz"])
            for (q, d, h, t, lat) in st:
                P.op("act", I("activation", out=q["ZT"], in_=q["z"], func=AF.Copy), reads=[q["bz"]], writes=[sb(q, "ZT")])
            for (q, d, h, t, lat) in st:
                _, _, r_, rt_ = q["cur"]
                q["w"], q["bw"] = pslot()
                mmq(q["w"], q["ZT"], q[r_], [sb(q, "ZT"), sb(q, r_)], q["bw"])
            for (q, d, h, t, lat) in st:
                _, _, r_, rt_ = q["cur"]
                P.op("dve", I("scalar_tensor_tensor", out=q["AinvT"], in0=q["w"], scalar=-1.0, in1=q[r_], op0=ALU.mult, op1=ALU.add),
                     reads=[q["bw"], sb(q, r_)], writes=[sb(q, "AinvT")])
            if KSUB < 9:
                return
            for (q, d, h, t, lat) in st:
                q["a"], q["ba"] = pslot()
                mmq(q["a"], q["kT"], q["Sbf"], [q["bk"], sb(q, "Sbf")], q["ba"])
            for (q, d, h, t, lat) in st:
                P.op("dve", I("scalar_tensor_tensor", out=q["r"], in0=q["a"], scalar=q["neg"], in1=q["vtok"], op0=ALU.mult, op1=ALU.add),
                     reads=[q["ba"], q["bg"], sb(q, "vtok")], writes=[sb(q, "r")])
            for (q, d, h, t, lat) in st:
                q["bb"], q["bbb"] = pslot()
                mmq(q["bb"], q["AinvT"], q["r"], [sb(q, "AinvT"), sb(q, "r")], q["bbb"])
            for (q, d, h, t, lat) in st:
                P.op("act", I("activation", out=q["vnew"], in_=q["bb"], func=AF.Copy, scale=q["beta"]), reads=[q["bbb"], q["bg"]], writes=[sb(q, "vnew")])
            for (q, d, h, t, lat) in st:
                if lat:
                    q["o1"], q["bo1"] = pslot()
                    mmq(q["o1"], q["qT"], q["Sbf"], [q["bq"], sb(q, "Sbf")], q["bo1"])
                    q["o2"], q["bo2"] = pslot()
                    mmq(q["o2"], q["QKd"], q["vnew"], [sb(q, "QKd"), sb(q, "vnew")], q["bo2"])
                q["sp"], q["bsp"] = pslot()
                mmq(q["sp"], q["kd"], q["vnew"], [sb(q, "kd"), sb(q, "vnew")], q["bsp"])
            for (q, d, h, t, lat) in st:
                if lat:
                    oa = o_acc[:, t - 2, h * 128:(h + 1) * 128]
                    bo = b_oacc[t - 2][h]
                    tmp = q["gmask"]
                    if not o_written[t - 2][h]:
                        P.op("act", I("activation", out=tmp, in_=q["o2"], func=AF.Copy), reads=[q["bo2"]], writes=[sb(q, "gmask")])
                        o_written[t - 2][h] = True
                    else:
                        P.op("dve", I("tensor_tensor", out=tmp, in0=q["o2"], in1=oa, op=ALU.add), reads=[q["bo2"], bo], writes=[sb(q, "gmask")])
                    P.op("dve", I("scalar_tensor_tensor", out=oa, in0=q["o1"], scalar=q["eg"], in1=tmp, op0=ALU.mult, op1=ALU.add),
                         reads=[q["bo1"], q["bg"], sb(q, "gmask")], writes=[bo])
                P.op("dve", I("scalar_tensor_tensor", out=q["S"], in0=q["S"], scalar=q["gl"], in1=q["sp"], op0=ALU.mult, op1=ALU.add),
                     reads=[sb(q, "S"), q["bg"], q["bsp"]], writes=[sb(q, "S")])
                P.op("act", I("activation", out=q["Sbf"], in_=q["S"], func=AF.Copy), reads=[sb(q, "S")], writes=[sb(q, "Sbf")])
        for step2 in range(min(36, KSTEPS)):
            scan_step(step2)
        P.barrier()

        if stop <= 6:
            return
        WB = Alloc(arena[:, RB:RB + 54 * KB], 54 * KB)
        WinA = WB([8, 1024], BF16); Wz = WB([8, 512], BF16); Wout = WB([8, 1024], BF16)
        b_w7 = P.buf()
        sets7 = []
        for i7 in range(2):
            d7 = {}
            if i7 == 0:
                d7["xTt"] = WB([8, 128], BF16); d7["u"] = WB([512], F32); d7["v"] = WB([512], F32); d7["vn"] = WB([512], BF16)
                d7["y"] = WB([1024], BF16); d7["yT"] = WB([8, 128], BF16); d7["sz"] = WB([512], F32); d7["st6"] = WB([8], F32); d7["ss4"] = WB([4], F32)
            else:
                d7["u"] = xin2[0][:, 0:512]; d7["v"] = xin2[0][:, 512:1024]
                d7["sz"] = xin[0][:, 0:512]
                d7["y"] = xin[0][:, 512:1024].bitcast(BF16)
                d7["xTt"] = V(arena, RD + 1152, [8, 128], BF16); d7["vn"] = V(arena, RD + 1152 + 2048, [512], BF16)
                d7["yT"] = V(arena, RD + 4224, [8, 128], BF16)
                d7["st6"] = WB([8], F32); d7["ss4"] = WB([4], F32)
            for nm in ("xTt", "u", "v", "vn", "y", "yT", "sz", "st"):
                d7["b_" + nm] = P.buf()
            sets7.append(d7)
        winv = win_d.rearrange("(kc p) n -> p kc n", p=128)
        P.dma("pool", sem_w[0], WinA, winv[:, :, 0:1024], writes=[b_w7])
        P.dma("pool", sem_w[1], Wz, winv[:, :, 2560:3072], writes=[b_w7])
        P.dma("pool", sem_w[2], Wout, wout_d.rearrange("(kc p) n -> p kc n", p=128), writes=[b_w7])
        make_bc(bcG, b, 2, b_bcG, xin[1], b_xin[1])
        def p7_a(tt, xTt, u_sb, v_sb, vn_bf, y_bf, yTt, sz, st6, ss4, b_xTt, b_u, b_v, b_vn, b_y, b_yT, b_sz, b_st):
            xt = x_res[:, tt, :]
            P.dma("sp", sem_x[tt % 2], xt, x_d[b, tt * 128:(tt + 1) * 128, :], writes=[b_xres[tt]])
            norm_mod_T(xt, b_xres[tt], bcA, bcS, xTt, [b_xTt], 0, xmb, b_xmb)
            for hf, bk in ((0, 1), (1, 2)):
                for kc in range(8):
                    P.op("pe", lambda e, kc=kc, hf=hf, bk=bk: e.matmul(banks[bk][:, :], xTt[:, kc, :], WinA[:, kc, hf * 512:(hf + 1) * 512],
                                                                     start=(kc == 0), stop=(kc == 7)), reads=[b_xTt, b_w7], writes=[b_ps[bk]])
            for kc in range(8):
                P.op("pe", lambda e, kc=kc: e.matmul(banks[3][:, :], xTt[:, kc, :], Wz[:, kc, :], start=(kc == 0), stop=(kc == 7)),
                     reads=[b_xTt, b_w7], writes=[b_ps[3]])
            P.op("act", lambda e: e.activation(out=u_sb, in_=banks[1][:, :], func=AF.Gelu_apprx_tanh), reads=[b_ps[1]], writes=[b_u])
            P.op("act", lambda e: e.activation(out=v_sb, in_=banks[2][:, :], func=AF.Gelu_apprx_tanh), reads=[b_ps[2]], writes=[b_v])
            P.op("act", lambda e: e.activation(out=sz, in_=banks[3][:, :], func=AF.Silu), reads=[b_ps[3]], writes=[b_sz])
            P.op("dve", lambda e: e.bn_stats(out=st6[:, 0:6], in_=v_sb), reads=[b_v], writes=[b_st])
            P.op("dve", lambda e: e.bn_aggr(out=st6[:, 6:8], in_=st6[:, 0:6]), reads=[b_st], writes=[b_st])
            P.op("dve", lambda e: e.tensor_scalar(out=st6[:, 7:8], in0=st6[:, 7:8], scalar1=EPS, scalar2=None, op0=ALU.add), reads=[b_st], writes=[b_st])
            P.op("act", lambda e: e.activation(out=st6[:, 7:8], in_=st6[:, 7:8], func=AF.Sqrt), reads=[b_st], writes=[b_st])
            P.op("dve", lambda e: e.reciprocal(out=st6[:, 7:8], in_=st6[:, 7:8]), reads=[b_st], writes=[b_st])
            P.op("dve", lambda e: e.tensor_scalar(out=v_sb, in0=v_sb, scalar1=st6[:, 6:7], scalar2=st6[:, 7:8], op0=ALU.subtract, op1=ALU.mult),
                 reads=[b_v, b_st], writes=[b_v])
            P.op("dve", lambda e: e.tensor_tensor(out=v_sb, in0=v_sb, in1=lng_bc, op=ALU.mult), reads=[b_v, b_par], writes=[b_v])
            P.op("dve", lambda e: e.tensor_tensor(out=vn_bf, in0=v_sb, in1=lnb_bc, op=ALU.add), reads=[b_v, b_par], writes=[b_vn])
        def p7_b(tt, xTt, u_sb, v_sb, vn_bf, y_bf, yTt, sz, st6, ss4, b_xTt, b_u, b_v, b_vn, b_y, b_yT, b_sz, b_st):
            xt = x_res[:, tt, :]
            for h in range(4):
                P.op("pe", lambda e, h=h: e.matmul(banks[4][:, h * 128:(h + 1) * 128], wsT[:, h, :], vn_bf[:, h * 128:(h + 1) * 128], start=True, stop=True),
                     reads=[b_vn, b_par], writes=[b_ps[4]])
            for h in range(4):
                hs = slice(h * 128, (h + 1) * 128)
                P.op("dve", lambda e, h=h, hs=hs: e.scalar_tensor_tensor(out=y_bf[:, hs], in0=banks[4][:, hs], scalar=bsT[:, h:h + 1], in1=u_sb[:, hs],
                                                                       op0=ALU.add, op1=ALU.mult), reads=[b_ps[4], b_par, b_u], writes=[b_y])
            for h in range(4):
                hs = slice(h * 128, (h + 1) * 128)
                P.op("act", lambda e, h=h, hs=hs, tt=tt: e.activation(out=u_sb[:, hs], in_=o_acc[:, tt, hs], func=AF.Square, accum_out=ss4[:, h:h + 1]),
                     reads=[b_oacc[tt][h], b_y], writes=[b_u, b_st])
            rstd_from_ss(ss4, 128.0, b_st)
            for h in range(4):
                hs = slice(h * 128, (h + 1) * 128)
                P.op("dve", lambda e, h=h, hs=hs, tt=tt: e.scalar_tensor_tensor(out=u_sb[:, hs], in0=o_acc[:, tt, hs], scalar=ss4[:, h:h + 1], in1=onorm_bc,
                                                                              op0=ALU.mult, op1=ALU.mult), reads=[b_oacc[tt][h], b_st, b_par, b_u], writes=[b_u])
            P.op("dve", lambda e: e.tensor_tensor(out=y_bf[:, 512:1024], in0=u_sb, in1=sz, op=ALU.mult), reads=[b_u, b_sz], writes=[b_y])
            pv = banks[5][:, 0:512].bitcast(BF16)
            for ch in range(8):
                P.op("pe", lambda e, ch=ch: e.transpose(out=pv[:, ch * 128:(ch + 1) * 128], in_=y_bf[:, ch * 128:(ch + 1) * 128], identity=ident_bf),
                     reads=[b_y, b_const], writes=[b_ps[5]])
            P.op("act", lambda e: e.activation(out=yTt, in_=pv.rearrange("p (a b) -> p a b", a=8), func=AF.Copy), reads=[b_ps[5]], writes=[b_yT])
            for hf in range(2):
                bk = 6 + hf
                for ch in range(8):
                    P.op("pe", lambda e, ch=ch, hf=hf, bk=bk: e.matmul(banks[bk][:, :], yTt[:, ch, :], Wout[:, ch, hf * 512:(hf + 1) * 512],
                                                                     start=(ch == 0), stop=(ch == 7)), reads=[b_yT, b_w7], writes=[b_ps[bk]])
            for hf in range(2):
                hs = slice(hf * 512, (hf + 1) * 512)
                P.op("dve", lambda e, hf=hf, hs=hs: e.tensor_tensor(out=junk_f32[:, hs], in0=banks[6 + hf][:, :], in1=bcG[:, hs], op=ALU.mult),
                     reads=[b_ps[6 + hf], b_bcG], writes=[b_jf])
            P.op("pool", lambda e, xt=xt: e.tensor_tensor(out=xt, in0=xt, in1=junk_f32, op=ALU.add), reads=[b_jf, b_xres[tt]], writes=[b_xres[tt]])
        def args7(tt):
            d7 = sets7[tt % 2]
            return (tt, d7["xTt"], d7["u"], d7["v"], d7["vn"], d7["y"], d7["yT"], d7["sz"], d7["st6"], d7["ss4"],
                    d7["b_xTt"], d7["b_u"], d7["b_v"], d7["b_vn"], d7["b_y"], d7["b_yT"], d7["b_sz"], d7["b_st"])
        def cap(fn, *a):
            P.capture = []
            fn(*a)
            lst = P.capture
            P.capture = None
            return lst
        P.replay_merged(cap(p7_a, *args7(0)), [])
        for tt in range(1, 16):
            P.replay_merged(cap(p7_a, *args7(tt)), cap(p7_b, *args7(tt - 1)))
        P.replay_merged([], cap(p7_b, *args7(15)))
        P.barrier()
        if dbg and b == 0:
            out_toks.append(P.dma("sp", sem_o[0], dbg_d, x_res, reads=b_xres))
            P.barrier()

        if stop <= 7:
            return
        MA = Alloc(arena[:, RB:ARENA], ARENA - RB)
        h2T = MA([8, 2048], BF16)
        b_h2T = [P.buf() for _ in range(16)]
        GU = [MA([8, 1024], BF16) for _ in range(2)]; DW = [MA([4, 1024], BF16) for _ in range(2)]
        b_GU = [P.buf() for _ in range(2)]; b_DW = [P.buf() for _ in range(2)]
        act_t = [MA([4, 512], BF16) for _ in range(2)]; b_act = [P.buf() for _ in range(2)]
        sg = [MA([512], F32) for _ in range(2)]; b_sg = [P.buf() for _ in range(2)]
        h2f = MA([1024], F32); f32T = MA([8, 128], F32); b_f32T = P.buf()
        cb = MA([16, 16], F32); b_cb = [P.buf() for _ in range(16)]
        lg = MA([20], F32); rt = MA([32], F32); b_rt = P.buf()
        small2 = MA([16], F32); b_small2 = P.buf()
        bc2A = MA([1024], F32); bc2S = MA([1024], F32); bc2G = MA([1024], F32); tb_ = MA([1024], F32)
        b_2A, b_2S, b_2G, b_tb = P.buf(), P.buf(), P.buf(), P.buf()
        jb = MA([1024], BF16); b_jb = P.buf()
        load(tb_, n2g_d.partition_broadcast(128), writes=[b_tb])
        P.op("dve", lambda e: e.tensor_copy(out=h2f, in_=tb_), reads=[b_tb], writes=[b_jf])
        g2_bc = h2f
        make_bc(bc2A, b, 4, b_2A, tb_, b_tb, g_bc=g2_bc, b_g=b_jf)
        make_bc(bc2S, b, 3, b_2S, tb_, b_tb)
        make_bc(bc2G, b, 5, b_2G, tb_, b_tb)
        def route_tile(tt, ev):
            small2, b_small2, jb, b_jb, h2f, b_jf, f32T, b_f32T, lg, rt, b_rt, bkA, bkB, bkC = ev
            ss = small2[:, 0:1]
            src = x_res[:, tt, :]
            P.op("act", lambda e, src=src: e.activation(out=jb, in_=src, func=AF.Square, accum_out=ss), reads=[b_xres[tt]], writes=[b_jb, b_small2])
            rstd_from_ss(ss, 1024.0, b_small2)
            P.op("dve", lambda e, src=src: e.scalar_tensor_tensor(out=h2f, in0=src, scalar=ss, in1=bc2A, op0=ALU.mult, op1=ALU.mult),
                 reads=[b_xres[tt], b_small2, b_2A], writes=[b_jf])
            P.op("dve", lambda e: e.tensor_tensor(out=h2f, in0=h2f, in1=bc2S, op=ALU.add), reads=[b_jf, b_2S], writes=[b_jf])
            for kc in range(8):
                bk = (bkA, bkB)[kc // 4]
                P.op("pe", lambda e, kc=kc, bk=bk: e.transpose(out=banks[bk][:, (kc % 4) * 128:(kc % 4 + 1) * 128], in_=h2f[:, kc * 128:(kc + 1) * 128], identity=ident),
                     reads=[b_jf, b_const], writes=[b_ps[bk]])
            for hf in range(2):
                P.op("act", lambda e, hf=hf, tt=tt: e.activation(out=h2T[:, hf * 4:(hf + 1) * 4, tt * 128:(tt + 1) * 128],
                                                                 in_=banks[(bkA, bkB)[hf]][:, :].rearrange("p (a b) -> p a b", a=4), func=AF.Copy),
                     reads=[b_ps[(bkA, bkB)[hf]]], writes=[b_h2T[tt]])
                P.op("dve", lambda e, hf=hf: e.tensor_copy(out=f32T[:, hf * 4:(hf + 1) * 4, :], in_=banks[(bkA, bkB)[hf]][:, :].rearrange("p (a b) -> p a b", a=4)),
                     reads=[b_ps[(bkA, bkB)[hf]]], writes=[b_f32T])
            for kc in range(8):
                P.op("pe", lambda e, kc=kc: e.matmul(banks[bkC][:, 0:20], f32T[:, kc, :], Wr32[:, kc, :], start=(kc == 0), stop=(kc == 7)),
                     reads=[b_f32T, b_par], writes=[b_ps[bkC]])
            R_ = [b_rt]

            def dv(fn, extra_r=()):
                P.op("dve", fn, reads=R_ + list(extra_r), writes=R_)
            gmx, ngm, gsum, pg, m1, m2, dd, w1g, w2g = (rt[:, i:i + 1] for i in range(9))
            ohg = rt[:, 12:16]; es = rt[:, 16:20]; oh1 = rt[:, 20:24]; es2 = rt[:, 24:28]; oh2 = rt[:, 28:32]
            P.op("dve", lambda e: e.tensor_tensor(out=lg, in0=banks[bkC][:, 0:20], in1=brt_bc, op=ALU.add), reads=[b_ps[bkC], b_par], writes=R_)
            dv(lambda e: e.tensor_reduce(out=gmx, in_=lg[:, 0:4], axis=AX.X, op=ALU.max))
            dv(lambda e: e.tensor_scalar(out=ohg, in0=lg[:, 0:4], scalar1=gmx, scalar2=None, op0=ALU.is_equal))
            dv(lambda e: e.tensor_scalar(out=ngm, in0=gmx, scalar1=-1.0, scalar2=None, op0=ALU.mult))
            P.op("act", lambda e: e.activation(out=es2, in_=lg[:, 0:4], func=AF.Exp, bias=ngm, accum_out=gsum), reads=R_, writes=R_)
            dv(lambda e: e.reciprocal(out=pg, in_=gsum))
            dv(lambda e: e.tensor_scalar(out=es, in0=lg[:, 4:8], scalar1=ohg[:, 0:1], scalar2=None, op0=ALU.mult))
            for g in range(1, 4):
                dv(lambda e, g=g: e.scalar_tensor_tensor(out=es, in0=lg[:, 4 + 4 * g:8 + 4 * g], scalar=ohg[:, g:g + 1], in1=es, op0=ALU.mult, op1=ALU.add))
            dv(lambda e: e.tensor_reduce(out=m1, in_=es, axis=AX.X, op=ALU.max))
            dv(lambda e: e.tensor_scalar(out=oh1, in0=es, scalar1=m1, scalar2=None, op0=ALU.is_equal))
            dv(lambda e: e.scalar_tensor_tensor(out=es2, in0=oh1, scalar=-1e30, in1=es, op0=ALU.mult, op1=ALU.add))
            dv(lambda e: e.tensor_reduce(out=m2, in_=es2, axis=AX.X, op=ALU.max))
            dv(lambda e: e.tensor_scalar(out=oh2, in0=es2, scalar1=m2, scalar2=None, op0=ALU.is_equal))
            dv(lambda e: e.tensor_tensor(out=dd, in0=m1, in1=m2, op=ALU.subtract))
            P.op("act", lambda e: e.activation(out=dd, in_=dd, func=AF.Sigmoid), reads=R_, writes=R_)
            dv(lambda e: e.tensor_tensor(out=w1g, in0=dd, in1=pg, op=ALU.mult))
            dv(lambda e: e.tensor_tensor(out=w2g, in0=pg, in1=w1g, op=ALU.subtract))
            dv(lambda e: e.tensor_scalar(out=es, in0=oh1, scalar1=w1g, scalar2=None, op0=ALU.mult))
            dv(lambda e: e.scalar_tensor_tensor(out=es, in0=oh2, scalar=w2g, in1=es, op0=ALU.mult, op1=ALU.add))
            for g in range(4):
                P.op("dve", lambda e, g=g, tt=tt: e.tensor_scalar(out=cb[:, tt, 4 * g:4 * g + 4], in0=es, scalar1=ohg[:, g:g + 1], scalar2=None, op0=ALU.mult),
                     reads=R_, writes=[b_cb[tt]])
        f32T2 = MA([8, 128], F32); jb2 = MA([1024], BF16); lg2 = MA([20], F32); rt2 = MA([32], F32); small3 = MA([16], F32)
        ev_r = [(small2, b_small2, jb, b_jb, h2f, b_jf, f32T, b_f32T, lg, rt, b_rt, 0, 1, 2),
                (small3, P.buf(), jb2, P.buf(), tb_, b_tb, f32T2, P.buf(), lg2, rt2, P.buf(), 3, 4, 5)]
        for tt in range(0, 16, 2):
            P.capture = []
            route_tile(tt, ev_r[0])
            A_ = P.capture
            P.capture = []
            route_tile(tt + 1, ev_r[1])
            B_ = P.capture
            P.capture = None
            P.replay_merged(A_, B_)
        if stop <= 8:
            return
        def emit_GU(ex, s, tb4, a_s):
            for fc in range(4):
                gs = fc % 2
                bg_, bu_ = (0, 1) if gs == 0 else (2, 3)
                for kc in range(8):
                    P.op("pe", I("matmul", banks[bg_][:, :], GU[s][:, kc, fc * 128:(fc + 1) * 128], h2T[:, kc, tb4 * 512:(tb4 + 1) * 512], start=(kc == 0), stop=(kc == 7)),
                         reads=[b_GU[s]] + b_h2T[tb4 * 4:tb4 * 4 + 4], writes=[b_ps[bg_]])
                for kc in range(8):
                    P.op("pe", I("matmul", banks[bu_][:, :], GU[s][:, kc, 512 + fc * 128:512 + (fc + 1) * 128], h2T[:, kc, tb4 * 512:(tb4 + 1) * 512], start=(kc == 0), stop=(kc == 7)),
                         reads=[b_GU[s]] + b_h2T[tb4 * 4:tb4 * 4 + 4], writes=[b_ps[bu_]])
                P.op("act", I("activation", out=sg[gs], in_=banks[bg_][:, :], func=AF.Silu), reads=[b_ps[bg_]], writes=[b_sg[gs]])
                P.op("dve", I("tensor_tensor", out=act_t[a_s][:, fc, :], in0=sg[gs], in1=banks[bu_][:, :], op=ALU.mult),
                     reads=[b_sg[gs], b_ps[bu_]], writes=[b_act[a_s]])

        def emit_DOWN(ex, s, tb4, a_s):
            for t4 in range(4):
                tt = tb4 * 4 + t4
                ds = t4 % 2
                for hf in range(2):
                    bk = 4 + ds * 2 + hf
                    for fc in range(4):
                        P.op("pe", I("matmul", banks[bk][:, :], act_t[a_s][:, fc, t4 * 128:(t4 + 1) * 128], DW[s][:, fc, hf * 512:(hf + 1) * 512], start=(fc == 0), stop=(fc == 3)),
                             reads=[b_act[a_s], b_DW[s]], writes=[b_ps[bk]])
                for hf in range(2):
                    bk = 4 + ds * 2 + hf
                    hs = slice(hf * 512, (hf + 1) * 512)
                    P.op("dve", I("scalar_tensor_tensor", out=x_res[:, tt, hs], in0=banks[bk][:, :], scalar=cb[:, tt, ex:ex + 1], in1=x_res[:, tt, hs], op0=ALU.mult, op1=ALU.add),
                         reads=[b_ps[bk], b_cb[tt], b_xres[tt]], writes=[b_xres[tt]])

        pending = None
        gcount = 0
        for ex in range(16):
            s = ex % 2
            P.dma("pool", sem_w[s], GU[s], wgu_d[ex].rearrange("(kc p) n -> p kc n", p=128), writes=[b_GU[s]])
            P.dma("pool", sem_w[2 + s], DW[s], wdn_d[ex].rearrange("(fc p) n -> p fc n", p=128), writes=[b_DW[s]])
            for fc in range(4):
                P.op("pool", I("tensor_tensor", out=DW[s][:, fc, :], in0=DW[s][:, fc, :], in1=bc2G, op=ALU.mult),
                     reads=[b_DW[s], b_2G], writes=[b_DW[s]])
            for tb4 in range(4):
                a_s = gcount % 2
                emit_GU(ex, s, tb4, a_s)
                if pending is not None:
                    emit_DOWN(*pending)
                pending = (ex, s, tb4, a_s)
                gcount += 1
        emit_DOWN(*pending)
        if stop <= 9:
            return
        load(tb_, fg_d.partition_broadcast(128), writes=[b_tb])
        def fin_tile(tt, small_, b_small_, jb_, b_jb_, ot_, b_ot_, sem_):
            ss = small_[:, 0:1]
            src = x_res[:, tt, :]
            P.op("act", I("activation", out=jb_, in_=src, func=AF.Square, accum_out=ss), reads=[b_xres[tt]], writes=[b_jb_, b_small_])
            rstd_from_ss(ss, 1024.0, b_small_)
            P.op("dve", I("scalar_tensor_tensor", out=ot_, in0=src, scalar=ss, in1=tb_, op0=ALU.mult, op1=ALU.mult),
                 reads=[b_xres[tt], b_small_, b_tb], writes=[b_ot_])
            P.dma("sp", sem_, out_d[b, tt * 128:(tt + 1) * 128, :], ot_, reads=[b_ot_])
        fe = [(small2, b_small2, jb, b_jb, bc2A, b_2A, sem_o[0]), (small3, ev_r[1][1], jb2, ev_r[1][3], bc2S, b_2S, sem_o[1])]
        for tt in range(0, 16, 2):
            P.capture = []
            fin_tile(tt, *fe[0])
            A_ = P.capture
            P.capture = []
            fin_tile(tt + 1, *fe[1])
            B_ = P.capture
            P.capture = None
            P.replay_merged(A_, B_)
        P.barrier()

    for b in range(NB):
        do_batch(b)
        P.barrier()

    P._emit_waits("sp", out_toks + [(k, P.dma_sem_cnt[k]) for k in sem_o if P.dma_sem_cnt[k] > 0])
    P.emit()
    P.close()
    return nc


_NC_CACHE = {}


def kernel(**inputs):
    NB = 2
    if "nc" not in _NC_CACHE:
        _NC_CACHE["nc"] = build(NB)
    nc = _NC_CACHE["nc"]
    f = lambda a: np.ascontiguousarray(np.asarray(a, dtype=np.float32))
    shared = {
        "c_ctx": f(inputs["c_ctx"]).reshape(1, 1024), "w_ada": f(inputs["w_ada"])[0], "b_ada": f(inputs["b_ada"]).reshape(1, 6144),
        "norm1_g": f(inputs["norm1_g"]).reshape(1, 1024), "w_in": f(inputs["w_in"])[0], "ln_a_g": f(inputs["ln_a_g"]).reshape(1, 512),
        "ln_a_b": f(inputs["ln_a_b"]).reshape(1, 512), "w_spatial": f(inputs["w_spatial"])[0], "b_spatial": f(inputs["b_spatial"])[0],
        "conv_qkv": f(inputs["conv_qkv"])[0], "a_log": f(inputs["a_log"]).reshape(1, 8), "dt_bias": f(inputs["dt_bias"]).reshape(1, 8),
        "onorm_g": f(inputs["onorm_g"]).reshape(1, 128), "w_out": f(inputs["w_out"])[0], "norm2_g": f(inputs["norm2_g"]).reshape(1, 1024),
        "w_group": f(inputs["w_group"])[0], "b_group": f(inputs["b_group"]).reshape(1, 4), "w_router": f(inputs["w_router"])[0],
        "b_router": f(inputs["b_router"]).reshape(1, 16), "w_gate_up": f(inputs["w_gate_up"])[0], "w_down": f(inputs["w_down"])[0],
        "final_g": f(inputs["final_g"]).reshape(1, 1024),
    }
    x = f(inputs["x"]); c = f(inputs["c"]); ctx = f(inputs["ctx"])
    in_maps = []
    for i in range(N_CORES):
        m = dict(shared)
        m["x"] = x[i * NB:(i + 1) * NB]; m["c"] = c[i * NB:(i + 1) * NB]; m["ctx"] = ctx[i * NB:(i + 1) * NB]
        in_maps.append(m)
    res = run_bass_kernel_spmd(nc, in_maps, core_ids=list(range(N_CORES)))
    return np.concatenate([r["out"] for r in res.results], axis=0).astype(np.float32)
```

```python
from contextlib import ExitStack
import os
import numpy as np
import concourse.bass as bass
import concourse.mybir as mybir
from concourse.bass_utils import run_bass_kernel_spmd

F32 = mybir.dt.float32
BF16 = mybir.dt.bfloat16
U8 = mybir.dt.uint8
AF = mybir.ActivationFunctionType
ALU = mybir.AluOpType
AX = mybir.AxisListType

N_CORES = 8
KLAT = int(os.environ.get('KLAT', '1'))
KNOQK = int(os.environ.get('KNOQK', '0'))
SAME_ENG_SYNC = int(os.environ.get('KSES', '1'))
EPS = 1e-6


class Buf:
    __slots__ = ("name", "last_w", "readers", "excl")

    def __init__(self, name, excl=False):
        self.name = name
        self.excl = excl
        self.last_w = None
        self.readers = []


class Prog:
    ENG = ("pe", "act", "dve", "pool", "sp")

    def __init__(self, nc):
        self.nc = nc
        self.es = ExitStack()
        self.ops = {e: [] for e in self.ENG}
        self.count = {e: 0 for e in self.ENG}
        self.sems = {}
        for e in self.ENG:
            self.sems[e] = self.es.enter_context(nc.semaphore("s_" + e))
        self.waited = {e: {} for e in self.ENG}
        self.dma_sem_cnt = {}
        self.nbuf = 0
        self.capture = None

    def sbuf(self, name, shape, dtype=F32):
        return self.es.enter_context(self.nc.sbuf_tensor(name, list(shape), dtype))

    def psum(self, name, shape, dtype=F32):
        return self.es.enter_context(self.nc.psum_tensor(name, list(shape), dtype))

    def buf(self, name=None, excl=False):
        self.nbuf += 1
        return Buf(name or f"b{self.nbuf}", excl)

    def dma_sem(self, name):
        key = "d_" + name
        self.sems[key] = self.es.enter_context(self.nc.semaphore(key))
        self.dma_sem_cnt[key] = 0
        return key

    def _deps(self, reads, writes):
        toks = []
        for b in reads:
            if b.last_w is not None:
                toks.append(b.last_w)
        for b in writes:
            if b.last_w is not None:
                toks.append(b.last_w)
            toks.extend(b.readers)
        return toks

    def _emit_waits(self, eng, toks):
        need = {}
        for (k, v) in toks:
            if k == eng and (eng in ("pe", "sp") or not SAME_ENG_SYNC):
                continue
            if v > need.get(k, 0):
                need[k] = v
        for k, v in need.items():
            if self.waited[eng].get(k, 0) >= v:
                continue
            self.waited[eng][k] = v
            self.ops[eng].append(("wait", self.sems[k], v))

    def _commit(self, tok, reads, writes):
        for b in writes:
            b.last_w = tok
            b.readers = []
        for b in reads:
            b.readers.append(tok)

    def op(self, eng, fn, reads=(), writes=()):
        if self.capture is not None:
            self.capture.append(("op", eng, fn, list(reads), list(writes)))
            return None
        writes = [b for b in writes if b is not None] + [b for b in reads if b is not None and b.excl]
        reads = [b for b in reads if b is not None and not b.excl]
        self._emit_waits(eng, self._deps(reads, writes))
        self.count[eng] += 1
        tok = (eng, self.count[eng])
        self.ops[eng].append(("op", fn, self.sems[eng], 1))
        self._commit(tok, reads, writes)
        return tok

    def dma(self, queue, semkey, out_ap, in_ap, reads=(), writes=()):
        if self.capture is not None:
            self.capture.append(("dma", queue, semkey, out_ap, in_ap, list(reads), list(writes)))
            return None
        reads = [b for b in reads if b is not None]
        writes = [b for b in writes if b is not None]
        self._emit_waits(queue, self._deps(reads, writes))
        if self.dma_sem_cnt[semkey] > 0:
            self._emit_waits(queue, [(semkey, self.dma_sem_cnt[semkey])])
        self.dma_sem_cnt[semkey] += 16
        tok = (semkey, self.dma_sem_cnt[semkey])

        def fn(e, out_ap=out_ap, in_ap=in_ap):
            return e.dma_start(out=out_ap, in_=in_ap)
        self.ops[queue].append(("op", fn, self.sems[semkey], 16))
        self._commit(tok, reads, writes)
        return tok

    def replay_merged(self, A, B):
        la, lb = len(A), len(B)
        ia = ib = 0
        while ia < la or ib < lb:
            if ib >= lb or (ia < la and ia * lb <= ib * la):
                it = A[ia]; ia += 1
            else:
                it = B[ib]; ib += 1
            if it[0] == "op":
                self.op(it[1], it[2], it[3], it[4])
            else:
                self.dma(it[1], it[2], it[3], it[4], it[5], it[6])

    def barrier(self):
        toks = [(e, self.count[e]) for e in self.ENG if e != "sp" and self.count[e] > 0]
        toks += [(k, v) for k, v in self.dma_sem_cnt.items() if v > 0]
        for e in self.ENG:
            self._emit_waits(e, toks)

    def emit(self):
        nc = self.nc
        P = self
        with nc.Block() as block:
            def run(e, engine):
                for item in P.ops[e]:
                    if item[0] == "wait":
                        engine.wait_ge(item[1], item[2])
                    else:
                        _, fn, sem, inc = item
                        fn(engine).then_inc(sem, inc)

            @block.sync
            def _(eng):
                run("sp", eng)

            @block.tensor
            def _(eng):
                run("pe", eng)

            @block.scalar
            def _(eng):
                run("act", eng)

            @block.vector
            def _(eng):
                run("dve", eng)

            @block.gpsimd
            def _(eng):
                run("pool", eng)

    def close(self):
        self.es.close()


def I(name, *a, **kw):
    return lambda e: getattr(e, name)(*a, **kw)


def build(NB=2, dbg=False, stop=99, KSTEPS=36, KSUB=99):
    nc = bass.Bass("TRN2", target_bir_lowering=False)

    def din(name, shape):
        return nc.dram_tensor(name, list(shape), F32, kind="ExternalInput").ap()
    x_d = din("x", [NB, 2048, 1024]); ctx_d = din("ctx", [NB, 256, 1024]); c_d = din("c", [NB, 1024])
    cctx_d = din("c_ctx", [1, 1024]); wada_d = din("w_ada", [1024, 6144]); bada_d = din("b_ada", [1, 6144])
    n1g_d = din("norm1_g", [1, 1024]); win_d = din("w_in", [1024, 3088]); lng_d = din("ln_a_g", [1, 512])
    lnb_d = din("ln_a_b", [1, 512]); wsp_d = din("w_spatial", [4, 128, 128]); bsp_d = din("b_spatial", [4, 128])
    conv_d = din("conv_qkv", [5, 1536]); alog_d = din("a_log", [1, 8]); dtb_d = din("dt_bias", [1, 8])
    ong_d = din("onorm_g", [1, 128]); wout_d = din("w_out", [1024, 1024]); n2g_d = din("norm2_g", [1, 1024])
    wgrp_d = din("w_group", [1024, 4]); bgrp_d = din("b_group", [1, 4]); wrt_d = din("w_router", [1024, 16])
    brt_d = din("b_router", [1, 16]); wgu_d = din("w_gate_up", [16, 1024, 1024]); wdn_d = din("w_down", [16, 512, 1024])
    fg_d = din("final_g", [1, 1024])
    out_d = nc.dram_tensor("out", [NB, 2048, 1024], F32, kind="ExternalOutput").ap()
    dbg_d = nc.dram_tensor("dbg", [128, 16, 1024], F32, kind="ExternalOutput").ap() if dbg else None

    P = Prog(nc)
    ARENA = 192 * 1024
    arena = P.sbuf("arena", [128, ARENA], U8)
    pers = P.sbuf("pers", [128, 15 * 1024], U8)
    banks = [P.psum(f"bank{i}", [128, 512]) for i in range(8)]

    def V(base, off, shape, dt, parts=128):
        esz = 2 if dt == BF16 else 4
        n = 1
        for s in shape:
            n *= s
        ap = base[0:parts, off:off + n * esz].bitcast(dt)
        if len(shape) == 2:
            ap = ap.rearrange("p (a b) -> p a b", a=shape[0])
        elif len(shape) == 3:
            ap = ap.rearrange("p (a b c) -> p a b c", a=shape[0], b=shape[1])
        return ap

    class Alloc:
        def __init__(self, base, size):
            self.base, self.size, self.off = base, size, 0

        def __call__(self, shape, dt, parts=128):
            esz = 2 if dt == BF16 else 4
            n = esz
            for s in shape:
                n *= s
            n = (n + 31) // 32 * 32
            off = self.off
            self.off += n
            assert self.off <= self.size, (self.off, self.size)
            return V(self.base, off, shape, dt, parts)

    PA = Alloc(pers, 15 * 1024)
    KB = 1024
    sem_ld = P.dma_sem("ld")
    ident = PA([128], F32); ident_bf = PA([128], BF16); ones_bf = PA([128], BF16)
    m_ui = PA([128], F32); m_us = PA([128], F32); m_li = PA([128], F32); m_ls = PA([128], F32); m_one = PA([128], F32)
    m_ubd = V(arena, 150 * 1024, [128], F32); m_ux = V(arena, 150 * 1024 + 512, [128], F32); m_lbd = V(arena, 150 * 1024 + 1024, [128], F32); m_lx = V(arena, 150 * 1024 + 1536, [128], F32)
    mb_ubd = PA([128], BF16); mb_ux = PA([128], BF16); mb_lbd = PA([128], BF16); mb_lx = PA([128], BF16)
    b_const = P.buf("const")

    def pool_op(fn, reads=(), writes=()):
        return P.op("pool", fn, reads, writes)

    def mk_mask(ap, pattern_step, chmul, cmp):
        pool_op(lambda e: e.memset(ap, 1.0), writes=[b_const])
        pool_op(lambda e: e.affine_select(out=ap, in_=ap, pattern=[[pattern_step, 128]], compare_op=cmp, fill=0.0,
                                          base=0, channel_multiplier=chmul), reads=[b_const], writes=[b_const])
    pool_op(lambda e: e.memset(ident, 0.0), writes=[b_const])
    pool_op(lambda e: e.affine_select(out=ident, in_=ident, pattern=[[-1, 128]], compare_op=ALU.not_equal, fill=1.0,
                                      base=0, channel_multiplier=1), reads=[b_const], writes=[b_const])
    pool_op(lambda e: e.tensor_copy(out=ident_bf, in_=ident), reads=[b_const], writes=[b_const])
    pool_op(lambda e: e.memset(ones_bf, 1.0), writes=[b_const])
    pool_op(lambda e: e.memset(m_one, 1.0), writes=[b_const])
    mk_mask(m_ui, 1, -1, ALU.is_ge)
    mk_mask(m_us, 1, -1, ALU.is_gt)
    mk_mask(m_li, -1, 1, ALU.is_ge)
    mk_mask(m_ls, -1, 1, ALU.is_gt)
    pool_op(lambda e: e.tensor_copy(out=m_ubd, in_=m_us), reads=[b_const], writes=[b_const])
    pool_op(lambda e: e.memset(m_ubd[0:64, 64:128], 0.0), reads=[b_const], writes=[b_const])
    pool_op(lambda e: e.memset(m_ux, 0.0), writes=[b_const])
    pool_op(lambda e: e.memset(m_ux[0:64, 64:128], 1.0), reads=[b_const], writes=[b_const])
    pool_op(lambda e: e.tensor_copy(out=m_lbd, in_=m_ls), reads=[b_const], writes=[b_const])
    pool_op(lambda e: e.memset(m_lbd[64:128, 0:64], 0.0), reads=[b_const], writes=[b_const])
    pool_op(lambda e: e.memset(m_lx, 0.0), writes=[b_const])
    pool_op(lambda e: e.memset(m_lx[64:128, 0:64], 1.0), reads=[b_const], writes=[b_const])

    mb_ui = PA([128], BF16); mb_us = PA([128], BF16); mb_li = PA([128], BF16); mb_ls = PA([128], BF16)
    for dst_, src_ in ((mb_ui, m_ui), (mb_us, m_us), (mb_li, m_li), (mb_ls, m_ls), (mb_ubd, m_ubd), (mb_ux, m_ux), (mb_lbd, m_lbd), (mb_lx, m_lx)):
        pool_op(lambda e, dst_=dst_, src_=src_: e.tensor_copy(out=dst_, in_=src_), reads=[b_const], writes=[b_const])
    eps_t = PA([1], F32)
    pool_op(lambda e: e.memset(eps_t, EPS), writes=[b_const])
    negA = PA([8], F32); dtb_bc = PA([8], F32); onorm_bc = PA([128], F32)
    lng_bc = PA([512], F32); lnb_bc = PA([512], F32)
    wsT = PA([4, 128], BF16); bsT = PA([4], F32); convw = PA([12, 5], F32)
    Wr32 = PA([8, 20], F32); brt_bc = PA([20], F32)
    modT = PA([48, 4], F32)
    b_par = P.buf("params")

    def load(dst, src, reads=(), writes=(), q="sp"):
        return P.dma(q, sem_ld, dst, src, reads=reads, writes=list(writes))

    load(negA, alog_d.partition_broadcast(128), writes=[b_par])
    load(dtb_bc, dtb_d.partition_broadcast(128), writes=[b_par])
    load(onorm_bc, ong_d.partition_broadcast(128), writes=[b_par])
    load(lng_bc, lng_d.partition_broadcast(128), writes=[b_par])
    load(lnb_bc, lnb_d.partition_broadcast(128), writes=[b_par])
    load(brt_bc[:, 0:4], bgrp_d.partition_broadcast(128), writes=[b_par])
    load(brt_bc[:, 4:20], brt_d.partition_broadcast(128), writes=[b_par])
    load(Wr32[:, :, 0:4], wgrp_d.rearrange("(kc p) n -> p kc n", p=128), writes=[b_par])
    load(Wr32[:, :, 4:20], wrt_d.rearrange("(kc p) n -> p kc n", p=128), writes=[b_par])
    P.op("act", lambda e: e.activation(out=negA, in_=negA, func=AF.Exp), reads=[b_par], writes=[b_par])
    P.op("dve", lambda e: e.tensor_scalar(out=negA, in0=negA, scalar1=-1.0, scalar2=None, op0=ALU.mult), reads=[b_par], writes=[b_par])

    A0 = Alloc(arena, ARENA)
    b_tmp = P.buf("setup_tmp")
    b_ps = [P.buf(f"bank{i}", excl=True) for i in range(8)]
    wsp_sb = A0([4, 128], F32); bsp_sb = A0([128], F32, parts=4); conv_sb = A0([1536], F32, parts=5)
    load(wsp_sb, wsp_d.rearrange("h i j -> i h j"), writes=[b_tmp])
    load(bsp_sb, bsp_d, writes=[b_tmp])
    load(conv_sb, conv_d, writes=[b_tmp])
    for h in range(4):
        P.op("pe", lambda e, h=h: e.transpose(out=banks[0][:, h * 128:(h + 1) * 128], in_=wsp_sb[:, h, :], identity=ident),
             reads=[b_tmp, b_const], writes=[b_ps[0]])
    P.op("act", lambda e: e.activation(out=wsT, in_=banks[0][:, 0:512].rearrange("p (a b) -> p a b", a=4), func=AF.Copy),
         reads=[b_ps[0]], writes=[b_par])
    P.op("pe", lambda e: e.transpose(out=banks[1][:, 0:4], in_=bsp_sb, identity=ident[0:4, 0:4]), reads=[b_tmp, b_const], writes=[b_ps[1]])
    P.op("act", lambda e: e.activation(out=bsT, in_=banks[1][:, 0:4], func=AF.Copy), reads=[b_ps[1]], writes=[b_par])
    for cc in range(12):
        P.op("pe", lambda e, cc=cc: e.transpose(out=banks[2][:, cc * 8:cc * 8 + 5], in_=conv_sb[:, cc * 128:(cc + 1) * 128],
                                                identity=ident[0:5, 0:5]), reads=[b_tmp, b_const], writes=[b_ps[2]])
    P.op("act", lambda e: e.activation(out=convw, in_=banks[2][:, 0:96].rearrange("p (a b) -> p a b", a=12)[:, :, 0:5], func=AF.Copy),
         reads=[b_ps[2]], writes=[b_par])

    cT = A0([3, 8], F32); cTb = A0([3, 8], BF16)
    for j in range(NB):
        load(cT[:, j, :], c_d[j].rearrange("(p kc) -> p kc", kc=8), writes=[b_tmp])
    if NB < 2:
        P.op("dve", lambda e: e.memset(cT[:, 1, :], 0.0), writes=[b_tmp])
    load(cT[:, 2, :], cctx_d[0].rearrange("(p kc) -> p kc", kc=8), writes=[b_tmp])
    P.op("act", lambda e: e.activation(out=cTb, in_=cT, func=AF.Silu), reads=[b_tmp], writes=[b_tmp])
    wada_v = wada_d.rearrange("(p kc) n -> p kc n", kc=8)
    wa = [A0([8, 512], BF16) for _ in range(2)]
    b_wa = [P.buf() for _ in range(2)]
    sem_wa = [P.dma_sem(f"wa{i}") for i in range(2)]
    for nb_ in range(12):
        s = nb_ % 2
        P.dma("pool", sem_wa[s], wa[s], wada_v[:, :, nb_ * 512:(nb_ + 1) * 512], writes=[b_wa[s]])
        for c4 in range(4):
            ch = nb_ * 4 + c4
            for kc in range(8):
                P.op("pe", lambda e, s=s, c4=c4, kc=kc, ch=ch: e.matmul(banks[3][:, ch * 4:ch * 4 + 3], wa[s][:, kc, c4 * 128:(c4 + 1) * 128],
                                                                      cTb[:, :, kc], start=(kc == 0), stop=(kc == 7)),
                     reads=[b_wa[s], b_tmp], writes=[b_ps[3]])
    P.op("act", lambda e: e.activation(out=modT[:, :, 0:3], in_=banks[3][:, 0:192].rearrange("p (a b) -> p a b", a=48)[:, :, 0:3], func=AF.Copy),
         reads=[b_ps[3]], writes=[b_par])
    P.barrier()

    def make_bc(dst, j, which, b_dst, tmp_bias, b_tmpb, g_bc=None, b_g=None):
        load(tmp_bias, bada_d[:, which * 1024:(which + 1) * 1024].partition_broadcast(128), writes=[b_tmpb])
        for c8 in range(8):
            ch = which * 8 + c8
            bk = 6 + c8 // 4
            P.op("pe", lambda e, c8=c8, ch=ch, bk=bk: e.matmul(banks[bk][:, (c8 % 4) * 128:(c8 % 4 + 1) * 128],
                                                               modT[:, ch, j:j + 1].to_broadcast([128, 128]), ident, start=True, stop=True),
                 reads=[b_par, b_const], writes=[b_ps[bk]])
        for hf in range(2):
            P.op("dve", lambda e, hf=hf: e.tensor_tensor(out=dst[:, hf * 512:(hf + 1) * 512], in0=banks[6 + hf][:, :],
                                                        in1=tmp_bias[:, hf * 512:(hf + 1) * 512], op=ALU.add),
                 reads=[b_ps[6 + hf], b_tmpb], writes=[b_dst])
        if g_bc is not None:
            P.op("dve", lambda e: e.scalar_tensor_tensor(out=dst, in0=dst, scalar=1.0, in1=g_bc, op0=ALU.add, op1=ALU.mult),
                 reads=[b_dst, b_g], writes=[b_dst])

    def rstd_from_ss(ss, n, b_s):
        P.op("dve", lambda e: e.tensor_scalar(out=ss, in0=ss, scalar1=1.0 / n, scalar2=EPS, op0=ALU.mult, op1=ALU.add), reads=[b_s], writes=[b_s])
        P.op("act", lambda e: e.activation(out=ss, in_=ss, func=AF.Sqrt), reads=[b_s], writes=[b_s])
        P.op("dve", lambda e: e.reciprocal(out=ss, in_=ss), reads=[b_s], writes=[b_s])

    sem_x = [P.dma_sem(f"x{i}") for i in range(2)]
    sem_w = [P.dma_sem(f"w{i}") for i in range(4)]
    sem_o = [P.dma_sem(f"o{i}") for i in range(2)]
    out_toks = []

    def do_batch(b):
        A = Alloc(arena, ARENA)
        RA = 0
        x_res = V(arena, RA, [16, 1024], F32)
        b_xres = [P.buf(f"xres{t}") for t in range(16)]
        xT = V(arena, RA, [8, 2304], BF16)
        b_xT = [P.buf(f"xT{t}") for t in range(18)]
        CT = RA + 36 * KB
        RB = 64 * KB
        qkvT = V(arena, RB, [12, 2304], BF16)
        b_qkv = [[P.buf() for _ in range(18)] for _ in range(12)]
        RC = RB + 54 * KB
        o_acc = V(arena, RC, [16, 512], F32)
        b_oacc = [[P.buf() for _ in range(4)] for _ in range(16)]
        Wqkv = V(arena, RC, [8, 1536], BF16)
        b_wqkv = P.buf()
        RD = RC + 32 * KB
        AD = Alloc(arena[:, RD:ARENA], ARENA - RD)
        gb = AD([18, 16], F32); cumE = AD([18, 40], F32); negE = AD([18, 40], F32)
        b_gb = [P.buf() for _ in range(18)]
        bcA = AD([1024], F32); bcS = AD([1024], F32); bcG = AD([1024], F32)
        b_bcA, b_bcS, b_bcG = P.buf(), P.buf(), P.buf()
        Wab = AD([8, 16], BF16); b_wab = P.buf()
        xin = [AD([1024], F32) for _ in range(2)]; b_xin = [P.buf() for _ in range(2)]
        xmb = AD([1024], BF16); b_xmb = P.buf()
        small = AD([16], F32); b_small0 = P.buf()
        g1_bc = xin[0]
        load(g1_bc, n1g_d.partition_broadcast(128), writes=[b_xin[0]])
        P.dma("pool", sem_w[0], Wab, win_d.rearrange("(kc p) n -> p kc n", p=128)[:, :, 3072:3088], writes=[b_wab])

        def norm_mod_T(src, b_src, A_bc, S_bc, dstT, b_dstT, bank, junk, b_junk, f32T=None, xm_f32=None, tmps=None):
            small_, b_small, junk_f32, b_jf = tmps if tmps is not None else (small, b_small0, junk_f320, b_jf0)
            ss = small_[:, 0:1]
            P.op("act", lambda e: e.activation(out=junk, in_=src, func=AF.Square, accum_out=ss), reads=[b_src], writes=[b_junk, b_small])
            rstd_from_ss(ss, 1024.0, b_small)
            if xm_f32 is None:
                tmp = junk_f32
                P.op("dve", lambda e: e.scalar_tensor_tensor(out=tmp, in0=src, scalar=ss, in1=A_bc, op0=ALU.mult, op1=ALU.mult),
                     reads=[b_src, b_small, b_bcA], writes=[b_jf])
                P.op("dve", lambda e: e.tensor_tensor(out=junk, in0=tmp, in1=S_bc, op=ALU.add), reads=[b_jf, b_bcS], writes=[b_junk])
                pv = banks[bank][:, 0:512].bitcast(BF16)
                for kc in range(8):
                    P.op("pe", lambda e, kc=kc: e.transpose(out=pv[:, kc * 128:(kc + 1) * 128], in_=junk[:, kc * 128:(kc + 1) * 128], identity=ident_bf),
                         reads=[b_junk, b_const], writes=[b_ps[bank]])
                P.op("act", lambda e: e.activation(out=dstT, in_=pv.rearrange("p (a b) -> p a b", a=8), func=AF.Copy),
                     reads=[b_ps[bank]], writes=b_dstT)
            else:
                P.op("dve", lambda e: e.scalar_tensor_tensor(out=xm_f32, in0=src, scalar=ss, in1=A_bc, op0=ALU.mult, op1=ALU.mult),
                     reads=[b_src, b_small, b_bcA], writes=[b_jf])
                P.op("dve", lambda e: e.tensor_tensor(out=xm_f32, in0=xm_f32, in1=S_bc, op=ALU.add), reads=[b_jf, b_bcS], writes=[b_jf])
                for kc in range(8):
                    bk = bank + kc // 4
                    P.op("pe", lambda e, kc=kc, bk=bk: e.transpose(out=banks[bk][:, (kc % 4) * 128:(kc % 4 + 1) * 128],
                                                                   in_=xm_f32[:, kc * 128:(kc + 1) * 128], identity=ident),
                         reads=[b_jf, b_const], writes=[b_ps[bk]])
                for hf in range(2):
                    P.op("act", lambda e, hf=hf: e.activation(out=dstT[:, hf * 4:(hf + 1) * 4, :], in_=banks[bank + hf][:, :].rearrange("p (a b) -> p a b", a=4), func=AF.Copy),
                         reads=[b_ps[bank + hf]], writes=b_dstT)
                    P.op("dve", lambda e, hf=hf: e.tensor_copy(out=f32T[:, hf * 4:(hf + 1) * 4, :], in_=banks[bank + hf][:, :].rearrange("p (a b) -> p a b", a=4)),
                         reads=[b_ps[bank + hf]], writes=[b_f32T])

        junk_f320 = AD([1024], F32); b_jf0 = P.buf()
        junk_f32 = junk_f320; b_jf = b_jf0

        def p1_tile(t, ev):
            if t < 2:
                src_d = ctx_d[b, t * 128:(t + 1) * 128, :]
            else:
                src_d = x_d[b, (t - 2) * 128:(t - 1) * 128, :]
            xt = ev["xin"]; bk0, bk1, bk2 = ev["banks"]
            ghf_ = ev["ghf"]
            P.dma("sp", ev["sem"], xt, src_d, writes=[ev["b_xin"]])
            norm_mod_T(xt, ev["b_xin"], bcA, bcS, xT[:, :, t * 128:(t + 1) * 128], [b_xT[t]], bk0, ev["xmb"], ev["b_xmb"], tmps=ev["tmps"])
            for kc in range(8):
                P.op("pe", I("matmul", banks[bk1][:, 0:16], xT[:, kc, t * 128:(t + 1) * 128], Wab[:, kc, :], start=(kc == 0), stop=(kc == 7)),
                     reads=[b_xT[t], b_wab], writes=[b_ps[bk1]])
            g8 = gb[:, t, 0:8]
            P.op("dve", I("tensor_tensor", out=g8, in0=banks[bk1][:, 0:8], in1=dtb_bc, op=ALU.add), reads=[b_ps[bk1], b_par], writes=[b_gb[t]])
            P.op("act", I("activation", out=gb[:, t, 8:16], in_=banks[bk1][:, 8:16], func=AF.Sigmoid), reads=[b_ps[bk1]], writes=[b_gb[t]])
            P.op("act", I("activation", out=g8, in_=g8, func=AF.Exp), reads=[b_gb[t]], writes=[b_gb[t]])
            P.op("act", I("activation", out=g8, in_=g8, func=AF.Ln, bias=1.0), reads=[b_gb[t]], writes=[b_gb[t]])
            P.op("dve", I("tensor_tensor", out=g8, in0=g8, in1=negA, op=ALU.mult), reads=[b_gb[t], b_par], writes=[b_gb[t]])
            P.op("dve", I("tensor_copy", out=ghb[:, t, 0:8], in_=g8), reads=[b_gb[t]], writes=[b_gb[t]])
            P.op("dve", I("tensor_copy", out=ghf_, in_=ghb[:, t, 0:8]), reads=[b_gb[t]], writes=[b_gb[t]])
            P.op("dve", I("tensor_tensor", out=ghb[:, t, 8:16], in0=g8, in1=ghf_, op=ALU.subtract), reads=[b_gb[t]], writes=[b_gb[t]])
            for mi, mk in enumerate((m_ui, m_ls, m_li, m_us, m_one)):
                P.op("pe", I("matmul", banks[bk2][:, mi * 8:(mi + 1) * 8], mk, g8, start=True, stop=True), reads=[b_gb[t], b_const], writes=[b_ps[bk2]])
            P.op("act", I("activation", out=cumE[:, t, :], in_=banks[bk2][:, 0:40], func=AF.Exp), reads=[b_ps[bk2]], writes=[b_gb[t]])
            P.op("dve", I("tensor_scalar", out=negE[:, t, :], in0=cumE[:, t, :], scalar1=-1.0, scalar2=None, op0=ALU.mult), reads=[b_gb[t]], writes=[b_gb[t]])

        def phase1_tiles(tiles, j):
            make_bc(bcA, j, 1, b_bcA, xin[1], b_xin[1], g_bc=g1_bc, b_g=b_xin[0])
            make_bc(bcS, j, 0, b_bcS, xin[1], b_xin[1])
            for i in range(0, len(tiles), 2):
                P.capture = []
                p1_tile(tiles[i], envs1[0])
                A_ = P.capture
                P.capture = []
                p1_tile(tiles[i + 1], envs1[1])
                B_ = P.capture
                P.capture = None
                P.replay_merged(A_, B_)

        if stop <= 0:
            return
        _x2 = AD([1024], F32); _bx2 = P.buf()
        xin2 = [_x2, _x2]; b_xin2 = [_bx2, _bx2]
        ghb = AD([18, 16], BF16); ghf = AD([8], F32)
        E1 = Alloc(arena[:, RB:RB + 16 * KB], 16 * KB)
        envs1 = [
            {"xin": xin2[0], "b_xin": b_xin2[0], "sem": sem_x[0], "xmb": xmb, "b_xmb": b_xmb, "tmps": None, "ghf": ghf, "banks": (0, 1, 2)},
            {"xin": E1([1024], F32), "b_xin": P.buf(), "sem": sem_x[1], "xmb": E1([1024], BF16), "b_xmb": P.buf(),
             "tmps": (E1([16], F32), P.buf(), E1([1024], F32), P.buf()), "ghf": E1([8], F32), "banks": (3, 4, 5)},
        ]
        phase1_tiles([0, 1], 2)
        phase1_tiles(list(range(2, 18)), b)

        if stop <= 1:
            return
        P.dma("pool", sem_w[1], Wqkv, win_d.rearrange("(kc p) n -> p kc n", p=128)[:, :, 1024:2560], writes=[b_wqkv])
        C5 = Alloc(arena[:, CT:CT + 28 * KB], 28 * KB)
        PTb = [C5([2320], BF16) for _ in range(2)]; b_PTb = [P.buf() for _ in range(2)]
        ACC = C5([2304], F32); b_ACC = P.buf()
        SQB = C5([2304], BF16); b_SQB = P.buf()
        RNb = [C5([512], F32) for _ in range(2)]; b_RNb = [P.buf() for _ in range(2)]
        DG = C5([5, 128], BF16); b_DG = P.buf()
        for i_ in range(2):
            P.op("pool", I("memset", PTb[i_], 0.0), writes=[b_PTb[i_]])
        blocks = [(0, 256)] + [(256 + i * 512, 512) for i in range(4)]
        for cc in range(12):
            pi_ = cc % 2
            PT_ = PTb[pi_]; bPT = b_PTb[pi_]
            for tp in range(5):
                P.op("dve", I("tensor_scalar", out=DG[:, tp, :], in0=ident_bf, scalar1=convw[:, cc, tp:tp + 1], scalar2=None, op0=ALU.mult),
                     reads=[b_par, b_const], writes=[b_DG])
            for bi, (t0, n) in enumerate(blocks):
                bk = bi % 2
                tl = list(range(t0 // 128, (t0 + n) // 128))
                for kc in range(8):
                    P.op("pe", I("matmul", banks[bk][:, 0:n], Wqkv[:, kc, cc * 128:(cc + 1) * 128], xT[:, kc, t0:t0 + n], start=(kc == 0), stop=(kc == 7)),
                         reads=[b_wqkv] + [b_xT[t] for t in tl], writes=[b_ps[bk]])
                po = (2 + t0) if t0 < 256 else (262 + t0 - 256)
                P.op("act", I("activation", out=PT_[:, po:po + n], in_=banks[bk][:, 0:n], func=AF.Copy), reads=[b_ps[bk]], writes=[bPT])
            allq = [b_qkv[cc][t] for t in range(18)]
            for bi, (t0, n) in enumerate(blocks):
                bk = 2 + bi % 2
                po = (2 + t0) if t0 < 256 else (262 + t0 - 256)
                for tp in range(5):
                    P.op("pe", I("matmul", banks[bk][:, 0:n], DG[:, tp, :], PT_[:, po - 2 + tp:po - 2 + tp + n], start=(tp == 0), stop=(tp == 4)),
                         reads=[b_DG, bPT], writes=[b_ps[bk]])
                if cc >= 8:
                    P.op("act", I("activation", out=qkvT[:, cc, t0:t0 + n], in_=banks[bk][:, 0:n], func=AF.Silu), reads=[b_ps[bk]], writes=allq)
                else:
                    P.op("act", I("activation", out=ACC[:, t0:t0 + n], in_=banks[bk][:, 0:n], func=AF.Silu), reads=[b_ps[bk]], writes=[b_ACC])
                    P.op("dve", I("tensor_tensor", out=SQB[:, t0:t0 + n], in0=ACC[:, t0:t0 + n], in1=ACC[:, t0:t0 + n], op=ALU.mult), reads=[b_ACC], writes=[b_SQB])
            if cc < 8:
                sc = (128.0 ** -0.5) if cc < 4 else 1.0
                for bi, (t0, n) in enumerate(blocks):
                    bk = 4 + bi % 2
                    ri = bi % 2
                    P.op("pe", I("matmul", banks[bk][:, 0:n], ones_bf, SQB[:, t0:t0 + n], start=True, stop=True), reads=[b_SQB, b_const], writes=[b_ps[bk]])
                    P.op("act", I("activation", out=RNb[ri][:, 0:n], in_=banks[bk][:, 0:n], func=AF.Sqrt, bias=eps_t), reads=[b_ps[bk], b_const], writes=[b_RNb[ri]])
                    P.op("dve", I("reciprocal", out=RNb[ri][:, 0:n], in_=RNb[ri][:, 0:n]), reads=[b_RNb[ri]], writes=[b_RNb[ri]])
                    P.op("dve", I("scalar_tensor_tensor", out=qkvT[:, cc, t0:t0 + n], in0=ACC[:, t0:t0 + n], scalar=sc, in1=RNb[ri][:, 0:n], op0=ALU.mult, op1=ALU.mult),
                         reads=[b_ACC, b_RNb[ri]], writes=allq)
        P.barrier()
        if stop <= 5:
            return
        SA = Alloc(arena[:, RA:RA + 64 * KB], 64 * KB)
        seqs = []
        for d in range(2):
            for h in range(4):
                q = {"d": d, "h": h}
                for nm in ("gmask", "decT", "dmbd", "dmx", "dmq", "vtok"):
                    q[nm] = SA([128], F32)
                for nm in ("Mbd", "Nbd", "XT", "QKd", "R0", "R1", "RT0", "RT1", "P0", "P1", "PT0", "PT1", "ZT", "AinvT", "kd", "r", "vnew", "Sbf"):
                    q[nm] = SA([128], BF16)
                q["S"] = SA([128], F32)
                q["b"] = {}
                seqs.append(q)

        def sb(q, nm):
            if nm not in q["b"]:
                q["b"][nm] = P.buf()
            return q["b"][nm]
        ring_i = {"prep": 0, "scan": 0}

        def pslot(kind="prep"):
            i = ring_i[kind]
            ring_i[kind] += 1
            if kind == "prep":
                i %= 20
                bk, c = i % 5, i // 5
            else:
                i %= 12
                bk, c = 5 + i % 3, i // 3
            return banks[bk][:, c * 128:(c + 1) * 128], b_ps[bk]

        for q in seqs:
            P.op("pool", I("memset", q["S"], 0.0), writes=[sb(q, "S")])
            P.op("pool", I("memset", q["Sbf"], 0.0), writes=[sb(q, "Sbf")])
        orders = [list(range(18)), [1, 0] + list(range(17, 1, -1))]
        o_written = [[False] * 4 for _ in range(16)]

        def mmq(out, lhsT, rhs, reads, bw):
            P.op("pe", lambda e: e.matmul(out, lhsT, rhs, start=True, stop=True), reads=reads, writes=[bw])

        def scan_step(step2, part):
            step = step2 // 2
            st = []
            for q in seqs:
                d, h = q["d"], q["h"]
                if d != step2 % 2:
                    continue
                t = orders[d][step]
                if KLAT == 0:
                    t = t % 2
                st.append((q, d, h, t, (t >= 2) and KNOQK == 0))
            tsl = lambda t: slice(t * 128, (t + 1) * 128)
            if part == "prep":
                for (q, d, h, t, lat) in st:
                    kT = qkvT[:, 4 + h, tsl(t)]; qT = qkvT[:, h, tsl(t)]; vT = qkvT[:, 8 + h, tsl(t)]
                    q["kT"], q["qT"] = kT, qT
                    q["bk"], q["bq"], q["bv"] = b_qkv[4 + h][t], b_qkv[h][t], b_qkv[8 + h][t]
                    col = d * 4 + h
                    q["beta"] = gb[:, t, 8 + col:9 + col]
                    q["gcol"] = gb[:, t, col:col + 1]
                    q["eg"] = cumE[:, t, (h if d == 0 else 20 + h):(h if d == 0 else 20 + h) + 1]
                    q["neg"] = negE[:, t, (h if d == 0 else 20 + h):(h if d == 0 else 20 + h) + 1]
                    q["ekd"] = cumE[:, t, (8 + h if d == 0 else 28 + h):(8 + h if d == 0 else 28 + h) + 1]
                    q["gl"] = cumE[:, t, 32 + col:33 + col]
                    q["bg"] = b_gb[t]
                    ks, q["bks"] = pslot(); q["ks"] = ks.bitcast(BF16)[:, 0:128]
                    vs, q["bvs"] = pslot(); q["vs"] = vs.bitcast(BF16)[:, 0:128]
                    P.op("pe", I("transpose", out=q["ks"], in_=kT, identity=ident_bf), reads=[q["bk"], b_const], writes=[q["bks"]])
                    P.op("pe", I("transpose", out=q["vs"], in_=vT, identity=ident_bf), reads=[q["bv"], b_const], writes=[q["bvs"]])
                    ml = mb_ls if d == 0 else mb_us
                    gmv = q["gmask"].bitcast(BF16)
                    q["gmh"], q["gml"] = gmv[:, 0:128], gmv[:, 128:256]
                    P.op("dve", I("tensor_scalar", out=q["gmh"], in0=ml, scalar1=ghb[:, t, col:col + 1], scalar2=None, op0=ALU.mult),
                         reads=[q["bg"], b_const], writes=[sb(q, "gmask")])
                    P.op("dve", I("tensor_scalar", out=q["gml"], in0=ml, scalar1=ghb[:, t, 8 + col:9 + col], scalar2=None, op0=ALU.mult),
                         reads=[q["bg"], b_const], writes=[sb(q, "gmask")])
                if KSUB < 2:
                    return
                for (q, d, h, t, lat) in st:
                    P.op("act", I("activation", out=q["vtok"], in_=q["vs"], func=AF.Copy), reads=[q["bvs"]], writes=[sb(q, "vtok")])
                    P.op("act", I("activation", out=q["kd"], in_=q["ks"], func=AF.Copy, scale=q["ekd"]), reads=[q["bks"], q["bg"]], writes=[sb(q, "kd")])
                for (q, d, h, t, lat) in st:
                    q["G"], q["bG"] = pslot()
                    mmq(q["G"], q["kT"], q["kT"], [q["bk"]], q["bG"])
                    if lat:
                        q["QK"], q["bQK"] = pslot()
                        mmq(q["QK"], q["kT"], q["qT"], [q["bk"], q["bq"]], q["bQK"])
                for (q, d, h, t, lat) in st:
                    mr = mb_ui if d == 0 else mb_li
                    q["df"], q["bdf"] = pslot()
                    P.op("pe", I("matmul", q["df"], q["gmh"], mr, start=True, stop=False), reads=[sb(q, "gmask"), b_const], writes=[q["bdf"]])
                    P.op("pe", I("matmul", q["df"], q["gml"], mr, start=False, stop=True), reads=[sb(q, "gmask"), b_const], writes=[q["bdf"]])
                if KSUB < 3:
                    return
                for (q, d, h, t, lat) in st:
                    P.op("act", I("activation", out=q["decT"], in_=q["df"], func=AF.Exp), reads=[q["bdf"]], writes=[sb(q, "decT")])
                if KSUB < 5:
                    return
                for (q, d, h, t, lat) in st:
                    mbd, mx, mi = (mb_ubd, mb_ux, mb_ui) if d == 0 else (mb_lbd, mb_lx, mb_li)
                    t1 = q["dmbd"].bitcast(BF16)[:, 0:128]
                    P.op("dve", I("scalar_tensor_tensor", out=t1, in0=q["G"], scalar=q["beta"], in1=q["decT"], op0=ALU.mult, op1=ALU.mult),
                         reads=[q["bG"], q["bg"], sb(q, "decT")], writes=[sb(q, "dmbd")])
                    P.op("dve", I("tensor_tensor", out=q["Mbd"], in0=t1, in1=mbd, op=ALU.mult), reads=[sb(q, "dmbd"), b_const], writes=[sb(q, "Mbd")])
                    P.op("dve", I("tensor_tensor", out=q["XT"], in0=t1, in1=mx, op=ALU.mult), reads=[sb(q, "dmbd"), b_const], writes=[sb(q, "XT")])
                    if lat:
                        t2 = q["dmq"].bitcast(BF16)[:, 0:128]
                        P.op("dve", I("tensor_tensor", out=t2, in0=q["QK"], in1=q["decT"], op=ALU.mult), reads=[q["bQK"], sb(q, "decT")], writes=[sb(q, "dmq")])
                        P.op("dve", I("tensor_tensor", out=q["QKd"], in0=t2, in1=mi, op=ALU.mult), reads=[sb(q, "dmq"), b_const], writes=[sb(q, "QKd")])
                if KSUB < 6:
                    return
                for (q, d, h, t, lat) in st:
                    ns, q["bns"] = pslot(); q["ns"] = ns.bitcast(BF16)[:, 0:128]
                    P.op("pe", I("transpose", out=q["ns"], in_=q["Mbd"], identity=ident_bf), reads=[sb(q, "Mbd"), b_const], writes=[q["bns"]])
                    P.op("act", I("activation", out=q["Nbd"], in_=q["ns"], func=AF.Copy), reads=[q["bns"]], writes=[sb(q, "Nbd")])
                    P.op("dve", I("tensor_tensor", out=q["R0"], in0=ident_bf, in1=q["Mbd"], op=ALU.subtract), reads=[sb(q, "Mbd"), b_const], writes=[sb(q, "R0")])
                    q["cur"] = ("Mbd", "Nbd", "R0", "RT0")
                if KSUB < 7:
                    return
                for lvl in range(5):
                    pn, ptn = ("P0", "PT0") if lvl % 2 == 0 else ("P1", "PT1")
                    rn_ = "R1" if lvl % 2 == 0 else "R0"
                    last = lvl == 4
                    for (q, d, h, t, lat) in st:
                        pw, pwt, r_, _ = q["cur"]
                        if not last:
                            q["p2"], q["bp2"] = pslot()
                            mmq(q["p2"], q[pwt], q[pw], [sb(q, pw), sb(q, pwt)], q["bp2"])
                        q["p2t"], q["bp2t"] = pslot()
                        mmq(q["p2t"], q[pw], q[pwt], [sb(q, pw), sb(q, pwt)], q["bp2t"])
                    for (q, d, h, t, lat) in st:
                        if not last:
                            P.op("act", I("activation", out=q[pn], in_=q["p2"], func=AF.Copy), reads=[q["bp2"]], writes=[sb(q, pn)])
                        P.op("act", I("activation", out=q[ptn], in_=q["p2t"], func=AF.Copy), reads=[q["bp2t"]], writes=[sb(q, ptn)])
                    for (q, d, h, t, lat) in st:
                        pw, pwt, r_, _ = q["cur"]
                        q["ra"], q["bra"] = pslot()
                        mmq(q["ra"], q[ptn], q[r_], [sb(q, r_), sb(q, ptn)], q["bra"])
                    for (q, d, h, t, lat) in st:
                        pw, pwt, r_, _ = q["cur"]
                        P.op("dve", I("tensor_tensor", out=q[rn_], in0=q["ra"], in1=q[r_], op=ALU.add),
                             reads=[q["bra"], sb(q, r_)], writes=[sb(q, rn_)])
                        q["cur"] = (pn, ptn, rn_, None)
                for (q, d, h, t, lat) in st:
                    _, _, r_, _ = q["cur"]
                    rts, q["brts"] = pslot(); q["rts"] = rts.bitcast(BF16)[:, 0:128]
                    P.op("pe", I("transpose", out=q["rts"], in_=q[r_], identity=ident_bf), reads=[sb(q, r_), b_const], writes=[q["brts"]])
                for (q, d, h, t, lat) in st:
                    _, _, r_, _ = q["cur"]
                    P.op("act", I("activation", out=q["RT0"], in_=q["rts"], func=AF.Copy), reads=[q["brts"]], writes=[sb(q, "RT0")])
                    q["cur"] = (None, None, r_, "RT0")
                if KSUB < 8:
                    return
                for (q, d, h, t, lat) in st:
                    _, _, r_, rt_ = q["cur"]
                    q["z"], q["bz"] = pslot()
                    mmq(q["z"], q["XT"], q[rt_], [sb(q, "XT"), sb(q, rt_)], q["bz"])
                for (q, d, h, t, lat) in st:
                    P.op("act", I("activation", out=q["ZT"], in_=q["z"], func=AF.Copy), reads=[q["bz"]], writes=[sb(q, "ZT")])
                for (q, d, h, t, lat) in st:
                    _, _, r_, rt_ = q["cur"]
                    q["w"], q["bw"] = pslot()
                    mmq(q["w"], q["ZT"], q[r_], [sb(q, "ZT"), sb(q, r_)], q["bw"])
                for (q, d, h, t, lat) in st:
                    _, _, r_, rt_ = q["cur"]
                    P.op("dve", I("scalar_tensor_tensor", out=q["AinvT"], in0=q["w"], scalar=-1.0, in1=q[r_], op0=ALU.mult, op1=ALU.add),
                         reads=[q["bw"], sb(q, r_)], writes=[sb(q, "AinvT")])
                if KSUB < 9:
                    return
                return
            for (q, d, h, t, lat) in st:
                q["a"], q["ba"] = pslot("scan")
                mmq(q["a"], q["kT"], q["Sbf"], [q["bk"], sb(q, "Sbf")], q["ba"])
            for (q, d, h, t, lat) in st:
                P.op("dve", I("scalar_tensor_tensor", out=q["r"], in0=q["a"], scalar=q["neg"], in1=q["vtok"], op0=ALU.mult, op1=ALU.add),
                     reads=[q["ba"], q["bg"], sb(q, "vtok")], writes=[sb(q, "r")])
            for (q, d, h, t, lat) in st:
                q["bb"], q["bbb"] = pslot("scan")
                mmq(q["bb"], q["AinvT"], q["r"], [sb(q, "AinvT"), sb(q, "r")], q["bbb"])
            for (q, d, h, t, lat) in st:
                P.op("act", I("activation", out=q["vnew"], in_=q["bb"], func=AF.Copy, scale=q["beta"]), reads=[q["bbb"], q["bg"]], writes=[sb(q, "vnew")])
            for (q, d, h, t, lat) in st:
                if lat:
                    q["o1"], q["bo1"] = pslot("scan")
                    mmq(q["o1"], q["qT"], q["Sbf"], [q["bq"], sb(q, "Sbf")], q["bo1"])
                    q["o2"], q["bo2"] = pslot("scan")
                    mmq(q["o2"], q["QKd"], q["vnew"], [sb(q, "QKd"), sb(q, "vnew")], q["bo2"])
                q["sp"], q["bsp"] = pslot("scan")
                mmq(q["sp"], q["kd"], q["vnew"], [sb(q, "kd"), sb(q, "vnew")], q["bsp"])
            for (q, d, h, t, lat) in st:
                if lat:
                    oa = o_acc[:, t - 2, h * 128:(h + 1) * 128]
                    bo = b_oacc[t - 2][h]
                    tmp = q["gmask"]
                    if not o_written[t - 2][h]:
                        P.op("act", I("activation", out=tmp, in_=q["o2"], func=AF.Copy), reads=[q["bo2"]], writes=[sb(q, "gmask")])
                        o_written[t - 2][h] = True
                    else:
                        P.op("dve", I("tensor_tensor", out=tmp, in0=q["o2"], in1=oa, op=ALU.add), reads=[q["bo2"], bo], writes=[sb(q, "gmask")])
                    P.op("dve", I("scalar_tensor_tensor", out=oa, in0=q["o1"], scalar=q["eg"], in1=tmp, op0=ALU.mult, op1=ALU.add),
                         reads=[q["bo1"], q["bg"], sb(q, "gmask")], writes=[bo])
                P.op("dve", I("scalar_tensor_tensor", out=q["S"], in0=q["S"], scalar=q["gl"], in1=q["sp"], op0=ALU.mult, op1=ALU.add),
                     reads=[sb(q, "S"), q["bg"], q["bsp"]], writes=[sb(q, "S")])
                P.op("act", I("activation", out=q["Sbf"], in_=q["S"], func=AF.Copy), reads=[sb(q, "S")], writes=[sb(q, "Sbf")])
        def cap2(step2, part):
            P.capture = []
            scan_step(step2, part)
            lst = P.capture
            P.capture = None
            return lst
        nst = min(36, KSTEPS)
        P.replay_merged(cap2(0, "prep"), [])
        for step2 in range(nst):
            nxt = cap2(step2 + 1, "prep") if step2 + 1 < nst else []
            P.replay_merged(nxt, cap2(step2, "scan"))
        P.barrier()

        if stop <= 6:
            return
        WB = Alloc(arena[:, RB:RB + 54 * KB], 54 * KB)
        WinA = WB([8, 1024], BF16); Wz = WB([8, 512], BF16); Wout = WB([8, 1024], BF16)
        b_w7 = P.buf()
        sets7 = []
        for i7 in range(2):
            d7 = {}
            if i7 == 0:
                d7["xTt"] = WB([8, 128], BF16); d7["u"] = WB([512], F32); d7["v"] = WB([512], F32); d7["vn"] = WB([512], BF16)
                d7["y"] = WB([1024], BF16); d7["yT"] = WB([8, 128], BF16); d7["sz"] = WB([512], F32); d7["st6"] = WB([8], F32); d7["ss4"] = WB([4], F32)
            else:
                d7["u"] = xin2[0][:, 0:512]; d7["v"] = xin2[0][:, 512:1024]
                d7["sz"] = xin[0][:, 0:512]
                d7["y"] = xin[0][:, 512:1024].bitcast(BF16)
                d7["xTt"] = V(arena, RD + 1152, [8, 128], BF16); d7["vn"] = V(arena, RD + 1152 + 2048, [512], BF16)
                d7["yT"] = V(arena, RD + 4224, [8, 128], BF16)
                d7["st6"] = WB([8], F32); d7["ss4"] = WB([4], F32)
            for nm in ("xTt", "u", "v", "vn", "y", "yT", "sz", "st"):
                d7["b_" + nm] = P.buf()
            sets7.append(d7)
        winv = win_d.rearrange("(kc p) n -> p kc n", p=128)
        P.dma("pool", sem_w[0], WinA, winv[:, :, 0:1024], writes=[b_w7])
        P.dma("pool", sem_w[1], Wz, winv[:, :, 2560:3072], writes=[b_w7])
        b_wo = P.buf()
        P.dma("pool", sem_w[2], Wout, wout_d.rearrange("(kc p) n -> p kc n", p=128), writes=[b_wo])
        make_bc(bcG, b, 2, b_bcG, xin[1], b_xin[1])
        for ch in range(8):
            P.op("pool", I("tensor_tensor", out=Wout[:, ch, :], in0=Wout[:, ch, :], in1=bcG, op=ALU.mult), reads=[b_wo, b_bcG], writes=[b_wo])
        def p7_a(tt, xTt, u_sb, v_sb, vn_bf, y_bf, yTt, sz, st6, ss4, b_xTt, b_u, b_v, b_vn, b_y, b_yT, b_sz, b_st):
            xt = x_res[:, tt, :]
            P.dma("sp", sem_x[tt % 2], xt, x_d[b, tt * 128:(tt + 1) * 128, :], writes=[b_xres[tt]])
            norm_mod_T(xt, b_xres[tt], bcA, bcS, xTt, [b_xTt], 0, xmb, b_xmb)
            for hf, bk in ((0, 1), (1, 2)):
                for kc in range(8):
                    P.op("pe", lambda e, kc=kc, hf=hf, bk=bk: e.matmul(banks[bk][:, :], xTt[:, kc, :], WinA[:, kc, hf * 512:(hf + 1) * 512],
                                                                     start=(kc == 0), stop=(kc == 7)), reads=[b_xTt, b_w7], writes=[b_ps[bk]])
            for kc in range(8):
                P.op("pe", lambda e, kc=kc: e.matmul(banks[3][:, :], xTt[:, kc, :], Wz[:, kc, :], start=(kc == 0), stop=(kc == 7)),
                     reads=[b_xTt, b_w7], writes=[b_ps[3]])
            P.op("act", lambda e: e.activation(out=u_sb, in_=banks[1][:, :], func=AF.Gelu_apprx_tanh), reads=[b_ps[1]], writes=[b_u])
            P.op("act", lambda e: e.activation(out=v_sb, in_=banks[2][:, :], func=AF.Gelu_apprx_tanh), reads=[b_ps[2]], writes=[b_v])
            P.op("act", lambda e: e.activation(out=sz, in_=banks[3][:, :], func=AF.Silu), reads=[b_ps[3]], writes=[b_sz])
            P.op("dve", lambda e: e.bn_stats(out=st6[:, 0:6], in_=v_sb), reads=[b_v], writes=[b_st])
            P.op("dve", lambda e: e.bn_aggr(out=st6[:, 6:8], in_=st6[:, 0:6]), reads=[b_st], writes=[b_st])
            P.op("dve", lambda e: e.tensor_scalar(out=st6[:, 7:8], in0=st6[:, 7:8], scalar1=EPS, scalar2=None, op0=ALU.add), reads=[b_st], writes=[b_st])
            P.op("act", lambda e: e.activation(out=st6[:, 7:8], in_=st6[:, 7:8], func=AF.Sqrt), reads=[b_st], writes=[b_st])
            P.op("dve", lambda e: e.reciprocal(out=st6[:, 7:8], in_=st6[:, 7:8]), reads=[b_st], writes=[b_st])
            P.op("dve", lambda e: e.tensor_scalar(out=v_sb, in0=v_sb, scalar1=st6[:, 6:7], scalar2=st6[:, 7:8], op0=ALU.subtract, op1=ALU.mult),
                 reads=[b_v, b_st], writes=[b_v])
            P.op("dve", lambda e: e.tensor_tensor(out=v_sb, in0=v_sb, in1=lng_bc, op=ALU.mult), reads=[b_v, b_par], writes=[b_v])
            P.op("dve", lambda e: e.tensor_tensor(out=vn_bf, in0=v_sb, in1=lnb_bc, op=ALU.add), reads=[b_v, b_par], writes=[b_vn])
        def p7_b(tt, xTt, u_sb, v_sb, vn_bf, y_bf, yTt, sz, st6, ss4, b_xTt, b_u, b_v, b_vn, b_y, b_yT, b_sz, b_st):
            xt = x_res[:, tt, :]
            for h in range(4):
                P.op("pe", lambda e, h=h: e.matmul(banks[4][:, h * 128:(h + 1) * 128], wsT[:, h, :], vn_bf[:, h * 128:(h + 1) * 128], start=True, stop=True),
                     reads=[b_vn, b_par], writes=[b_ps[4]])
            for h in range(4):
                hs = slice(h * 128, (h + 1) * 128)
                P.op("dve", lambda e, h=h, hs=hs: e.scalar_tensor_tensor(out=y_bf[:, hs], in0=banks[4][:, hs], scalar=bsT[:, h:h + 1], in1=u_sb[:, hs],
                                                                       op0=ALU.add, op1=ALU.mult), reads=[b_ps[4], b_par, b_u], writes=[b_y])
            for h in range(4):
                hs = slice(h * 128, (h + 1) * 128)
                P.op("act", lambda e, h=h, hs=hs, tt=tt: e.activation(out=u_sb[:, hs], in_=o_acc[:, tt, hs], func=AF.Square, accum_out=ss4[:, h:h + 1]),
                     reads=[b_oacc[tt][h], b_y], writes=[b_u, b_st])
            rstd_from_ss(ss4, 128.0, b_st)
            for h in range(4):
                hs = slice(h * 128, (h + 1) * 128)
                P.op("dve", lambda e, h=h, hs=hs, tt=tt: e.scalar_tensor_tensor(out=u_sb[:, hs], in0=o_acc[:, tt, hs], scalar=ss4[:, h:h + 1], in1=onorm_bc,
                                                                              op0=ALU.mult, op1=ALU.mult), reads=[b_oacc[tt][h], b_st, b_par, b_u], writes=[b_u])
            P.op("dve", lambda e: e.tensor_tensor(out=y_bf[:, 512:1024], in0=u_sb, in1=sz, op=ALU.mult), reads=[b_u, b_sz], writes=[b_y])
            pv = banks[5][:, 0:512].bitcast(BF16)
            for ch in range(8):
                P.op("pe", lambda e, ch=ch: e.transpose(out=pv[:, ch * 128:(ch + 1) * 128], in_=y_bf[:, ch * 128:(ch + 1) * 128], identity=ident_bf),
                     reads=[b_y, b_const], writes=[b_ps[5]])
            P.op("act", lambda e: e.activation(out=yTt, in_=pv.rearrange("p (a b) -> p a b", a=8), func=AF.Copy), reads=[b_ps[5]], writes=[b_yT])
            for hf in range(2):
                bk = 6 + hf
                for ch in range(8):
                    P.op("pe", lambda e, ch=ch, hf=hf, bk=bk: e.matmul(banks[bk][:, :], yTt[:, ch, :], Wout[:, ch, hf * 512:(hf + 1) * 512],
                                                                     start=(ch == 0), stop=(ch == 7)), reads=[b_yT, b_wo], writes=[b_ps[bk]])
            for hf in range(2):
                hs = slice(hf * 512, (hf + 1) * 512)
                P.op("dve", I("tensor_tensor", out=xt[:, hs], in0=banks[6 + hf][:, :], in1=xt[:, hs], op=ALU.add),
                     reads=[b_ps[6 + hf], b_xres[tt]], writes=[b_xres[tt]])
        def args7(tt):
            d7 = sets7[tt % 2]
            return (tt, d7["xTt"], d7["u"], d7["v"], d7["vn"], d7["y"], d7["yT"], d7["sz"], d7["st6"], d7["ss4"],
                    d7["b_xTt"], d7["b_u"], d7["b_v"], d7["b_vn"], d7["b_y"], d7["b_yT"], d7["b_sz"], d7["b_st"])
        def cap(fn, *a):
            P.capture = []
            fn(*a)
            lst = P.capture
            P.capture = None
            return lst
        P.replay_merged(cap(p7_a, *args7(0)), [])
        for tt in range(1, 16):
            P.replay_merged(cap(p7_a, *args7(tt)), cap(p7_b, *args7(tt - 1)))
        P.replay_merged([], cap(p7_b, *args7(15)))
        P.barrier()
        if dbg and b == 0:
            out_toks.append(P.dma("sp", sem_o[0], dbg_d, x_res, reads=b_xres))
            P.barrier()

        if stop <= 7:
            return
        MA = Alloc(arena[:, RB:ARENA], ARENA - RB)
        h2T = MA([8, 2048], BF16)
        b_h2T = [P.buf() for _ in range(16)]
        GU = [MA([8, 1024], BF16) for _ in range(2)]; DW = [MA([4, 1024], BF16) for _ in range(2)]
        b_GU = [P.buf() for _ in range(2)]; b_DW = [P.buf() for _ in range(2)]
        act_t = [MA([4, 512], BF16) for _ in range(2)]; b_act = [P.buf() for _ in range(2)]
        sg = [MA([512], F32) for _ in range(2)]; b_sg = [P.buf() for _ in range(2)]
        h2f = MA([1024], F32); f32T = MA([8, 128], F32); b_f32T = P.buf()
        cb = MA([16, 16], F32); b_cb = [P.buf() for _ in range(16)]
        lg = MA([20], F32); rt = MA([32], F32); b_rt = P.buf()
        small2 = MA([16], F32); b_small2 = P.buf()
        bc2A = MA([1024], F32); bc2S = MA([1024], F32); bc2G = MA([1024], F32); tb_ = MA([1024], F32)
        b_2A, b_2S, b_2G, b_tb = P.buf(), P.buf(), P.buf(), P.buf()
        jb = MA([1024], BF16); b_jb = P.buf()
        load(tb_, n2g_d.partition_broadcast(128), writes=[b_tb])
        P.op("dve", lambda e: e.tensor_copy(out=h2f, in_=tb_), reads=[b_tb], writes=[b_jf])
        g2_bc = h2f
        make_bc(bc2A, b, 4, b_2A, tb_, b_tb, g_bc=g2_bc, b_g=b_jf)
        make_bc(bc2S, b, 3, b_2S, tb_, b_tb)
        make_bc(bc2G, b, 5, b_2G, tb_, b_tb)
        def route_tile(tt, ev):
            small2, b_small2, jb, b_jb, h2f, b_jf, f32T, b_f32T, lg, rt, b_rt, bkA, bkB, bkC = ev
            ss = small2[:, 0:1]
            src = x_res[:, tt, :]
            P.op("act", lambda e, src=src: e.activation(out=jb, in_=src, func=AF.Square, accum_out=ss), reads=[b_xres[tt]], writes=[b_jb, b_small2])
            rstd_from_ss(ss, 1024.0, b_small2)
            P.op("dve", lambda e, src=src: e.scalar_tensor_tensor(out=h2f, in0=src, scalar=ss, in1=bc2A, op0=ALU.mult, op1=ALU.mult),
                 reads=[b_xres[tt], b_small2, b_2A], writes=[b_jf])
            P.op("dve", lambda e: e.tensor_tensor(out=h2f, in0=h2f, in1=bc2S, op=ALU.add), reads=[b_jf, b_2S], writes=[b_jf])
            for kc in range(8):
                bk = (bkA, bkB)[kc // 4]
                P.op("pe", lambda e, kc=kc, bk=bk: e.transpose(out=banks[bk][:, (kc % 4) * 128:(kc % 4 + 1) * 128], in_=h2f[:, kc * 128:(kc + 1) * 128], identity=ident),
                     reads=[b_jf, b_const], writes=[b_ps[bk]])
            for hf in range(2):
                P.op("act", lambda e, hf=hf, tt=tt: e.activation(out=h2T[:, hf * 4:(hf + 1) * 4, tt * 128:(tt + 1) * 128],
                                                                 in_=banks[(bkA, bkB)[hf]][:, :].rearrange("p (a b) -> p a b", a=4), func=AF.Copy),
                     reads=[b_ps[(bkA, bkB)[hf]]], writes=[b_h2T[tt]])
                P.op("dve", lambda e, hf=hf: e.tensor_copy(out=f32T[:, hf * 4:(hf + 1) * 4, :], in_=banks[(bkA, bkB)[hf]][:, :].rearrange("p (a b) -> p a b", a=4)),
                     reads=[b_ps[(bkA, bkB)[hf]]], writes=[b_f32T])
            for kc in range(8):
                P.op("pe", lambda e, kc=kc: e.matmul(banks[bkC][:, 0:20], f32T[:, kc, :], Wr32[:, kc, :], start=(kc == 0), stop=(kc == 7)),
                     reads=[b_f32T, b_par], writes=[b_ps[bkC]])
            R_ = [b_rt]

            def dv(fn, extra_r=()):
                P.op("dve", fn, reads=R_ + list(extra_r), writes=R_)
            gmx, ngm, gsum, pg, m1, m2, dd, w1g, w2g = (rt[:, i:i + 1] for i in range(9))
            ohg = rt[:, 12:16]; es = rt[:, 16:20]; oh1 = rt[:, 20:24]; es2 = rt[:, 24:28]; oh2 = rt[:, 28:32]
            P.op("dve", lambda e: e.tensor_tensor(out=lg, in0=banks[bkC][:, 0:20], in1=brt_bc, op=ALU.add), reads=[b_ps[bkC], b_par], writes=R_)
            dv(lambda e: e.tensor_reduce(out=gmx, in_=lg[:, 0:4], axis=AX.X, op=ALU.max))
            dv(lambda e: e.tensor_scalar(out=ohg, in0=lg[:, 0:4], scalar1=gmx, scalar2=None, op0=ALU.is_equal))
            dv(lambda e: e.tensor_scalar(out=ngm, in0=gmx, scalar1=-1.0, scalar2=None, op0=ALU.mult))
            P.op("act", lambda e: e.activation(out=es2, in_=lg[:, 0:4], func=AF.Exp, bias=ngm, accum_out=gsum), reads=R_, writes=R_)
            dv(lambda e: e.reciprocal(out=pg, in_=gsum))
            dv(lambda e: e.tensor_scalar(out=es, in0=lg[:, 4:8], scalar1=ohg[:, 0:1], scalar2=None, op0=ALU.mult))
            for g in range(1, 4):
                dv(lambda e, g=g: e.scalar_tensor_tensor(out=es, in0=lg[:, 4 + 4 * g:8 + 4 * g], scalar=ohg[:, g:g + 1], in1=es, op0=ALU.mult, op1=ALU.add))
            dv(lambda e: e.tensor_reduce(out=m1, in_=es, axis=AX.X, op=ALU.max))
            dv(lambda e: e.tensor_scalar(out=oh1, in0=es, scalar1=m1, scalar2=None, op0=ALU.is_equal))
            dv(lambda e: e.scalar_tensor_tensor(out=es2, in0=oh1, scalar=-1e30, in1=es, op0=ALU.mult, op1=ALU.add))
            dv(lambda e: e.tensor_reduce(out=m2, in_=es2, axis=AX.X, op=ALU.max))
            dv(lambda e: e.tensor_scalar(out=oh2, in0=es2, scalar1=m2, scalar2=None, op0=ALU.is_equal))
            dv(lambda e: e.tensor_tensor(out=dd, in0=m1, in1=m2, op=ALU.subtract))
            P.op("act", lambda e: e.activation(out=dd, in_=dd, func=AF.Sigmoid), reads=R_, writes=R_)
            dv(lambda e: e.tensor_tensor(out=w1g, in0=dd, in1=pg, op=ALU.mult))
            dv(lambda e: e.tensor_tensor(out=w2g, in0=pg, in1=w1g, op=ALU.subtract))
            dv(lambda e: e.tensor_scalar(out=es, in0=oh1, scalar1=w1g, scalar2=None, op0=ALU.mult))
            dv(lambda e: e.scalar_tensor_tensor(out=es, in0=oh2, scalar=w2g, in1=es, op0=ALU.mult, op1=ALU.add))
            for g in range(4):
                P.op("dve", lambda e, g=g, tt=tt: e.tensor_scalar(out=cb[:, tt, 4 * g:4 * g + 4], in0=es, scalar1=ohg[:, g:g + 1], scalar2=None, op0=ALU.mult),
                     reads=R_, writes=[b_cb[tt]])
        f32T2 = MA([8, 128], F32); jb2 = MA([1024], BF16); lg2 = MA([20], F32); rt2 = MA([32], F32); small3 = MA([16], F32)
        ev_r = [(small2, b_small2, jb, b_jb, h2f, b_jf, f32T, b_f32T, lg, rt, b_rt, 0, 1, 2),
                (small3, P.buf(), jb2, P.buf(), tb_, b_tb, f32T2, P.buf(), lg2, rt2, P.buf(), 3, 4, 5)]
        for tt in range(0, 16, 2):
            P.capture = []
            route_tile(tt, ev_r[0])
            A_ = P.capture
            P.capture = []
            route_tile(tt + 1, ev_r[1])
            B_ = P.capture
            P.capture = None
            P.replay_merged(A_, B_)
        if stop <= 8:
            return
        def emit_GU(ex, s, tb4, a_s):
            for fc in range(4):
                gs = fc % 2
                bg_, bu_ = (0, 1) if gs == 0 else (2, 3)
                for kc in range(8):
                    P.op("pe", I("matmul", banks[bg_][:, :], GU[s][:, kc, fc * 128:(fc + 1) * 128], h2T[:, kc, tb4 * 512:(tb4 + 1) * 512], start=(kc == 0), stop=(kc == 7)),
                         reads=[b_GU[s]] + b_h2T[tb4 * 4:tb4 * 4 + 4], writes=[b_ps[bg_]])
                for kc in range(8):
                    P.op("pe", I("matmul", banks[bu_][:, :], GU[s][:, kc, 512 + fc * 128:512 + (fc + 1) * 128], h2T[:, kc, tb4 * 512:(tb4 + 1) * 512], start=(kc == 0), stop=(kc == 7)),
                         reads=[b_GU[s]] + b_h2T[tb4 * 4:tb4 * 4 + 4], writes=[b_ps[bu_]])
                P.op("act", I("activation", out=sg[gs], in_=banks[bg_][:, :], func=AF.Silu), reads=[b_ps[bg_]], writes=[b_sg[gs]])
                P.op("dve", I("tensor_tensor", out=act_t[a_s][:, fc, :], in0=sg[gs], in1=banks[bu_][:, :], op=ALU.mult),
                     reads=[b_sg[gs], b_ps[bu_]], writes=[b_act[a_s]])

        def emit_DOWN(ex, s, tb4, a_s):
            for t4 in range(4):
                tt = tb4 * 4 + t4
                ds = t4 % 2
                for hf in range(2):
                    bk = 4 + ds * 2 + hf
                    for fc in range(4):
                        P.op("pe", I("matmul", banks[bk][:, :], act_t[a_s][:, fc, t4 * 128:(t4 + 1) * 128], DW[s][:, fc, hf * 512:(hf + 1) * 512], start=(fc == 0), stop=(fc == 3)),
                             reads=[b_act[a_s], b_DW[s]], writes=[b_ps[bk]])
                for hf in range(2):
                    bk = 4 + ds * 2 + hf
                    hs = slice(hf * 512, (hf + 1) * 512)
                    P.op("dve", I("scalar_tensor_tensor", out=x_res[:, tt, hs], in0=banks[bk][:, :], scalar=cb[:, tt, ex:ex + 1], in1=x_res[:, tt, hs], op0=ALU.mult, op1=ALU.add),
                         reads=[b_ps[bk], b_cb[tt], b_xres[tt]], writes=[b_xres[tt]])

        pending = None
        gcount = 0
        for ex in range(16):
            s = ex % 2
            P.dma("pool", sem_w[s], GU[s], wgu_d[ex].rearrange("(kc p) n -> p kc n", p=128), writes=[b_GU[s]])
            P.dma("pool", sem_w[2 + s], DW[s], wdn_d[ex].rearrange("(fc p) n -> p fc n", p=128), writes=[b_DW[s]])
            for fc in range(4):
                P.op("pool", I("tensor_tensor", out=DW[s][:, fc, :], in0=DW[s][:, fc, :], in1=bc2G, op=ALU.mult),
                     reads=[b_DW[s], b_2G], writes=[b_DW[s]])
            for tb4 in range(4):
                a_s = gcount % 2
                emit_GU(ex, s, tb4, a_s)
                if pending is not None:
                    emit_DOWN(*pending)
                pending = (ex, s, tb4, a_s)
                gcount += 1
        emit_DOWN(*pending)
        if stop <= 9:
            return
        load(tb_, fg_d.partition_broadcast(128), writes=[b_tb])
        def fin_tile(tt, small_, b_small_, jb_, b_jb_, ot_, b_ot_, sem_):
            ss = small_[:, 0:1]
            src = x_res[:, tt, :]
            P.op("act", I("activation", out=jb_, in_=src, func=AF.Square, accum_out=ss), reads=[b_xres[tt]], writes=[b_jb_, b_small_])
            rstd_from_ss(ss, 1024.0, b_small_)
            P.op("dve", I("scalar_tensor_tensor", out=ot_, in0=src, scalar=ss, in1=tb_, op0=ALU.mult, op1=ALU.mult),
                 reads=[b_xres[tt], b_small_, b_tb], writes=[b_ot_])
            P.dma("sp", sem_, out_d[b, tt * 128:(tt + 1) * 128, :], ot_, reads=[b_ot_])
        fe = [(small2, b_small2, jb, b_jb, bc2A, b_2A, sem_o[0]), (small3, ev_r[1][1], jb2, ev_r[1][3], bc2S, b_2S, sem_o[1])]
        for tt in range(0, 16, 2):
            P.capture = []
            fin_tile(tt, *fe[0])
            A_ = P.capture
            P.capture = []
            fin_tile(tt + 1, *fe[1])
            B_ = P.capture
            P.capture = None
            P.replay_merged(A_, B_)
        P.barrier()

    for b in range(NB):
        do_batch(b)
        P.barrier()

    P._emit_waits("sp", out_toks + [(k, P.dma_sem_cnt[k]) for k in sem_o if P.dma_sem_cnt[k] > 0])
    P.emit()
    P.close()
    return nc


_NC_CACHE = {}


def kernel(**inputs):
    NB = 2
    if "nc" not in _NC_CACHE:
        _NC_CACHE["nc"] = build(NB)
    nc = _NC_CACHE["nc"]
    f = lambda a: np.ascontiguousarray(np.asarray(a, dtype=np.float32))
    shared = {
        "c_ctx": f(inputs["c_ctx"]).reshape(1, 1024), "w_ada": f(inputs["w_ada"])[0], "b_ada": f(inputs["b_ada"]).reshape(1, 6144),
        "norm1_g": f(inputs["norm1_g"]).reshape(1, 1024), "w_in": f(inputs["w_in"])[0], "ln_a_g": f(inputs["ln_a_g"]).reshape(1, 512),
        "ln_a_b": f(inputs["ln_a_b"]).reshape(1, 512), "w_spatial": f(inputs["w_spatial"])[0], "b_spatial": f(inputs["b_spatial"])[0],
        "conv_qkv": f(inputs["conv_qkv"])[0], "a_log": f(inputs["a_log"]).reshape(1, 8), "dt_bias": f(inputs["dt_bias"]).reshape(1, 8),
        "onorm_g": f(inputs["onorm_g"]).reshape(1, 128), "w_out": f(inputs["w_out"])[0], "norm2_g": f(inputs["norm2_g"]).reshape(1, 1024),
        "w_group": f(inputs["w_group"])[0], "b_group": f(inputs["b_group"]).reshape(1, 4), "w_router": f(inputs["w_router"])[0],
        "b_router": f(inputs["b_router"]).reshape(1, 16), "w_gate_up": f(inputs["w_gate_up"])[0], "w_down": f(inputs["w_down"])[0],
        "final_g": f(inputs["final_g"]).reshape(1, 1024),
    }
    x = f(inputs["x"]); c = f(inputs["c"]); ctx = f(inputs["ctx"])
    in_maps = []
    for i in range(N_CORES):
        m = dict(shared)
        m["x"] = x[i * NB:(i + 1) * NB]; m["c"] = c[i * NB:(i + 1) * NB]; m["ctx"] = ctx[i * NB:(i + 1) * NB]
        in_maps.append(m)
    res = run_bass_kernel_spmd(nc, in_maps, core_ids=list(range(N_CORES)))
    return np.concatenate([r["out"] for r in res.results], axis=0).astype(np.float32)
```

```python
from contextlib import ExitStack
import os
import numpy as np
import concourse.bass as bass
import concourse.mybir as mybir
from concourse.bass_utils import run_bass_kernel_spmd

F32 = mybir.dt.float32
BF16 = mybir.dt.bfloat16
U8 = mybir.dt.uint8
AF = mybir.ActivationFunctionType
ALU = mybir.AluOpType
AX = mybir.AxisListType

N_CORES = 8
KLAT = int(os.environ.get('KLAT', '1'))
KNOQK = int(os.environ.get('KNOQK', '0'))
SAME_ENG_SYNC = int(os.environ.get('KSES', '1'))
EPS = 1e-6


class Buf:
    __slots__ = ("name", "last_w", "readers", "excl")

    def __init__(self, name, excl=False):
        self.name = name
        self.excl = excl
        self.last_w = None
        self.readers = []


class Prog:
    ENG = ("pe", "act", "dve", "pool", "sp")

    def __init__(self, nc):
        self.nc = nc
        self.es = ExitStack()
        self.ops = {e: [] for e in self.ENG}
        self.count = {e: 0 for e in self.ENG}
        self.sems = {}
        for e in self.ENG:
            self.sems[e] = self.es.enter_context(nc.semaphore("s_" + e))
        self.waited = {e: {} for e in self.ENG}
        self.dma_sem_cnt = {}
        self.nbuf = 0
        self.capture = None

    def sbuf(self, name, shape, dtype=F32):
        return self.es.enter_context(self.nc.sbuf_tensor(name, list(shape), dtype))

    def psum(self, name, shape, dtype=F32):
        return self.es.enter_context(self.nc.psum_tensor(name, list(shape), dtype))

    def buf(self, name=None, excl=False):
        self.nbuf += 1
        return Buf(name or f"b{self.nbuf}", excl)

    def dma_sem(self, name):
        key = "d_" + name
        self.sems[key] = self.es.enter_context(self.nc.semaphore(key))
        self.dma_sem_cnt[key] = 0
        return key

    def _deps(self, reads, writes):
        toks = []
        for b in reads:
            if b.last_w is not None:
                toks.append(b.last_w)
        for b in writes:
            if b.last_w is not None:
                toks.append(b.last_w)
            toks.extend(b.readers)
        return toks

    def _emit_waits(self, eng, toks):
        need = {}
        for (k, v) in toks:
            if k == eng and (eng in ("pe", "sp") or not SAME_ENG_SYNC):
                continue
            if v > need.get(k, 0):
                need[k] = v
        for k, v in need.items():
            if self.waited[eng].get(k, 0) >= v:
                continue
            self.waited[eng][k] = v
            self.ops[eng].append(("wait", self.sems[k], v))

    def _commit(self, tok, reads, writes):
        for b in writes:
            b.last_w = tok
            b.readers = []
        for b in reads:
            b.readers.append(tok)

    def op(self, eng, fn, reads=(), writes=()):
        if self.capture is not None:
            self.capture.append(("op", eng, fn, list(reads), list(writes)))
            return None
        writes = [b for b in writes if b is not None] + [b for b in reads if b is not None and b.excl]
        reads = [b for b in reads if b is not None and not b.excl]
        self._emit_waits(eng, self._deps(reads, writes))
        self.count[eng] += 1
        tok = (eng, self.count[eng])
        self.ops[eng].append(("op", fn, self.sems[eng], 1))
        self._commit(tok, reads, writes)
        return tok

    def dma(self, queue, semkey, out_ap, in_ap, reads=(), writes=()):
        if self.capture is not None:
            self.capture.append(("dma", queue, semkey, out_ap, in_ap, list(reads), list(writes)))
            return None
        reads = [b for b in reads if b is not None]
        writes = [b for b in writes if b is not None]
        self._emit_waits(queue, self._deps(reads, writes))
        if self.dma_sem_cnt[semkey] > 0:
            self._emit_waits(queue, [(semkey, self.dma_sem_cnt[semkey])])
        self.dma_sem_cnt[semkey] += 16
        tok = (semkey, self.dma_sem_cnt[semkey])

        def fn(e, out_ap=out_ap, in_ap=in_ap):
            return e.dma_start(out=out_ap, in_=in_ap)
        self.ops[queue].append(("op", fn, self.sems[semkey], 16))
        self._commit(tok, reads, writes)
        return tok

    def replay_merged(self, A, B):
        la, lb = len(A), len(B)
        ia = ib = 0
        while ia < la or ib < lb:
            if ib >= lb or (ia < la and ia * lb <= ib * la):
                it = A[ia]; ia += 1
            else:
                it = B[ib]; ib += 1
            if it[0] == "op":
                self.op(it[1], it[2], it[3], it[4])
            else:
                self.dma(it[1], it[2], it[3], it[4], it[5], it[6])

    def barrier(self):
        toks = [(e, self.count[e]) for e in self.ENG if e != "sp" and self.count[e] > 0]
        toks += [(k, v) for k, v in self.dma_sem_cnt.items() if v > 0]
        for e in self.ENG:
            self._emit_waits(e, toks)

    def emit(self):
        nc = self.nc
        P = self
        with nc.Block() as block:
            def run(e, engine):
                for item in P.ops[e]:
                    if item[0] == "wait":
                        engine.wait_ge(item[1], item[2])
                    else:
                        _, fn, sem, inc = item
                        fn(engine).then_inc(sem, inc)

            @block.sync
            def _(eng):
                run("sp", eng)

            @block.tensor
            def _(eng):
                run("pe", eng)

            @block.scalar
            def _(eng):
                run("act", eng)

            @block.vector
            def _(eng):
                run("dve", eng)

            @block.gpsimd
            def _(eng):
                run("pool", eng)

    def close(self):
        self.es.close()


def I(name, *a, **kw):
    return lambda e: getattr(e, name)(*a, **kw)


def build(NB=2, dbg=False, stop=99, KSTEPS=36, KSUB=99):
    nc = bass.Bass("TRN2", target_bir_lowering=False)

    def din(name, shape):
        return nc.dram_tensor(name, list(shape), F32, kind="ExternalInput").ap()
    x_d = din("x", [NB, 2048, 1024]); ctx_d = din("ctx", [NB, 256, 1024]); c_d = din("c", [NB, 1024])
    cctx_d = din("c_ctx", [1, 1024]); wada_d = din("w_ada", [1024, 6144]); bada_d = din("b_ada", [1, 6144])
    n1g_d = din("norm1_g", [1, 1024]); win_d = din("w_in", [1024, 3088]); lng_d = din("ln_a_g", [1, 512])
    lnb_d = din("ln_a_b", [1, 512]); wsp_d = din("w_spatial", [4, 128, 128]); bsp_d = din("b_spatial", [4, 128])
    conv_d = din("conv_qkv", [5, 1536]); alog_d = din("a_log", [1, 8]); dtb_d = din("dt_bias", [1, 8])
    ong_d = din("onorm_g", [1, 128]); wout_d = din("w_out", [1024, 1024]); n2g_d = din("norm2_g", [1, 1024])
    wgrp_d = din("w_group", [1024, 4]); bgrp_d = din("b_group", [1, 4]); wrt_d = din("w_router", [1024, 16])
    brt_d = din("b_router", [1, 16]); wgu_d = din("w_gate_up", [16, 1024, 1024]); wdn_d = din("w_down", [16, 512, 1024])
    fg_d = din("final_g", [1, 1024])
    out_d = nc.dram_tensor("out", [NB, 2048, 1024], F32, kind="ExternalOutput").ap()
    dbg_d = nc.dram_tensor("dbg", [128, 16, 1024], F32, kind="ExternalOutput").ap() if dbg else None

    P = Prog(nc)
    ARENA = 192 * 1024
    arena = P.sbuf("arena", [128, ARENA], U8)
    pers = P.sbuf("pers", [128, 15 * 1024], U8)
    banks = [P.psum(f"bank{i}", [128, 512]) for i in range(8)]

    def V(base, off, shape, dt, parts=128):
        esz = 2 if dt == BF16 else 4
        n = 1
        for s in shape:
            n *= s
        ap = base[0:parts, off:off + n * esz].bitcast(dt)
        if len(shape) == 2:
            ap = ap.rearrange("p (a b) -> p a b", a=shape[0])
        elif len(shape) == 3:
            ap = ap.rearrange("p (a b c) -> p a b c", a=shape[0], b=shape[1])
        return ap

    class Alloc:
        def __init__(self, base, size):
            self.base, self.size, self.off = base, size, 0

        def __call__(self, shape, dt, parts=128):
            esz = 2 if dt == BF16 else 4
            n = esz
            for s in shape:
                n *= s
            n = (n + 31) // 32 * 32
            off = self.off
            self.off += n
            assert self.off <= self.size, (self.off, self.size)
            return V(self.base, off, shape, dt, parts)

    PA = Alloc(pers, 15 * 1024)
    KB = 1024
    sem_ld = P.dma_sem("ld")
    ident = PA([128], F32); ident_bf = PA([128], BF16); ones_bf = PA([128], BF16)
    m_ui = PA([128], F32); m_us = PA([128], F32); m_li = PA([128], F32); m_ls = PA([128], F32); m_one = PA([128], F32)
    m_ubd = V(arena, 150 * 1024, [128], F32); m_ux = V(arena, 150 * 1024 + 512, [128], F32); m_lbd = V(arena, 150 * 1024 + 1024, [128], F32); m_lx = V(arena, 150 * 1024 + 1536, [128], F32)
    mb_ubd = PA([128], BF16); mb_ux = PA([128], BF16); mb_lbd = PA([128], BF16); mb_lx = PA([128], BF16)
    b_const = P.buf("const")

    def pool_op(fn, reads=(), writes=()):
        return P.op("pool", fn, reads, writes)

    def mk_mask(ap, pattern_step, chmul, cmp):
        pool_op(lambda e: e.memset(ap, 1.0), writes=[b_const])
        pool_op(lambda e: e.affine_select(out=ap, in_=ap, pattern=[[pattern_step, 128]], compare_op=cmp, fill=0.0,
                                          base=0, channel_multiplier=chmul), reads=[b_const], writes=[b_const])
    pool_op(lambda e: e.memset(ident, 0.0), writes=[b_const])
    pool_op(lambda e: e.affine_select(out=ident, in_=ident, pattern=[[-1, 128]], compare_op=ALU.not_equal, fill=1.0,
                                      base=0, channel_multiplier=1), reads=[b_const], writes=[b_const])
    pool_op(lambda e: e.tensor_copy(out=ident_bf, in_=ident), reads=[b_const], writes=[b_const])
    pool_op(lambda e: e.memset(ones_bf, 1.0), writes=[b_const])
    pool_op(lambda e: e.memset(m_one, 1.0), writes=[b_const])
    mk_mask(m_ui, 1, -1, ALU.is_ge)
    mk_mask(m_us, 1, -1, ALU.is_gt)
    mk_mask(m_li, -1, 1, ALU.is_ge)
    mk_mask(m_ls, -1, 1, ALU.is_gt)
    pool_op(lambda e: e.tensor_copy(out=m_ubd, in_=m_us), reads=[b_const], writes=[b_const])
    pool_op(lambda e: e.memset(m_ubd[0:64, 64:128], 0.0), reads=[b_const], writes=[b_const])
    pool_op(lambda e: e.memset(m_ux, 0.0), writes=[b_const])
    pool_op(lambda e: e.memset(m_ux[0:64, 64:128], 1.0), reads=[b_const], writes=[b_const])
    pool_op(lambda e: e.tensor_copy(out=m_lbd, in_=m_ls), reads=[b_const], writes=[b_const])
    pool_op(lambda e: e.memset(m_lbd[64:128, 0:64], 0.0), reads=[b_const], writes=[b_const])
    pool_op(lambda e: e.memset(m_lx, 0.0), writes=[b_const])
    pool_op(lambda e: e.memset(m_lx[64:128, 0:64], 1.0), reads=[b_const], writes=[b_const])

    mb_ui = PA([128], BF16); mb_us = PA([128], BF16); mb_li = PA([128], BF16); mb_ls = PA([128], BF16)
    for dst_, src_ in ((mb_ui, m_ui), (mb_us, m_us), (mb_li, m_li), (mb_ls, m_ls), (mb_ubd, m_ubd), (mb_ux, m_ux), (mb_lbd, m_lbd), (mb_lx, m_lx)):
        pool_op(lambda e, dst_=dst_, src_=src_: e.tensor_copy(out=dst_, in_=src_), reads=[b_const], writes=[b_const])
    eps_t = PA([1], F32)
    pool_op(lambda e: e.memset(eps_t, EPS), writes=[b_const])
    negA = PA([8], F32); dtb_bc = PA([8], F32); onorm_bc = PA([128], F32)
    lng_bc = PA([512], F32); lnb_bc = PA([512], F32)
    wsT = PA([4, 128], BF16); bsT = PA([4], F32); convw = PA([12, 5], F32)
    Wr32 = PA([8, 20], F32); brt_bc = PA([20], F32)
    modT = PA([48, 4], F32)
    b_par = P.buf("params")

    def load(dst, src, reads=(), writes=(), q="sp"):
        return P.dma(q, sem_ld, dst, src, reads=reads, writes=list(writes))

    load(negA, alog_d.partition_broadcast(128), writes=[b_par])
    load(dtb_bc, dtb_d.partition_broadcast(128), writes=[b_par])
    load(onorm_bc, ong_d.partition_broadcast(128), writes=[b_par])
    load(lng_bc, lng_d.partition_broadcast(128), writes=[b_par])
    load(lnb_bc, lnb_d.partition_broadcast(128), writes=[b_par])
    load(brt_bc[:, 0:4], bgrp_d.partition_broadcast(128), writes=[b_par])
    load(brt_bc[:, 4:20], brt_d.partition_broadcast(128), writes=[b_par])
    load(Wr32[:, :, 0:4], wgrp_d.rearrange("(kc p) n -> p kc n", p=128), writes=[b_par])
    load(Wr32[:, :, 4:20], wrt_d.rearrange("(kc p) n -> p kc n", p=128), writes=[b_par])
    P.op("act", lambda e: e.activation(out=negA, in_=negA, func=AF.Exp), reads=[b_par], writes=[b_par])
    P.op("dve", lambda e: e.tensor_scalar(out=negA, in0=negA, scalar1=-1.0, scalar2=None, op0=ALU.mult), reads=[b_par], writes=[b_par])

    A0 = Alloc(arena, ARENA)
    b_tmp = P.buf("setup_tmp")
    b_ps = [P.buf(f"bank{i}", excl=True) for i in range(8)]
    wsp_sb = A0([4, 128], F32); bsp_sb = A0([128], F32, parts=4); conv_sb = A0([1536], F32, parts=5)
    load(wsp_sb, wsp_d.rearrange("h i j -> i h j"), writes=[b_tmp])
    load(bsp_sb, bsp_d, writes=[b_tmp])
    load(conv_sb, conv_d, writes=[b_tmp])
    for h in range(4):
        P.op("pe", lambda e, h=h: e.transpose(out=banks[0][:, h * 128:(h + 1) * 128], in_=wsp_sb[:, h, :], identity=ident),
             reads=[b_tmp, b_const], writes=[b_ps[0]])
    P.op("act", lambda e: e.activation(out=wsT, in_=banks[0][:, 0:512].rearrange("p (a b) -> p a b", a=4), func=AF.Copy),
         reads=[b_ps[0]], writes=[b_par])
    P.op("pe", lambda e: e.transpose(out=banks[1][:, 0:4], in_=bsp_sb, identity=ident[0:4, 0:4]), reads=[b_tmp, b_const], writes=[b_ps[1]])
    P.op("act", lambda e: e.activation(out=bsT, in_=banks[1][:, 0:4], func=AF.Copy), reads=[b_ps[1]], writes=[b_par])
    for cc in range(12):
        P.op("pe", lambda e, cc=cc: e.transpose(out=banks[2][:, cc * 8:cc * 8 + 5], in_=conv_sb[:, cc * 128:(cc + 1) * 128],
                                                identity=ident[0:5, 0:5]), reads=[b_tmp, b_const], writes=[b_ps[2]])
    P.op("act", lambda e: e.activation(out=convw, in_=banks[2][:, 0:96].rearrange("p (a b) -> p a b", a=12)[:, :, 0:5], func=AF.Copy),
         reads=[b_ps[2]], writes=[b_par])

    cT = A0([3, 8], F32); cTb = A0([3, 8], BF16)
    for j in range(NB):
        load(cT[:, j, :], c_d[j].rearrange("(p kc) -> p kc", kc=8), writes=[b_tmp])
    if NB < 2:
        P.op("dve", lambda e: e.memset(cT[:, 1, :], 0.0), writes=[b_tmp])
    load(cT[:, 2, :], cctx_d[0].rearrange("(p kc) -> p kc", kc=8), writes=[b_tmp])
    P.op("act", lambda e: e.activation(out=cTb, in_=cT, func=AF.Silu), reads=[b_tmp], writes=[b_tmp])
    wada_v = wada_d.rearrange("(p kc) n -> p kc n", kc=8)
    wa = [A0([8, 512], BF16) for _ in range(2)]
    b_wa = [P.buf() for _ in range(2)]
    sem_wa = [P.dma_sem(f"wa{i}") for i in range(2)]
    for nb_ in range(12):
        s = nb_ % 2
        P.dma("pool", sem_wa[s], wa[s], wada_v[:, :, nb_ * 512:(nb_ + 1) * 512], writes=[b_wa[s]])
        for c4 in range(4):
            ch = nb_ * 4 + c4
            for kc in range(8):
                P.op("pe", lambda e, s=s, c4=c4, kc=kc, ch=ch: e.matmul(banks[3][:, ch * 4:ch * 4 + 3], wa[s][:, kc, c4 * 128:(c4 + 1) * 128],
                                                                      cTb[:, :, kc], start=(kc == 0), stop=(kc == 7)),
                     reads=[b_wa[s], b_tmp], writes=[b_ps[3]])
    P.op("act", lambda e: e.activation(out=modT[:, :, 0:3], in_=banks[3][:, 0:192].rearrange("p (a b) -> p a b", a=48)[:, :, 0:3], func=AF.Copy),
         reads=[b_ps[3]], writes=[b_par])
    P.barrier()

    def make_bc(dst, j, which, b_dst, tmp_bias, b_tmpb, g_bc=None, b_g=None):
        load(tmp_bias, bada_d[:, which * 1024:(which + 1) * 1024].partition_broadcast(128), writes=[b_tmpb])
        for c8 in range(8):
            ch = which * 8 + c8
            bk = 6 + c8 // 4
            P.op("pe", lambda e, c8=c8, ch=ch, bk=bk: e.matmul(banks[bk][:, (c8 % 4) * 128:(c8 % 4 + 1) * 128],
                                                               modT[:, ch, j:j + 1].to_broadcast([128, 128]), ident, start=True, stop=True),
                 reads=[b_par, b_const], writes=[b_ps[bk]])
        for hf in range(2):
            P.op("dve", lambda e, hf=hf: e.tensor_tensor(out=dst[:, hf * 512:(hf + 1) * 512], in0=banks[6 + hf][:, :],
                                                        in1=tmp_bias[:, hf * 512:(hf + 1) * 512], op=ALU.add),
                 reads=[b_ps[6 + hf], b_tmpb], writes=[b_dst])
        if g_bc is not None:
            P.op("dve", lambda e: e.scalar_tensor_tensor(out=dst, in0=dst, scalar=1.0, in1=g_bc, op0=ALU.add, op1=ALU.mult),
                 reads=[b_dst, b_g], writes=[b_dst])

    def rstd_from_ss(ss, n, b_s):
        P.op("dve", lambda e: e.tensor_scalar(out=ss, in0=ss, scalar1=1.0 / n, scalar2=EPS, op0=ALU.mult, op1=ALU.add), reads=[b_s], writes=[b_s])
        P.op("act", lambda e: e.activation(out=ss, in_=ss, func=AF.Sqrt), reads=[b_s], writes=[b_s])
        P.op("dve", lambda e: e.reciprocal(out=ss, in_=ss), reads=[b_s], writes=[b_s])

    sem_x = [P.dma_sem(f"x{i}") for i in range(2)]
    sem_w = [P.dma_sem(f"w{i}") for i in range(4)]
    sem_o = [P.dma_sem(f"o{i}") for i in range(2)]
    out_toks = []

    def do_batch(b):
        A = Alloc(arena, ARENA)
        RA = 0
        x_res = V(arena, RA, [16, 1024], F32)
        b_xres = [P.buf(f"xres{t}") for t in range(16)]
        xT = V(arena, RA, [8, 2304], BF16)
        b_xT = [P.buf(f"xT{t}") for t in range(18)]
        CT = RA + 36 * KB
        RB = 64 * KB
        qkvT = V(arena, RB, [12, 2304], BF16)
        b_qkv = [[P.buf() for _ in range(18)] for _ in range(12)]
        RC = RB + 54 * KB
        o_acc = V(arena, RC, [16, 512], F32)
        b_oacc = [[P.buf() for _ in range(4)] for _ in range(16)]
        Wqkv = V(arena, RC, [8, 1536], BF16)
        b_wqkv = P.buf()
        RD = RC + 32 * KB
        AD = Alloc(arena[:, RD:ARENA], ARENA - RD)
        gb = AD([18, 16], F32); cumE = AD([18, 40], F32); negE = AD([18, 40], F32)
        b_gb = [P.buf() for _ in range(18)]
        bcA = AD([1024], F32); bcS = AD([1024], F32); bcG = AD([1024], F32)
        b_bcA, b_bcS, b_bcG = P.buf(), P.buf(), P.buf()
        Wab = AD([8, 16], BF16); b_wab = P.buf()
        xin = [AD([1024], F32) for _ in range(2)]; b_xin = [P.buf() for _ in range(2)]
        xmb = AD([1024], BF16); b_xmb = P.buf()
        small = AD([16], F32); b_small0 = P.buf()
        g1_bc = xin[0]
        load(g1_bc, n1g_d.partition_broadcast(128), writes=[b_xin[0]])
        P.dma("pool", sem_w[0], Wab, win_d.rearrange("(kc p) n -> p kc n", p=128)[:, :, 3072:3088], writes=[b_wab])

        def norm_mod_T(src, b_src, A_bc, S_bc, dstT, b_dstT, bank, junk, b_junk, f32T=None, xm_f32=None, tmps=None):
            small_, b_small, junk_f32, b_jf = tmps if tmps is not None else (small, b_small0, junk_f320, b_jf0)
            ss = small_[:, 0:1]
            P.op("act", lambda e: e.activation(out=junk, in_=src, func=AF.Square, accum_out=ss), reads=[b_src], writes=[b_junk, b_small])
            rstd_from_ss(ss, 1024.0, b_small)
            if xm_f32 is None:
                tmp = junk_f32
                P.op("dve", lambda e: e.scalar_tensor_tensor(out=tmp, in0=src, scalar=ss, in1=A_bc, op0=ALU.mult, op1=ALU.mult),
                     reads=[b_src, b_small, b_bcA], writes=[b_jf])
                P.op("dve", lambda e: e.tensor_tensor(out=junk, in0=tmp, in1=S_bc, op=ALU.add), reads=[b_jf, b_bcS], writes=[b_junk])
                pv = banks[bank][:, 0:512].bitcast(BF16)
                for kc in range(8):
                    P.op("pe", lambda e, kc=kc: e.transpose(out=pv[:, kc * 128:(kc + 1) * 128], in_=junk[:, kc * 128:(kc + 1) * 128], identity=ident_bf),
                         reads=[b_junk, b_const], writes=[b_ps[bank]])
                P.op("act", lambda e: e.activation(out=dstT, in_=pv.rearrange("p (a b) -> p a b", a=8), func=AF.Copy),
                     reads=[b_ps[bank]], writes=b_dstT)
            else:
                P.op("dve", lambda e: e.scalar_tensor_tensor(out=xm_f32, in0=src, scalar=ss, in1=A_bc, op0=ALU.mult, op1=ALU.mult),
                     reads=[b_src, b_small, b_bcA], writes=[b_jf])
                P.op("dve", lambda e: e.tensor_tensor(out=xm_f32, in0=xm_f32, in1=S_bc, op=ALU.add), reads=[b_jf, b_bcS], writes=[b_jf])
                for kc in range(8):
                    bk = bank + kc // 4
                    P.op("pe", lambda e, kc=kc, bk=bk: e.transpose(out=banks[bk][:, (kc % 4) * 128:(kc % 4 + 1) * 128],
                                                                   in_=xm_f32[:, kc * 128:(kc + 1) * 128], identity=ident),
                         reads=[b_jf, b_const], writes=[b_ps[bk]])
                for hf in range(2):
                    P.op("act", lambda e, hf=hf: e.activation(out=dstT[:, hf * 4:(hf + 1) * 4, :], in_=banks[bank + hf][:, :].rearrange("p (a b) -> p a b", a=4), func=AF.Copy),
                         reads=[b_ps[bank + hf]], writes=b_dstT)
                    P.op("dve", lambda e, hf=hf: e.tensor_copy(out=f32T[:, hf * 4:(hf + 1) * 4, :], in_=banks[bank + hf][:, :].rearrange("p (a b) -> p a b", a=4)),
                         reads=[b_ps[bank + hf]], writes=[b_f32T])

        junk_f320 = AD([1024], F32); b_jf0 = P.buf()
        junk_f32 = junk_f320; b_jf = b_jf0

        def p1_tile(t, ev):
            if t < 2:
                src_d = ctx_d[b, t * 128:(t + 1) * 128, :]
            else:
                src_d = x_d[b, (t - 2) * 128:(t - 1) * 128, :]
            xt = ev["xin"]; bk0, bk1, bk2 = ev["banks"]
            ghf_ = ev["ghf"]
            P.dma("sp", ev["sem"], xt, src_d, writes=[ev["b_xin"]])
            norm_mod_T(xt, ev["b_xin"], bcA, bcS, xT[:, :, t * 128:(t + 1) * 128], [b_xT[t]], bk0, ev["xmb"], ev["b_xmb"], tmps=ev["tmps"])
            for kc in range(8):
                P.op("pe", I("matmul", banks[bk1][:, 0:16], xT[:, kc, t * 128:(t + 1) * 128], Wab[:, kc, :], start=(kc == 0), stop=(kc == 7)),
                     reads=[b_xT[t], b_wab], writes=[b_ps[bk1]])
            g8 = gb[:, t, 0:8]
            P.op("dve", I("tensor_tensor", out=g8, in0=banks[bk1][:, 0:8], in1=dtb_bc, op=ALU.add), reads=[b_ps[bk1], b_par], writes=[b_gb[t]])
            P.op("act", I("activation", out=gb[:, t, 8:16], in_=banks[bk1][:, 8:16], func=AF.Sigmoid), reads=[b_ps[bk1]], writes=[b_gb[t]])
            P.op("act", I("activation", out=g8, in_=g8, func=AF.Exp), reads=[b_gb[t]], writes=[b_gb[t]])
            P.op("act", I("activation", out=g8, in_=g8, func=AF.Ln, bias=1.0), reads=[b_gb[t]], writes=[b_gb[t]])
            P.op("dve", I("tensor_tensor", out=g8, in0=g8, in1=negA, op=ALU.mult), reads=[b_gb[t], b_par], writes=[b_gb[t]])
            P.op("dve", I("tensor_copy", out=ghb[:, t, 0:8], in_=g8), reads=[b_gb[t]], writes=[b_gb[t]])
            P.op("dve", I("tensor_copy", out=ghf_, in_=ghb[:, t, 0:8]), reads=[b_gb[t]], writes=[b_gb[t]])
            P.op("dve", I("tensor_tensor", out=ghb[:, t, 8:16], in0=g8, in1=ghf_, op=ALU.subtract), reads=[b_gb[t]], writes=[b_gb[t]])
            for mi, mk in enumerate((m_ui, m_ls, m_li, m_us, m_one)):
                P.op("pe", I("matmul", banks[bk2][:, mi * 8:(mi + 1) * 8], mk, g8, start=True, stop=True), reads=[b_gb[t], b_const], writes=[b_ps[bk2]])
            P.op("act", I("activation", out=cumE[:, t, :], in_=banks[bk2][:, 0:40], func=AF.Exp), reads=[b_ps[bk2]], writes=[b_gb[t]])
            P.op("dve", I("tensor_scalar", out=negE[:, t, :], in0=cumE[:, t, :], scalar1=-1.0, scalar2=None, op0=ALU.mult), reads=[b_gb[t]], writes=[b_gb[t]])

        def phase1_tiles(tiles, j):
            make_bc(bcA, j, 1, b_bcA, xin[1], b_xin[1], g_bc=g1_bc, b_g=b_xin[0])
            make_bc(bcS, j, 0, b_bcS, xin[1], b_xin[1])
            for i in range(0, len(tiles), 2):
                P.capture = []
                p1_tile(tiles[i], envs1[0])
                A_ = P.capture
                P.capture = []
                p1_tile(tiles[i + 1], envs1[1])
                B_ = P.capture
                P.capture = None
                P.replay_merged(A_, B_)

        if stop <= 0:
            return
        _x2 = AD([1024], F32); _bx2 = P.buf()
        xin2 = [_x2, _x2]; b_xin2 = [_bx2, _bx2]
        ghb = AD([18, 16], BF16); ghf = AD([8], F32)
        E1 = Alloc(arena[:, RB:RB + 16 * KB], 16 * KB)
        envs1 = [
            {"xin": xin2[0], "b_xin": b_xin2[0], "sem": sem_x[0], "xmb": xmb, "b_xmb": b_xmb, "tmps": None, "ghf": ghf, "banks": (0, 1, 2)},
            {"xin": E1([1024], F32), "b_xin": P.buf(), "sem": sem_x[1], "xmb": E1([1024], BF16), "b_xmb": P.buf(),
             "tmps": (E1([16], F32), P.buf(), E1([1024], F32), P.buf()), "ghf": E1([8], F32), "banks": (3, 4, 5)},
        ]
        phase1_tiles([0, 1], 2)
        phase1_tiles(list(range(2, 18)), b)

        if stop <= 1:
            return
        P.dma("pool", sem_w[1], Wqkv, win_d.rearrange("(kc p) n -> p kc n", p=128)[:, :, 1024:2560], writes=[b_wqkv])
        C5 = Alloc(arena[:, CT:CT + 28 * KB], 28 * KB)
        PTb = [C5([2320], BF16) for _ in range(2)]; b_PTb = [P.buf() for _ in range(2)]
        ACC = C5([2304], F32); b_ACC = P.buf()
        SQB = C5([2304], BF16); b_SQB = P.buf()
        RNb = [C5([512], F32) for _ in range(2)]; b_RNb = [P.buf() for _ in range(2)]
        DG = C5([5, 128], BF16); b_DG = P.buf()
        for i_ in range(2):
            P.op("pool", I("memset", PTb[i_], 0.0), writes=[b_PTb[i_]])
        blocks = [(0, 256)] + [(256 + i * 512, 512) for i in range(4)]
        for cc in range(12):
            pi_ = cc % 2
            PT_ = PTb[pi_]; bPT = b_PTb[pi_]
            for tp in range(5):
                P.op("dve", I("tensor_scalar", out=DG[:, tp, :], in0=ident_bf, scalar1=convw[:, cc, tp:tp + 1], scalar2=None, op0=ALU.mult),
                     reads=[b_par, b_const], writes=[b_DG])
            for bi, (t0, n) in enumerate(blocks):
                bk = bi % 2
                tl = list(range(t0 // 128, (t0 + n) // 128))
                for kc in range(8):
                    P.op("pe", I("matmul", banks[bk][:, 0:n], Wqkv[:, kc, cc * 128:(cc + 1) * 128], xT[:, kc, t0:t0 + n], start=(kc == 0), stop=(kc == 7)),
                         reads=[b_wqkv] + [b_xT[t] for t in tl], writes=[b_ps[bk]])
                po = (2 + t0) if t0 < 256 else (262 + t0 - 256)
                P.op("act", I("activation", out=PT_[:, po:po + n], in_=banks[bk][:, 0:n], func=AF.Copy), reads=[b_ps[bk]], writes=[bPT])
            allq = [b_qkv[cc][t] for t in range(18)]
            for bi, (t0, n) in enumerate(blocks):
                bk = 2 + bi % 2
                po = (2 + t0) if t0 < 256 else (262 + t0 - 256)
                for tp in range(5):
                    P.op("pe", I("matmul", banks[bk][:, 0:n], DG[:, tp, :], PT_[:, po - 2 + tp:po - 2 + tp + n], start=(tp == 0), stop=(tp == 4)),
                         reads=[b_DG, bPT], writes=[b_ps[bk]])
                if cc >= 8:
                    P.op("act", I("activation", out=qkvT[:, cc, t0:t0 + n], in_=banks[bk][:, 0:n], func=AF.Silu), reads=[b_ps[bk]], writes=allq)
                else:
                    P.op("act", I("activation", out=ACC[:, t0:t0 + n], in_=banks[bk][:, 0:n], func=AF.Silu), reads=[b_ps[bk]], writes=[b_ACC])
                    P.op("dve", I("tensor_tensor", out=SQB[:, t0:t0 + n], in0=ACC[:, t0:t0 + n], in1=ACC[:, t0:t0 + n], op=ALU.mult), reads=[b_ACC], writes=[b_SQB])
            if cc < 8:
                sc = (128.0 ** -0.5) if cc < 4 else 1.0
                for bi, (t0, n) in enumerate(blocks):
                    bk = 4 + bi % 2
                    ri = bi % 2
                    P.op("pe", I("matmul", banks[bk][:, 0:n], ones_bf, SQB[:, t0:t0 + n], start=True, stop=True), reads=[b_SQB, b_const], writes=[b_ps[bk]])
                    P.op("act", I("activation", out=RNb[ri][:, 0:n], in_=banks[bk][:, 0:n], func=AF.Sqrt, bias=eps_t), reads=[b_ps[bk], b_const], writes=[b_RNb[ri]])
                    P.op("dve", I("reciprocal", out=RNb[ri][:, 0:n], in_=RNb[ri][:, 0:n]), reads=[b_RNb[ri]], writes=[b_RNb[ri]])
                    P.op("dve", I("scalar_tensor_tensor", out=qkvT[:, cc, t0:t0 + n], in0=ACC[:, t0:t0 + n], scalar=sc, in1=RNb[ri][:, 0:n], op0=ALU.mult, op1=ALU.mult),
                         reads=[b_ACC, b_RNb[ri]], writes=allq)
        P.barrier()
        if stop <= 5:
            return
        SA = Alloc(arena[:, RA:RA + 64 * KB], 64 * KB)
        seqs = []
        for d in range(2):
            for h in range(4):
                q = {"d": d, "h": h}
                for nm in ("gmask", "decT", "dmbd", "dmx", "dmq", "vtok"):
                    q[nm] = SA([128], F32)
                for nm in ("Mbd", "Nbd", "XT", "QKd", "R0", "R1", "RT0", "RT1", "P0", "P1", "PT0", "PT1", "ZT", "AinvT", "kd", "r", "vnew", "Sbf"):
                    q[nm] = SA([128], BF16)
                q["S"] = SA([128], F32)
                q["b"] = {}
                seqs.append(q)

        def sb(q, nm):
            if nm not in q["b"]:
                q["b"][nm] = P.buf()
            return q["b"][nm]
        ring_i = {"prep": 0, "scan": 0}

        def pslot(kind="prep"):
            i = ring_i[kind]
            ring_i[kind] += 1
            if kind == "prep":
                i %= 20
                bk, c = i % 5, i // 5
            else:
                i %= 12
                bk, c = 5 + i % 3, i // 3
            return banks[bk][:, c * 128:(c + 1) * 128], b_ps[bk]

        for q in seqs:
            P.op("pool", I("memset", q["S"], 0.0), writes=[sb(q, "S")])
            P.op("pool", I("memset", q["Sbf"], 0.0), writes=[sb(q, "Sbf")])
        orders = [list(range(18)), [1, 0] + list(range(17, 1, -1))]
        o_written = [[False] * 4 for _ in range(16)]

        def mmq(out, lhsT, rhs, reads, bw):
            P.op("pe", lambda e: e.matmul(out, lhsT, rhs, start=True, stop=True), reads=reads, writes=[bw])

        def scan_step(step2, part):
            step = step2 // 2
            st = []
            for q in seqs:
                d, h = q["d"], q["h"]
                if d != step2 % 2:
                    continue
                t = orders[d][step]
                if KLAT == 0:
                    t = t % 2
                st.append((q, d, h, t, (t >= 2) and KNOQK == 0))
            tsl = lambda t: slice(t * 128, (t + 1) * 128)
            if part == "prep":
                for (q, d, h, t, lat) in st:
                    kT = qkvT[:, 4 + h, tsl(t)]; qT = qkvT[:, h, tsl(t)]; vT = qkvT[:, 8 + h, tsl(t)]
                    q["kT"], q["qT"] = kT, qT
                    q["bk"], q["bq"], q["bv"] = b_qkv[4 + h][t], b_qkv[h][t], b_qkv[8 + h][t]
                    col = d * 4 + h
                    q["beta"] = gb[:, t, 8 + col:9 + col]
                    q["gcol"] = gb[:, t, col:col + 1]
                    q["eg"] = cumE[:, t, (h if d == 0 else 20 + h):(h if d == 0 else 20 + h) + 1]
                    q["neg"] = negE[:, t, (h if d == 0 else 20 + h):(h if d == 0 else 20 + h) + 1]
                    q["ekd"] = cumE[:, t, (8 + h if d == 0 else 28 + h):(8 + h if d == 0 else 28 + h) + 1]
                    q["gl"] = cumE[:, t, 32 + col:33 + col]
                    q["bg"] = b_gb[t]
                    ks, q["bks"] = pslot(); q["ks"] = ks.bitcast(BF16)[:, 0:128]
                    vs, q["bvs"] = pslot(); q["vs"] = vs.bitcast(BF16)[:, 0:128]
                    P.op("pe", I("transpose", out=q["ks"], in_=kT, identity=ident_bf), reads=[q["bk"], b_const], writes=[q["bks"]])
                    P.op("pe", I("transpose", out=q["vs"], in_=vT, identity=ident_bf), reads=[q["bv"], b_const], writes=[q["bvs"]])
                    ml = mb_ls if d == 0 else mb_us
                    gmv = q["gmask"].bitcast(BF16)
                    q["gmh"], q["gml"] = gmv[:, 0:128], gmv[:, 128:256]
                    P.op("dve", I("tensor_scalar", out=q["gmh"], in0=ml, scalar1=ghb[:, t, col:col + 1], scalar2=None, op0=ALU.mult),
                         reads=[q["bg"], b_const], writes=[sb(q, "gmask")])
                    P.op("dve", I("tensor_scalar", out=q["gml"], in0=ml, scalar1=ghb[:, t, 8 + col:9 + col], scalar2=None, op0=ALU.mult),
                         reads=[q["bg"], b_const], writes=[sb(q, "gmask")])
                if KSUB < 2:
                    return
                for (q, d, h, t, lat) in st:
                    P.op("act", I("activation", out=q["vtok"], in_=q["vs"], func=AF.Copy), reads=[q["bvs"]], writes=[sb(q, "vtok")])
                    P.op("act", I("activation", out=q["kd"], in_=q["ks"], func=AF.Copy, scale=q["ekd"]), reads=[q["bks"], q["bg"]], writes=[sb(q, "kd")])
                for (q, d, h, t, lat) in st:
                    q["G"], q["bG"] = pslot()
                    mmq(q["G"], q["kT"], q["kT"], [q["bk"]], q["bG"])
                    if lat:
                        q["QK"], q["bQK"] = pslot()
                        mmq(q["QK"], q["kT"], q["qT"], [q["bk"], q["bq"]], q["bQK"])
                for (q, d, h, t, lat) in st:
                    mr = mb_ui if d == 0 else mb_li
                    q["df"], q["bdf"] = pslot()
                    P.op("pe", I("matmul", q["df"], q["gmh"], mr, start=True, stop=False), reads=[sb(q, "gmask"), b_const], writes=[q["bdf"]])
                    P.op("pe", I("matmul", q["df"], q["gml"], mr, start=False, stop=True), reads=[sb(q, "gmask"), b_const], writes=[q["bdf"]])
                if KSUB < 3:
                    return
                for (q, d, h, t, lat) in st:
                    P.op("act", I("activation", out=q["decT"], in_=q["df"], func=AF.Exp), reads=[q["bdf"]], writes=[sb(q, "decT")])
                if KSUB < 5:
                    return
                for (q, d, h, t, lat) in st:
                    mbd, mx, mi = (mb_ubd, mb_ux, mb_ui) if d == 0 else (mb_lbd, mb_lx, mb_li)
                    t1 = q["dmbd"].bitcast(BF16)[:, 0:128]
                    P.op("dve", I("scalar_tensor_tensor", out=t1, in0=q["G"], scalar=q["beta"], in1=q["decT"], op0=ALU.mult, op1=ALU.mult),
                         reads=[q["bG"], q["bg"], sb(q, "decT")], writes=[sb(q, "dmbd")])
                    P.op("dve", I("tensor_tensor", out=q["Mbd"], in0=t1, in1=mbd, op=ALU.mult), reads=[sb(q, "dmbd"), b_const], writes=[sb(q, "Mbd")])
                    P.op("dve", I("tensor_tensor", out=q["XT"], in0=t1, in1=mx, op=ALU.mult), reads=[sb(q, "dmbd"), b_const], writes=[sb(q, "XT")])
                    if lat:
                        t2 = q["dmq"].bitcast(BF16)[:, 0:128]
                        P.op("dve", I("tensor_tensor", out=t2, in0=q["QK"], in1=q["decT"], op=ALU.mult), reads=[q["bQK"], sb(q, "decT")], writes=[sb(q, "dmq")])
                        P.op("dve", I("tensor_tensor", out=q["QKd"], in0=t2, in1=mi, op=ALU.mult), reads=[sb(q, "dmq"), b_const], writes=[sb(q, "QKd")])
                if KSUB < 6:
                    return
                for (q, d, h, t, lat) in st:
                    ns, q["bns"] = pslot(); q["ns"] = ns.bitcast(BF16)[:, 0:128]
                    P.op("pe", I("transpose", out=q["ns"], in_=q["Mbd"], identity=ident_bf), reads=[sb(q, "Mbd"), b_const], writes=[q["bns"]])
                    P.op("act", I("activation", out=q["Nbd"], in_=q["ns"], func=AF.Copy), reads=[q["bns"]], writes=[sb(q, "Nbd")])
                    P.op("dve", I("tensor_tensor", out=q["R0"], in0=ident_bf, in1=q["Mbd"], op=ALU.subtract), reads=[sb(q, "Mbd"), b_const], writes=[sb(q, "R0")])
                    q["cur"] = ("Mbd", "Nbd", "R0", "RT0")
                if KSUB < 7:
                    return
                for lvl in range(5):
                    pn, ptn = ("P0", "PT0") if lvl % 2 == 0 else ("P1", "PT1")
                    rn_ = "R1" if lvl % 2 == 0 else "R0"
                    last = lvl == 4
                    for (q, d, h, t, lat) in st:
                        pw, pwt, r_, _ = q["cur"]
                        if not last:
                            q["p2"], q["bp2"] = pslot()
                            mmq(q["p2"], q[pwt], q[pw], [sb(q, pw), sb(q, pwt)], q["bp2"])
                        q["p2t"], q["bp2t"] = pslot()
                        mmq(q["p2t"], q[pw], q[pwt], [sb(q, pw), sb(q, pwt)], q["bp2t"])
                    for (q, d, h, t, lat) in st:
                        if not last:
                            P.op("act", I("activation", out=q[pn], in_=q["p2"], func=AF.Copy), reads=[q["bp2"]], writes=[sb(q, pn)])
                        P.op("act", I("activation", out=q[ptn], in_=q["p2t"], func=AF.Copy), reads=[q["bp2t"]], writes=[sb(q, ptn)])
                    for (q, d, h, t, lat) in st:
                        pw, pwt, r_, _ = q["cur"]
                        q["ra"], q["bra"] = pslot()
                        mmq(q["ra"], q[ptn], q[r_], [sb(q, r_), sb(q, ptn)], q["bra"])
                    for (q, d, h, t, lat) in st:
                        pw, pwt, r_, _ = q["cur"]
                        P.op("dve", I("tensor_tensor", out=q[rn_], in0=q["ra"], in1=q[r_], op=ALU.add),
                             reads=[q["bra"], sb(q, r_)], writes=[sb(q, rn_)])
                        q["cur"] = (pn, ptn, rn_, None)
                for (q, d, h, t, lat) in st:
                    _, _, r_, _ = q["cur"]
                    rts, q["brts"] = pslot(); q["rts"] = rts.bitcast(BF16)[:, 0:128]
                    P.op("pe", I("transpose", out=q["rts"], in_=q[r_], identity=ident_bf), reads=[sb(q, r_), b_const], writes=[q["brts"]])
                for (q, d, h, t, lat) in st:
                    _, _, r_, _ = q["cur"]
                    P.op("act", I("activation", out=q["RT0"], in_=q["rts"], func=AF.Copy), reads=[q["brts"]], writes=[sb(q, "RT0")])
                    q["cur"] = (None, None, r_, "RT0")
                if KSUB < 8:
                    return
                for (q, d, h, t, lat) in st:
                    _, _, r_, rt_ = q["cur"]
                    q["z"], q["bz"] = pslot()
                    mmq(q["z"], q["XT"], q[rt_], [sb(q, "XT"), sb(q, rt_)], q["bz"])
                for (q, d, h, t, lat) in st:
                    P.op("act", I("activation", out=q["ZT"], in_=q["z"], func=AF.Copy), reads=[q["bz"]], writes=[sb(q, "ZT")])
                for (q, d, h, t, lat) in st:
                    _, _, r_, rt_ = q["cur"]
                    q["w"], q["bw"] = pslot()
                    mmq(q["w"], q["ZT"], q[r_], [sb(q, "ZT"), sb(q, r_)], q["bw"])
                for (q, d, h, t, lat) in st:
                    _, _, r_, rt_ = q["cur"]
                    P.op("dve", I("scalar_tensor_tensor", out=q["AinvT"], in0=q["w"], scalar=-1.0, in1=q[r_], op0=ALU.mult, op1=ALU.add),
                         reads=[q["bw"], sb(q, r_)], writes=[sb(q, "AinvT")])
                if KSUB < 9:
                    return
                return
            for (q, d, h, t, lat) in st:
                q["a"], q["ba"] = pslot("scan")
                mmq(q["a"], q["kT"], q["Sbf"], [q["bk"], sb(q, "Sbf")], q["ba"])
            for (q, d, h, t, lat) in st:
                P.op("dve", I("scalar_tensor_tensor", out=q["r"], in0=q["a"], scalar=q["neg"], in1=q["vtok"], op0=ALU.mult, op1=ALU.add),
                     reads=[q["ba"], q["bg"], sb(q, "vtok")], writes=[sb(q, "r")])
            for (q, d, h, t, lat) in st:
                q["bb"], q["bbb"] = pslot("scan")
                mmq(q["bb"], q["AinvT"], q["r"], [sb(q, "AinvT"), sb(q, "r")], q["bbb"])
            for (q, d, h, t, lat) in st:
                P.op("act", I("activation", out=q["vnew"], in_=q["bb"], func=AF.Copy, scale=q["beta"]), reads=[q["bbb"], q["bg"]], writes=[sb(q, "vnew")])
            for (q, d, h, t, lat) in st:
                if lat:
                    q["o1"], q["bo1"] = pslot("scan")
                    mmq(q["o1"], q["qT"], q["Sbf"], [q["bq"], sb(q, "Sbf")], q["bo1"])
                    q["o2"], q["bo2"] = pslot("scan")
                    mmq(q["o2"], q["QKd"], q["vnew"], [sb(q, "QKd"), sb(q, "vnew")], q["bo2"])
                q["sp"], q["bsp"] = pslot("scan")
                mmq(q["sp"], q["kd"], q["vnew"], [sb(q, "kd"), sb(q, "vnew")], q["bsp"])
            for (q, d, h, t, lat) in st:
                if lat:
                    oa = o_acc[:, t - 2, h * 128:(h + 1) * 128]
                    bo = b_oacc[t - 2][h]
                    tmp = q["gmask"]
                    if not o_written[t - 2][h]:
                        P.op("act", I("activation", out=tmp, in_=q["o2"], func=AF.Copy), reads=[q["bo2"]], writes=[sb(q, "gmask")])
                        o_written[t - 2][h] = True
                    else:
                        P.op("dve", I("tensor_tensor", out=tmp, in0=q["o2"], in1=oa, op=ALU.add), reads=[q["bo2"], bo], writes=[sb(q, "gmask")])
                    P.op("dve", I("scalar_tensor_tensor", out=oa, in0=q["o1"], scalar=q["eg"], in1=tmp, op0=ALU.mult, op1=ALU.add),
                         reads=[q["bo1"], q["bg"], sb(q, "gmask")], writes=[bo])
                P.op("dve", I("scalar_tensor_tensor", out=q["S"], in0=q["S"], scalar=q["gl"], in1=q["sp"], op0=ALU.mult, op1=ALU.add),
                     reads=[sb(q, "S"), q["bg"], q["bsp"]], writes=[sb(q, "S")])
                P.op("act", I("activation", out=q["Sbf"], in_=q["S"], func=AF.Copy), reads=[sb(q, "S")], writes=[sb(q, "Sbf")])
        def cap2(step2, part):
            P.capture = []
            scan_step(step2, part)
            lst = P.capture
            P.capture = None
            return lst
        nst = min(36, KSTEPS)
        P.replay_merged(cap2(0, "prep"), [])
        for step2 in range(nst):
            nxt = cap2(step2 + 1, "prep") if step2 + 1 < nst else []
            P.replay_merged(nxt, cap2(step2, "scan"))
        P.barrier()

        if stop <= 6:
            return
        WB = Alloc(arena[:, RB:RB + 54 * KB], 54 * KB)
        WinA = WB([8, 1024], BF16); Wz = WB([8, 512], BF16); Wout = WB([8, 1024], BF16)
        b_w7 = P.buf()
        sets7 = []
        for i7 in range(2):
            d7 = {}
            if i7 == 0:
                d7["xTt"] = WB([8, 128], BF16); d7["u"] = WB([512], F32); d7["v"] = WB([512], F32); d7["vn"] = WB([512], BF16)
                d7["y"] = WB([1024], BF16); d7["yT"] = WB([8, 128], BF16); d7["sz"] = WB([512], F32); d7["st6"] = WB([8], F32); d7["ss4"] = WB([4], F32)
            else:
                d7["u"] = xin2[0][:, 0:512]; d7["v"] = xin2[0][:, 512:1024]
                d7["sz"] = xin[0][:, 0:512]
                d7["y"] = xin[0][:, 512:1024].bitcast(BF16)
                d7["xTt"] = V(arena, RD + 1152, [8, 128], BF16); d7["vn"] = V(arena, RD + 1152 + 2048, [512], BF16)
                d7["yT"] = V(arena, RD + 4224, [8, 128], BF16)
                d7["st6"] = WB([8], F32); d7["ss4"] = WB([4], F32)
            for nm in ("xTt", "u", "v", "vn", "y", "yT", "sz", "st"):
                d7["b_" + nm] = P.buf()
            sets7.append(d7)
        winv = win_d.rearrange("(kc p) n -> p kc n", p=128)
        P.dma("pool", sem_w[0], WinA, winv[:, :, 0:1024], writes=[b_w7])
        b_wz = P.buf()
        P.dma("pool", sem_w[1], Wz, winv[:, :, 2560:3072], writes=[b_wz])
        b_wo = P.buf()
        P.dma("pool", sem_w[2], Wout, wout_d.rearrange("(kc p) n -> p kc n", p=128), writes=[b_wo])
        make_bc(bcG, b, 2, b_bcG, xin[1], b_xin[1])
        for ch in range(8):
            P.op("pool", I("tensor_tensor", out=Wout[:, ch, :], in0=Wout[:, ch, :], in1=bcG, op=ALU.mult), reads=[b_wo, b_bcG], writes=[b_wo])
        def p7_a(tt, xTt, u_sb, v_sb, vn_bf, y_bf, yTt, sz, st6, ss4, b_xTt, b_u, b_v, b_vn, b_y, b_yT, b_sz, b_st):
            xt = x_res[:, tt, :]
            P.dma("sp", sem_x[tt % 2], xt, x_d[b, tt * 128:(tt + 1) * 128, :], writes=[b_xres[tt]])
            norm_mod_T(xt, b_xres[tt], bcA, bcS, xTt, [b_xTt], 0, xmb, b_xmb)
            for hf, bk in ((0, 1), (1, 2)):
                for kc in range(8):
                    P.op("pe", lambda e, kc=kc, hf=hf, bk=bk: e.matmul(banks[bk][:, :], xTt[:, kc, :], WinA[:, kc, hf * 512:(hf + 1) * 512],
                                                                     start=(kc == 0), stop=(kc == 7)), reads=[b_xTt, b_w7], writes=[b_ps[bk]])
            for kc in range(8):
                P.op("pe", lambda e, kc=kc: e.matmul(banks[3][:, :], xTt[:, kc, :], Wz[:, kc, :], start=(kc == 0), stop=(kc == 7)),
                     reads=[b_xTt, b_wz], writes=[b_ps[3]])
            P.op("act", lambda e: e.activation(out=u_sb, in_=banks[1][:, :], func=AF.Gelu_apprx_tanh), reads=[b_ps[1]], writes=[b_u])
            P.op("act", lambda e: e.activation(out=v_sb, in_=banks[2][:, :], func=AF.Gelu_apprx_tanh), reads=[b_ps[2]], writes=[b_v])
            P.op("act", lambda e: e.activation(out=sz, in_=banks[3][:, :], func=AF.Silu), reads=[b_ps[3]], writes=[b_sz])
            P.op("dve", lambda e: e.bn_stats(out=st6[:, 0:6], in_=v_sb), reads=[b_v], writes=[b_st])
            P.op("dve", lambda e: e.bn_aggr(out=st6[:, 6:8], in_=st6[:, 0:6]), reads=[b_st], writes=[b_st])
            P.op("dve", lambda e: e.tensor_scalar(out=st6[:, 7:8], in0=st6[:, 7:8], scalar1=EPS, scalar2=None, op0=ALU.add), reads=[b_st], writes=[b_st])
            P.op("act", lambda e: e.activation(out=st6[:, 7:8], in_=st6[:, 7:8], func=AF.Sqrt), reads=[b_st], writes=[b_st])
            P.op("dve", lambda e: e.reciprocal(out=st6[:, 7:8], in_=st6[:, 7:8]), reads=[b_st], writes=[b_st])
            P.op("dve", lambda e: e.tensor_scalar(out=v_sb, in0=v_sb, scalar1=st6[:, 6:7], scalar2=st6[:, 7:8], op0=ALU.subtract, op1=ALU.mult),
                 reads=[b_v, b_st], writes=[b_v])
            P.op("dve", lambda e: e.tensor_tensor(out=v_sb, in0=v_sb, in1=lng_bc, op=ALU.mult), reads=[b_v, b_par], writes=[b_v])
            P.op("dve", lambda e: e.tensor_tensor(out=vn_bf, in0=v_sb, in1=lnb_bc, op=ALU.add), reads=[b_v, b_par], writes=[b_vn])
        def p7_b(tt, xTt, u_sb, v_sb, vn_bf, y_bf, yTt, sz, st6, ss4, b_xTt, b_u, b_v, b_vn, b_y, b_yT, b_sz, b_st):
            xt = x_res[:, tt, :]
            for h in range(4):
                P.op("pe", lambda e, h=h: e.matmul(banks[4][:, h * 128:(h + 1) * 128], wsT[:, h, :], vn_bf[:, h * 128:(h + 1) * 128], start=True, stop=True),
                     reads=[b_vn, b_par], writes=[b_ps[4]])
            for h in range(4):
                hs = slice(h * 128, (h + 1) * 128)
                P.op("dve", lambda e, h=h, hs=hs: e.scalar_tensor_tensor(out=y_bf[:, hs], in0=banks[4][:, hs], scalar=bsT[:, h:h + 1], in1=u_sb[:, hs],
                                                                       op0=ALU.add, op1=ALU.mult), reads=[b_ps[4], b_par, b_u], writes=[b_y])
            for h in range(4):
                hs = slice(h * 128, (h + 1) * 128)
                P.op("act", lambda e, h=h, hs=hs, tt=tt: e.activation(out=u_sb[:, hs], in_=o_acc[:, tt, hs], func=AF.Square, accum_out=ss4[:, h:h + 1]),
                     reads=[b_oacc[tt][h], b_y], writes=[b_u, b_st])
            rstd_from_ss(ss4, 128.0, b_st)
            for h in range(4):
                hs = slice(h * 128, (h + 1) * 128)
                P.op("dve", lambda e, h=h, hs=hs, tt=tt: e.scalar_tensor_tensor(out=u_sb[:, hs], in0=o_acc[:, tt, hs], scalar=ss4[:, h:h + 1], in1=onorm_bc,
                                                                              op0=ALU.mult, op1=ALU.mult), reads=[b_oacc[tt][h], b_st, b_par, b_u], writes=[b_u])
            P.op("dve", lambda e: e.tensor_tensor(out=y_bf[:, 512:1024], in0=u_sb, in1=sz, op=ALU.mult), reads=[b_u, b_sz], writes=[b_y])
            pv = banks[5][:, 0:512].bitcast(BF16)
            for ch in range(8):
                P.op("pe", lambda e, ch=ch: e.transpose(out=pv[:, ch * 128:(ch + 1) * 128], in_=y_bf[:, ch * 128:(ch + 1) * 128], identity=ident_bf),
                     reads=[b_y, b_const], writes=[b_ps[5]])
            P.op("act", lambda e: e.activation(out=yTt, in_=pv.rearrange("p (a b) -> p a b", a=8), func=AF.Copy), reads=[b_ps[5]], writes=[b_yT])
            for hf in range(2):
                bk = 6 + hf
                for ch in range(8):
                    P.op("pe", lambda e, ch=ch, hf=hf, bk=bk: e.matmul(banks[bk][:, :], yTt[:, ch, :], Wout[:, ch, hf * 512:(hf + 1) * 512],
                                                                     start=(ch == 0), stop=(ch == 7)), reads=[b_yT, b_wo], writes=[b_ps[bk]])
            for hf in range(2):
                hs = slice(hf * 512, (hf + 1) * 512)
                P.op("dve", I("tensor_tensor", out=xt[:, hs], in0=banks[6 + hf][:, :], in1=xt[:, hs], op=ALU.add),
                     reads=[b_ps[6 + hf], b_xres[tt]], writes=[b_xres[tt]])
        def args7(tt):
            d7 = sets7[tt % 2]
            return (tt, d7["xTt"], d7["u"], d7["v"], d7["vn"], d7["y"], d7["yT"], d7["sz"], d7["st6"], d7["ss4"],
                    d7["b_xTt"], d7["b_u"], d7["b_v"], d7["b_vn"], d7["b_y"], d7["b_yT"], d7["b_sz"], d7["b_st"])
        def cap(fn, *a):
            P.capture = []
            fn(*a)
            lst = P.capture
            P.capture = None
            return lst
        P.replay_merged(cap(p7_a, *args7(0)), [])
        for tt in range(1, 16):
            P.replay_merged(cap(p7_a, *args7(tt)), cap(p7_b, *args7(tt - 1)))
        P.replay_merged([], cap(p7_b, *args7(15)))
        P.barrier()
        if dbg and b == 0:
            out_toks.append(P.dma("sp", sem_o[0], dbg_d, x_res, reads=b_xres))
            P.barrier()

        if stop <= 7:
            return
        MA = Alloc(arena[:, RB:ARENA], ARENA - RB)
        h2T = MA([8, 2048], BF16)
        b_h2T = [P.buf() for _ in range(16)]
        GU = [MA([8, 1024], BF16) for _ in range(2)]; DW = [MA([4, 1024], BF16) for _ in range(2)]
        b_GU = [P.buf() for _ in range(2)]; b_DW = [P.buf() for _ in range(2)]
        act_t = [MA([4, 512], BF16) for _ in range(2)]; b_act = [P.buf() for _ in range(2)]
        sg = [MA([512], F32) for _ in range(2)]; b_sg = [P.buf() for _ in range(2)]
        h2f = MA([1024], F32); f32T = MA([8, 128], F32); b_f32T = P.buf()
        cb = MA([16, 16], F32); b_cb = [P.buf() for _ in range(16)]
        lg = MA([20], F32); rt = MA([32], F32); b_rt = P.buf()
        small2 = MA([16], F32); b_small2 = P.buf()
        bc2A = MA([1024], F32); bc2S = MA([1024], F32); bc2G = MA([1024], F32); tb_ = MA([1024], F32)
        b_2A, b_2S, b_2G, b_tb = P.buf(), P.buf(), P.buf(), P.buf()
        jb = MA([1024], BF16); b_jb = P.buf()
        load(tb_, n2g_d.partition_broadcast(128), writes=[b_tb])
        P.op("dve", lambda e: e.tensor_copy(out=h2f, in_=tb_), reads=[b_tb], writes=[b_jf])
        g2_bc = h2f
        make_bc(bc2A, b, 4, b_2A, tb_, b_tb, g_bc=g2_bc, b_g=b_jf)
        make_bc(bc2S, b, 3, b_2S, tb_, b_tb)
        make_bc(bc2G, b, 5, b_2G, tb_, b_tb)
        def route_tile(tt, ev):
            small2, b_small2, jb, b_jb, h2f, b_jf, f32T, b_f32T, lg, rt, b_rt, bkA, bkB, bkC = ev
            ss = small2[:, 0:1]
            src = x_res[:, tt, :]
            P.op("act", lambda e, src=src: e.activation(out=jb, in_=src, func=AF.Square, accum_out=ss), reads=[b_xres[tt]], writes=[b_jb, b_small2])
            rstd_from_ss(ss, 1024.0, b_small2)
            P.op("dve", lambda e, src=src: e.scalar_tensor_tensor(out=h2f, in0=src, scalar=ss, in1=bc2A, op0=ALU.mult, op1=ALU.mult),
                 reads=[b_xres[tt], b_small2, b_2A], writes=[b_jf])
            P.op("dve", lambda e: e.tensor_tensor(out=h2f, in0=h2f, in1=bc2S, op=ALU.add), reads=[b_jf, b_2S], writes=[b_jf])
            for kc in range(8):
                bk = (bkA, bkB)[kc // 4]
                P.op("pe", lambda e, kc=kc, bk=bk: e.transpose(out=banks[bk][:, (kc % 4) * 128:(kc % 4 + 1) * 128], in_=h2f[:, kc * 128:(kc + 1) * 128], identity=ident),
                     reads=[b_jf, b_const], writes=[b_ps[bk]])
            for hf in range(2):
                P.op("act", lambda e, hf=hf, tt=tt: e.activation(out=h2T[:, hf * 4:(hf + 1) * 4, tt * 128:(tt + 1) * 128],
                                                                 in_=banks[(bkA, bkB)[hf]][:, :].rearrange("p (a b) -> p a b", a=4), func=AF.Copy),
                     reads=[b_ps[(bkA, bkB)[hf]]], writes=[b_h2T[tt]])
                P.op("dve", lambda e, hf=hf: e.tensor_copy(out=f32T[:, hf * 4:(hf + 1) * 4, :], in_=banks[(bkA, bkB)[hf]][:, :].rearrange("p (a b) -> p a b", a=4)),
                     reads=[b_ps[(bkA, bkB)[hf]]], writes=[b_f32T])
            for kc in range(8):
                P.op("pe", lambda e, kc=kc: e.matmul(banks[bkC][:, 0:20], f32T[:, kc, :], Wr32[:, kc, :], start=(kc == 0), stop=(kc == 7)),
                     reads=[b_f32T, b_par], writes=[b_ps[bkC]])
            R_ = [b_rt]

            def dv(fn, extra_r=()):
                P.op("dve", fn, reads=R_ + list(extra_r), writes=R_)
            gmx, ngm, gsum, pg, m1, m2, dd, w1g, w2g = (rt[:, i:i + 1] for i in range(9))
            ohg = rt[:, 12:16]; es = rt[:, 16:20]; oh1 = rt[:, 20:24]; es2 = rt[:, 24:28]; oh2 = rt[:, 28:32]
            P.op("dve", lambda e: e.tensor_tensor(out=lg, in0=banks[bkC][:, 0:20], in1=brt_bc, op=ALU.add), reads=[b_ps[bkC], b_par], writes=R_)
            dv(lambda e: e.tensor_reduce(out=gmx, in_=lg[:, 0:4], axis=AX.X, op=ALU.max))
            dv(lambda e: e.tensor_scalar(out=ohg, in0=lg[:, 0:4], scalar1=gmx, scalar2=None, op0=ALU.is_equal))
            dv(lambda e: e.tensor_scalar(out=ngm, in0=gmx, scalar1=-1.0, scalar2=None, op0=ALU.mult))
            P.op("act", lambda e: e.activation(out=es2, in_=lg[:, 0:4], func=AF.Exp, bias=ngm, accum_out=gsum), reads=R_, writes=R_)
            dv(lambda e: e.reciprocal(out=pg, in_=gsum))
            dv(lambda e: e.tensor_scalar(out=es, in0=lg[:, 4:8], scalar1=ohg[:, 0:1], scalar2=None, op0=ALU.mult))
            for g in range(1, 4):
                dv(lambda e, g=g: e.scalar_tensor_tensor(out=es, in0=lg[:, 4 + 4 * g:8 + 4 * g], scalar=ohg[:, g:g + 1], in1=es, op0=ALU.mult, op1=ALU.add))
            dv(lambda e: e.tensor_reduce(out=m1, in_=es, axis=AX.X, op=ALU.max))
            dv(lambda e: e.tensor_scalar(out=oh1, in0=es, scalar1=m1, scalar2=None, op0=ALU.is_equal))
            dv(lambda e: e.scalar_tensor_tensor(out=es2, in0=oh1, scalar=-1e30, in1=es, op0=ALU.mult, op1=ALU.add))
            dv(lambda e: e.tensor_reduce(out=m2, in_=es2, axis=AX.X, op=ALU.max))
            dv(lambda e: e.tensor_scalar(out=oh2, in0=es2, scalar1=m2, scalar2=None, op0=ALU.is_equal))
            dv(lambda e: e.tensor_tensor(out=dd, in0=m1, in1=m2, op=ALU.subtract))
            P.op("act", lambda e: e.activation(out=dd, in_=dd, func=AF.Sigmoid), reads=R_, writes=R_)
            dv(lambda e: e.tensor_tensor(out=w1g, in0=dd, in1=pg, op=ALU.mult))
            dv(lambda e: e.tensor_tensor(out=w2g, in0=pg, in1=w1g, op=ALU.subtract))
            dv(lambda e: e.tensor_scalar(out=es, in0=oh1, scalar1=w1g, scalar2=None, op0=ALU.mult))
            dv(lambda e: e.scalar_tensor_tensor(out=es, in0=oh2, scalar=w2g, in1=es, op0=ALU.mult, op1=ALU.add))
            for g in range(4):
                P.op("dve", lambda e, g=g, tt=tt: e.tensor_scalar(out=cb[:, tt, 4 * g:4 * g + 4], in0=es, scalar1=ohg[:, g:g + 1], scalar2=None, op0=ALU.mult),
                     reads=R_, writes=[b_cb[tt]])
        f32T2 = MA([8, 128], F32); jb2 = MA([1024], BF16); lg2 = MA([20], F32); rt2 = MA([32], F32); small3 = MA([16], F32)
        ev_r = [(small2, b_small2, jb, b_jb, h2f, b_jf, f32T, b_f32T, lg, rt, b_rt, 0, 1, 2),
                (small3, P.buf(), jb2, P.buf(), tb_, b_tb, f32T2, P.buf(), lg2, rt2, P.buf(), 3, 4, 5)]
        for tt in range(0, 16, 2):
            P.capture = []
            route_tile(tt, ev_r[0])
            A_ = P.capture
            P.capture = []
            route_tile(tt + 1, ev_r[1])
            B_ = P.capture
            P.capture = None
            P.replay_merged(A_, B_)
        if stop <= 8:
            return
        def emit_GU(ex, s, tb4, a_s):
            for fc in range(4):
                gs = fc % 2
                bg_, bu_ = (0, 1) if gs == 0 else (2, 3)
                for kc in range(8):
                    P.op("pe", I("matmul", banks[bg_][:, :], GU[s][:, kc, fc * 128:(fc + 1) * 128], h2T[:, kc, tb4 * 512:(tb4 + 1) * 512], start=(kc == 0), stop=(kc == 7)),
                         reads=[b_GU[s]] + b_h2T[tb4 * 4:tb4 * 4 + 4], writes=[b_ps[bg_]])
                for kc in range(8):
                    P.op("pe", I("matmul", banks[bu_][:, :], GU[s][:, kc, 512 + fc * 128:512 + (fc + 1) * 128], h2T[:, kc, tb4 * 512:(tb4 + 1) * 512], start=(kc == 0), stop=(kc == 7)),
                         reads=[b_GU[s]] + b_h2T[tb4 * 4:tb4 * 4 + 4], writes=[b_ps[bu_]])
                P.op("act", I("activation", out=sg[gs], in_=banks[bg_][:, :], func=AF.Silu), reads=[b_ps[bg_]], writes=[b_sg[gs]])
                P.op("dve", I("tensor_tensor", out=act_t[a_s][:, fc, :], in0=sg[gs], in1=banks[bu_][:, :], op=ALU.mult),
                     reads=[b_sg[gs], b_ps[bu_]], writes=[b_act[a_s]])

        def emit_DOWN(ex, s, tb4, a_s):
            for t4 in range(4):
                tt = tb4 * 4 + t4
                ds = t4 % 2
                for hf in range(2):
                    bk = 4 + ds * 2 + hf
                    for fc in range(4):
                        P.op("pe", I("matmul", banks[bk][:, :], act_t[a_s][:, fc, t4 * 128:(t4 + 1) * 128], DW[s][:, fc, hf * 512:(hf + 1) * 512], start=(fc == 0), stop=(fc == 3)),
                             reads=[b_act[a_s], b_DW[s]], writes=[b_ps[bk]])
                for hf in range(2):
                    bk = 4 + ds * 2 + hf
                    hs = slice(hf * 512, (hf + 1) * 512)
                    P.op("dve", I("scalar_tensor_tensor", out=x_res[:, tt, hs], in0=banks[bk][:, :], scalar=cb[:, tt, ex:ex + 1], in1=x_res[:, tt, hs], op0=ALU.mult, op1=ALU.add),
                         reads=[b_ps[bk], b_cb[tt], b_xres[tt]], writes=[b_xres[tt]])

        pending = None
        gcount = 0
        for ex in range(16):
            s = ex % 2
            P.dma("pool", sem_w[s], GU[s], wgu_d[ex].rearrange("(kc p) n -> p kc n", p=128), writes=[b_GU[s]])
            P.dma("pool", sem_w[2 + s], DW[s], wdn_d[ex].rearrange("(fc p) n -> p fc n", p=128), writes=[b_DW[s]])
            for fc in range(4):
                P.op("pool", I("tensor_tensor", out=DW[s][:, fc, :], in0=DW[s][:, fc, :], in1=bc2G, op=ALU.mult),
                     reads=[b_DW[s], b_2G], writes=[b_DW[s]])
            for tb4 in range(4):
                a_s = gcount % 2
                emit_GU(ex, s, tb4, a_s)
                if pending is not None:
                    emit_DOWN(*pending)
                pending = (ex, s, tb4, a_s)
                gcount += 1
        emit_DOWN(*pending)
        if stop <= 9:
            return
        load(tb_, fg_d.partition_broadcast(128), writes=[b_tb])
        def fin_tile(tt, small_, b_small_, jb_, b_jb_, ot_, b_ot_, sem_):
            ss = small_[:, 0:1]
            src = x_res[:, tt, :]
            P.op("act", I("activation", out=jb_, in_=src, func=AF.Square, accum_out=ss), reads=[b_xres[tt]], writes=[b_jb_, b_small_])
            rstd_from_ss(ss, 1024.0, b_small_)
            P.op("dve", I("scalar_tensor_tensor", out=ot_, in0=src, scalar=ss, in1=tb_, op0=ALU.mult, op1=ALU.mult),
                 reads=[b_xres[tt], b_small_, b_tb], writes=[b_ot_])
            P.dma("sp", sem_, out_d[b, tt * 128:(tt + 1) * 128, :], ot_, reads=[b_ot_])
        fe = [(small2, b_small2, jb, b_jb, bc2A, b_2A, sem_o[0]), (small3, ev_r[1][1], jb2, ev_r[1][3], bc2S, b_2S, sem_o[1])]
        for tt in range(0, 16, 2):
            P.capture = []
            fin_tile(tt, *fe[0])
            A_ = P.capture
            P.capture = []
            fin_tile(tt + 1, *fe[1])
            B_ = P.capture
            P.capture = None
            P.replay_merged(A_, B_)
        P.barrier()

    for b in range(NB):
        do_batch(b)
        P.barrier()

    P._emit_waits("sp", out_toks + [(k, P.dma_sem_cnt[k]) for k in sem_o if P.dma_sem_cnt[k] > 0])
    P.emit()
    P.close()
    return nc


_NC_CACHE = {}


def kernel(**inputs):
    NB = 2
    if "nc" not in _NC_CACHE:
        _NC_CACHE["nc"] = build(NB)
    nc = _NC_CACHE["nc"]
    f = lambda a: np.ascontiguousarray(np.asarray(a, dtype=np.float32))
    shared = {
        "c_ctx": f(inputs["c_ctx"]).reshape(1, 1024), "w_ada": f(inputs["w_ada"])[0], "b_ada": f(inputs["b_ada"]).reshape(1, 6144),
        "norm1_g": f(inputs["norm1_g"]).reshape(1, 1024), "w_in": f(inputs["w_in"])[0], "ln_a_g": f(inputs["ln_a_g"]).reshape(1, 512),
        "ln_a_b": f(inputs["ln_a_b"]).reshape(1, 512), "w_spatial": f(inputs["w_spatial"])[0], "b_spatial": f(inputs["b_spatial"])[0],
        "conv_qkv": f(inputs["conv_qkv"])[0], "a_log": f(inputs["a_log"]).reshape(1, 8), "dt_bias": f(inputs["dt_bias"]).reshape(1, 8),
        "onorm_g": f(inputs["onorm_g"]).reshape(1, 128), "w_out": f(inputs["w_out"])[0], "norm2_g": f(inputs["norm2_g"]).reshape(1, 1024),
        "w_group": f(inputs["w_group"])[0], "b_group": f(inputs["b_group"]).reshape(1, 4), "w_router": f(inputs["w_router"])[0],
        "b_router": f(inputs["b_router"]).reshape(1, 16), "w_gate_up": f(inputs["w_gate_up"])[0], "w_down": f(inputs["w_down"])[0],
        "final_g": f(inputs["final_g"]).reshape(1, 1024),
    }
    x = f(inputs["x"]); c = f(inputs["c"]); ctx = f(inputs["ctx"])
    in_maps = []
    for i in range(N_CORES):
        m = dict(shared)
        m["x"] = x[i * NB:(i + 1) * NB]; m["c"] = c[i * NB:(i + 1) * NB]; m["ctx"] = ctx[i * NB:(i + 1) * NB]
        in_maps.append(m)
    res = run_bass_kernel_spmd(nc, in_maps, core_ids=list(range(N_CORES)))
    return np.concatenate([r["out"] for r in res.results], axis=0).astype(np.float32)
```

```python
from contextlib import ExitStack
import os
import numpy as np
import concourse.bass as bass
import concourse.mybir as mybir
from concourse.bass_utils import run_bass_kernel_spmd

F32 = mybir.dt.float32
BF16 = mybir.dt.bfloat16
U8 = mybir.dt.uint8
AF = mybir.ActivationFunctionType
ALU = mybir.AluOpType
AX = mybir.AxisListType

N_CORES = 8
KLAT = int(os.environ.get('KLAT', '1'))
KNOQK = int(os.environ.get('KNOQK', '0'))
SAME_ENG_SYNC = int(os.environ.get('KSES', '1'))
EPS = 1e-6


class Buf:
    __slots__ = ("name", "last_w", "readers", "excl")

    def __init__(self, name, excl=False):
        self.name = name
        self.excl = excl
        self.last_w = None
        self.readers = []


class Prog:
    ENG = ("pe", "act", "dve", "pool", "sp")

    def __init__(self, nc):
        self.nc = nc
        self.es = ExitStack()
        self.ops = {e: [] for e in self.ENG}
        self.count = {e: 0 for e in self.ENG}
        self.sems = {}
        for e in self.ENG:
            self.sems[e] = self.es.enter_context(nc.semaphore("s_" + e))
        self.waited = {e: {} for e in self.ENG}
        self.dma_sem_cnt = {}
        self.nbuf = 0
        self.capture = None

    def sbuf(self, name, shape, dtype=F32):
        return self.es.enter_context(self.nc.sbuf_tensor(name, list(shape), dtype))

    def psum(self, name, shape, dtype=F32):
        return self.es.enter_context(self.nc.psum_tensor(name, list(shape), dtype))

    def buf(self, name=None, excl=False):
        self.nbuf += 1
        return Buf(name or f"b{self.nbuf}", excl)

    def dma_sem(self, name):
        key = "d_" + name
        self.sems[key] = self.es.enter_context(self.nc.semaphore(key))
        self.dma_sem_cnt[key] = 0
        return key

    def _deps(self, reads, writes):
        toks = []
        for b in reads:
            if b.last_w is not None:
                toks.append(b.last_w)
        for b in writes:
            if b.last_w is not None:
                toks.append(b.last_w)
            toks.extend(b.readers)
        return toks

    def _emit_waits(self, eng, toks):
        need = {}
        for (k, v) in toks:
            if k == eng and (eng in ("pe", "sp") or not SAME_ENG_SYNC):
                continue
            if v > need.get(k, 0):
                need[k] = v
        for k, v in need.items():
            if self.waited[eng].get(k, 0) >= v:
                continue
            self.waited[eng][k] = v
            self.ops[eng].append(("wait", self.sems[k], v))

    def _commit(self, tok, reads, writes):
        for b in writes:
            b.last_w = tok
            b.readers = []
        for b in reads:
            b.readers.append(tok)

    def op(self, eng, fn, reads=(), writes=()):
        if self.capture is not None:
            self.capture.append(("op", eng, fn, list(reads), list(writes)))
            return None
        writes = [b for b in writes if b is not None] + [b for b in reads if b is not None and b.excl]
        reads = [b for b in reads if b is not None and not b.excl]
        self._emit_waits(eng, self._deps(reads, writes))
        self.count[eng] += 1
        tok = (eng, self.count[eng])
        self.ops[eng].append(("op", fn, self.sems[eng], 1))
        self._commit(tok, reads, writes)
        return tok

    def dma(self, queue, semkey, out_ap, in_ap, reads=(), writes=()):
        if self.capture is not None:
            self.capture.append(("dma", queue, semkey, out_ap, in_ap, list(reads), list(writes)))
            return None
        reads = [b for b in reads if b is not None]
        writes = [b for b in writes if b is not None]
        self._emit_waits(queue, self._deps(reads, writes))
        if self.dma_sem_cnt[semkey] > 0:
            self._emit_waits(queue, [(semkey, self.dma_sem_cnt[semkey])])
        self.dma_sem_cnt[semkey] += 16
        tok = (semkey, self.dma_sem_cnt[semkey])

        def fn(e, out_ap=out_ap, in_ap=in_ap):
            return e.dma_start(out=out_ap, in_=in_ap)
        self.ops[queue].append(("op", fn, self.sems[semkey], 16))
        self._commit(tok, reads, writes)
        return tok

    def replay_merged(self, A, B):
        la, lb = len(A), len(B)
        ia = ib = 0
        while ia < la or ib < lb:
            if ib >= lb or (ia < la and ia * lb <= ib * la):
                it = A[ia]; ia += 1
            else:
                it = B[ib]; ib += 1
            if it[0] == "op":
                self.op(it[1], it[2], it[3], it[4])
            else:
                self.dma(it[1], it[2], it[3], it[4], it[5], it[6])

    def barrier(self):
        toks = [(e, self.count[e]) for e in self.ENG if e != "sp" and self.count[e] > 0]
        toks += [(k, v) for k, v in self.dma_sem_cnt.items() if v > 0]
        for e in self.ENG:
            self._emit_waits(e, toks)

    def emit(self):
        nc = self.nc
        P = self
        with nc.Block() as block:
            def run(e, engine):
                for item in P.ops[e]:
                    if item[0] == "wait":
                        engine.wait_ge(item[1], item[2])
                    else:
                        _, fn, sem, inc = item
                        fn(engine).then_inc(sem, inc)

            @block.sync
            def _(eng):
                run("sp", eng)

            @block.tensor
            def _(eng):
                run("pe", eng)

            @block.scalar
            def _(eng):
                run("act", eng)

            @block.vector
            def _(eng):
                run("dve", eng)

            @block.gpsimd
            def _(eng):
                run("pool", eng)

    def close(self):
        self.es.close()


def I(name, *a, **kw):
    return lambda e: getattr(e, name)(*a, **kw)


def build(NB=2, dbg=False, stop=99, KSTEPS=36, KSUB=99):
    nc = bass.Bass("TRN2", target_bir_lowering=False)

    def din(name, shape):
        return nc.dram_tensor(name, list(shape), F32, kind="ExternalInput").ap()
    x_d = din("x", [NB, 2048, 1024]); ctx_d = din("ctx", [NB, 256, 1024]); c_d = din("c", [NB, 1024])
    cctx_d = din("c_ctx", [1, 1024]); wada_d = din("w_ada", [1024, 6144]); bada_d = din("b_ada", [1, 6144])
    n1g_d = din("norm1_g", [1, 1024]); win_d = din("w_in", [1024, 3088]); lng_d = din("ln_a_g", [1, 512])
    lnb_d = din("ln_a_b", [1, 512]); wsp_d = din("w_spatial", [4, 128, 128]); bsp_d = din("b_spatial", [4, 128])
    conv_d = din("conv_qkv", [5, 1536]); alog_d = din("a_log", [1, 8]); dtb_d = din("dt_bias", [1, 8])
    ong_d = din("onorm_g", [1, 128]); wout_d = din("w_out", [1024, 1024]); n2g_d = din("norm2_g", [1, 1024])
    wgrp_d = din("w_group", [1024, 4]); bgrp_d = din("b_group", [1, 4]); wrt_d = din("w_router", [1024, 16])
    brt_d = din("b_router", [1, 16]); wgu_d = din("w_gate_up", [16, 1024, 1024]); wdn_d = din("w_down", [16, 512, 1024])
    fg_d = din("final_g", [1, 1024])
    out_d = nc.dram_tensor("out", [NB, 2048, 1024], F32, kind="ExternalOutput").ap()
    dbg_d = nc.dram_tensor("dbg", [128, 16, 1024], F32, kind="ExternalOutput").ap() if dbg else None

    P = Prog(nc)
    ARENA = 192 * 1024
    arena = P.sbuf("arena", [128, ARENA], U8)
    pers = P.sbuf("pers", [128, 15 * 1024], U8)
    banks = [P.psum(f"bank{i}", [128, 512]) for i in range(8)]

    def V(base, off, shape, dt, parts=128):
        esz = 2 if dt == BF16 else 4
        n = 1
        for s in shape:
            n *= s
        ap = base[0:parts, off:off + n * esz].bitcast(dt)
        if len(shape) == 2:
            ap = ap.rearrange("p (a b) -> p a b", a=shape[0])
        elif len(shape) == 3:
            ap = ap.rearrange("p (a b c) -> p a b c", a=shape[0], b=shape[1])
        return ap

    class Alloc:
        def __init__(self, base, size):
            self.base, self.size, self.off = base, size, 0

        def __call__(self, shape, dt, parts=128):
            esz = 2 if dt == BF16 else 4
            n = esz
            for s in shape:
                n *= s
            n = (n + 31) // 32 * 32
            off = self.off
            self.off += n
            assert self.off <= self.size, (self.off, self.size)
            return V(self.base, off, shape, dt, parts)

    PA = Alloc(pers, 15 * 1024)
    KB = 1024
    sem_ld = P.dma_sem("ld")
    ident = PA([128], F32); ident_bf = PA([128], BF16); ones_bf = PA([128], BF16)
    m_ui = PA([128], F32); m_us = PA([128], F32); m_li = PA([128], F32); m_ls = PA([128], F32); m_one = PA([128], F32)
    m_ubd = V(arena, 150 * 1024, [128], F32); m_ux = V(arena, 150 * 1024 + 512, [128], F32); m_lbd = V(arena, 150 * 1024 + 1024, [128], F32); m_lx = V(arena, 150 * 1024 + 1536, [128], F32)
    mb_ubd = PA([128], BF16); mb_ux = PA([128], BF16); mb_lbd = PA([128], BF16); mb_lx = PA([128], BF16)
    b_const = P.buf("const")

    def pool_op(fn, reads=(), writes=()):
        return P.op("pool", fn, reads, writes)

    def mk_mask(ap, pattern_step, chmul, cmp):
        pool_op(lambda e: e.memset(ap, 1.0), writes=[b_const])
        pool_op(lambda e: e.affine_select(out=ap, in_=ap, pattern=[[pattern_step, 128]], compare_op=cmp, fill=0.0,
                                          base=0, channel_multiplier=chmul), reads=[b_const], writes=[b_const])
    pool_op(lambda e: e.memset(ident, 0.0), writes=[b_const])
    pool_op(lambda e: e.affine_select(out=ident, in_=ident, pattern=[[-1, 128]], compare_op=ALU.not_equal, fill=1.0,
                                      base=0, channel_multiplier=1), reads=[b_const], writes=[b_const])
    pool_op(lambda e: e.tensor_copy(out=ident_bf, in_=ident), reads=[b_const], writes=[b_const])
    pool_op(lambda e: e.memset(ones_bf, 1.0), writes=[b_const])
    pool_op(lambda e: e.memset(m_one, 1.0), writes=[b_const])
    mk_mask(m_ui, 1, -1, ALU.is_ge)
    mk_mask(m_us, 1, -1, ALU.is_gt)
    mk_mask(m_li, -1, 1, ALU.is_ge)
    mk_mask(m_ls, -1, 1, ALU.is_gt)
    pool_op(lambda e: e.tensor_copy(out=m_ubd, in_=m_us), reads=[b_const], writes=[b_const])
    pool_op(lambda e: e.memset(m_ubd[0:64, 64:128], 0.0), reads=[b_const], writes=[b_const])
    pool_op(lambda e: e.memset(m_ux, 0.0), writes=[b_const])
    pool_op(lambda e: e.memset(m_ux[0:64, 64:128], 1.0), reads=[b_const], writes=[b_const])
    pool_op(lambda e: e.tensor_copy(out=m_lbd, in_=m_ls), reads=[b_const], writes=[b_const])
    pool_op(lambda e: e.memset(m_lbd[64:128, 0:64], 0.0), reads=[b_const], writes=[b_const])
    pool_op(lambda e: e.memset(m_lx, 0.0), writes=[b_const])
    pool_op(lambda e: e.memset(m_lx[64:128, 0:64], 1.0), reads=[b_const], writes=[b_const])

    mb_ui = PA([128], BF16); mb_us = PA([128], BF16); mb_li = PA([128], BF16); mb_ls = PA([128], BF16)
    for dst_, src_ in ((mb_ui, m_ui), (mb_us, m_us), (mb_li, m_li), (mb_ls, m_ls), (mb_ubd, m_ubd), (mb_ux, m_ux), (mb_lbd, m_lbd), (mb_lx, m_lx)):
        pool_op(lambda e, dst_=dst_, src_=src_: e.tensor_copy(out=dst_, in_=src_), reads=[b_const], writes=[b_const])
    eps_t = PA([1], F32)
    pool_op(lambda e: e.memset(eps_t, EPS), writes=[b_const])
    negA = PA([8], F32); dtb_bc = PA([8], F32); onorm_bc = PA([128], F32)
    lng_bc = PA([512], F32); lnb_bc = PA([512], F32)
    wsT = PA([4, 128], BF16); bsT = PA([4], F32); convw = PA([12, 5], F32)
    Wr32 = PA([8, 20], F32); brt_bc = PA([20], F32)
    modT = PA([48, 4], F32)
    b_par = P.buf("params")

    def load(dst, src, reads=(), writes=(), q="sp"):
        return P.dma(q, sem_ld, dst, src, reads=reads, writes=list(writes))

    load(negA, alog_d.partition_broadcast(128), writes=[b_par])
    load(dtb_bc, dtb_d.partition_broadcast(128), writes=[b_par])
    load(onorm_bc, ong_d.partition_broadcast(128), writes=[b_par])
    load(lng_bc, lng_d.partition_broadcast(128), writes=[b_par])
    load(lnb_bc, lnb_d.partition_broadcast(128), writes=[b_par])
    load(brt_bc[:, 0:4], bgrp_d.partition_broadcast(128), writes=[b_par])
    load(brt_bc[:, 4:20], brt_d.partition_broadcast(128), writes=[b_par])
    load(Wr32[:, :, 0:4], wgrp_d.rearrange("(kc p) n -> p kc n", p=128), writes=[b_par])
    load(Wr32[:, :, 4:20], wrt_d.rearrange("(kc p) n -> p kc n", p=128), writes=[b_par])
    P.op("act", lambda e: e.activation(out=negA, in_=negA, func=AF.Exp), reads=[b_par], writes=[b_par])
    P.op("dve", lambda e: e.tensor_scalar(out=negA, in0=negA, scalar1=-1.0, scalar2=None, op0=ALU.mult), reads=[b_par], writes=[b_par])

    A0 = Alloc(arena, ARENA)
    b_tmp = P.buf("setup_tmp")
    b_ps = [P.buf(f"bank{i}", excl=True) for i in range(8)]
    wsp_sb = A0([4, 128], F32); bsp_sb = A0([128], F32, parts=4); conv_sb = A0([1536], F32, parts=5)
    load(wsp_sb, wsp_d.rearrange("h i j -> i h j"), writes=[b_tmp])
    load(bsp_sb, bsp_d, writes=[b_tmp])
    load(conv_sb, conv_d, writes=[b_tmp])
    for h in range(4):
        P.op("pe", lambda e, h=h: e.transpose(out=banks[0][:, h * 128:(h + 1) * 128], in_=wsp_sb[:, h, :], identity=ident),
             reads=[b_tmp, b_const], writes=[b_ps[0]])
    P.op("act", lambda e: e.activation(out=wsT, in_=banks[0][:, 0:512].rearrange("p (a b) -> p a b", a=4), func=AF.Copy),
         reads=[b_ps[0]], writes=[b_par])
    P.op("pe", lambda e: e.transpose(out=banks[1][:, 0:4], in_=bsp_sb, identity=ident[0:4, 0:4]), reads=[b_tmp, b_const], writes=[b_ps[1]])
    P.op("act", lambda e: e.activation(out=bsT, in_=banks[1][:, 0:4], func=AF.Copy), reads=[b_ps[1]], writes=[b_par])
    for cc in range(12):
        P.op("pe", lambda e, cc=cc: e.transpose(out=banks[2][:, cc * 8:cc * 8 + 5], in_=conv_sb[:, cc * 128:(cc + 1) * 128],
                                                identity=ident[0:5, 0:5]), reads=[b_tmp, b_const], writes=[b_ps[2]])
    P.op("act", lambda e: e.activation(out=convw, in_=banks[2][:, 0:96].rearrange("p (a b) -> p a b", a=12)[:, :, 0:5], func=AF.Copy),
         reads=[b_ps[2]], writes=[b_par])

    cT = A0([3, 8], F32); cTb = A0([3, 8], BF16)
    for j in range(NB):
        load(cT[:, j, :], c_d[j].rearrange("(p kc) -> p kc", kc=8), writes=[b_tmp])
    if NB < 2:
        P.op("dve", lambda e: e.memset(cT[:, 1, :], 0.0), writes=[b_tmp])
    load(cT[:, 2, :], cctx_d[0].rearrange("(p kc) -> p kc", kc=8), writes=[b_tmp])
    P.op("act", lambda e: e.activation(out=cTb, in_=cT, func=AF.Silu), reads=[b_tmp], writes=[b_tmp])
    wada_v = wada_d.rearrange("(p kc) n -> p kc n", kc=8)
    wa = [A0([8, 512], BF16) for _ in range(2)]
    b_wa = [P.buf() for _ in range(2)]
    sem_wa = [P.dma_sem(f"wa{i}") for i in range(2)]
    for nb_ in range(12):
        s = nb_ % 2
        P.dma("pool", sem_wa[s], wa[s], wada_v[:, :, nb_ * 512:(nb_ + 1) * 512], writes=[b_wa[s]])
        for c4 in range(4):
            ch = nb_ * 4 + c4
            for kc in range(8):
                P.op("pe", lambda e, s=s, c4=c4, kc=kc, ch=ch: e.matmul(banks[3][:, ch * 4:ch * 4 + 3], wa[s][:, kc, c4 * 128:(c4 + 1) * 128],
                                                                      cTb[:, :, kc], start=(kc == 0), stop=(kc == 7)),
                     reads=[b_wa[s], b_tmp], writes=[b_ps[3]])
    P.op("act", lambda e: e.activation(out=modT[:, :, 0:3], in_=banks[3][:, 0:192].rearrange("p (a b) -> p a b", a=48)[:, :, 0:3], func=AF.Copy),
         reads=[b_ps[3]], writes=[b_par])
    P.barrier()

    def make_bc(dst, j, which, b_dst, tmp_bias, b_tmpb, g_bc=None, b_g=None):
        load(tmp_bias, bada_d[:, which * 1024:(which + 1) * 1024].partition_broadcast(128), writes=[b_tmpb])
        for c8 in range(8):
            ch = which * 8 + c8
            bk = 6 + c8 // 4
            P.op("pe", lambda e, c8=c8, ch=ch, bk=bk: e.matmul(banks[bk][:, (c8 % 4) * 128:(c8 % 4 + 1) * 128],
                                                               modT[:, ch, j:j + 1].to_broadcast([128, 128]), ident, start=True, stop=True),
                 reads=[b_par, b_const], writes=[b_ps[bk]])
        for hf in range(2):
            P.op("dve", lambda e, hf=hf: e.tensor_tensor(out=dst[:, hf * 512:(hf + 1) * 512], in0=banks[6 + hf][:, :],
                                                        in1=tmp_bias[:, hf * 512:(hf + 1) * 512], op=ALU.add),
                 reads=[b_ps[6 + hf], b_tmpb], writes=[b_dst])
        if g_bc is not None:
            P.op("dve", lambda e: e.scalar_tensor_tensor(out=dst, in0=dst, scalar=1.0, in1=g_bc, op0=ALU.add, op1=ALU.mult),
                 reads=[b_dst, b_g], writes=[b_dst])

    def rstd_from_ss(ss, n, b_s):
        P.op("dve", lambda e: e.tensor_scalar(out=ss, in0=ss, scalar1=1.0 / n, scalar2=EPS, op0=ALU.mult, op1=ALU.add), reads=[b_s], writes=[b_s])
        P.op("act", lambda e: e.activation(out=ss, in_=ss, func=AF.Sqrt), reads=[b_s], writes=[b_s])
        P.op("dve", lambda e: e.reciprocal(out=ss, in_=ss), reads=[b_s], writes=[b_s])

    sem_x = [P.dma_sem(f"x{i}") for i in range(2)]
    sem_w = [P.dma_sem(f"w{i}") for i in range(4)]
    sem_o = [P.dma_sem(f"o{i}") for i in range(2)]
    out_toks = []

    def do_batch(b):
        A = Alloc(arena, ARENA)
        RA = 0
        x_res = V(arena, RA, [16, 1024], F32)
        b_xres = [P.buf(f"xres{t}") for t in range(16)]
        xT = V(arena, RA, [8, 2304], BF16)
        b_xT = [P.buf(f"xT{t}") for t in range(18)]
        CT = RA + 36 * KB
        RB = 64 * KB
        qkvT = V(arena, RB, [12, 2304], BF16)
        b_qkv = [[P.buf() for _ in range(18)] for _ in range(12)]
        RC = RB + 54 * KB
        o_acc = V(arena, RC, [16, 512], F32)
        b_oacc = [[P.buf() for _ in range(4)] for _ in range(16)]
        Wqkv = V(arena, RC, [8, 1536], BF16)
        b_wqkv = P.buf()
        RD = RC + 32 * KB
        AD = Alloc(arena[:, RD:ARENA], ARENA - RD)
        gb = AD([18, 16], F32); cumE = AD([18, 40], F32); negE = AD([18, 40], F32)
        b_gb = [P.buf() for _ in range(18)]
        bcA = AD([1024], F32); bcS = AD([1024], F32); bcG = AD([1024], F32)
        b_bcA, b_bcS, b_bcG = P.buf(), P.buf(), P.buf()
        Wab = AD([8, 16], BF16); b_wab = P.buf()
        xin = [AD([1024], F32) for _ in range(2)]; b_xin = [P.buf() for _ in range(2)]
        xmb = AD([1024], BF16); b_xmb = P.buf()
        small = AD([16], F32); b_small0 = P.buf()
        g1_bc = xin[0]
        load(g1_bc, n1g_d.partition_broadcast(128), writes=[b_xin[0]])
        P.dma("pool", sem_w[0], Wab, win_d.rearrange("(kc p) n -> p kc n", p=128)[:, :, 3072:3088], writes=[b_wab])

        def norm_mod_T(src, b_src, A_bc, S_bc, dstT, b_dstT, bank, junk, b_junk, f32T=None, xm_f32=None, tmps=None):
            small_, b_small, junk_f32, b_jf = tmps if tmps is not None else (small, b_small0, junk_f320, b_jf0)
            ss = small_[:, 0:1]
            P.op("act", lambda e: e.activation(out=junk, in_=src, func=AF.Square, accum_out=ss), reads=[b_src], writes=[b_junk, b_small])
            rstd_from_ss(ss, 1024.0, b_small)
            if xm_f32 is None:
                tmp = junk_f32
                P.op("dve", lambda e: e.scalar_tensor_tensor(out=tmp, in0=src, scalar=ss, in1=A_bc, op0=ALU.mult, op1=ALU.mult),
                     reads=[b_src, b_small, b_bcA], writes=[b_jf])
                P.op("dve", lambda e: e.tensor_tensor(out=junk, in0=tmp, in1=S_bc, op=ALU.add), reads=[b_jf, b_bcS], writes=[b_junk])
                pv = banks[bank][:, 0:512].bitcast(BF16)
                for kc in range(8):
                    P.op("pe", lambda e, kc=kc: e.transpose(out=pv[:, kc * 128:(kc + 1) * 128], in_=junk[:, kc * 128:(kc + 1) * 128], identity=ident_bf),
                         reads=[b_junk, b_const], writes=[b_ps[bank]])
                P.op("act", lambda e: e.activation(out=dstT, in_=pv.rearrange("p (a b) -> p a b", a=8), func=AF.Copy),
                     reads=[b_ps[bank]], writes=b_dstT)
            else:
                P.op("dve", lambda e: e.scalar_tensor_tensor(out=xm_f32, in0=src, scalar=ss, in1=A_bc, op0=ALU.mult, op1=ALU.mult),
                     reads=[b_src, b_small, b_bcA], writes=[b_jf])
                P.op("dve", lambda e: e.tensor_tensor(out=xm_f32, in0=xm_f32, in1=S_bc, op=ALU.add), reads=[b_jf, b_bcS], writes=[b_jf])
                for kc in range(8):
                    bk = bank + kc // 4
                    P.op("pe", lambda e, kc=kc, bk=bk: e.transpose(out=banks[bk][:, (kc % 4) * 128:(kc % 4 + 1) * 128],
                                                                   in_=xm_f32[:, kc * 128:(kc + 1) * 128], identity=ident),
                         reads=[b_jf, b_const], writes=[b_ps[bk]])
                for hf in range(2):
                    P.op("act", lambda e, hf=hf: e.activation(out=dstT[:, hf * 4:(hf + 1) * 4, :], in_=banks[bank + hf][:, :].rearrange("p (a b) -> p a b", a=4), func=AF.Copy),
                         reads=[b_ps[bank + hf]], writes=b_dstT)
                    P.op("dve", lambda e, hf=hf: e.tensor_copy(out=f32T[:, hf * 4:(hf + 1) * 4, :], in_=banks[bank + hf][:, :].rearrange("p (a b) -> p a b", a=4)),
                         reads=[b_ps[bank + hf]], writes=[b_f32T])

        junk_f320 = AD([1024], F32); b_jf0 = P.buf()
        junk_f32 = junk_f320; b_jf = b_jf0

        def p1_tile(t, ev):
            if t < 2:
                src_d = ctx_d[b, t * 128:(t + 1) * 128, :]
            else:
                src_d = x_d[b, (t - 2) * 128:(t - 1) * 128, :]
            xt = ev["xin"]; bk0, bk1, bk2 = ev["banks"]
            ghf_ = ev["ghf"]
            P.dma("sp", ev["sem"], xt, src_d, writes=[ev["b_xin"]])
            norm_mod_T(xt, ev["b_xin"], bcA, bcS, xT[:, :, t * 128:(t + 1) * 128], [b_xT[t]], bk0, ev["xmb"], ev["b_xmb"], tmps=ev["tmps"])
            for kc in range(8):
                P.op("pe", I("matmul", banks[bk1][:, 0:16], xT[:, kc, t * 128:(t + 1) * 128], Wab[:, kc, :], start=(kc == 0), stop=(kc == 7)),
                     reads=[b_xT[t], b_wab], writes=[b_ps[bk1]])
            g8 = gb[:, t, 0:8]
            P.op("dve", I("tensor_tensor", out=g8, in0=banks[bk1][:, 0:8], in1=dtb_bc, op=ALU.add), reads=[b_ps[bk1], b_par], writes=[b_gb[t]])
            P.op("act", I("activation", out=gb[:, t, 8:16], in_=banks[bk1][:, 8:16], func=AF.Sigmoid), reads=[b_ps[bk1]], writes=[b_gb[t]])
            P.op("act", I("activation", out=g8, in_=g8, func=AF.Exp), reads=[b_gb[t]], writes=[b_gb[t]])
            P.op("act", I("activation", out=g8, in_=g8, func=AF.Ln, bias=1.0), reads=[b_gb[t]], writes=[b_gb[t]])
            P.op("dve", I("tensor_tensor", out=g8, in0=g8, in1=negA, op=ALU.mult), reads=[b_gb[t], b_par], writes=[b_gb[t]])
            P.op("dve", I("tensor_copy", out=ghb[:, t, 0:8], in_=g8), reads=[b_gb[t]], writes=[b_gb[t]])
            P.op("dve", I("tensor_copy", out=ghf_, in_=ghb[:, t, 0:8]), reads=[b_gb[t]], writes=[b_gb[t]])
            P.op("dve", I("tensor_tensor", out=ghb[:, t, 8:16], in0=g8, in1=ghf_, op=ALU.subtract), reads=[b_gb[t]], writes=[b_gb[t]])
            for mi, mk in enumerate((m_ui, m_ls, m_li, m_us, m_one)):
                P.op("pe", I("matmul", banks[bk2][:, mi * 8:(mi + 1) * 8], mk, g8, start=True, stop=True), reads=[b_gb[t], b_const], writes=[b_ps[bk2]])
            P.op("act", I("activation", out=cumE[:, t, :], in_=banks[bk2][:, 0:40], func=AF.Exp), reads=[b_ps[bk2]], writes=[b_gb[t]])
            P.op("dve", I("tensor_scalar", out=negE[:, t, :], in0=cumE[:, t, :], scalar1=-1.0, scalar2=None, op0=ALU.mult), reads=[b_gb[t]], writes=[b_gb[t]])

        def phase1_tiles(tiles, j):
            make_bc(bcA, j, 1, b_bcA, xin[1], b_xin[1], g_bc=g1_bc, b_g=b_xin[0])
            make_bc(bcS, j, 0, b_bcS, xin[1], b_xin[1])
            for i in range(0, len(tiles), 2):
                P.capture = []
                p1_tile(tiles[i], envs1[0])
                A_ = P.capture
                P.capture = []
                p1_tile(tiles[i + 1], envs1[1])
                B_ = P.capture
                P.capture = None
                P.replay_merged(A_, B_)

        if stop <= 0:
            return
        _x2 = AD([1024], F32); _bx2 = P.buf()
        xin2 = [_x2, _x2]; b_xin2 = [_bx2, _bx2]
        ghb = AD([18, 16], BF16); ghf = AD([8], F32)
        E1 = Alloc(arena[:, RB:RB + 16 * KB], 16 * KB)
        envs1 = [
            {"xin": xin2[0], "b_xin": b_xin2[0], "sem": sem_x[0], "xmb": xmb, "b_xmb": b_xmb, "tmps": None, "ghf": ghf, "banks": (0, 1, 2)},
            {"xin": E1([1024], F32), "b_xin": P.buf(), "sem": sem_x[1], "xmb": E1([1024], BF16), "b_xmb": P.buf(),
             "tmps": (E1([16], F32), P.buf(), E1([1024], F32), P.buf()), "ghf": E1([8], F32), "banks": (3, 4, 5)},
        ]
        phase1_tiles([0, 1], 2)
        phase1_tiles(list(range(2, 18)), b)

        if stop <= 1:
            return
        P.dma("pool", sem_w[1], Wqkv, win_d.rearrange("(kc p) n -> p kc n", p=128)[:, :, 1024:2560], writes=[b_wqkv])
        C5 = Alloc(arena[:, CT:CT + 28 * KB], 28 * KB)
        PTb = [C5([2320], BF16) for _ in range(2)]; b_PTb = [P.buf() for _ in range(2)]
        ACC = C5([2304], F32); b_ACC = P.buf()
        SQB = C5([2304], BF16); b_SQB = P.buf()
        RNb = [C5([512], F32) for _ in range(2)]; b_RNb = [P.buf() for _ in range(2)]
        DG = C5([5, 128], BF16); b_DG = P.buf()
        for i_ in range(2):
            P.op("pool", I("memset", PTb[i_], 0.0), writes=[b_PTb[i_]])
        blocks = [(0, 256)] + [(256 + i * 512, 512) for i in range(4)]
        for cc in range(12):
            pi_ = cc % 2
            PT_ = PTb[pi_]; bPT = b_PTb[pi_]
            for tp in range(5):
                P.op("dve", I("tensor_scalar", out=DG[:, tp, :], in0=ident_bf, scalar1=convw[:, cc, tp:tp + 1], scalar2=None, op0=ALU.mult),
                     reads=[b_par, b_const], writes=[b_DG])
            for bi, (t0, n) in enumerate(blocks):
                bk = bi % 2
                tl = list(range(t0 // 128, (t0 + n) // 128))
                for kc in range(8):
                    P.op("pe", I("matmul", banks[bk][:, 0:n], Wqkv[:, kc, cc * 128:(cc + 1) * 128], xT[:, kc, t0:t0 + n], start=(kc == 0), stop=(kc == 7)),
                         reads=[b_wqkv] + [b_xT[t] for t in tl], writes=[b_ps[bk]])
                po = (2 + t0) if t0 < 256 else (262 + t0 - 256)
                P.op("act", I("activation", out=PT_[:, po:po + n], in_=banks[bk][:, 0:n], func=AF.Copy), reads=[b_ps[bk]], writes=[bPT])
            allq = [b_qkv[cc][t] for t in range(18)]
            for bi, (t0, n) in enumerate(blocks):
                bk = 2 + bi % 2
                po = (2 + t0) if t0 < 256 else (262 + t0 - 256)
                for tp in range(5):
                    P.op("pe", I("matmul", banks[bk][:, 0:n], DG[:, tp, :], PT_[:, po - 2 + tp:po - 2 + tp + n], start=(tp == 0), stop=(tp == 4)),
                         reads=[b_DG, bPT], writes=[b_ps[bk]])
                if cc >= 8:
                    P.op("act", I("activation", out=qkvT[:, cc, t0:t0 + n], in_=banks[bk][:, 0:n], func=AF.Silu), reads=[b_ps[bk]], writes=allq)
                else:
                    P.op("act", I("activation", out=ACC[:, t0:t0 + n], in_=banks[bk][:, 0:n], func=AF.Silu), reads=[b_ps[bk]], writes=[b_ACC])
                    P.op("dve", I("tensor_tensor", out=SQB[:, t0:t0 + n], in0=ACC[:, t0:t0 + n], in1=ACC[:, t0:t0 + n], op=ALU.mult), reads=[b_ACC], writes=[b_SQB])
            if cc < 8:
                sc = (128.0 ** -0.5) if cc < 4 else 1.0
                for bi, (t0, n) in enumerate(blocks):
                    bk = 4 + bi % 2
                    ri = bi % 2
                    P.op("pe", I("matmul", banks[bk][:, 0:n], ones_bf, SQB[:, t0:t0 + n], start=True, stop=True), reads=[b_SQB, b_const], writes=[b_ps[bk]])
                    P.op("act", I("activation", out=RNb[ri][:, 0:n], in_=banks[bk][:, 0:n], func=AF.Sqrt, bias=eps_t), reads=[b_ps[bk], b_const], writes=[b_RNb[ri]])
                    P.op("dve", I("reciprocal", out=RNb[ri][:, 0:n], in_=RNb[ri][:, 0:n]), reads=[b_RNb[ri]], writes=[b_RNb[ri]])
                    P.op("dve", I("scalar_tensor_tensor", out=qkvT[:, cc, t0:t0 + n], in0=ACC[:, t0:t0 + n], scalar=sc, in1=RNb[ri][:, 0:n], op0=ALU.mult, op1=ALU.mult),
                         reads=[b_ACC, b_RNb[ri]], writes=allq)
        P.barrier()
        if stop <= 5:
            return
        SA = Alloc(arena[:, RA:RA + 64 * KB], 64 * KB)
        seqs = []
        for d in range(2):
            for h in range(4):
                q = {"d": d, "h": h}
                for nm in ("gmask", "decT", "dmbd", "dmx", "dmq", "vtok"):
                    q[nm] = SA([128], F32)
                for nm in ("Mbd", "Nbd", "XT", "QKd", "R0", "R1", "RT0", "RT1", "P0", "P1", "PT0", "PT1", "ZT", "AinvT", "kd", "r", "vnew", "Sbf"):
                    q[nm] = SA([128], BF16)
                q["S"] = SA([128], F32)
                q["b"] = {}
                seqs.append(q)

        def sb(q, nm):
            if nm not in q["b"]:
                q["b"][nm] = P.buf()
            return q["b"][nm]
        ring_i = {"prep": 0, "scan": 0}

        def pslot(kind="prep"):
            i = ring_i[kind]
            ring_i[kind] += 1
            if kind == "prep":
                i %= 20
                bk, c = i % 5, i // 5
            else:
                i %= 12
                bk, c = 5 + i % 3, i // 3
            return banks[bk][:, c * 128:(c + 1) * 128], b_ps[bk]

        for q in seqs:
            P.op("pool", I("memset", q["S"], 0.0), writes=[sb(q, "S")])
            P.op("pool", I("memset", q["Sbf"], 0.0), writes=[sb(q, "Sbf")])
        orders = [list(range(18)), [1, 0] + list(range(17, 1, -1))]
        o_written = [[False] * 4 for _ in range(16)]

        def mmq(out, lhsT, rhs, reads, bw):
            P.op("pe", lambda e: e.matmul(out, lhsT, rhs, start=True, stop=True), reads=reads, writes=[bw])

        def scan_step(step2, part):
            step = step2 // 2
            st = []
            for q in seqs:
                d, h = q["d"], q["h"]
                if d != step2 % 2:
                    continue
                t = orders[d][step]
                if KLAT == 0:
                    t = t % 2
                st.append((q, d, h, t, (t >= 2) and KNOQK == 0))
            tsl = lambda t: slice(t * 128, (t + 1) * 128)
            if part == "prep":
                for (q, d, h, t, lat) in st:
                    kT = qkvT[:, 4 + h, tsl(t)]; qT = qkvT[:, h, tsl(t)]; vT = qkvT[:, 8 + h, tsl(t)]
                    q["kT"], q["qT"] = kT, qT
                    q["bk"], q["bq"], q["bv"] = b_qkv[4 + h][t], b_qkv[h][t], b_qkv[8 + h][t]
                    col = d * 4 + h
                    q["beta"] = gb[:, t, 8 + col:9 + col]
                    q["gcol"] = gb[:, t, col:col + 1]
                    q["eg"] = cumE[:, t, (h if d == 0 else 20 + h):(h if d == 0 else 20 + h) + 1]
                    q["neg"] = negE[:, t, (h if d == 0 else 20 + h):(h if d == 0 else 20 + h) + 1]
                    q["ekd"] = cumE[:, t, (8 + h if d == 0 else 28 + h):(8 + h if d == 0 else 28 + h) + 1]
                    q["gl"] = cumE[:, t, 32 + col:33 + col]
                    q["bg"] = b_gb[t]
                    ks, q["bks"] = pslot(); q["ks"] = ks.bitcast(BF16)[:, 0:128]
                    vs, q["bvs"] = pslot(); q["vs"] = vs.bitcast(BF16)[:, 0:128]
                    P.op("pe", I("transpose", out=q["ks"], in_=kT, identity=ident_bf), reads=[q["bk"], b_const], writes=[q["bks"]])
                    P.op("pe", I("transpose", out=q["vs"], in_=vT, identity=ident_bf), reads=[q["bv"], b_const], writes=[q["bvs"]])
                    ml = mb_ls if d == 0 else mb_us
                    gmv = q["gmask"].bitcast(BF16)
                    q["gmh"], q["gml"] = gmv[:, 0:128], gmv[:, 128:256]
                    P.op("dve", I("tensor_scalar", out=q["gmh"], in0=ml, scalar1=ghb[:, t, col:col + 1], scalar2=None, op0=ALU.mult),
                         reads=[q["bg"], b_const], writes=[sb(q, "gmask")])
                    P.op("dve", I("tensor_scalar", out=q["gml"], in0=ml, scalar1=ghb[:, t, 8 + col:9 + col], scalar2=None, op0=ALU.mult),
                         reads=[q["bg"], b_const], writes=[sb(q, "gmask")])
                if KSUB < 2:
                    return
                for (q, d, h, t, lat) in st:
                    P.op("act", I("activation", out=q["vtok"], in_=q["vs"], func=AF.Copy), reads=[q["bvs"]], writes=[sb(q, "vtok")])
                    P.op("act", I("activation", out=q["kd"], in_=q["ks"], func=AF.Copy, scale=q["ekd"]), reads=[q["bks"], q["bg"]], writes=[sb(q, "kd")])
                for (q, d, h, t, lat) in st:
                    q["G"], q["bG"] = pslot()
                    mmq(q["G"], q["kT"], q["kT"], [q["bk"]], q["bG"])
                    if lat:
                        q["QK"], q["bQK"] = pslot()
                        mmq(q["QK"], q["kT"], q["qT"], [q["bk"], q["bq"]], q["bQK"])
                for (q, d, h, t, lat) in st:
                    mr = mb_ui if d == 0 else mb_li
                    q["df"], q["bdf"] = pslot()
                    P.op("pe", I("matmul", q["df"], q["gmh"], mr, start=True, stop=False), reads=[sb(q, "gmask"), b_const], writes=[q["bdf"]])
                    P.op("pe", I("matmul", q["df"], q["gml"], mr, start=False, stop=True), reads=[sb(q, "gmask"), b_const], writes=[q["bdf"]])
                if KSUB < 3:
                    return
                for (q, d, h, t, lat) in st:
                    P.op("act", I("activation", out=q["decT"], in_=q["df"], func=AF.Exp), reads=[q["bdf"]], writes=[sb(q, "decT")])
                if KSUB < 5:
                    return
                for (q, d, h, t, lat) in st:
                    mbd, mx, mi = (mb_ubd, mb_ux, mb_ui) if d == 0 else (mb_lbd, mb_lx, mb_li)
                    t1 = q["dmbd"].bitcast(BF16)[:, 0:128]
                    P.op("dve", I("scalar_tensor_tensor", out=t1, in0=q["G"], scalar=q["beta"], in1=q["decT"], op0=ALU.mult, op1=ALU.mult),
                         reads=[q["bG"], q["bg"], sb(q, "decT")], writes=[sb(q, "dmbd")])
                    P.op("dve", I("tensor_tensor", out=q["Mbd"], in0=t1, in1=mbd, op=ALU.mult), reads=[sb(q, "dmbd"), b_const], writes=[sb(q, "Mbd")])
                    P.op("dve", I("tensor_tensor", out=q["XT"], in0=t1, in1=mx, op=ALU.mult), reads=[sb(q, "dmbd"), b_const], writes=[sb(q, "XT")])
                    if lat:
                        t2 = q["dmq"].bitcast(BF16)[:, 0:128]
                        P.op("dve", I("tensor_tensor", out=t2, in0=q["QK"], in1=q["decT"], op=ALU.mult), reads=[q["bQK"], sb(q, "decT")], writes=[sb(q, "dmq")])
                        P.op("dve", I("tensor_tensor", out=q["QKd"], in0=t2, in1=mi, op=ALU.mult), reads=[sb(q, "dmq"), b_const], writes=[sb(q, "QKd")])
                if KSUB < 6:
                    return
                for (q, d, h, t, lat) in st:
                    ns, q["bns"] = pslot(); q["ns"] = ns.bitcast(BF16)[:, 0:128]
                    P.op("pe", I("transpose", out=q["ns"], in_=q["Mbd"], identity=ident_bf), reads=[sb(q, "Mbd"), b_const], writes=[q["bns"]])
                    P.op("act", I("activation", out=q["Nbd"], in_=q["ns"], func=AF.Copy), reads=[q["bns"]], writes=[sb(q, "Nbd")])
                    P.op("dve", I("tensor_tensor", out=q["R0"], in0=ident_bf, in1=q["Mbd"], op=ALU.subtract), reads=[sb(q, "Mbd"), b_const], writes=[sb(q, "R0")])
                    q["cur"] = ("Mbd", "Nbd", "R0", "RT0")
                if KSUB < 7:
                    return
                for lvl in range(5):
                    pn, ptn = ("P0", "PT0") if lvl % 2 == 0 else ("P1", "PT1")
                    rn_ = "R1" if lvl % 2 == 0 else "R0"
                    last = lvl == 4
                    for (q, d, h, t, lat) in st:
                        pw, pwt, r_, _ = q["cur"]
                        if not last:
                            q["p2"], q["bp2"] = pslot()
                            mmq(q["p2"], q[pwt], q[pw], [sb(q, pw), sb(q, pwt)], q["bp2"])
                        q["p2t"], q["bp2t"] = pslot()
                        mmq(q["p2t"], q[pw], q[pwt], [sb(q, pw), sb(q, pwt)], q["bp2t"])
                    for (q, d, h, t, lat) in st:
                        if not last:
                            P.op("dve", I("tensor_copy", out=q[pn], in_=q["p2"]), reads=[q["bp2"]], writes=[sb(q, pn)])
                        P.op("act", I("activation", out=q[ptn], in_=q["p2t"], func=AF.Copy), reads=[q["bp2t"]], writes=[sb(q, ptn)])
                    for (q, d, h, t, lat) in st:
                        pw, pwt, r_, _ = q["cur"]
                        q["ra"], q["bra"] = pslot()
                        mmq(q["ra"], q[ptn], q[r_], [sb(q, r_), sb(q, ptn)], q["bra"])
                    for (q, d, h, t, lat) in st:
                        pw, pwt, r_, _ = q["cur"]
                        P.op("dve", I("tensor_tensor", out=q[rn_], in0=q["ra"], in1=q[r_], op=ALU.add),
                             reads=[q["bra"], sb(q, r_)], writes=[sb(q, rn_)])
                        q["cur"] = (pn, ptn, rn_, None)
                for (q, d, h, t, lat) in st:
                    _, _, r_, _ = q["cur"]
                    rts, q["brts"] = pslot(); q["rts"] = rts.bitcast(BF16)[:, 0:128]
                    P.op("pe", I("transpose", out=q["rts"], in_=q[r_], identity=ident_bf), reads=[sb(q, r_), b_const], writes=[q["brts"]])
                for (q, d, h, t, lat) in st:
                    _, _, r_, _ = q["cur"]
                    P.op("act", I("activation", out=q["RT0"], in_=q["rts"], func=AF.Copy), reads=[q["brts"]], writes=[sb(q, "RT0")])
                    q["cur"] = (None, None, r_, "RT0")
                if KSUB < 8:
                    return
                for (q, d, h, t, lat) in st:
                    _, _, r_, rt_ = q["cur"]
                    q["z"], q["bz"] = pslot()
                    mmq(q["z"], q["XT"], q[rt_], [sb(q, "XT"), sb(q, rt_)], q["bz"])
                for (q, d, h, t, lat) in st:
                    P.op("act", I("activation", out=q["ZT"], in_=q["z"], func=AF.Copy), reads=[q["bz"]], writes=[sb(q, "ZT")])
                for (q, d, h, t, lat) in st:
                    _, _, r_, rt_ = q["cur"]
                    q["w"], q["bw"] = pslot()
                    mmq(q["w"], q["ZT"], q[r_], [sb(q, "ZT"), sb(q, r_)], q["bw"])
                for (q, d, h, t, lat) in st:
                    _, _, r_, rt_ = q["cur"]
                    P.op("dve", I("scalar_tensor_tensor", out=q["AinvT"], in0=q["w"], scalar=-1.0, in1=q[r_], op0=ALU.mult, op1=ALU.add),
                         reads=[q["bw"], sb(q, r_)], writes=[sb(q, "AinvT")])
                if KSUB < 9:
                    return
                return
            for (q, d, h, t, lat) in st:
                q["a"], q["ba"] = pslot("scan")
                mmq(q["a"], q["kT"], q["Sbf"], [q["bk"], sb(q, "Sbf")], q["ba"])
            for (q, d, h, t, lat) in st:
                P.op("dve", I("scalar_tensor_tensor", out=q["r"], in0=q["a"], scalar=q["neg"], in1=q["vtok"], op0=ALU.mult, op1=ALU.add),
                     reads=[q["ba"], q["bg"], sb(q, "vtok")], writes=[sb(q, "r")])
            for (q, d, h, t, lat) in st:
                q["bb"], q["bbb"] = pslot("scan")
                mmq(q["bb"], q["AinvT"], q["r"], [sb(q, "AinvT"), sb(q, "r")], q["bbb"])
            for (q, d, h, t, lat) in st:
                P.op("act", I("activation", out=q["vnew"], in_=q["bb"], func=AF.Copy, scale=q["beta"]), reads=[q["bbb"], q["bg"]], writes=[sb(q, "vnew")])
            for (q, d, h, t, lat) in st:
                if lat:
                    q["o1"], q["bo1"] = pslot("scan")
                    mmq(q["o1"], q["qT"], q["Sbf"], [q["bq"], sb(q, "Sbf")], q["bo1"])
                    q["o2"], q["bo2"] = pslot("scan")
                    mmq(q["o2"], q["QKd"], q["vnew"], [sb(q, "QKd"), sb(q, "vnew")], q["bo2"])
                q["sp"], q["bsp"] = pslot("scan")
                mmq(q["sp"], q["kd"], q["vnew"], [sb(q, "kd"), sb(q, "vnew")], q["bsp"])
            for (q, d, h, t, lat) in st:
                if lat:
                    oa = o_acc[:, t - 2, h * 128:(h + 1) * 128]
                    bo = b_oacc[t - 2][h]
                    tmp = q["gmask"]
                    if not o_written[t - 2][h]:
                        P.op("act", I("activation", out=tmp, in_=q["o2"], func=AF.Copy), reads=[q["bo2"]], writes=[sb(q, "gmask")])
                        o_written[t - 2][h] = True
                    else:
                        P.op("dve", I("tensor_tensor", out=tmp, in0=q["o2"], in1=oa, op=ALU.add), reads=[q["bo2"], bo], writes=[sb(q, "gmask")])
                    P.op("dve", I("scalar_tensor_tensor", out=oa, in0=q["o1"], scalar=q["eg"], in1=tmp, op0=ALU.mult, op1=ALU.add),
                         reads=[q["bo1"], q["bg"], sb(q, "gmask")], writes=[bo])
                P.op("dve", I("scalar_tensor_tensor", out=q["S"], in0=q["S"], scalar=q["gl"], in1=q["sp"], op0=ALU.mult, op1=ALU.add),
                     reads=[sb(q, "S"), q["bg"], q["bsp"]], writes=[sb(q, "S")])
                P.op("act", I("activation", out=q["Sbf"], in_=q["S"], func=AF.Copy), reads=[sb(q, "S")], writes=[sb(q, "Sbf")])
        def cap2(step2, part):
            P.capture = []
            scan_step(step2, part)
            lst = P.capture
            P.capture = None
            return lst
        nst = min(36, KSTEPS)
        P.replay_merged(cap2(0, "prep"), [])
        for step2 in range(nst):
            nxt = cap2(step2 + 1, "prep") if step2 + 1 < nst else []
            P.replay_merged(nxt, cap2(step2, "scan"))
        P.barrier()

        if stop <= 6:
            return
        WB = Alloc(arena[:, RB:RB + 54 * KB], 54 * KB)
        WinA = WB([8, 1024], BF16); Wz = WB([8, 512], BF16); Wout = WB([8, 1024], BF16)
        b_w7 = P.buf()
        sets7 = []
        for i7 in range(2):
            d7 = {}
            if i7 == 0:
                d7["xTt"] = WB([8, 128], BF16); d7["u"] = WB([512], F32); d7["v"] = WB([512], F32); d7["vn"] = WB([512], BF16)
                d7["y"] = WB([1024], BF16); d7["yT"] = WB([8, 128], BF16); d7["sz"] = WB([512], F32); d7["st6"] = WB([8], F32); d7["ss4"] = WB([4], F32)
            else:
                d7["u"] = xin2[0][:, 0:512]; d7["v"] = xin2[0][:, 512:1024]
                d7["sz"] = xin[0][:, 0:512]
                d7["y"] = xin[0][:, 512:1024].bitcast(BF16)
                d7["xTt"] = V(arena, RD + 1152, [8, 128], BF16); d7["vn"] = V(arena, RD + 1152 + 2048, [512], BF16)
                d7["yT"] = V(arena, RD + 4224, [8, 128], BF16)
                d7["st6"] = WB([8], F32); d7["ss4"] = WB([4], F32)
            for nm in ("xTt", "u", "v", "vn", "y", "yT", "sz", "st"):
                d7["b_" + nm] = P.buf()
            sets7.append(d7)
        winv = win_d.rearrange("(kc p) n -> p kc n", p=128)
        P.dma("pool", sem_w[0], WinA, winv[:, :, 0:1024], writes=[b_w7])
        b_wz = P.buf()
        P.dma("pool", sem_w[1], Wz, winv[:, :, 2560:3072], writes=[b_wz])
        b_wo = P.buf()
        P.dma("pool", sem_w[2], Wout, wout_d.rearrange("(kc p) n -> p kc n", p=128), writes=[b_wo])
        make_bc(bcG, b, 2, b_bcG, xin[1], b_xin[1])
        for ch in range(8):
            P.op("pool", I("tensor_tensor", out=Wout[:, ch, :], in0=Wout[:, ch, :], in1=bcG, op=ALU.mult), reads=[b_wo, b_bcG], writes=[b_wo])
        def p7_a(tt, xTt, u_sb, v_sb, vn_bf, y_bf, yTt, sz, st6, ss4, b_xTt, b_u, b_v, b_vn, b_y, b_yT, b_sz, b_st):
            xt = x_res[:, tt, :]
            P.dma("sp", sem_x[tt % 2], xt, x_d[b, tt * 128:(tt + 1) * 128, :], writes=[b_xres[tt]])
            norm_mod_T(xt, b_xres[tt], bcA, bcS, xTt, [b_xTt], 0, xmb, b_xmb)
            for hf, bk in ((0, 1), (1, 2)):
                for kc in range(8):
                    P.op("pe", lambda e, kc=kc, hf=hf, bk=bk: e.matmul(banks[bk][:, :], xTt[:, kc, :], WinA[:, kc, hf * 512:(hf + 1) * 512],
                                                                     start=(kc == 0), stop=(kc == 7)), reads=[b_xTt, b_w7], writes=[b_ps[bk]])
            for kc in range(8):
                P.op("pe", lambda e, kc=kc: e.matmul(banks[3][:, :], xTt[:, kc, :], Wz[:, kc, :], start=(kc == 0), stop=(kc == 7)),
                     reads=[b_xTt, b_wz], writes=[b_ps[3]])
            P.op("act", lambda e: e.activation(out=u_sb, in_=banks[1][:, :], func=AF.Gelu_apprx_tanh), reads=[b_ps[1]], writes=[b_u])
            P.op("act", lambda e: e.activation(out=v_sb, in_=banks[2][:, :], func=AF.Gelu_apprx_tanh), reads=[b_ps[2]], writes=[b_v])
            P.op("act", lambda e: e.activation(out=sz, in_=banks[3][:, :], func=AF.Silu), reads=[b_ps[3]], writes=[b_sz])
            P.op("dve", lambda e: e.bn_stats(out=st6[:, 0:6], in_=v_sb), reads=[b_v], writes=[b_st])
            P.op("dve", lambda e: e.bn_aggr(out=st6[:, 6:8], in_=st6[:, 0:6]), reads=[b_st], writes=[b_st])
            P.op("dve", lambda e: e.tensor_scalar(out=st6[:, 7:8], in0=st6[:, 7:8], scalar1=EPS, scalar2=None, op0=ALU.add), reads=[b_st], writes=[b_st])
            P.op("act", lambda e: e.activation(out=st6[:, 7:8], in_=st6[:, 7:8], func=AF.Sqrt), reads=[b_st], writes=[b_st])
            P.op("dve", lambda e: e.reciprocal(out=st6[:, 7:8], in_=st6[:, 7:8]), reads=[b_st], writes=[b_st])
            P.op("dve", lambda e: e.tensor_scalar(out=v_sb, in0=v_sb, scalar1=st6[:, 6:7], scalar2=st6[:, 7:8], op0=ALU.subtract, op1=ALU.mult),
                 reads=[b_v, b_st], writes=[b_v])
            P.op("dve", lambda e: e.tensor_tensor(out=v_sb, in0=v_sb, in1=lng_bc, op=ALU.mult), reads=[b_v, b_par], writes=[b_v])
            P.op("dve", lambda e: e.tensor_tensor(out=vn_bf, in0=v_sb, in1=lnb_bc, op=ALU.add), reads=[b_v, b_par], writes=[b_vn])
        def p7_b(tt, xTt, u_sb, v_sb, vn_bf, y_bf, yTt, sz, st6, ss4, b_xTt, b_u, b_v, b_vn, b_y, b_yT, b_sz, b_st):
            xt = x_res[:, tt, :]
            for h in range(4):
                P.op("pe", lambda e, h=h: e.matmul(banks[4][:, h * 128:(h + 1) * 128], wsT[:, h, :], vn_bf[:, h * 128:(h + 1) * 128], start=True, stop=True),
                     reads=[b_vn, b_par], writes=[b_ps[4]])
            for h in range(4):
                hs = slice(h * 128, (h + 1) * 128)
                P.op("dve", lambda e, h=h, hs=hs: e.scalar_tensor_tensor(out=y_bf[:, hs], in0=banks[4][:, hs], scalar=bsT[:, h:h + 1], in1=u_sb[:, hs],
                                                                       op0=ALU.add, op1=ALU.mult), reads=[b_ps[4], b_par, b_u], writes=[b_y])
            for h in range(4):
                hs = slice(h * 128, (h + 1) * 128)
                P.op("act", lambda e, h=h, hs=hs, tt=tt: e.activation(out=u_sb[:, hs], in_=o_acc[:, tt, hs], func=AF.Square, accum_out=ss4[:, h:h + 1]),
                     reads=[b_oacc[tt][h], b_y], writes=[b_u, b_st])
            rstd_from_ss(ss4, 128.0, b_st)
            for h in range(4):
                hs = slice(h * 128, (h + 1) * 128)
                P.op("dve", lambda e, h=h, hs=hs, tt=tt: e.scalar_tensor_tensor(out=u_sb[:, hs], in0=o_acc[:, tt, hs], scalar=ss4[:, h:h + 1], in1=onorm_bc,
                                                                              op0=ALU.mult, op1=ALU.mult), reads=[b_oacc[tt][h], b_st, b_par, b_u], writes=[b_u])
            P.op("dve", lambda e: e.tensor_tensor(out=y_bf[:, 512:1024], in0=u_sb, in1=sz, op=ALU.mult), reads=[b_u, b_sz], writes=[b_y])
            pv = banks[5][:, 0:512].bitcast(BF16)
            for ch in range(8):
                P.op("pe", lambda e, ch=ch: e.transpose(out=pv[:, ch * 128:(ch + 1) * 128], in_=y_bf[:, ch * 128:(ch + 1) * 128], identity=ident_bf),
                     reads=[b_y, b_const], writes=[b_ps[5]])
            P.op("act", lambda e: e.activation(out=yTt, in_=pv.rearrange("p (a b) -> p a b", a=8), func=AF.Copy), reads=[b_ps[5]], writes=[b_yT])
            for hf in range(2):
                bk = 6 + hf
                for ch in range(8):
                    P.op("pe", lambda e, ch=ch, hf=hf, bk=bk: e.matmul(banks[bk][:, :], yTt[:, ch, :], Wout[:, ch, hf * 512:(hf + 1) * 512],
                                                                     start=(ch == 0), stop=(ch == 7)), reads=[b_yT, b_wo], writes=[b_ps[bk]])
            for hf in range(2):
                hs = slice(hf * 512, (hf + 1) * 512)
                P.op("dve", I("tensor_tensor", out=xt[:, hs], in0=banks[6 + hf][:, :], in1=xt[:, hs], op=ALU.add),
                     reads=[b_ps[6 + hf], b_xres[tt]], writes=[b_xres[tt]])
        def args7(tt):
            d7 = sets7[tt % 2]
            return (tt, d7["xTt"], d7["u"], d7["v"], d7["vn"], d7["y"], d7["yT"], d7["sz"], d7["st6"], d7["ss4"],
                    d7["b_xTt"], d7["b_u"], d7["b_v"], d7["b_vn"], d7["b_y"], d7["b_yT"], d7["b_sz"], d7["b_st"])
        def cap(fn, *a):
            P.capture = []
            fn(*a)
            lst = P.capture
            P.capture = None
            return lst
        P.replay_merged(cap(p7_a, *args7(0)), [])
        for tt in range(1, 16):
            P.replay_merged(cap(p7_a, *args7(tt)), cap(p7_b, *args7(tt - 1)))
        P.replay_merged([], cap(p7_b, *args7(15)))
        P.barrier()
        if dbg and b == 0:
            out_toks.append(P.dma("sp", sem_o[0], dbg_d, x_res, reads=b_xres))
            P.barrier()

        if stop <= 7:
            return
        MA = Alloc(arena[:, RB:ARENA], ARENA - RB)
        h2T = MA([8, 2048], BF16)
        b_h2T = [P.buf() for _ in range(16)]
        GU = [MA([8, 1024], BF16) for _ in range(2)]; DW = [MA([4, 1024], BF16) for _ in range(2)]
        b_GU = [P.buf() for _ in range(2)]; b_DW = [P.buf() for _ in range(2)]
        act_t = [MA([4, 512], BF16) for _ in range(2)]; b_act = [P.buf() for _ in range(2)]
        sg = [MA([512], F32) for _ in range(2)]; b_sg = [P.buf() for _ in range(2)]
        h2f = MA([1024], F32); f32T = MA([8, 128], F32); b_f32T = P.buf()
        cb = MA([16, 16], F32); b_cb = [P.buf() for _ in range(16)]
        lg = MA([20], F32); rt = MA([32], F32); b_rt = P.buf()
        small2 = MA([16], F32); b_small2 = P.buf()
        bc2A = MA([1024], F32); bc2S = MA([1024], F32); bc2G = MA([1024], F32); tb_ = MA([1024], F32)
        b_2A, b_2S, b_2G, b_tb = P.buf(), P.buf(), P.buf(), P.buf()
        jb = MA([1024], BF16); b_jb = P.buf()
        load(tb_, n2g_d.partition_broadcast(128), writes=[b_tb])
        P.op("dve", lambda e: e.tensor_copy(out=h2f, in_=tb_), reads=[b_tb], writes=[b_jf])
        g2_bc = h2f
        make_bc(bc2A, b, 4, b_2A, tb_, b_tb, g_bc=g2_bc, b_g=b_jf)
        make_bc(bc2S, b, 3, b_2S, tb_, b_tb)
        make_bc(bc2G, b, 5, b_2G, tb_, b_tb)
        def route_tile(tt, ev):
            small2, b_small2, jb, b_jb, h2f, b_jf, f32T, b_f32T, lg, rt, b_rt, bkA, bkB, bkC = ev
            ss = small2[:, 0:1]
            src = x_res[:, tt, :]
            P.op("act", lambda e, src=src: e.activation(out=jb, in_=src, func=AF.Square, accum_out=ss), reads=[b_xres[tt]], writes=[b_jb, b_small2])
            rstd_from_ss(ss, 1024.0, b_small2)
            P.op("dve", lambda e, src=src: e.scalar_tensor_tensor(out=h2f, in0=src, scalar=ss, in1=bc2A, op0=ALU.mult, op1=ALU.mult),
                 reads=[b_xres[tt], b_small2, b_2A], writes=[b_jf])
            P.op("dve", lambda e: e.tensor_tensor(out=h2f, in0=h2f, in1=bc2S, op=ALU.add), reads=[b_jf, b_2S], writes=[b_jf])
            for kc in range(8):
                bk = (bkA, bkB)[kc // 4]
                P.op("pe", lambda e, kc=kc, bk=bk: e.transpose(out=banks[bk][:, (kc % 4) * 128:(kc % 4 + 1) * 128], in_=h2f[:, kc * 128:(kc + 1) * 128], identity=ident),
                     reads=[b_jf, b_const], writes=[b_ps[bk]])
            for hf in range(2):
                P.op("act", lambda e, hf=hf, tt=tt: e.activation(out=h2T[:, hf * 4:(hf + 1) * 4, tt * 128:(tt + 1) * 128],
                                                                 in_=banks[(bkA, bkB)[hf]][:, :].rearrange("p (a b) -> p a b", a=4), func=AF.Copy),
                     reads=[b_ps[(bkA, bkB)[hf]]], writes=[b_h2T[tt]])
                P.op("dve", lambda e, hf=hf: e.tensor_copy(out=f32T[:, hf * 4:(hf + 1) * 4, :], in_=banks[(bkA, bkB)[hf]][:, :].rearrange("p (a b) -> p a b", a=4)),
                     reads=[b_ps[(bkA, bkB)[hf]]], writes=[b_f32T])
            for kc in range(8):
                P.op("pe", lambda e, kc=kc: e.matmul(banks[bkC][:, 0:20], f32T[:, kc, :], Wr32[:, kc, :], start=(kc == 0), stop=(kc == 7)),
                     reads=[b_f32T, b_par], writes=[b_ps[bkC]])
            R_ = [b_rt]

            def dv(fn, extra_r=()):
                P.op("dve", fn, reads=R_ + list(extra_r), writes=R_)
            gmx, ngm, gsum, pg, m1, m2, dd, w1g, w2g = (rt[:, i:i + 1] for i in range(9))
            ohg = rt[:, 12:16]; es = rt[:, 16:20]; oh1 = rt[:, 20:24]; es2 = rt[:, 24:28]; oh2 = rt[:, 28:32]
            P.op("dve", lambda e: e.tensor_tensor(out=lg, in0=banks[bkC][:, 0:20], in1=brt_bc, op=ALU.add), reads=[b_ps[bkC], b_par], writes=R_)
            dv(lambda e: e.tensor_reduce(out=gmx, in_=lg[:, 0:4], axis=AX.X, op=ALU.max))
            dv(lambda e: e.tensor_scalar(out=ohg, in0=lg[:, 0:4], scalar1=gmx, scalar2=None, op0=ALU.is_equal))
            dv(lambda e: e.tensor_scalar(out=ngm, in0=gmx, scalar1=-1.0, scalar2=None, op0=ALU.mult))
            P.op("act", lambda e: e.activation(out=es2, in_=lg[:, 0:4], func=AF.Exp, bias=ngm, accum_out=gsum), reads=R_, writes=R_)
            dv(lambda e: e.reciprocal(out=pg, in_=gsum))
            dv(lambda e: e.tensor_scalar(out=es, in0=lg[:, 4:8], scalar1=ohg[:, 0:1], scalar2=None, op0=ALU.mult))
            for g in range(1, 4):
                dv(lambda e, g=g: e.scalar_tensor_tensor(out=es, in0=lg[:, 4 + 4 * g:8 + 4 * g], scalar=ohg[:, g:g + 1], in1=es, op0=ALU.mult, op1=ALU.add))
            dv(lambda e: e.tensor_reduce(out=m1, in_=es, axis=AX.X, op=ALU.max))
            dv(lambda e: e.tensor_scalar(out=oh1, in0=es, scalar1=m1, scalar2=None, op0=ALU.is_equal))
            dv(lambda e: e.scalar_tensor_tensor(out=es2, in0=oh1, scalar=-1e30, in1=es, op0=ALU.mult, op1=ALU.add))
            dv(lambda e: e.tensor_reduce(out=m2, in_=es2, axis=AX.X, op=ALU.max))
            dv(lambda e: e.tensor_scalar(out=oh2, in0=es2, scalar1=m2, scalar2=None, op0=ALU.is_equal))
            dv(lambda e: e.tensor_tensor(out=dd, in0=m1, in1=m2, op=ALU.subtract))
            P.op("act", lambda e: e.activation(out=dd, in_=dd, func=AF.Sigmoid), reads=R_, writes=R_)
            dv(lambda e: e.tensor_tensor(out=w1g, in0=dd, in1=pg, op=ALU.mult))
            dv(lambda e: e.tensor_tensor(out=w2g, in0=pg, in1=w1g, op=ALU.subtract))
            dv(lambda e: e.tensor_scalar(out=es, in0=oh1, scalar1=w1g, scalar2=None, op0=ALU.mult))
            dv(lambda e: e.scalar_tensor_tensor(out=es, in0=oh2, scalar=w2g, in1=es, op0=ALU.mult, op1=ALU.add))
            for g in range(4):
                P.op("dve", lambda e, g=g, tt=tt: e.tensor_scalar(out=cb[:, tt, 4 * g:4 * g + 4], in0=es, scalar1=ohg[:, g:g + 1], scalar2=None, op0=ALU.mult),
                     reads=R_, writes=[b_cb[tt]])
        f32T2 = MA([8, 128], F32); jb2 = MA([1024], BF16); lg2 = MA([20], F32); rt2 = MA([32], F32); small3 = MA([16], F32)
        ev_r = [(small2, b_small2, jb, b_jb, h2f, b_jf, f32T, b_f32T, lg, rt, b_rt, 0, 1, 2),
                (small3, P.buf(), jb2, P.buf(), tb_, b_tb, f32T2, P.buf(), lg2, rt2, P.buf(), 3, 4, 5)]
        for tt in range(0, 16, 2):
            P.capture = []
            route_tile(tt, ev_r[0])
            A_ = P.capture
            P.capture = []
            route_tile(tt + 1, ev_r[1])
            B_ = P.capture
            P.capture = None
            P.replay_merged(A_, B_)
        if stop <= 8:
            return
        def emit_GU(ex, s, tb4, a_s):
            for fc in range(4):
                gs = fc % 2
                bg_, bu_ = (0, 1) if gs == 0 else (2, 3)
                for kc in range(8):
                    P.op("pe", I("matmul", banks[bg_][:, :], GU[s][:, kc, fc * 128:(fc + 1) * 128], h2T[:, kc, tb4 * 512:(tb4 + 1) * 512], start=(kc == 0), stop=(kc == 7)),
                         reads=[b_GU[s]] + b_h2T[tb4 * 4:tb4 * 4 + 4], writes=[b_ps[bg_]])
                for kc in range(8):
                    P.op("pe", I("matmul", banks[bu_][:, :], GU[s][:, kc, 512 + fc * 128:512 + (fc + 1) * 128], h2T[:, kc, tb4 * 512:(tb4 + 1) * 512], start=(kc == 0), stop=(kc == 7)),
                         reads=[b_GU[s]] + b_h2T[tb4 * 4:tb4 * 4 + 4], writes=[b_ps[bu_]])
                P.op("act", I("activation", out=sg[gs], in_=banks[bg_][:, :], func=AF.Silu), reads=[b_ps[bg_]], writes=[b_sg[gs]])
                P.op("dve", I("tensor_tensor", out=act_t[a_s][:, fc, :], in0=sg[gs], in1=banks[bu_][:, :], op=ALU.mult),
                     reads=[b_sg[gs], b_ps[bu_]], writes=[b_act[a_s]])

        def emit_DOWN(ex, s, tb4, a_s):
            for t4 in range(4):
                tt = tb4 * 4 + t4
                ds = t4 % 2
                for hf in range(2):
                    bk = 4 + ds * 2 + hf
                    for fc in range(4):
                        P.op("pe", I("matmul", banks[bk][:, :], act_t[a_s][:, fc, t4 * 128:(t4 + 1) * 128], DW[s][:, fc, hf * 512:(hf + 1) * 512], start=(fc == 0), stop=(fc == 3)),
                             reads=[b_act[a_s], b_DW[s]], writes=[b_ps[bk]])
                for hf in range(2):
                    bk = 4 + ds * 2 + hf
                    hs = slice(hf * 512, (hf + 1) * 512)
                    P.op("dve", I("scalar_tensor_tensor", out=x_res[:, tt, hs], in0=banks[bk][:, :], scalar=cb[:, tt, ex:ex + 1], in1=x_res[:, tt, hs], op0=ALU.mult, op1=ALU.add),
                         reads=[b_ps[bk], b_cb[tt], b_xres[tt]], writes=[b_xres[tt]])

        pending = None
        gcount = 0
        for ex in range(16):
            s = ex % 2
            P.dma("pool", sem_w[s], GU[s], wgu_d[ex].rearrange("(kc p) n -> p kc n", p=128), writes=[b_GU[s]])
            P.dma("pool", sem_w[2 + s], DW[s], wdn_d[ex].rearrange("(fc p) n -> p fc n", p=128), writes=[b_DW[s]])
            for fc in range(4):
                P.op("pool", I("tensor_tensor", out=DW[s][:, fc, :], in0=DW[s][:, fc, :], in1=bc2G, op=ALU.mult),
                     reads=[b_DW[s], b_2G], writes=[b_DW[s]])
            for tb4 in range(4):
                a_s = gcount % 2
                emit_GU(ex, s, tb4, a_s)
                if pending is not None:
                    emit_DOWN(*pending)
                pending = (ex, s, tb4, a_s)
                gcount += 1
        emit_DOWN(*pending)
        if stop <= 9:
            return
        load(tb_, fg_d.partition_broadcast(128), writes=[b_tb])
        def fin_tile(tt, small_, b_small_, jb_, b_jb_, ot_, b_ot_, sem_):
            ss = small_[:, 0:1]
            src = x_res[:, tt, :]
            P.op("act", I("activation", out=jb_, in_=src, func=AF.Square, accum_out=ss), reads=[b_xres[tt]], writes=[b_jb_, b_small_])
            rstd_from_ss(ss, 1024.0, b_small_)
            P.op("dve", I("scalar_tensor_tensor", out=ot_, in0=src, scalar=ss, in1=tb_, op0=ALU.mult, op1=ALU.mult),
                 reads=[b_xres[tt], b_small_, b_tb], writes=[b_ot_])
            P.dma("sp", sem_, out_d[b, tt * 128:(tt + 1) * 128, :], ot_, reads=[b_ot_])
        fe = [(small2, b_small2, jb, b_jb, bc2A, b_2A, sem_o[0]), (small3, ev_r[1][1], jb2, ev_r[1][3], bc2S, b_2S, sem_o[1])]
        for tt in range(0, 16, 2):
            P.capture = []
            fin_tile(tt, *fe[0])
            A_ = P.capture
            P.capture = []
            fin_tile(tt + 1, *fe[1])
            B_ = P.capture
            P.capture = None
            P.replay_merged(A_, B_)
        P.barrier()

    for b in range(NB):
        do_batch(b)
        P.barrier()

    P._emit_waits("sp", out_toks + [(k, P.dma_sem_cnt[k]) for k in sem_o if P.dma_sem_cnt[k] > 0])
    P.emit()
    P.close()
    return nc


_NC_CACHE = {}


def kernel(**inputs):
    NB = 2
    if "nc" not in _NC_CACHE:
        _NC_CACHE["nc"] = build(NB)
    nc = _NC_CACHE["nc"]
    f = lambda a: np.ascontiguousarray(np.asarray(a, dtype=np.float32))
    shared = {
        "c_ctx": f(inputs["c_ctx"]).reshape(1, 1024), "w_ada": f(inputs["w_ada"])[0], "b_ada": f(inputs["b_ada"]).reshape(1, 6144),
        "norm1_g": f(inputs["norm1_g"]).reshape(1, 1024), "w_in": f(inputs["w_in"])[0], "ln_a_g": f(inputs["ln_a_g"]).reshape(1, 512),
        "ln_a_b": f(inputs["ln_a_b"]).reshape(1, 512), "w_spatial": f(inputs["w_spatial"])[0], "b_spatial": f(inputs["b_spatial"])[0],
        "conv_qkv": f(inputs["conv_qkv"])[0], "a_log": f(inputs["a_log"]).reshape(1, 8), "dt_bias": f(inputs["dt_bias"]).reshape(1, 8),
        "onorm_g": f(inputs["onorm_g"]).reshape(1, 128), "w_out": f(inputs["w_out"])[0], "norm2_g": f(inputs["norm2_g"]).reshape(1, 1024),
        "w_group": f(inputs["w_group"])[0], "b_group": f(inputs["b_group"]).reshape(1, 4), "w_router": f(inputs["w_router"])[0],
        "b_router": f(inputs["b_router"]).reshape(1, 16), "w_gate_up": f(inputs["w_gate_up"])[0], "w_down": f(inputs["w_down"])[0],
        "final_g": f(inputs["final_g"]).reshape(1, 1024),
    }
    x = f(inputs["x"]); c = f(inputs["c"]); ctx = f(inputs["ctx"])
    in_maps = []
    for i in range(N_CORES):
        m = dict(shared)
        m["x"] = x[i * NB:(i + 1) * NB]; m["c"] = c[i * NB:(i + 1) * NB]; m["ctx"] = ctx[i * NB:(i + 1) * NB]
        in_maps.append(m)
    res = run_bass_kernel_spmd(nc, in_maps, core_ids=list(range(N_CORES)))
    return np.concatenate([r["out"] for r in res.results], axis=0).astype(np.float32)
```

```python
from contextlib import ExitStack
import os
import numpy as np
import concourse.bass as bass
import concourse.mybir as mybir
from concourse.bass_utils import run_bass_kernel_spmd

F32 = mybir.dt.float32
BF16 = mybir.dt.bfloat16
U8 = mybir.dt.uint8
AF = mybir.ActivationFunctionType
ALU = mybir.AluOpType
AX = mybir.AxisListType

N_CORES = 8
KLAT = int(os.environ.get('KLAT', '1'))
KNOQK = int(os.environ.get('KNOQK', '0'))
SAME_ENG_SYNC = int(os.environ.get('KSES', '1'))
EPS = 1e-6


class Buf:
    __slots__ = ("name", "last_w", "readers", "excl")

    def __init__(self, name, excl=False):
        self.name = name
        self.excl = excl
        self.last_w = None
        self.readers = []


class Prog:
    ENG = ("pe", "act", "dve", "pool", "sp")

    def __init__(self, nc):
        self.nc = nc
        self.es = ExitStack()
        self.ops = {e: [] for e in self.ENG}
        self.count = {e: 0 for e in self.ENG}
        self.sems = {}
        for e in self.ENG:
            self.sems[e] = self.es.enter_context(nc.semaphore("s_" + e))
        self.waited = {e: {} for e in self.ENG}
        self.dma_sem_cnt = {}
        self.nbuf = 0
        self.capture = None

    def sbuf(self, name, shape, dtype=F32):
        return self.es.enter_context(self.nc.sbuf_tensor(name, list(shape), dtype))

    def psum(self, name, shape, dtype=F32):
        return self.es.enter_context(self.nc.psum_tensor(name, list(shape), dtype))

    def buf(self, name=None, excl=False):
        self.nbuf += 1
        return Buf(name or f"b{self.nbuf}", excl)

    def dma_sem(self, name):
        key = "d_" + name
        self.sems[key] = self.es.enter_context(self.nc.semaphore(key))
        self.dma_sem_cnt[key] = 0
        return key

    def _deps(self, reads, writes):
        toks = []
        for b in reads:
            if b.last_w is not None:
                toks.append(b.last_w)
        for b in writes:
            if b.last_w is not None:
                toks.append(b.last_w)
            toks.extend(b.readers)
        return toks

    def _emit_waits(self, eng, toks):
        need = {}
        for (k, v) in toks:
            if k == eng and (eng in ("pe", "sp") or not SAME_ENG_SYNC):
                continue
            if v > need.get(k, 0):
                need[k] = v
        for k, v in need.items():
            if self.waited[eng].get(k, 0) >= v:
                continue
            self.waited[eng][k] = v
            self.ops[eng].append(("wait", self.sems[k], v))

    def _commit(self, tok, reads, writes):
        for b in writes:
            b.last_w = tok
            b.readers = []
        for b in reads:
            b.readers.append(tok)

    def op(self, eng, fn, reads=(), writes=()):
        if self.capture is not None:
            self.capture.append(("op", eng, fn, list(reads), list(writes)))
            return None
        writes = [b for b in writes if b is not None] + [b for b in reads if b is not None and b.excl]
        reads = [b for b in reads if b is not None and not b.excl]
        self._emit_waits(eng, self._deps(reads, writes))
        self.count[eng] += 1
        tok = (eng, self.count[eng])
        self.ops[eng].append(("op", fn, self.sems[eng], 1))
        self._commit(tok, reads, writes)
        return tok

    def dma(self, queue, semkey, out_ap, in_ap, reads=(), writes=()):
        if self.capture is not None:
            self.capture.append(("dma", queue, semkey, out_ap, in_ap, list(reads), list(writes)))
            return None
        reads = [b for b in reads if b is not None]
        writes = [b for b in writes if b is not None]
        self._emit_waits(queue, self._deps(reads, writes))
        if self.dma_sem_cnt[semkey] > 0:
            self._emit_waits(queue, [(semkey, self.dma_sem_cnt[semkey])])
        self.dma_sem_cnt[semkey] += 16
        tok = (semkey, self.dma_sem_cnt[semkey])

        def fn(e, out_ap=out_ap, in_ap=in_ap):
            return e.dma_start(out=out_ap, in_=in_ap)
        self.ops[queue].append(("op", fn, self.sems[semkey], 16))
        self._commit(tok, reads, writes)
        return tok

    def replay_merged(self, A, B):
        la, lb = len(A), len(B)
        ia = ib = 0
        while ia < la or ib < lb:
            if ib >= lb or (ia < la and ia * lb <= ib * la):
                it = A[ia]; ia += 1
            else:
                it = B[ib]; ib += 1
            if it[0] == "op":
                self.op(it[1], it[2], it[3], it[4])
            else:
                self.dma(it[1], it[2], it[3], it[4], it[5], it[6])

    def barrier(self):
        toks = [(e, self.count[e]) for e in self.ENG if e != "sp" and self.count[e] > 0]
        toks += [(k, v) for k, v in self.dma_sem_cnt.items() if v > 0]
        for e in self.ENG:
            self._emit_waits(e, toks)

    def emit(self):
        nc = self.nc
        P = self
        with nc.Block() as block:
            def run(e, engine):
                for item in P.ops[e]:
                    if item[0] == "wait":
                        engine.wait_ge(item[1], item[2])
                    else:
                        _, fn, sem, inc = item
                        fn(engine).then_inc(sem, inc)

            @block.sync
            def _(eng):
                run("sp", eng)

            @block.tensor
            def _(eng):
                run("pe", eng)

            @block.scalar
            def _(eng):
                run("act", eng)

            @block.vector
            def _(eng):
                run("dve", eng)

            @block.gpsimd
            def _(eng):
                run("pool", eng)

    def close(self):
        self.es.close()


def I(name, *a, **kw):
    return lambda e: getattr(e, name)(*a, **kw)


def build(NB=2, dbg=False, stop=99, KSTEPS=36, KSUB=99):
    nc = bass.Bass("TRN2", target_bir_lowering=False)

    def din(name, shape):
        return nc.dram_tensor(name, list(shape), F32, kind="ExternalInput").ap()
    x_d = din("x", [NB, 2048, 1024]); ctx_d = din("ctx", [NB, 256, 1024]); c_d = din("c", [NB, 1024])
    cctx_d = din("c_ctx", [1, 1024]); wada_d = din("w_ada", [1024, 6144]); bada_d = din("b_ada", [1, 6144])
    n1g_d = din("norm1_g", [1, 1024]); win_d = din("w_in", [1024, 3088]); lng_d = din("ln_a_g", [1, 512])
    lnb_d = din("ln_a_b", [1, 512]); wsp_d = din("w_spatial", [4, 128, 128]); bsp_d = din("b_spatial", [4, 128])
    conv_d = din("conv_qkv", [5, 1536]); alog_d = din("a_log", [1, 8]); dtb_d = din("dt_bias", [1, 8])
    ong_d = din("onorm_g", [1, 128]); wout_d = din("w_out", [1024, 1024]); n2g_d = din("norm2_g", [1, 1024])
    wgrp_d = din("w_group", [1024, 4]); bgrp_d = din("b_group", [1, 4]); wrt_d = din("w_router", [1024, 16])
    brt_d = din("b_router", [1, 16]); wgu_d = din("w_gate_up", [16, 1024, 1024]); wdn_d = din("w_down", [16, 512, 1024])
    fg_d = din("final_g", [1, 1024])
    out_d = nc.dram_tensor("out", [NB, 2048, 1024], F32, kind="ExternalOutput").ap()
    dbg_d = nc.dram_tensor("dbg", [128, 16, 1024], F32, kind="ExternalOutput").ap() if dbg else None

    P = Prog(nc)
    ARENA = 192 * 1024
    arena = P.sbuf("arena", [128, ARENA], U8)
    pers = P.sbuf("pers", [128, 15 * 1024], U8)
    banks = [P.psum(f"bank{i}", [128, 512]) for i in range(8)]

    def V(base, off, shape, dt, parts=128):
        esz = 2 if dt == BF16 else 4
        n = 1
        for s in shape:
            n *= s
        ap = base[0:parts, off:off + n * esz].bitcast(dt)
        if len(shape) == 2:
            ap = ap.rearrange("p (a b) -> p a b", a=shape[0])
        elif len(shape) == 3:
            ap = ap.rearrange("p (a b c) -> p a b c", a=shape[0], b=shape[1])
        return ap

    class Alloc:
        def __init__(self, base, size):
            self.base, self.size, self.off = base, size, 0

        def __call__(self, shape, dt, parts=128):
            esz = 2 if dt == BF16 else 4
            n = esz
            for s in shape:
                n *= s
            n = (n + 31) // 32 * 32
            off = self.off
            self.off += n
            assert self.off <= self.size, (self.off, self.size)
            return V(self.base, off, shape, dt, parts)

    PA = Alloc(pers, 15 * 1024)
    KB = 1024
    sem_ld = P.dma_sem("ld")
    ident = PA([128], F32); ident_bf = PA([128], BF16); ones_bf = PA([128], BF16)
    m_ui = PA([128], F32); m_us = PA([128], F32); m_li = PA([128], F32); m_ls = PA([128], F32); m_one = PA([128], F32)
    m_ubd = V(arena, 150 * 1024, [128], F32); m_ux = V(arena, 150 * 1024 + 512, [128], F32); m_lbd = V(arena, 150 * 1024 + 1024, [128], F32); m_lx = V(arena, 150 * 1024 + 1536, [128], F32)
    mb_ubd = PA([128], BF16); mb_ux = PA([128], BF16); mb_lbd = PA([128], BF16); mb_lx = PA([128], BF16)
    b_const = P.buf("const")

    def pool_op(fn, reads=(), writes=()):
        return P.op("pool", fn, reads, writes)

    def mk_mask(ap, pattern_step, chmul, cmp):
        pool_op(lambda e: e.memset(ap, 1.0), writes=[b_const])
        pool_op(lambda e: e.affine_select(out=ap, in_=ap, pattern=[[pattern_step, 128]], compare_op=cmp, fill=0.0,
                                          base=0, channel_multiplier=chmul), reads=[b_const], writes=[b_const])
    pool_op(lambda e: e.memset(ident, 0.0), writes=[b_const])
    pool_op(lambda e: e.affine_select(out=ident, in_=ident, pattern=[[-1, 128]], compare_op=ALU.not_equal, fill=1.0,
                                      base=0, channel_multiplier=1), reads=[b_const], writes=[b_const])
    pool_op(lambda e: e.tensor_copy(out=ident_bf, in_=ident), reads=[b_const], writes=[b_const])
    pool_op(lambda e: e.memset(ones_bf, 1.0), writes=[b_const])
    pool_op(lambda e: e.memset(m_one, 1.0), writes=[b_const])
    mk_mask(m_ui, 1, -1, ALU.is_ge)
    mk_mask(m_us, 1, -1, ALU.is_gt)
    mk_mask(m_li, -1, 1, ALU.is_ge)
    mk_mask(m_ls, -1, 1, ALU.is_gt)
    pool_op(lambda e: e.tensor_copy(out=m_ubd, in_=m_us), reads=[b_const], writes=[b_const])
    pool_op(lambda e: e.memset(m_ubd[0:64, 64:128], 0.0), reads=[b_const], writes=[b_const])
    pool_op(lambda e: e.memset(m_ux, 0.0), writes=[b_const])
    pool_op(lambda e: e.memset(m_ux[0:64, 64:128], 1.0), reads=[b_const], writes=[b_const])
    pool_op(lambda e: e.tensor_copy(out=m_lbd, in_=m_ls), reads=[b_const], writes=[b_const])
    pool_op(lambda e: e.memset(m_lbd[64:128, 0:64], 0.0), reads=[b_const], writes=[b_const])
    pool_op(lambda e: e.memset(m_lx, 0.0), writes=[b_const])
    pool_op(lambda e: e.memset(m_lx[64:128, 0:64], 1.0), reads=[b_const], writes=[b_const])

    mb_ui = PA([128], BF16); mb_us = PA([128], BF16); mb_li = PA([128], BF16); mb_ls = PA([128], BF16)
    for dst_, src_ in ((mb_ui, m_ui), (mb_us, m_us), (mb_li, m_li), (mb_ls, m_ls), (mb_ubd, m_ubd), (mb_ux, m_ux), (mb_lbd, m_lbd), (mb_lx, m_lx)):
        pool_op(lambda e, dst_=dst_, src_=src_: e.tensor_copy(out=dst_, in_=src_), reads=[b_const], writes=[b_const])
    eps_t = PA([1], F32)
    pool_op(lambda e: e.memset(eps_t, EPS), writes=[b_const])
    negA = PA([8], F32); dtb_bc = PA([8], F32); onorm_bc = PA([128], F32)
    lng_bc = PA([512], F32); lnb_bc = PA([512], F32)
    wsT = PA([4, 128], BF16); bsT = PA([4], F32); convw = PA([12, 5], F32)
    Wr32 = PA([8, 20], F32); brt_bc = PA([20], F32)
    modT = PA([48, 4], F32)
    b_par = P.buf("params")

    def load(dst, src, reads=(), writes=(), q="sp"):
        return P.dma(q, sem_ld, dst, src, reads=reads, writes=list(writes))

    load(negA, alog_d.partition_broadcast(128), writes=[b_par])
    load(dtb_bc, dtb_d.partition_broadcast(128), writes=[b_par])
    load(onorm_bc, ong_d.partition_broadcast(128), writes=[b_par])
    load(lng_bc, lng_d.partition_broadcast(128), writes=[b_par])
    load(lnb_bc, lnb_d.partition_broadcast(128), writes=[b_par])
    load(brt_bc[:, 0:4], bgrp_d.partition_broadcast(128), writes=[b_par])
    load(brt_bc[:, 4:20], brt_d.partition_broadcast(128), writes=[b_par])
    load(Wr32[:, :, 0:4], wgrp_d.rearrange("(kc p) n -> p kc n", p=128), writes=[b_par])
    load(Wr32[:, :, 4:20], wrt_d.rearrange("(kc p) n -> p kc n", p=128), writes=[b_par])
    P.op("act", lambda e: e.activation(out=negA, in_=negA, func=AF.Exp), reads=[b_par], writes=[b_par])
    P.op("dve", lambda e: e.tensor_scalar(out=negA, in0=negA, scalar1=-1.0, scalar2=None, op0=ALU.mult), reads=[b_par], writes=[b_par])

    A0 = Alloc(arena, ARENA)
    b_tmp = P.buf("setup_tmp")
    b_ps = [P.buf(f"bank{i}", excl=True) for i in range(8)]
    wsp_sb = A0([4, 128], F32); bsp_sb = A0([128], F32, parts=4); conv_sb = A0([1536], F32, parts=5)
    load(wsp_sb, wsp_d.rearrange("h i j -> i h j"), writes=[b_tmp])
    load(bsp_sb, bsp_d, writes=[b_tmp])
    load(conv_sb, conv_d, writes=[b_tmp])
    for h in range(4):
        P.op("pe", lambda e, h=h: e.transpose(out=banks[0][:, h * 128:(h + 1) * 128], in_=wsp_sb[:, h, :], identity=ident),
             reads=[b_tmp, b_const], writes=[b_ps[0]])
    P.op("act", lambda e: e.activation(out=wsT, in_=banks[0][:, 0:512].rearrange("p (a b) -> p a b", a=4), func=AF.Copy),
         reads=[b_ps[0]], writes=[b_par])
    P.op("pe", lambda e: e.transpose(out=banks[1][:, 0:4], in_=bsp_sb, identity=ident[0:4, 0:4]), reads=[b_tmp, b_const], writes=[b_ps[1]])
    P.op("act", lambda e: e.activation(out=bsT, in_=banks[1][:, 0:4], func=AF.Copy), reads=[b_ps[1]], writes=[b_par])
    for cc in range(12):
        P.op("pe", lambda e, cc=cc: e.transpose(out=banks[2][:, cc * 8:cc * 8 + 5], in_=conv_sb[:, cc * 128:(cc + 1) * 128],
                                                identity=ident[0:5, 0:5]), reads=[b_tmp, b_const], writes=[b_ps[2]])
    P.op("act", lambda e: e.activation(out=convw, in_=banks[2][:, 0:96].rearrange("p (a b) -> p a b", a=12)[:, :, 0:5], func=AF.Copy),
         reads=[b_ps[2]], writes=[b_par])

    cT = A0([3, 8], F32); cTb = A0([3, 8], BF16)
    for j in range(NB):
        load(cT[:, j, :], c_d[j].rearrange("(p kc) -> p kc", kc=8), writes=[b_tmp])
    if NB < 2:
        P.op("dve", lambda e: e.memset(cT[:, 1, :], 0.0), writes=[b_tmp])
    load(cT[:, 2, :], cctx_d[0].rearrange("(p kc) -> p kc", kc=8), writes=[b_tmp])
    P.op("act", lambda e: e.activation(out=cTb, in_=cT, func=AF.Silu), reads=[b_tmp], writes=[b_tmp])
    wada_v = wada_d.rearrange("(p kc) n -> p kc n", kc=8)
    wa = [A0([8, 512], BF16) for _ in range(2)]
    b_wa = [P.buf() for _ in range(2)]
    sem_wa = [P.dma_sem(f"wa{i}") for i in range(2)]
    for nb_ in range(12):
        s = nb_ % 2
        P.dma("pool", sem_wa[s], wa[s], wada_v[:, :, nb_ * 512:(nb_ + 1) * 512], writes=[b_wa[s]])
        for c4 in range(4):
            ch = nb_ * 4 + c4
            for kc in range(8):
                P.op("pe", lambda e, s=s, c4=c4, kc=kc, ch=ch: e.matmul(banks[3][:, ch * 4:ch * 4 + 3], wa[s][:, kc, c4 * 128:(c4 + 1) * 128],
                                                                      cTb[:, :, kc], start=(kc == 0), stop=(kc == 7)),
                     reads=[b_wa[s], b_tmp], writes=[b_ps[3]])
    P.op("act", lambda e: e.activation(out=modT[:, :, 0:3], in_=banks[3][:, 0:192].rearrange("p (a b) -> p a b", a=48)[:, :, 0:3], func=AF.Copy),
         reads=[b_ps[3]], writes=[b_par])
    P.barrier()

    def make_bc(dst, j, which, b_dst, tmp_bias, b_tmpb, g_bc=None, b_g=None):
        load(tmp_bias, bada_d[:, which * 1024:(which + 1) * 1024].partition_broadcast(128), writes=[b_tmpb])
        for c8 in range(8):
            ch = which * 8 + c8
            bk = 6 + c8 // 4
            P.op("pe", lambda e, c8=c8, ch=ch, bk=bk: e.matmul(banks[bk][:, (c8 % 4) * 128:(c8 % 4 + 1) * 128],
                                                               modT[:, ch, j:j + 1].to_broadcast([128, 128]), ident, start=True, stop=True),
                 reads=[b_par, b_const], writes=[b_ps[bk]])
        for hf in range(2):
            P.op("dve", lambda e, hf=hf: e.tensor_tensor(out=dst[:, hf * 512:(hf + 1) * 512], in0=banks[6 + hf][:, :],
                                                        in1=tmp_bias[:, hf * 512:(hf + 1) * 512], op=ALU.add),
                 reads=[b_ps[6 + hf], b_tmpb], writes=[b_dst])
        if g_bc is not None:
            P.op("dve", lambda e: e.scalar_tensor_tensor(out=dst, in0=dst, scalar=1.0, in1=g_bc, op0=ALU.add, op1=ALU.mult),
                 reads=[b_dst, b_g], writes=[b_dst])

    def rstd_from_ss(ss, n, b_s):
        P.op("dve", lambda e: e.tensor_scalar(out=ss, in0=ss, scalar1=1.0 / n, scalar2=EPS, op0=ALU.mult, op1=ALU.add), reads=[b_s], writes=[b_s])
        P.op("act", lambda e: e.activation(out=ss, in_=ss, func=AF.Sqrt), reads=[b_s], writes=[b_s])
        P.op("dve", lambda e: e.reciprocal(out=ss, in_=ss), reads=[b_s], writes=[b_s])

    sem_x = [P.dma_sem(f"x{i}") for i in range(2)]
    sem_w = [P.dma_sem(f"w{i}") for i in range(4)]
    sem_o = [P.dma_sem(f"o{i}") for i in range(2)]
    out_toks = []

    def do_batch(b):
        A = Alloc(arena, ARENA)
        RA = 0
        x_res = V(arena, RA, [16, 1024], F32)
        b_xres = [P.buf(f"xres{t}") for t in range(16)]
        xT = V(arena, RA, [8, 2304], BF16)
        b_xT = [P.buf(f"xT{t}") for t in range(18)]
        CT = RA + 36 * KB
        RB = 64 * KB
        qkvT = V(arena, RB, [12, 2304], BF16)
        b_qkv = [[P.buf() for _ in range(18)] for _ in range(12)]
        RC = RB + 54 * KB
        o_acc = V(arena, RC, [16, 512], F32)
        b_oacc = [[P.buf() for _ in range(4)] for _ in range(16)]
        Wqkv = V(arena, RC, [8, 1536], BF16)
        b_wqkv = P.buf()
        RD = RC + 32 * KB
        AD = Alloc(arena[:, RD:ARENA], ARENA - RD)
        gb = AD([18, 16], F32); cumE = AD([18, 40], F32); negE = AD([18, 40], F32)
        b_gb = [P.buf() for _ in range(18)]
        bcA = AD([1024], F32); bcS = AD([1024], F32); bcG = AD([1024], F32)
        b_bcA, b_bcS, b_bcG = P.buf(), P.buf(), P.buf()
        Wab = AD([8, 16], BF16); b_wab = P.buf()
        xin = [AD([1024], F32) for _ in range(2)]; b_xin = [P.buf() for _ in range(2)]
        xmb = AD([1024], BF16); b_xmb = P.buf()
        small = AD([16], F32); b_small0 = P.buf()
        g1_bc = xin[0]
        load(g1_bc, n1g_d.partition_broadcast(128), writes=[b_xin[0]])
        P.dma("pool", sem_w[0], Wab, win_d.rearrange("(kc p) n -> p kc n", p=128)[:, :, 3072:3088], writes=[b_wab])

        def norm_mod_T(src, b_src, A_bc, S_bc, dstT, b_dstT, bank, junk, b_junk, f32T=None, xm_f32=None, tmps=None):
            small_, b_small, junk_f32, b_jf = tmps if tmps is not None else (small, b_small0, junk_f320, b_jf0)
            ss = small_[:, 0:1]
            P.op("act", lambda e: e.activation(out=junk, in_=src, func=AF.Square, accum_out=ss), reads=[b_src], writes=[b_junk, b_small])
            rstd_from_ss(ss, 1024.0, b_small)
            if xm_f32 is None:
                tmp = junk_f32
                P.op("dve", lambda e: e.scalar_tensor_tensor(out=tmp, in0=src, scalar=ss, in1=A_bc, op0=ALU.mult, op1=ALU.mult),
                     reads=[b_src, b_small, b_bcA], writes=[b_jf])
                P.op("dve", lambda e: e.tensor_tensor(out=junk, in0=tmp, in1=S_bc, op=ALU.add), reads=[b_jf, b_bcS], writes=[b_junk])
                pv = banks[bank][:, 0:512].bitcast(BF16)
                for kc in range(8):
                    P.op("pe", lambda e, kc=kc: e.transpose(out=pv[:, kc * 128:(kc + 1) * 128], in_=junk[:, kc * 128:(kc + 1) * 128], identity=ident_bf),
                         reads=[b_junk, b_const], writes=[b_ps[bank]])
                P.op("act", lambda e: e.activation(out=dstT, in_=pv.rearrange("p (a b) -> p a b", a=8), func=AF.Copy),
                     reads=[b_ps[bank]], writes=b_dstT)
            else:
                P.op("dve", lambda e: e.scalar_tensor_tensor(out=xm_f32, in0=src, scalar=ss, in1=A_bc, op0=ALU.mult, op1=ALU.mult),
                     reads=[b_src, b_small, b_bcA], writes=[b_jf])
                P.op("dve", lambda e: e.tensor_tensor(out=xm_f32, in0=xm_f32, in1=S_bc, op=ALU.add), reads=[b_jf, b_bcS], writes=[b_jf])
                for kc in range(8):
                    bk = bank + kc // 4
                    P.op("pe", lambda e, kc=kc, bk=bk: e.transpose(out=banks[bk][:, (kc % 4) * 128:(kc % 4 + 1) * 128],
                                                                   in_=xm_f32[:, kc * 128:(kc + 1) * 128], identity=ident),
                         reads=[b_jf, b_const], writes=[b_ps[bk]])
                for hf in range(2):
                    P.op("act", lambda e, hf=hf: e.activation(out=dstT[:, hf * 4:(hf + 1) * 4, :], in_=banks[bank + hf][:, :].rearrange("p (a b) -> p a b", a=4), func=AF.Copy),
                         reads=[b_ps[bank + hf]], writes=b_dstT)
                    P.op("dve", lambda e, hf=hf: e.tensor_copy(out=f32T[:, hf * 4:(hf + 1) * 4, :], in_=banks[bank + hf][:, :].rearrange("p (a b) -> p a b", a=4)),
                         reads=[b_ps[bank + hf]], writes=[b_f32T])

        junk_f320 = AD([1024], F32); b_jf0 = P.buf()
        junk_f32 = junk_f320; b_jf = b_jf0

        def p1_tile(t, ev):
            if t < 2:
                src_d = ctx_d[b, t * 128:(t + 1) * 128, :]
            else:
                src_d = x_d[b, (t - 2) * 128:(t - 1) * 128, :]
            xt = ev["xin"]; bk0, bk1, bk2 = ev["banks"]
            ghf_ = ev["ghf"]
            P.dma("sp", ev["sem"], xt, src_d, writes=[ev["b_xin"]])
            norm_mod_T(xt, ev["b_xin"], bcA, bcS, xT[:, :, t * 128:(t + 1) * 128], [b_xT[t]], bk0, ev["xmb"], ev["b_xmb"], tmps=ev["tmps"])
            for kc in range(8):
                P.op("pe", I("matmul", banks[bk1][:, 0:16], xT[:, kc, t * 128:(t + 1) * 128], Wab[:, kc, :], start=(kc == 0), stop=(kc == 7)),
                     reads=[b_xT[t], b_wab], writes=[b_ps[bk1]])
            g8 = gb[:, t, 0:8]
            P.op("dve", I("tensor_tensor", out=g8, in0=banks[bk1][:, 0:8], in1=dtb_bc, op=ALU.add), reads=[b_ps[bk1], b_par], writes=[b_gb[t]])
            P.op("act", I("activation", out=gb[:, t, 8:16], in_=banks[bk1][:, 8:16], func=AF.Sigmoid), reads=[b_ps[bk1]], writes=[b_gb[t]])
            P.op("act", I("activation", out=g8, in_=g8, func=AF.Exp), reads=[b_gb[t]], writes=[b_gb[t]])
            P.op("act", I("activation", out=g8, in_=g8, func=AF.Ln, bias=1.0), reads=[b_gb[t]], writes=[b_gb[t]])
            P.op("dve", I("tensor_tensor", out=g8, in0=g8, in1=negA, op=ALU.mult), reads=[b_gb[t], b_par], writes=[b_gb[t]])
            P.op("dve", I("tensor_copy", out=ghb[:, t, 0:8], in_=g8), reads=[b_gb[t]], writes=[b_gb[t]])
            P.op("dve", I("tensor_copy", out=ghf_, in_=ghb[:, t, 0:8]), reads=[b_gb[t]], writes=[b_gb[t]])
            P.op("dve", I("tensor_tensor", out=ghb[:, t, 8:16], in0=g8, in1=ghf_, op=ALU.subtract), reads=[b_gb[t]], writes=[b_gb[t]])
            for mi, mk in enumerate((m_ui, m_ls, m_li, m_us, m_one)):
                P.op("pe", I("matmul", banks[bk2][:, mi * 8:(mi + 1) * 8], mk, g8, start=True, stop=True), reads=[b_gb[t], b_const], writes=[b_ps[bk2]])
            P.op("act", I("activation", out=cumE[:, t, :], in_=banks[bk2][:, 0:40], func=AF.Exp), reads=[b_ps[bk2]], writes=[b_gb[t]])
            P.op("dve", I("tensor_scalar", out=negE[:, t, :], in0=cumE[:, t, :], scalar1=-1.0, scalar2=None, op0=ALU.mult), reads=[b_gb[t]], writes=[b_gb[t]])

        def phase1_tiles(tiles, j):
            make_bc(bcA, j, 1, b_bcA, xin[1], b_xin[1], g_bc=g1_bc, b_g=b_xin[0])
            make_bc(bcS, j, 0, b_bcS, xin[1], b_xin[1])
            for i in range(0, len(tiles), 2):
                P.capture = []
                p1_tile(tiles[i], envs1[0])
                A_ = P.capture
                P.capture = []
                p1_tile(tiles[i + 1], envs1[1])
                B_ = P.capture
                P.capture = None
                P.replay_merged(A_, B_)

        if stop <= 0:
            return
        _x2 = AD([1024], F32); _bx2 = P.buf()
        xin2 = [_x2, _x2]; b_xin2 = [_bx2, _bx2]
        ghb = AD([18, 16], BF16); ghf = AD([8], F32)
        E1 = Alloc(arena[:, RB:RB + 16 * KB], 16 * KB)
        envs1 = [
            {"xin": xin2[0], "b_xin": b_xin2[0], "sem": sem_x[0], "xmb": xmb, "b_xmb": b_xmb, "tmps": None, "ghf": ghf, "banks": (0, 1, 2)},
            {"xin": E1([1024], F32), "b_xin": P.buf(), "sem": sem_x[1], "xmb": E1([1024], BF16), "b_xmb": P.buf(),
             "tmps": (E1([16], F32), P.buf(), E1([1024], F32), P.buf()), "ghf": E1([8], F32), "banks": (3, 4, 5)},
        ]
        phase1_tiles([0, 1], 2)
        phase1_tiles(list(range(2, 18)), b)

        if stop <= 1:
            return
        P.dma("pool", sem_w[1], Wqkv, win_d.rearrange("(kc p) n -> p kc n", p=128)[:, :, 1024:2560], writes=[b_wqkv])
        C5 = Alloc(arena[:, CT:CT + 28 * KB], 28 * KB)
        PTb = [C5([2320], BF16) for _ in range(2)]; b_PTb = [P.buf() for _ in range(2)]
        ACC = C5([2304], F32); b_ACC = P.buf()
        SQB = C5([2304], BF16); b_SQB = P.buf()
        RNb = [C5([512], F32) for _ in range(2)]; b_RNb = [P.buf() for _ in range(2)]
        DG = C5([5, 128], BF16); b_DG = P.buf()
        for i_ in range(2):
            P.op("pool", I("memset", PTb[i_], 0.0), writes=[b_PTb[i_]])
        blocks = [(0, 256)] + [(256 + i * 512, 512) for i in range(4)]
        for cc in range(12):
            pi_ = cc % 2
            PT_ = PTb[pi_]; bPT = b_PTb[pi_]
            for tp in range(5):
                P.op("dve", I("tensor_scalar", out=DG[:, tp, :], in0=ident_bf, scalar1=convw[:, cc, tp:tp + 1], scalar2=None, op0=ALU.mult),
                     reads=[b_par, b_const], writes=[b_DG])
            for bi, (t0, n) in enumerate(blocks):
                bk = bi % 2
                tl = list(range(t0 // 128, (t0 + n) // 128))
                for kc in range(8):
                    P.op("pe", I("matmul", banks[bk][:, 0:n], Wqkv[:, kc, cc * 128:(cc + 1) * 128], xT[:, kc, t0:t0 + n], start=(kc == 0), stop=(kc == 7)),
                         reads=[b_wqkv] + [b_xT[t] for t in tl], writes=[b_ps[bk]])
                po = (2 + t0) if t0 < 256 else (262 + t0 - 256)
                P.op("act", I("activation", out=PT_[:, po:po + n], in_=banks[bk][:, 0:n], func=AF.Copy), reads=[b_ps[bk]], writes=[bPT])
            allq = [b_qkv[cc][t] for t in range(18)]
            for bi, (t0, n) in enumerate(blocks):
                bk = 2 + bi % 2
                po = (2 + t0) if t0 < 256 else (262 + t0 - 256)
                for tp in range(5):
                    P.op("pe", I("matmul", banks[bk][:, 0:n], DG[:, tp, :], PT_[:, po - 2 + tp:po - 2 + tp + n], start=(tp == 0), stop=(tp == 4)),
                         reads=[b_DG, bPT], writes=[b_ps[bk]])
                if cc >= 8:
                    P.op("act", I("activation", out=qkvT[:, cc, t0:t0 + n], in_=banks[bk][:, 0:n], func=AF.Silu), reads=[b_ps[bk]], writes=allq)
                else:
                    P.op("act", I("activation", out=ACC[:, t0:t0 + n], in_=banks[bk][:, 0:n], func=AF.Silu), reads=[b_ps[bk]], writes=[b_ACC])
                    P.op("dve", I("tensor_tensor", out=SQB[:, t0:t0 + n], in0=ACC[:, t0:t0 + n], in1=ACC[:, t0:t0 + n], op=ALU.mult), reads=[b_ACC], writes=[b_SQB])
            if cc < 8:
                sc = (128.0 ** -0.5) if cc < 4 else 1.0
                for bi, (t0, n) in enumerate(blocks):
                    bk = 4 + bi % 2
                    ri = bi % 2
                    P.op("pe", I("matmul", banks[bk][:, 0:n], ones_bf, SQB[:, t0:t0 + n], start=True, stop=True), reads=[b_SQB, b_const], writes=[b_ps[bk]])
                    P.op("act", I("activation", out=RNb[ri][:, 0:n], in_=banks[bk][:, 0:n], func=AF.Sqrt, bias=eps_t), reads=[b_ps[bk], b_const], writes=[b_RNb[ri]])
                    P.op("dve", I("reciprocal", out=RNb[ri][:, 0:n], in_=RNb[ri][:, 0:n]), reads=[b_RNb[ri]], writes=[b_RNb[ri]])
                    P.op("dve", I("scalar_tensor_tensor", out=qkvT[:, cc, t0:t0 + n], in0=ACC[:, t0:t0 + n], scalar=sc, in1=RNb[ri][:, 0:n], op0=ALU.mult, op1=ALU.mult),
                         reads=[b_ACC, b_RNb[ri]], writes=allq)
        P.barrier()
        if stop <= 5:
            return
        SA = Alloc(arena[:, RA:RA + 64 * KB], 64 * KB)
        seqs = []
        for d in range(2):
            for h in range(4):
                q = {"d": d, "h": h}
                for nm in ("gmask", "decT", "dmbd", "dmx", "dmq", "vtok"):
                    q[nm] = SA([128], F32)
                for nm in ("Mbd", "Nbd", "XT", "QKd", "R0", "R1", "RT0", "RT1", "P0", "P1", "PT0", "PT1", "ZT", "AinvT", "kd", "r", "vnew", "Sbf"):
                    q[nm] = SA([128], BF16)
                q["S"] = SA([128], F32)
                q["b"] = {}
                seqs.append(q)

        def sb(q, nm):
            if nm not in q["b"]:
                q["b"][nm] = P.buf()
            return q["b"][nm]
        ring_i = {"prep": 0, "scan": 0}

        def pslot(kind="prep"):
            i = ring_i[kind]
            ring_i[kind] += 1
            if kind == "prep":
                i %= 20
                bk, c = i % 5, i // 5
            else:
                i %= 12
                bk, c = 5 + i % 3, i // 3
            return banks[bk][:, c * 128:(c + 1) * 128], b_ps[bk]

        for q in seqs:
            P.op("pool", I("memset", q["S"], 0.0), writes=[sb(q, "S")])
            P.op("pool", I("memset", q["Sbf"], 0.0), writes=[sb(q, "Sbf")])
        orders = [list(range(18)), [1, 0] + list(range(17, 1, -1))]
        o_written = [[False] * 4 for _ in range(16)]

        def mmq(out, lhsT, rhs, reads, bw):
            P.op("pe", lambda e: e.matmul(out, lhsT, rhs, start=True, stop=True), reads=reads, writes=[bw])

        def scan_step(step2, part):
            step = step2 // 2
            st = []
            for q in seqs:
                d, h = q["d"], q["h"]
                if d != step2 % 2:
                    continue
                t = orders[d][step]
                if KLAT == 0:
                    t = t % 2
                st.append((q, d, h, t, (t >= 2) and KNOQK == 0))
            tsl = lambda t: slice(t * 128, (t + 1) * 128)
            if part == "prep":
                for (q, d, h, t, lat) in st:
                    kT = qkvT[:, 4 + h, tsl(t)]; qT = qkvT[:, h, tsl(t)]; vT = qkvT[:, 8 + h, tsl(t)]
                    q["kT"], q["qT"] = kT, qT
                    q["bk"], q["bq"], q["bv"] = b_qkv[4 + h][t], b_qkv[h][t], b_qkv[8 + h][t]
                    col = d * 4 + h
                    q["beta"] = gb[:, t, 8 + col:9 + col]
                    q["gcol"] = gb[:, t, col:col + 1]
                    q["eg"] = cumE[:, t, (h if d == 0 else 20 + h):(h if d == 0 else 20 + h) + 1]
                    q["neg"] = negE[:, t, (h if d == 0 else 20 + h):(h if d == 0 else 20 + h) + 1]
                    q["ekd"] = cumE[:, t, (8 + h if d == 0 else 28 + h):(8 + h if d == 0 else 28 + h) + 1]
                    q["gl"] = cumE[:, t, 32 + col:33 + col]
                    q["bg"] = b_gb[t]
                    ks, q["bks"] = pslot(); q["ks"] = ks.bitcast(BF16)[:, 0:128]
                    vs, q["bvs"] = pslot(); q["vs"] = vs.bitcast(BF16)[:, 0:128]
                    P.op("pe", I("transpose", out=q["ks"], in_=kT, identity=ident_bf), reads=[q["bk"], b_const], writes=[q["bks"]])
                    P.op("pe", I("transpose", out=q["vs"], in_=vT, identity=ident_bf), reads=[q["bv"], b_const], writes=[q["bvs"]])
                    ml = mb_ls if d == 0 else mb_us
                    gmv = q["gmask"].bitcast(BF16)
                    q["gmh"], q["gml"] = gmv[:, 0:128], gmv[:, 128:256]
                    P.op("dve", I("tensor_scalar", out=q["gmh"], in0=ml, scalar1=ghb[:, t, col:col + 1], scalar2=None, op0=ALU.mult),
                         reads=[q["bg"], b_const], writes=[sb(q, "gmask")])
                    P.op("dve", I("tensor_scalar", out=q["gml"], in0=ml, scalar1=ghb[:, t, 8 + col:9 + col], scalar2=None, op0=ALU.mult),
                         reads=[q["bg"], b_const], writes=[sb(q, "gmask")])
                if KSUB < 2:
                    return
                for (q, d, h, t, lat) in st:
                    P.op("dve", I("tensor_copy", out=q["vtok"], in_=q["vs"]), reads=[q["bvs"]], writes=[sb(q, "vtok")])
                    P.op("act", I("activation", out=q["kd"], in_=q["ks"], func=AF.Copy, scale=q["ekd"]), reads=[q["bks"], q["bg"]], writes=[sb(q, "kd")])
                for (q, d, h, t, lat) in st:
                    q["G"], q["bG"] = pslot()
                    mmq(q["G"], q["kT"], q["kT"], [q["bk"]], q["bG"])
                    if lat:
                        q["QK"], q["bQK"] = pslot()
                        mmq(q["QK"], q["kT"], q["qT"], [q["bk"], q["bq"]], q["bQK"])
                for (q, d, h, t, lat) in st:
                    mr = mb_ui if d == 0 else mb_li
                    q["df"], q["bdf"] = pslot()
                    P.op("pe", I("matmul", q["df"], q["gmh"], mr, start=True, stop=False), reads=[sb(q, "gmask"), b_const], writes=[q["bdf"]])
                    P.op("pe", I("matmul", q["df"], q["gml"], mr, start=False, stop=True), reads=[sb(q, "gmask"), b_const], writes=[q["bdf"]])
                if KSUB < 3:
                    return
                for (q, d, h, t, lat) in st:
                    P.op("act", I("activation", out=q["decT"], in_=q["df"], func=AF.Exp), reads=[q["bdf"]], writes=[sb(q, "decT")])
                if KSUB < 5:
                    return
                for (q, d, h, t, lat) in st:
                    mbd, mx, mi = (mb_ubd, mb_ux, mb_ui) if d == 0 else (mb_lbd, mb_lx, mb_li)
                    t1 = q["dmbd"].bitcast(BF16)[:, 0:128]
                    P.op("dve", I("scalar_tensor_tensor", out=t1, in0=q["G"], scalar=q["beta"], in1=q["decT"], op0=ALU.mult, op1=ALU.mult),
                         reads=[q["bG"], q["bg"], sb(q, "decT")], writes=[sb(q, "dmbd")])
                    P.op("dve", I("tensor_tensor", out=q["Mbd"], in0=t1, in1=mbd, op=ALU.mult), reads=[sb(q, "dmbd"), b_const], writes=[sb(q, "Mbd")])
                    P.op("dve", I("tensor_tensor", out=q["XT"], in0=t1, in1=mx, op=ALU.mult), reads=[sb(q, "dmbd"), b_const], writes=[sb(q, "XT")])
                    if lat:
                        t2 = q["dmq"].bitcast(BF16)[:, 0:128]
                        P.op("dve", I("tensor_tensor", out=t2, in0=q["QK"], in1=q["decT"], op=ALU.mult), reads=[q["bQK"], sb(q, "decT")], writes=[sb(q, "dmq")])
                        P.op("dve", I("tensor_tensor", out=q["QKd"], in0=t2, in1=mi, op=ALU.mult), reads=[sb(q, "dmq"), b_const], writes=[sb(q, "QKd")])
                if KSUB < 6:
                    return
                for (q, d, h, t, lat) in st:
                    ns, q["bns"] = pslot(); q["ns"] = ns.bitcast(BF16)[:, 0:128]
                    P.op("pe", I("transpose", out=q["ns"], in_=q["Mbd"], identity=ident_bf), reads=[sb(q, "Mbd"), b_const], writes=[q["bns"]])
                    P.op("dve", I("tensor_copy", out=q["Nbd"], in_=q["ns"]), reads=[q["bns"]], writes=[sb(q, "Nbd")])
                    P.op("dve", I("tensor_tensor", out=q["R0"], in0=ident_bf, in1=q["Mbd"], op=ALU.subtract), reads=[sb(q, "Mbd"), b_const], writes=[sb(q, "R0")])
                    q["cur"] = ("Mbd", "Nbd", "R0", "RT0")
                if KSUB < 7:
                    return
                for lvl in range(5):
                    pn, ptn = ("P0", "PT0") if lvl % 2 == 0 else ("P1", "PT1")
                    rn_ = "R1" if lvl % 2 == 0 else "R0"
                    last = lvl == 4
                    for (q, d, h, t, lat) in st:
                        pw, pwt, r_, _ = q["cur"]
                        if not last:
                            q["p2"], q["bp2"] = pslot()
                            mmq(q["p2"], q[pwt], q[pw], [sb(q, pw), sb(q, pwt)], q["bp2"])
                        q["p2t"], q["bp2t"] = pslot()
                        mmq(q["p2t"], q[pw], q[pwt], [sb(q, pw), sb(q, pwt)], q["bp2t"])
                    for (q, d, h, t, lat) in st:
                        if not last:
                            P.op("dve", I("tensor_copy", out=q[pn], in_=q["p2"]), reads=[q["bp2"]], writes=[sb(q, pn)])
                        P.op("act", I("activation", out=q[ptn], in_=q["p2t"], func=AF.Copy), reads=[q["bp2t"]], writes=[sb(q, ptn)])
                    for (q, d, h, t, lat) in st:
                        pw, pwt, r_, _ = q["cur"]
                        q["ra"], q["bra"] = pslot()
                        mmq(q["ra"], q[ptn], q[r_], [sb(q, r_), sb(q, ptn)], q["bra"])
                    for (q, d, h, t, lat) in st:
                        pw, pwt, r_, _ = q["cur"]
                        P.op("dve", I("tensor_tensor", out=q[rn_], in0=q["ra"], in1=q[r_], op=ALU.add),
                             reads=[q["bra"], sb(q, r_)], writes=[sb(q, rn_)])
                        q["cur"] = (pn, ptn, rn_, None)
                for (q, d, h, t, lat) in st:
                    _, _, r_, _ = q["cur"]
                    rts, q["brts"] = pslot(); q["rts"] = rts.bitcast(BF16)[:, 0:128]
                    P.op("pe", I("transpose", out=q["rts"], in_=q[r_], identity=ident_bf), reads=[sb(q, r_), b_const], writes=[q["brts"]])
                for (q, d, h, t, lat) in st:
                    _, _, r_, _ = q["cur"]
                    P.op("act", I("activation", out=q["RT0"], in_=q["rts"], func=AF.Copy), reads=[q["brts"]], writes=[sb(q, "RT0")])
                    q["cur"] = (None, None, r_, "RT0")
                if KSUB < 8:
                    return
                for (q, d, h, t, lat) in st:
                    _, _, r_, rt_ = q["cur"]
                    q["z"], q["bz"] = pslot()
                    mmq(q["z"], q["XT"], q[rt_], [sb(q, "XT"), sb(q, rt_)], q["bz"])
                for (q, d, h, t, lat) in st:
                    P.op("act", I("activation", out=q["ZT"], in_=q["z"], func=AF.Copy), reads=[q["bz"]], writes=[sb(q, "ZT")])
                for (q, d, h, t, lat) in st:
                    _, _, r_, rt_ = q["cur"]
                    q["w"], q["bw"] = pslot()
                    mmq(q["w"], q["ZT"], q[r_], [sb(q, "ZT"), sb(q, r_)], q["bw"])
                for (q, d, h, t, lat) in st:
                    _, _, r_, rt_ = q["cur"]
                    P.op("dve", I("scalar_tensor_tensor", out=q["AinvT"], in0=q["w"], scalar=-1.0, in1=q[r_], op0=ALU.mult, op1=ALU.add),
                         reads=[q["bw"], sb(q, r_)], writes=[sb(q, "AinvT")])
                if KSUB < 9:
                    return
                return
            for (q, d, h, t, lat) in st:
                q["a"], q["ba"] = pslot("scan")
                mmq(q["a"], q["kT"], q["Sbf"], [q["bk"], sb(q, "Sbf")], q["ba"])
            for (q, d, h, t, lat) in st:
                P.op("dve", I("scalar_tensor_tensor", out=q["r"], in0=q["a"], scalar=q["neg"], in1=q["vtok"], op0=ALU.mult, op1=ALU.add),
                     reads=[q["ba"], q["bg"], sb(q, "vtok")], writes=[sb(q, "r")])
            for (q, d, h, t, lat) in st:
                q["bb"], q["bbb"] = pslot("scan")
                mmq(q["bb"], q["AinvT"], q["r"], [sb(q, "AinvT"), sb(q, "r")], q["bbb"])
            for (q, d, h, t, lat) in st:
                P.op("act", I("activation", out=q["vnew"], in_=q["bb"], func=AF.Copy, scale=q["beta"]), reads=[q["bbb"], q["bg"]], writes=[sb(q, "vnew")])
            for (q, d, h, t, lat) in st:
                if lat:
                    q["o1"], q["bo1"] = pslot("scan")
                    mmq(q["o1"], q["qT"], q["Sbf"], [q["bq"], sb(q, "Sbf")], q["bo1"])
                    q["o2"], q["bo2"] = pslot("scan")
                    mmq(q["o2"], q["QKd"], q["vnew"], [sb(q, "QKd"), sb(q, "vnew")], q["bo2"])
                q["sp"], q["bsp"] = pslot("scan")
                mmq(q["sp"], q["kd"], q["vnew"], [sb(q, "kd"), sb(q, "vnew")], q["bsp"])
            for (q, d, h, t, lat) in st:
                if lat:
                    oa = o_acc[:, t - 2, h * 128:(h + 1) * 128]
                    bo = b_oacc[t - 2][h]
                    tmp = q["gmask"]
                    if not o_written[t - 2][h]:
                        P.op("act", I("activation", out=tmp, in_=q["o2"], func=AF.Copy), reads=[q["bo2"]], writes=[sb(q, "gmask")])
                        o_written[t - 2][h] = True
                    else:
                        P.op("dve", I("tensor_tensor", out=tmp, in0=q["o2"], in1=oa, op=ALU.add), reads=[q["bo2"], bo], writes=[sb(q, "gmask")])
                    P.op("dve", I("scalar_tensor_tensor", out=oa, in0=q["o1"], scalar=q["eg"], in1=tmp, op0=ALU.mult, op1=ALU.add),
                         reads=[q["bo1"], q["bg"], sb(q, "gmask")], writes=[bo])
                P.op("dve", I("scalar_tensor_tensor", out=q["S"], in0=q["S"], scalar=q["gl"], in1=q["sp"], op0=ALU.mult, op1=ALU.add),
                     reads=[sb(q, "S"), q["bg"], q["bsp"]], writes=[sb(q, "S")])
                P.op("act", I("activation", out=q["Sbf"], in_=q["S"], func=AF.Copy), reads=[sb(q, "S")], writes=[sb(q, "Sbf")])
        def cap2(step2, part):
            P.capture = []
            scan_step(step2, part)
            lst = P.capture
            P.capture = None
            return lst
        nst = min(36, KSTEPS)
        P.replay_merged(cap2(0, "prep"), [])
        for step2 in range(nst):
            nxt = cap2(step2 + 1, "prep") if step2 + 1 < nst else []
            P.replay_merged(nxt, cap2(step2, "scan"))
        P.barrier()

        if stop <= 6:
            return
        WB = Alloc(arena[:, RB:RB + 54 * KB], 54 * KB)
        WinA = WB([8, 1024], BF16); Wz = WB([8, 512], BF16); Wout = WB([8, 1024], BF16)
        b_w7 = P.buf()
        sets7 = []
        for i7 in range(2):
            d7 = {}
            if i7 == 0:
                d7["xTt"] = WB([8, 128], BF16); d7["u"] = WB([512], F32); d7["v"] = WB([512], F32); d7["vn"] = WB([512], BF16)
                d7["y"] = WB([1024], BF16); d7["yT"] = WB([8, 128], BF16); d7["sz"] = WB([512], F32); d7["st6"] = WB([8], F32); d7["ss4"] = WB([4], F32)
            else:
                d7["u"] = xin2[0][:, 0:512]; d7["v"] = xin2[0][:, 512:1024]
                d7["sz"] = xin[0][:, 0:512]
                d7["y"] = xin[0][:, 512:1024].bitcast(BF16)
                d7["xTt"] = V(arena, RD + 1152, [8, 128], BF16); d7["vn"] = V(arena, RD + 1152 + 2048, [512], BF16)
                d7["yT"] = V(arena, RD + 4224, [8, 128], BF16)
                d7["st6"] = WB([8], F32); d7["ss4"] = WB([4], F32)
            for nm in ("xTt", "u", "v", "vn", "y", "yT", "sz", "st"):
                d7["b_" + nm] = P.buf()
            sets7.append(d7)
        winv = win_d.rearrange("(kc p) n -> p kc n", p=128)
        P.dma("pool", sem_w[0], WinA, winv[:, :, 0:1024], writes=[b_w7])
        b_wz = P.buf()
        P.dma("pool", sem_w[1], Wz, winv[:, :, 2560:3072], writes=[b_wz])
        b_wo = P.buf()
        P.dma("pool", sem_w[2], Wout, wout_d.rearrange("(kc p) n -> p kc n", p=128), writes=[b_wo])
        make_bc(bcG, b, 2, b_bcG, xin[1], b_xin[1])
        for ch in range(8):
            P.op("pool", I("tensor_tensor", out=Wout[:, ch, :], in0=Wout[:, ch, :], in1=bcG, op=ALU.mult), reads=[b_wo, b_bcG], writes=[b_wo])
        def p7_a(tt, xTt, u_sb, v_sb, vn_bf, y_bf, yTt, sz, st6, ss4, b_xTt, b_u, b_v, b_vn, b_y, b_yT, b_sz, b_st):
            xt = x_res[:, tt, :]
            P.dma("sp", sem_x[tt % 2], xt, x_d[b, tt * 128:(tt + 1) * 128, :], writes=[b_xres[tt]])
            norm_mod_T(xt, b_xres[tt], bcA, bcS, xTt, [b_xTt], 0, xmb, b_xmb)
            for hf, bk in ((0, 1), (1, 2)):
                for kc in range(8):
                    P.op("pe", lambda e, kc=kc, hf=hf, bk=bk: e.matmul(banks[bk][:, :], xTt[:, kc, :], WinA[:, kc, hf * 512:(hf + 1) * 512],
                                                                     start=(kc == 0), stop=(kc == 7)), reads=[b_xTt, b_w7], writes=[b_ps[bk]])
            for kc in range(8):
                P.op("pe", lambda e, kc=kc: e.matmul(banks[3][:, :], xTt[:, kc, :], Wz[:, kc, :], start=(kc == 0), stop=(kc == 7)),
                     reads=[b_xTt, b_wz], writes=[b_ps[3]])
            P.op("act", lambda e: e.activation(out=u_sb, in_=banks[1][:, :], func=AF.Gelu_apprx_tanh), reads=[b_ps[1]], writes=[b_u])
            P.op("act", lambda e: e.activation(out=v_sb, in_=banks[2][:, :], func=AF.Gelu_apprx_tanh), reads=[b_ps[2]], writes=[b_v])
            P.op("act", lambda e: e.activation(out=sz, in_=banks[3][:, :], func=AF.Silu), reads=[b_ps[3]], writes=[b_sz])
            P.op("dve", lambda e: e.bn_stats(out=st6[:, 0:6], in_=v_sb), reads=[b_v], writes=[b_st])
            P.op("dve", lambda e: e.bn_aggr(out=st6[:, 6:8], in_=st6[:, 0:6]), reads=[b_st], writes=[b_st])
            P.op("dve", lambda e: e.tensor_scalar(out=st6[:, 7:8], in0=st6[:, 7:8], scalar1=EPS, scalar2=None, op0=ALU.add), reads=[b_st], writes=[b_st])
            P.op("act", lambda e: e.activation(out=st6[:, 7:8], in_=st6[:, 7:8], func=AF.Sqrt), reads=[b_st], writes=[b_st])
            P.op("dve", lambda e: e.reciprocal(out=st6[:, 7:8], in_=st6[:, 7:8]), reads=[b_st], writes=[b_st])
            P.op("dve", lambda e: e.tensor_scalar(out=v_sb, in0=v_sb, scalar1=st6[:, 6:7], scalar2=st6[:, 7:8], op0=ALU.subtract, op1=ALU.mult),
                 reads=[b_v, b_st], writes=[b_v])
            P.op("dve", lambda e: e.tensor_tensor(out=v_sb, in0=v_sb, in1=lng_bc, op=ALU.mult), reads=[b_v, b_par], writes=[b_v])
            P.op("dve", lambda e: e.tensor_tensor(out=vn_bf, in0=v_sb, in1=lnb_bc, op=ALU.add), reads=[b_v, b_par], writes=[b_vn])
        def p7_b(tt, xTt, u_sb, v_sb, vn_bf, y_bf, yTt, sz, st6, ss4, b_xTt, b_u, b_v, b_vn, b_y, b_yT, b_sz, b_st):
            xt = x_res[:, tt, :]
            for h in range(4):
                P.op("pe", lambda e, h=h: e.matmul(banks[4][:, h * 128:(h + 1) * 128], wsT[:, h, :], vn_bf[:, h * 128:(h + 1) * 128], start=True, stop=True),
                     reads=[b_vn, b_par], writes=[b_ps[4]])
            for h in range(4):
                hs = slice(h * 128, (h + 1) * 128)
                P.op("dve", lambda e, h=h, hs=hs: e.scalar_tensor_tensor(out=y_bf[:, hs], in0=banks[4][:, hs], scalar=bsT[:, h:h + 1], in1=u_sb[:, hs],
                                                                       op0=ALU.add, op1=ALU.mult), reads=[b_ps[4], b_par, b_u], writes=[b_y])
            for h in range(4):
                hs = slice(h * 128, (h + 1) * 128)
                P.op("act", lambda e, h=h, hs=hs, tt=tt: e.activation(out=u_sb[:, hs], in_=o_acc[:, tt, hs], func=AF.Square, accum_out=ss4[:, h:h + 1]),
                     reads=[b_oacc[tt][h], b_y], writes=[b_u, b_st])
            rstd_from_ss(ss4, 128.0, b_st)
            for h in range(4):
                hs = slice(h * 128, (h + 1) * 128)
                P.op("dve", lambda e, h=h, hs=hs, tt=tt: e.scalar_tensor_tensor(out=u_sb[:, hs], in0=o_acc[:, tt, hs], scalar=ss4[:, h:h + 1], in1=onorm_bc,
                                                                              op0=ALU.mult, op1=ALU.mult), reads=[b_oacc[tt][h], b_st, b_par, b_u], writes=[b_u])
            P.op("dve", lambda e: e.tensor_tensor(out=y_bf[:, 512:1024], in0=u_sb, in1=sz, op=ALU.mult), reads=[b_u, b_sz], writes=[b_y])
            pv = banks[5][:, 0:512].bitcast(BF16)
            for ch in range(8):
                P.op("pe", lambda e, ch=ch: e.transpose(out=pv[:, ch * 128:(ch + 1) * 128], in_=y_bf[:, ch * 128:(ch + 1) * 128], identity=ident_bf),
                     reads=[b_y, b_const], writes=[b_ps[5]])
            P.op("act", lambda e: e.activation(out=yTt, in_=pv.rearrange("p (a b) -> p a b", a=8), func=AF.Copy), reads=[b_ps[5]], writes=[b_yT])
            for hf in range(2):
                bk = 6 + hf
                for ch in range(8):
                    P.op("pe", lambda e, ch=ch, hf=hf, bk=bk: e.matmul(banks[bk][:, :], yTt[:, ch, :], Wout[:, ch, hf * 512:(hf + 1) * 512],
                                                                     start=(ch == 0), stop=(ch == 7)), reads=[b_yT, b_wo], writes=[b_ps[bk]])
            for hf in range(2):
                hs = slice(hf * 512, (hf + 1) * 512)
                P.op("dve", I("tensor_tensor", out=xt[:, hs], in0=banks[6 + hf][:, :], in1=xt[:, hs], op=ALU.add),
                     reads=[b_ps[6 + hf], b_xres[tt]], writes=[b_xres[tt]])
        def args7(tt):
            d7 = sets7[tt % 2]
            return (tt, d7["xTt"], d7["u"], d7["v"], d7["vn"], d7["y"], d7["yT"], d7["sz"], d7["st6"], d7["ss4"],
                    d7["b_xTt"], d7["b_u"], d7["b_v"], d7["b_vn"], d7["b_y"], d7["b_yT"], d7["b_sz"], d7["b_st"])
        def cap(fn, *a):
            P.capture = []
            fn(*a)
            lst = P.capture
            P.capture = None
            return lst
        P.replay_merged(cap(p7_a, *args7(0)), [])
        for tt in range(1, 16):
            P.replay_merged(cap(p7_a, *args7(tt)), cap(p7_b, *args7(tt - 1)))
        P.replay_merged([], cap(p7_b, *args7(15)))
        P.barrier()
        if dbg and b == 0:
            out_toks.append(P.dma("sp", sem_o[0], dbg_d, x_res, reads=b_xres))
            P.barrier()

        if stop <= 7:
            return
        MA = Alloc(arena[:, RB:ARENA], ARENA - RB)
        h2T = MA([8, 2048], BF16)
        b_h2T = [P.buf() for _ in range(16)]
        GU = [MA([8, 1024], BF16) for _ in range(2)]; DW = [MA([4, 1024], BF16) for _ in range(2)]
        b_GU = [P.buf() for _ in range(2)]; b_DW = [P.buf() for _ in range(2)]
        act_t = [MA([4, 512], BF16) for _ in range(2)]; b_act = [P.buf() for _ in range(2)]
        sg = [MA([512], F32) for _ in range(2)]; b_sg = [P.buf() for _ in range(2)]
        h2f = MA([1024], F32); f32T = MA([8, 128], F32); b_f32T = P.buf()
        cb = MA([16, 16], F32); b_cb = [P.buf() for _ in range(16)]
        lg = MA([20], F32); rt = MA([32], F32); b_rt = P.buf()
        small2 = MA([16], F32); b_small2 = P.buf()
        bc2A = MA([1024], F32); bc2S = MA([1024], F32); bc2G = MA([1024], F32); tb_ = MA([1024], F32)
        b_2A, b_2S, b_2G, b_tb = P.buf(), P.buf(), P.buf(), P.buf()
        jb = MA([1024], BF16); b_jb = P.buf()
        load(tb_, n2g_d.partition_broadcast(128), writes=[b_tb])
        P.op("dve", lambda e: e.tensor_copy(out=h2f, in_=tb_), reads=[b_tb], writes=[b_jf])
        g2_bc = h2f
        make_bc(bc2A, b, 4, b_2A, tb_, b_tb, g_bc=g2_bc, b_g=b_jf)
        make_bc(bc2S, b, 3, b_2S, tb_, b_tb)
        make_bc(bc2G, b, 5, b_2G, tb_, b_tb)
        def route_tile(tt, ev):
            small2, b_small2, jb, b_jb, h2f, b_jf, f32T, b_f32T, lg, rt, b_rt, bkA, bkB, bkC = ev
            ss = small2[:, 0:1]
            src = x_res[:, tt, :]
            P.op("act", lambda e, src=src: e.activation(out=jb, in_=src, func=AF.Square, accum_out=ss), reads=[b_xres[tt]], writes=[b_jb, b_small2])
            rstd_from_ss(ss, 1024.0, b_small2)
            P.op("dve", lambda e, src=src: e.scalar_tensor_tensor(out=h2f, in0=src, scalar=ss, in1=bc2A, op0=ALU.mult, op1=ALU.mult),
                 reads=[b_xres[tt], b_small2, b_2A], writes=[b_jf])
            P.op("dve", lambda e: e.tensor_tensor(out=h2f, in0=h2f, in1=bc2S, op=ALU.add), reads=[b_jf, b_2S], writes=[b_jf])
            for kc in range(8):
                bk = (bkA, bkB)[kc // 4]
                P.op("pe", lambda e, kc=kc, bk=bk: e.transpose(out=banks[bk][:, (kc % 4) * 128:(kc % 4 + 1) * 128], in_=h2f[:, kc * 128:(kc + 1) * 128], identity=ident),
                     reads=[b_jf, b_const], writes=[b_ps[bk]])
            for hf in range(2):
                P.op("act", lambda e, hf=hf, tt=tt: e.activation(out=h2T[:, hf * 4:(hf + 1) * 4, tt * 128:(tt + 1) * 128],
                                                                 in_=banks[(bkA, bkB)[hf]][:, :].rearrange("p (a b) -> p a b", a=4), func=AF.Copy),
                     reads=[b_ps[(bkA, bkB)[hf]]], writes=[b_h2T[tt]])
                P.op("dve", lambda e, hf=hf: e.tensor_copy(out=f32T[:, hf * 4:(hf + 1) * 4, :], in_=banks[(bkA, bkB)[hf]][:, :].rearrange("p (a b) -> p a b", a=4)),
                     reads=[b_ps[(bkA, bkB)[hf]]], writes=[b_f32T])
            for kc in range(8):
                P.op("pe", lambda e, kc=kc: e.matmul(banks[bkC][:, 0:20], f32T[:, kc, :], Wr32[:, kc, :], start=(kc == 0), stop=(kc == 7)),
                     reads=[b_f32T, b_par], writes=[b_ps[bkC]])
            R_ = [b_rt]

            def dv(fn, extra_r=()):
                P.op("dve", fn, reads=R_ + list(extra_r), writes=R_)
            gmx, ngm, gsum, pg, m1, m2, dd, w1g, w2g = (rt[:, i:i + 1] for i in range(9))
            ohg = rt[:, 12:16]; es = rt[:, 16:20]; oh1 = rt[:, 20:24]; es2 = rt[:, 24:28]; oh2 = rt[:, 28:32]
            P.op("dve", lambda e: e.tensor_tensor(out=lg, in0=banks[bkC][:, 0:20], in1=brt_bc, op=ALU.add), reads=[b_ps[bkC], b_par], writes=R_)
            dv(lambda e: e.tensor_reduce(out=gmx, in_=lg[:, 0:4], axis=AX.X, op=ALU.max))
            dv(lambda e: e.tensor_scalar(out=ohg, in0=lg[:, 0:4], scalar1=gmx, scalar2=None, op0=ALU.is_equal))
            dv(lambda e: e.tensor_scalar(out=ngm, in0=gmx, scalar1=-1.0, scalar2=None, op0=ALU.mult))
            P.op("act", lambda e: e.activation(out=es2, in_=lg[:, 0:4], func=AF.Exp, bias=ngm, accum_out=gsum), reads=R_, writes=R_)
            dv(lambda e: e.reciprocal(out=pg, in_=gsum))
            dv(lambda e: e.tensor_scalar(out=es, in0=lg[:, 4:8], scalar1=ohg[:, 0:1], scalar2=None, op0=ALU.mult))
            for g in range(1, 4):
                dv(lambda e, g=g: e.scalar_tensor_tensor(out=es, in0=lg[:, 4 + 4 * g:8 + 4 * g], scalar=ohg[:, g:g + 1], in1=es, op0=ALU.mult, op1=ALU.add))
            dv(lambda e: e.tensor_reduce(out=m1, in_=es, axis=AX.X, op=ALU.max))
            dv(lambda e: e.tensor_scalar(out=oh1, in0=es, scalar1=m1, scalar2=None, op0=ALU.is_equal))
            dv(lambda e: e.scalar_tensor_tensor(out=es2, in0=oh1, scalar=-1e30, in1=es, op0=ALU.mult, op1=ALU.add))
            dv(lambda e: e.tensor_reduce(out=m2, in_=es2, axis=AX.X, op=ALU.max))
            dv(lambda e: e.tensor_scalar(out=oh2, in0=es2, scalar1=m2, scalar2=None, op0=ALU.is_equal))
            dv(lambda e: e.tensor_tensor(out=dd, in0=m1, in1=m2, op=ALU.subtract))
            P.op("act", lambda e: e.activation(out=dd, in_=dd, func=AF.Sigmoid), reads=R_, writes=R_)
            dv(lambda e: e.tensor_tensor(out=w1g, in0=dd, in1=pg, op=ALU.mult))
            dv(lambda e: e.tensor_tensor(out=w2g, in0=pg, in1=w1g, op=ALU.subtract))
            dv(lambda e: e.tensor_scalar(out=es, in0=oh1, scalar1=w1g, scalar2=None, op0=ALU.mult))
            dv(lambda e: e.scalar_tensor_tensor(out=es, in0=oh2, scalar=w2g, in1=es, op0=ALU.mult, op1=ALU.add))
            for g in range(4):
                P.op("dve", lambda e, g=g, tt=tt: e.tensor_scalar(out=cb[:, tt, 4 * g:4 * g + 4], in0=es, scalar1=ohg[:, g:g + 1], scalar2=None, op0=ALU.mult),
                     reads=R_, writes=[b_cb[tt]])
        f32T2 = MA([8, 128], F32); jb2 = MA([1024], BF16); lg2 = MA([20], F32); rt2 = MA([32], F32); small3 = MA([16], F32)
        ev_r = [(small2, b_small2, jb, b_jb, h2f, b_jf, f32T, b_f32T, lg, rt, b_rt, 0, 1, 2),
                (small3, P.buf(), jb2, P.buf(), tb_, b_tb, f32T2, P.buf(), lg2, rt2, P.buf(), 3, 4, 5)]
        for tt in range(0, 16, 2):
            P.capture = []
            route_tile(tt, ev_r[0])
            A_ = P.capture
            P.capture = []
            route_tile(tt + 1, ev_r[1])
            B_ = P.capture
            P.capture = None
            P.replay_merged(A_, B_)
        if stop <= 8:
            return
        def emit_GU(ex, s, tb4, a_s):
            for fc in range(4):
                gs = fc % 2
                bg_, bu_ = (0, 1) if gs == 0 else (2, 3)
                for kc in range(8):
                    P.op("pe", I("matmul", banks[bg_][:, :], GU[s][:, kc, fc * 128:(fc + 1) * 128], h2T[:, kc, tb4 * 512:(tb4 + 1) * 512], start=(kc == 0), stop=(kc == 7)),
                         reads=[b_GU[s]] + b_h2T[tb4 * 4:tb4 * 4 + 4], writes=[b_ps[bg_]])
                for kc in range(8):
                    P.op("pe", I("matmul", banks[bu_][:, :], GU[s][:, kc, 512 + fc * 128:512 + (fc + 1) * 128], h2T[:, kc, tb4 * 512:(tb4 + 1) * 512], start=(kc == 0), stop=(kc == 7)),
                         reads=[b_GU[s]] + b_h2T[tb4 * 4:tb4 * 4 + 4], writes=[b_ps[bu_]])
                P.op("act", I("activation", out=sg[gs], in_=banks[bg_][:, :], func=AF.Silu), reads=[b_ps[bg_]], writes=[b_sg[gs]])
                P.op("dve", I("tensor_tensor", out=act_t[a_s][:, fc, :], in0=sg[gs], in1=banks[bu_][:, :], op=ALU.mult),
                     reads=[b_sg[gs], b_ps[bu_]], writes=[b_act[a_s]])

        def emit_DOWN(ex, s, tb4, a_s):
            for t4 in range(4):
                tt = tb4 * 4 + t4
                ds = t4 % 2
                for hf in range(2):
                    bk = 4 + ds * 2 + hf
                    for fc in range(4):
                        P.op("pe", I("matmul", banks[bk][:, :], act_t[a_s][:, fc, t4 * 128:(t4 + 1) * 128], DW[s][:, fc, hf * 512:(hf + 1) * 512], start=(fc == 0), stop=(fc == 3)),
                             reads=[b_act[a_s], b_DW[s]], writes=[b_ps[bk]])
                for hf in range(2):
                    bk = 4 + ds * 2 + hf
                    hs = slice(hf * 512, (hf + 1) * 512)
                    P.op("dve", I("scalar_tensor_tensor", out=x_res[:, tt, hs], in0=banks[bk][:, :], scalar=cb[:, tt, ex:ex + 1], in1=x_res[:, tt, hs], op0=ALU.mult, op1=ALU.add),
                         reads=[b_ps[bk], b_cb[tt], b_xres[tt]], writes=[b_xres[tt]])

        pending = None
        gcount = 0
        for ex in range(16):
            s = ex % 2
            P.dma("pool", sem_w[s], GU[s], wgu_d[ex].rearrange("(kc p) n -> p kc n", p=128), writes=[b_GU[s]])
            P.dma("pool", sem_w[2 + s], DW[s], wdn_d[ex].rearrange("(fc p) n -> p fc n", p=128), writes=[b_DW[s]])
            for fc in range(4):
                P.op("pool", I("tensor_tensor", out=DW[s][:, fc, :], in0=DW[s][:, fc, :], in1=bc2G, op=ALU.mult),
                     reads=[b_DW[s], b_2G], writes=[b_DW[s]])
            for tb4 in range(4):
                a_s = gcount % 2
                emit_GU(ex, s, tb4, a_s)
                if pending is not None:
                    emit_DOWN(*pending)
                pending = (ex, s, tb4, a_s)
                gcount += 1
        emit_DOWN(*pending)
        if stop <= 9:
            return
        load(tb_, fg_d.partition_broadcast(128), writes=[b_tb])
        def fin_tile(tt, small_, b_small_, jb_, b_jb_, ot_, b_ot_, sem_):
            ss = small_[:, 0:1]
            src = x_res[:, tt, :]
            P.op("act", I("activation", out=jb_, in_=src, func=AF.Square, accum_out=ss), reads=[b_xres[tt]], writes=[b_jb_, b_small_])
            rstd_from_ss(ss, 1024.0, b_small_)
            P.op("dve", I("scalar_tensor_tensor", out=ot_, in0=src, scalar=ss, in1=tb_, op0=ALU.mult, op1=ALU.mult),
                 reads=[b_xres[tt], b_small_, b_tb], writes=[b_ot_])
            P.dma("sp", sem_, out_d[b, tt * 128:(tt + 1) * 128, :], ot_, reads=[b_ot_])
        fe = [(small2, b_small2, jb, b_jb, bc2A, b_2A, sem_o[0]), (small3, ev_r[1][1], jb2, ev_r[1][3], bc2S, b_2S, sem_o[1])]
        for tt in range(0, 16, 2):
            P.capture = []
            fin_tile(tt, *fe[0])
            A_ = P.capture
            P.capture = []
            fin_tile(tt + 1, *fe[1])
            B_ = P.capture
            P.capture = None
            P.replay_merged(A_, B_)
        P.barrier()

    for b in range(NB):
        do_batch(b)
        P.barrier()

    P._emit_waits("sp", out_toks + [(k, P.dma_sem_cnt[k]) for k in sem_o if P.dma_sem_cnt[k] > 0])
    P.emit()
    P.close()
    return nc


_NC_CACHE = {}


def kernel(**inputs):
    NB = 2
    if "nc" not in _NC_CACHE:
        _NC_CACHE["nc"] = build(NB)
    nc = _NC_CACHE["nc"]
    f = lambda a: np.ascontiguousarray(np.asarray(a, dtype=np.float32))
    shared = {
        "c_ctx": f(inputs["c_ctx"]).reshape(1, 1024), "w_ada": f(inputs["w_ada"])[0], "b_ada": f(inputs["b_ada"]).reshape(1, 6144),
        "norm1_g": f(inputs["norm1_g"]).reshape(1, 1024), "w_in": f(inputs["w_in"])[0], "ln_a_g": f(inputs["ln_a_g"]).reshape(1, 512),
        "ln_a_b": f(inputs["ln_a_b"]).reshape(1, 512), "w_spatial": f(inputs["w_spatial"])[0], "b_spatial": f(inputs["b_spatial"])[0],
        "conv_qkv": f(inputs["conv_qkv"])[0], "a_log": f(inputs["a_log"]).reshape(1, 8), "dt_bias": f(inputs["dt_bias"]).reshape(1, 8),
        "onorm_g": f(inputs["onorm_g"]).reshape(1, 128), "w_out": f(inputs["w_out"])[0], "norm2_g": f(inputs["norm2_g"]).reshape(1, 1024),
        "w_group": f(inputs["w_group"])[0], "b_group": f(inputs["b_group"]).reshape(1, 4), "w_router": f(inputs["w_router"])[0],
        "b_router": f(inputs["b_router"]).reshape(1, 16), "w_gate_up": f(inputs["w_gate_up"])[0], "w_down": f(inputs["w_down"])[0],
        "final_g": f(inputs["final_g"]).reshape(1, 1024),
    }
    x = f(inputs["x"]); c = f(inputs["c"]); ctx = f(inputs["ctx"])
    in_maps = []
    for i in range(N_CORES):
        m = dict(shared)
        m["x"] = x[i * NB:(i + 1) * NB]; m["c"] = c[i * NB:(i + 1) * NB]; m["ctx"] = ctx[i * NB:(i + 1) * NB]
        in_maps.append(m)
    res = run_bass_kernel_spmd(nc, in_maps, core_ids=list(range(N_CORES)))
    return np.concatenate([r["out"] for r in res.results], axis=0).astype(np.float32)
```
